# Optimizing a Trainium2 kernel written in Bass

```python
import jax, jax.numpy as jnp
from jax import lax
import numpy as np

D_MODEL = 1024
BATCH = 4
SEQ = 8192
DEPTH = 1

CHUNK = 64
D_MIX = D_MODEL
D_MLSTM = D_MIX // 2
N_MLSTM_HEADS = 4
DH_MLSTM = D_MLSTM // N_MLSTM_HEADS
CONV_W = 4
D_GMLP = D_MIX - D_MLSTM
N_GMLP_GROUPS = 8
DG_GMLP = D_GMLP // N_GMLP_GROUPS
GMLP_BLOCK = 128
D_IN = 4 * D_MLSTM + 2 * N_MLSTM_HEADS + 2 * D_GMLP
N_EXPERT_GROUPS = 4
EXPERTS_PER_GROUP = 8
N_EXPERTS = N_EXPERT_GROUPS * EXPERTS_PER_GROUP
TOP_K = 2
D_FF_EXPERT = 512
MOE_BLOCK = 128
EPS = 1e-6

kernel_name = "hymba_mlstm_gmlp_hiermoe"


def rms_norm(x, g):
    xf = x.astype(jnp.float32)
    y = xf * lax.rsqrt(jnp.mean(xf * xf, axis=-1, keepdims=True) + EPS)
    return (y * g.astype(jnp.float32)).astype(x.dtype)


def head_rms(x):
    xf = x.astype(jnp.float32)
    return xf * lax.rsqrt(jnp.mean(xf * xf, axis=-1, keepdims=True) + EPS)


def causal_depthwise_conv(x, w):
    k = w.shape[0]
    s = x.shape[1]
    xp = jnp.pad(x, ((0, 0), (k - 1, 0), (0, 0)))
    y = w[0] * xp[:, 0:s]
    for i in range(1, k):
        y = y + w[i] * xp[:, i:i + s]
    return y


def mlstm_chunkwise(q, k, v, i_pre, f_pre):
    b_, s_, h_, dh = q.shape
    nc = s_ // CHUNK

    def to_chunks(a):
        a = a.reshape((b_, nc, CHUNK, h_) + a.shape[3:])
        return jnp.moveaxis(a, (1, 3), (0, 2))

    log_f = jax.nn.log_sigmoid(f_pre)
    xs = (to_chunks(q), to_chunks(k), to_chunks(v), to_chunks(i_pre), to_chunks(log_f))
    causal = jnp.tril(jnp.ones((CHUNK, CHUNK), dtype=bool))

    def step(carry, inp):
        c_st, n_st, m_st = carry
        qc, kc, vc, ic, lfc = inp
        bcum = jnp.cumsum(lfc, axis=-1)
        d = jnp.where(causal, bcum[..., :, None] - bcum[..., None, :] + ic[..., None, :], -jnp.inf)
        inter = bcum + m_st[..., None]
        m_t = jnp.maximum(inter, jnp.max(d, axis=-1))
        s = jnp.einsum('bhld,bhsd->bhls', qc, kc) * jnp.exp(d - m_t[..., None])
        w_inter = jnp.exp(inter - m_t)
        numer = jnp.einsum('bhls,bhsd->bhld', s, vc) + w_inter[..., None] * jnp.einsum('bhvd,bhld->bhlv', c_st, qc)
        nq = jnp.sum(s, axis=-1) + w_inter * jnp.einsum('bhd,bhld->bhl', n_st, qc)
        h = numer / jnp.maximum(jnp.abs(nq), jnp.exp(-m_t))[..., None]
        b_last = bcum[..., -1]
        w_state = b_last[..., None] - bcum + ic
        m_new = jnp.maximum(b_last + m_st, jnp.max(w_state, axis=-1))
        decay = jnp.exp(b_last + m_st - m_new)
        ws = jnp.exp(w_state - m_new[..., None])
        c_new = decay[..., None, None] * c_st + jnp.einsum('bhl,bhlv,bhld->bhvd', ws, vc, kc)
        n_new = decay[..., None] * n_st + jnp.einsum('bhl,bhld->bhd', ws, kc)
        return (c_new, n_new, m_new), h

    init = (jnp.zeros((b_, h_, dh, dh), jnp.float32), jnp.zeros((b_, h_, dh), jnp.float32),
            jnp.zeros((b_, h_), jnp.float32))
    _, hs = lax.scan(step, init, xs)
    return jnp.moveaxis(hs, (0, 2), (1, 3)).reshape(b_, s_, h_, dh)


def hybrid_mixer(xn, w_in, conv_qk, b_igate, b_fgate, g_mlstm_out, g_gmlp_v, w_spatial, b_spatial, g_gmlp_out, w_out):
    b_, s_, _ = xn.shape
    hh = N_MLSTM_HEADS
    proj = jnp.einsum('bsd,de->bse', xn, w_in)
    cuts = [2 * D_MLSTM, 3 * D_MLSTM, 4 * D_MLSTM, 4 * D_MLSTM + 2 * hh, 4 * D_MLSTM + 2 * hh + D_GMLP]
    qk, v_m, o_pre, if_pre, u_g, v_g = jnp.split(proj, cuts, axis=-1)

    qk = jax.nn.silu(causal_depthwise_conv(qk, conv_qk))
    q, k = jnp.split(qk.astype(jnp.float32), 2, axis=-1)
    q = q.reshape(b_, s_, hh, DH_MLSTM)
    k = k.reshape(b_, s_, hh, DH_MLSTM) * (DH_MLSTM ** -0.5)
    vm = v_m.astype(jnp.float32).reshape(b_, s_, hh, DH_MLSTM)
    i_pre = if_pre[..., :hh].astype(jnp.float32) + b_igate.astype(jnp.float32)
    f_pre = if_pre[..., hh:].astype(jnp.float32) + b_fgate.astype(jnp.float32)
    h_m = mlstm_chunkwise(q, k, vm, i_pre, f_pre)
    h_m = jax.nn.sigmoid(o_pre.astype(jnp.float32)).reshape(b_, s_, hh, DH_MLSTM) * h_m
    y_m = (head_rms(h_m).reshape(b_, s_, D_MLSTM) * g_mlstm_out.astype(jnp.float32)).astype(xn.dtype)

    nb = s_ // GMLP_BLOCK
    u = jax.nn.gelu(u_g)
    vg = rms_norm(jax.nn.gelu(v_g), g_gmlp_v)
    vb = vg.reshape(b_, nb, GMLP_BLOCK, N_GMLP_GROUPS, DG_GMLP)
    tri = jnp.tril(jnp.ones((GMLP_BLOCK, GMLP_BLOCK), dtype=w_spatial.dtype))
    mix = jnp.einsum('gts,bnsgc->bntgc', w_spatial * tri, vb) + b_spatial.T[:, :, None]
    gated = u.reshape(b_, nb, GMLP_BLOCK, N_GMLP_GROUPS, DG_GMLP) * mix
    y_g = (head_rms(gated).reshape(b_, s_, D_GMLP) * g_gmlp_out.astype(jnp.float32)).astype(xn.dtype)

    y = jnp.concatenate([y_m, y_g], axis=-1)
    return jnp.einsum('bse,ed->bsd', y, w_out)


def hierarchical_moe(xn, w_rg, b_rg, w_re, b_re, w_gate, w_up, w_down):
    b_, s_, d = xn.shape
    t = b_ * s_
    xf = xn.reshape(t, d)
    p_g = jax.nn.softmax((xf @ w_rg).astype(jnp.float32) + b_rg.astype(jnp.float32), axis=-1)
    g_sel = jnp.argmax(p_g, axis=-1)
    p_gsel = jnp.take_along_axis(p_g, g_sel[:, None], axis=-1)[:, 0]
    le = ((xf @ w_re).astype(jnp.float32) + b_re.astype(jnp.float32)).reshape(t, N_EXPERT_GROUPS, EXPERTS_PER_GROUP)
    le = jnp.take_along_axis(le, g_sel[:, None, None], axis=1)[:, 0]
    top_p, top_i = lax.top_k(jax.nn.softmax(le, axis=-1), TOP_K)
    top_p = top_p / jnp.sum(top_p, axis=-1, keepdims=True)
    weights = p_gsel[:, None] * top_p
    experts = (g_sel[:, None] * EXPERTS_PER_GROUP + top_i).astype(jnp.int32)

    n_pairs = t * TOP_K
    e_flat = experts.reshape(-1)
    tok_flat = jnp.repeat(jnp.arange(t, dtype=jnp.int32), TOP_K)
    w_flat = weights.reshape(-1)
    order = jnp.argsort(e_flat)
    e_sorted = e_flat[order]
    counts = jnp.zeros((N_EXPERTS,), jnp.int32).at[e_flat].add(1)
    padded = (counts + MOE_BLOCK - 1) // MOE_BLOCK * MOE_BLOCK
    starts = jnp.cumsum(counts) - counts
    pad_ends = jnp.cumsum(padded)
    pad_starts = pad_ends - padded
    dest = pad_starts[e_sorted] + jnp.arange(n_pairs, dtype=jnp.int32) - starts[e_sorted]
    n_blocks = n_pairs // MOE_BLOCK + N_EXPERTS
    n_rows = n_blocks * MOE_BLOCK
    row_tok = jnp.full((n_rows,), t, jnp.int32).at[dest].set(tok_flat[order])
    row_w = jnp.zeros((n_rows,), jnp.float32).at[dest].set(w_flat[order])
    block_e = jnp.minimum(jnp.searchsorted(pad_ends, jnp.arange(n_blocks, dtype=jnp.int32) * MOE_BLOCK, side='right'),
                          N_EXPERTS - 1)
    x_pad = jnp.concatenate([xf, jnp.zeros((1, d), xf.dtype)], axis=0)
    xb = x_pad[row_tok].reshape(n_blocks, MOE_BLOCK, d)

    def expert_block(args):
        xblk, e = args
        hid = jax.nn.silu(xblk @ w_gate[e]) * (xblk @ w_up[e])
        return hid @ w_down[e]

    yb = lax.map(expert_block, (xb, block_e)).reshape(n_rows, d)
    y = jax.ops.segment_sum(yb * row_w[:, None].astype(yb.dtype), row_tok, num_segments=t + 1)[:t]
    return y.reshape(b_, s_, d)


def setup_inputs(seed: int = 0) -> dict:
    key = jax.random.key(seed)
    ks = jax.random.split(key, 20)
    f32 = jnp.float32
    nrm = lambda k, shape, scale: jax.random.normal(k, shape, f32) * scale
    return {
        "x": nrm(ks[0], (BATCH, SEQ, D_MODEL), 1.0),
        "norm1_g": 1.0 + nrm(ks[1], (DEPTH, D_MODEL), 0.02),
        "w_in": nrm(ks[2], (DEPTH, D_MODEL, D_IN), D_MODEL ** -0.5),
        "conv_qk": nrm(ks[3], (DEPTH, CONV_W, 2 * D_MLSTM), CONV_W ** -0.5),
        "b_igate": nrm(ks[4], (DEPTH, N_MLSTM_HEADS), 0.1),
        "b_fgate": jnp.linspace(3.0, 6.0, N_MLSTM_HEADS, dtype=f32)[None, :] + nrm(ks[5], (DEPTH, N_MLSTM_HEADS), 0.1),
        "g_mlstm_out": 1.0 + nrm(ks[6], (DEPTH, D_MLSTM), 0.02),
        "g_gmlp_v": 1.0 + nrm(ks[7], (DEPTH, D_GMLP), 0.02),
        "w_spatial": nrm(ks[8], (DEPTH, N_GMLP_GROUPS, GMLP_BLOCK, GMLP_BLOCK), 0.5 * GMLP_BLOCK ** -0.5),
        "b_spatial": 1.0 + nrm(ks[9], (DEPTH, N_GMLP_GROUPS, GMLP_BLOCK), 0.01),
        "g_gmlp_out": 1.0 + nrm(ks[10], (DEPTH, D_GMLP), 0.02),
        "w_out": nrm(ks[11], (DEPTH, D_MIX, D_MODEL), D_MIX ** -0.5),
        "norm2_g": 1.0 + nrm(ks[12], (DEPTH, D_MODEL), 0.02),
        "w_router_group": nrm(ks[13], (DEPTH, D_MODEL, N_EXPERT_GROUPS), D_MODEL ** -0.5),
        "b_router_group": nrm(ks[14], (DEPTH, N_EXPERT_GROUPS), 0.01),
        "w_router_expert": nrm(ks[15], (DEPTH, D_MODEL, N_EXPERTS), D_MODEL ** -0.5),
        "b_router_expert": nrm(ks[16], (DEPTH, N_EXPERTS), 0.01),
        "w_gate": nrm(ks[17], (DEPTH, N_EXPERTS, D_MODEL, D_FF_EXPERT), D_MODEL ** -0.5),
        "w_up": nrm(ks[18], (DEPTH, N_EXPERTS, D_MODEL, D_FF_EXPERT), D_MODEL ** -0.5),
        "w_down": nrm(ks[19], (DEPTH, N_EXPERTS, D_FF_EXPERT, D_MODEL), D_FF_EXPERT ** -0.5),
        "final_g": 1.0 + nrm(jax.random.fold_in(key, 99), (D_MODEL,), 0.02),
    }


def reference(x, norm1_g, w_in, conv_qk, b_igate, b_fgate, g_mlstm_out, g_gmlp_v, w_spatial, b_spatial,
              g_gmlp_out, w_out, norm2_g, w_router_group, b_router_group, w_router_expert, b_router_expert,
              w_gate, w_up, w_down, final_g):
    h = x
    for l in range(DEPTH):
        h = h + hybrid_mixer(rms_norm(h, norm1_g[l]), w_in[l], conv_qk[l], b_igate[l], b_fgate[l],
                             g_mlstm_out[l], g_gmlp_v[l], w_spatial[l], b_spatial[l], g_gmlp_out[l], w_out[l])
        h = h + hierarchical_moe(rms_norm(h, norm2_g[l]), w_router_group[l], b_router_group[l],
                                 w_router_expert[l], b_router_expert[l], w_gate[l], w_up[l], w_down[l])
    return rms_norm(h, final_g)
```

```python
import contextlib
import numpy as np
import concourse.bass as bass
import concourse.mybir as mybir
from concourse.bass_utils import run_bass_kernel_spmd

F32 = mybir.dt.float32
BF16 = mybir.dt.bfloat16
I32 = mybir.dt.int32
AF = mybir.ActivationFunctionType
ALU = mybir.AluOpType
AX = mybir.AxisListType

ENGS = ("pe", "act", "dve", "pool", "sp")
SEM_CH = 8000
D = 1024
EPS = 1e-6
NE = 32
NBLK_MAX = 96
NSLOT = NBLK_MAX * 128


class Sched:
    def __init__(self, nc, stack):
        self.nc = nc
        self.stack = stack
        self.ops = []
        self.last_w = {}
        self.readers = {}
        self.dma_count = {}
        self.nbuf = 0
        self.flushed = 0
        self.sems = {}
        self.eng_seq = {e: 0 for e in ENGS}
        self.waited = {e: {} for e in ENGS}
        self.cur = stack

    def sb(self, shape, dt, name=None, persist=False):
        self.nbuf += 1
        return (self.stack if persist else self.cur).enter_context(self.nc.sbuf_tensor(name or f"sb{self.nbuf}", list(shape), dt))

    def ps(self, shape, dt, name=None):
        self.nbuf += 1
        return self.stack.enter_context(self.nc.psum_tensor(name or f"ps{self.nbuf}", list(shape), dt))

    def op(self, eng, fn, reads=(), writes=(), accw=(), dma=None):
        i = len(self.ops)
        deps = set()
        for t in reads:
            deps.update(self.last_w.get(t, ()))
        for t in writes:
            deps.update(self.last_w.get(t, ()))
            deps.update(self.readers.get(t, ()))
        for t in accw:
            deps.update(self.readers.get(t, ()))
        for t in reads:
            self.readers.setdefault(t, []).append(i)
        for t in writes:
            self.last_w[t] = [i]
            self.readers[t] = []
        for t in accw:
            if self.readers.get(t):
                self.last_w[t] = []
                self.readers[t] = []
            self.last_w.setdefault(t, []).append(i)
        deps.discard(i)
        self.ops.append(dict(eng=eng, fn=fn, deps=sorted(deps), dma=dma, sig=None))
        return i

    def flush(self):
        nc = self.nc
        ops = self.ops
        lo = self.flushed
        last = {}
        for i in range(lo, len(ops)):
            o = ops[i]
            key = ("dma", o["dma"]) if o["dma"] is not None else ("eng", o["eng"])
            last[key] = i
        bdeps = sorted(last.values())
        for en in ENGS:
            self.ops.append(dict(eng=en, fn=lambda e: e.nop(), deps=list(bdeps), dma=None, sig=None, barrier=True))
        hi = len(ops)

        def pe_pair(a, b):
            return (a["eng"] == "pe" and b["eng"] == "pe" and a["dma"] is None and b["dma"] is None
                    and not b.get("barrier"))

        needed = set()
        for i in range(lo, hi):
            o = ops[i]
            for d in o["deps"]:
                if pe_pair(ops[d], o):
                    continue
                needed.add(d)
        for i in range(lo, hi):
            o = ops[i]
            if o["dma"] is not None:
                k = ("dma", o["dma"])
                self.dma_count[k] = self.dma_count.get(k, 0) + 1
                o["sig"] = (k, 16 * self.dma_count[k])
                self.get_sem(k)
            elif i in needed:
                e = o["eng"]
                n = self.eng_seq[e]
                self.eng_seq[e] += 1
                k = ("eng", e, n // SEM_CH)
                o["sig"] = (k, n % SEM_CH + 1)
                self.get_sem(k)
        sems = self.sems
        waited_all = self.waited

        def run(engname):
            def body(e):
                waited = waited_all[engname]
                for i in range(lo, hi):
                    o = ops[i]
                    if o["eng"] != engname:
                        continue
                    for d in o["deps"]:
                        od = ops[d]
                        if od["sig"] is None or pe_pair(od, o):
                            continue
                        k, v = od["sig"]
                        if waited.get(k, 0) >= v:
                            continue
                        e.wait_ge(sems[k], v)
                        waited[k] = v
                    ins = o["fn"](e)
                    if o["sig"] is not None:
                        k, v = o["sig"]
                        ins.then_inc(sems[k], 16 if o["dma"] is not None else 1)
            return body

        with nc.Block() as block:
            block.tensor(run("pe"))
            block.scalar(run("act"))
            block.vector(run("dve"))
            block.gpsimd(run("pool"))
            block.sync(run("sp"))
        self.flushed = hi
        self.last_w = {}
        self.readers = {}

    def get_sem(self, key):
        if key not in self.sems:
            self.sems[key] = self.stack.enter_context(self.nc.semaphore(f"s_{len(self.sems)}"))
        return self.sems[key]


def build(NST=8, NPRE=8, dbg=None):
    nc = bass.Bass("TRN2", target_bir_lowering=False)
    NT = NST * 4
    TOK = NST * 512

    def din(name, shape, dt=F32):
        return nc.dram_tensor(name, list(shape), dt, kind="ExternalInput")

    x_main = din("x_main", [TOK, D])
    x_pre = din("x_pre", [max(NPRE, 1) * 512, D])
    norm1_g = din("norm1_g", [D])
    w_in = din("w_in", [D, 3080])
    conv_qk = din("conv_qk", [4, 1024])
    b_if = din("b_if", [8])
    g_mlstm = din("g_mlstm_out", [512])
    g_gv = din("g_gmlp_v", [512])
    w_sp = din("w_spatial", [8, 128, 128])
    b_sp = din("b_spatial", [8, 128])
    g_go = din("g_gmlp_out", [512])
    w_out = din("w_out", [D, D])
    norm2_g = din("norm2_g", [D])
    w_r = din("w_router", [D, 36])
    b_r = din("b_router", [36])
    wcat = din("wcat", [NE * 128, 12288])
    final_g = din("final_g", [D])
    out = nc.dram_tensor("out", [TOK, D], F32, kind="ExternalOutput")
    h1buf = nc.dram_tensor("h1buf", [TOK, D], F32)
    dbg_t = {}
    if dbg:
        for k, shp in dbg.items():
            dbg_t[k] = nc.dram_tensor("dbg_" + k, list(shp), F32, kind="ExternalOutput")

    with contextlib.ExitStack() as st:
        S = Sched(nc, st)
        finals = []

        ident_f = S.sb([128, 128], F32, "ident_f")
        ident_b = S.sb([128, 128], BF16, "ident_b")
        triu_f = S.sb([128, 128], F32, "triu_f")
        triu_b = S.sb([128, 128], BF16, "triu_b")
        tril_f = S.sb([128, 128], F32, "tril_f")
        ones_f = S.sb([128, 128], F32, "ones_f")
        S.op("pool", lambda e: e.memset(ident_f[:], 0.0), writes=["ident_f"])
        S.op("pool", lambda e: e.affine_select(out=ident_f[:], in_=ident_f[:], pattern=[[-1, 128]],
                                               compare_op=ALU.not_equal, fill=1.0, base=0, channel_multiplier=1),
             reads=["ident_f"], writes=["ident_f"])
        S.op("pool", lambda e: e.memset(ones_f[:], 1.0), writes=["ones_f"])
        S.op("pool", lambda e: e.affine_select(out=triu_f[:], in_=ones_f[:], pattern=[[1, 128]],
                                               compare_op=ALU.is_ge, fill=0.0, base=0, channel_multiplier=-1),
             reads=["ones_f"], writes=["triu_f"])
        S.op("pool", lambda e: e.affine_select(out=tril_f[:], in_=ones_f[:], pattern=[[-1, 128]],
                                               compare_op=ALU.is_ge, fill=0.0, base=0, channel_multiplier=1),
             reads=["ones_f"], writes=["tril_f"])
        S.op("dve", lambda e: e.tensor_copy(out=ident_b[:], in_=ident_f[:]), reads=["ident_f"], writes=["ident_b"])
        S.op("dve", lambda e: e.tensor_copy(out=triu_b[:], in_=triu_f[:]), reads=["triu_f"], writes=["triu_b"])

        def bload(name, src, n, eng="sp"):
            t = S.sb([128, n], F32, name)
            S.op(eng, lambda e: e.dma_start(out=t[:], in_=bass.AP(src, 0, [[0, 128], [1, n]])),
                 writes=[name], dma=name)
            return t

        gml_b = bload("gml_b", g_mlstm, 512)
        ggv_b = bload("ggv_b", g_gv, 512)
        ggo_b = bload("ggo_b", g_go, 512)
        bif_b = bload("bif_b", b_if, 8)

        g1col = S.sb([128, 8], F32, "g1col")
        cw = S.sb([128, 4, 8], F32, "cw")
        bsp = S.sb([128, 8], F32, "bsp")
        S.op("sp", lambda e: e.dma_start(out=g1col[:], in_=norm1_g.ap().rearrange("(c p) -> p c", p=128),
                                         allow_slow_non_contiguous=True), writes=["g1col"], dma="g1col")
        for i in range(4):
            S.op("sp", lambda e, i=i: e.dma_start(out=cw[:, i, :], in_=conv_qk.ap()[i, :].rearrange("(c p) -> p c", p=128),
                                                  allow_slow_non_contiguous=True), accw=["cw"], dma=("cw", i))
        S.op("sp", lambda e: e.dma_start(out=bsp[:], in_=b_sp.ap().rearrange("g t -> t g"),
                                         allow_slow_non_contiguous=True), writes=["bsp"], dma="bsp")

        ptr = [S.ps([128, 1024], BF16, f"ptr{i}") for i in range(2)]
        pfb = [S.ps([128, 512], F32, f"pf{i}") for i in range(6)]
        rr = {"t": 0, "f": 0, "A": 0, "B": 0}

        def tbank():
            i = rr["t"] % 2
            rr["t"] += 1
            return ptr[i], ("ptr", i)

        def fbank():
            i = rr["f"] % 6
            rr["f"] += 1
            return pfb[i], ("pf", i)

        def fbankA():
            i = rr["A"] % 3
            rr["A"] += 1
            return pfb[i], ("pf", i)

        def fbankB():
            i = 3 + rr["B"] % 2
            rr["B"] += 1
            return pfb[i], ("pf", i)

        def fbankC():
            return pfb[5], ("pf", 5)

        junk = S.sb([128, 1024], BF16, "junk", persist=True)
        base_b = S.sb([128, 32], F32, "base_b", persist=True)
        E1s = S.sb([128, NT, 32], BF16, "E1s", persist=True)
        E2s = S.sb([128, NT, 32], BF16, "E2s", persist=True)
        pw = S.sb([128, NT, 4], F32, "pw", persist=True)
        stA = contextlib.ExitStack()
        S.cur = stA
        xbuf = [S.sb([128, 4, 1024], F32, f"xbuf{i}") for i in range(2)]
        wqk = S.sb([128, 8, 1024], BF16, "wqk")
        wvo = S.sb([128, 8, 1024], BF16, "wvo")
        wuv = S.sb([128, 8, 1024], BF16, "wuv")
        wif = S.sb([128, 8, 8], BF16, "wif")
        wout = S.sb([128, 8, 1024], BF16, "wout")
        for kc in range(8):
            sl = kc % 2
            stg = xbuf[sl][:].rearrange("p a b -> p (a b)")
            S.op("sp", lambda e, stg=stg, kc=kc: e.dma_start(out=stg[:, 0:3080], in_=w_in.ap()[kc * 128:(kc + 1) * 128, :]),
                 writes=[("x", sl)], dma=("x", sl))
            for (dst, c0, n, tok) in ((wqk, 0, 1024, "wqk"), (wvo, 1024, 1024, "wvo"), (wif, 2048, 8, "wif"), (wuv, 2056, 1024, "wuv")):
                S.op("act", lambda e, dst=dst, c0=c0, n=n, kc=kc, stg=stg: e.mul(dst[:, kc, 0:n], stg[:, c0:c0 + n], g1col[:, kc:kc + 1]),
                     reads=[("x", sl), "g1col"], accw=[tok])
        S.op("pool", lambda e: e.dma_start(out=wout[:], in_=w_out.ap().rearrange("(c p) n -> p c n", p=128)),
             writes=["wout"], dma="wout")

        wsp_f = xbuf[1][:, 3, :].rearrange("p (g s) -> p g s", g=8)
        wspT = S.sb([128, 8, 128], BF16, "wspT")
        S.op("sp", lambda e: e.dma_start(out=wsp_f, in_=w_sp.ap().rearrange("g t s -> t g s")), writes=[("x", 1)], dma=("x", 1))
        S.op("dve", lambda e: e.tensor_tensor(out=wsp_f, in0=wsp_f, in1=tril_f[:].unsqueeze(1).broadcast_to([128, 8, 128]), op=ALU.mult),
             reads=[("x", 1), "tril_f"], writes=[("x", 1)])
        for half in range(2):
            pb, pt = fbank()
            for g4 in range(4):
                g = half * 4 + g4
                S.op("pe", lambda e, pb=pb, g=g, g4=g4: e.transpose(out=pb[:, g4 * 128:(g4 + 1) * 128], in_=wsp_f[:, g, :], identity=ident_f[:]),
                     reads=[("x", 1), "ident_f"], accw=[pt])
            S.op("dve", lambda e, pb=pb, half=half: e.tensor_copy(out=wspT[:, half * 4:(half + 1) * 4, :].rearrange("p a b -> p (a b)"), in_=pb[:]),
                 reads=[pt], accw=["wspT"])

        C32 = S.sb([128, 4, 129], F32, "C32")
        Csb = S.sb([128, 4, 129], BF16, "Csb")
        m_st = S.sb([4, 1], F32, "m_st")
        halo = S.sb([128, 8, 3], F32, "halo")
        S.op("pool", lambda e: e.memset(C32[:], 0.0), writes=[("C32", h) for h in range(4)])
        S.op("pool", lambda e: e.memset(m_st[:], 0.0), writes=["m_st"])
        S.op("pool", lambda e: e.memset(halo[:], 0.0), writes=[("halo", c) for c in range(8)])

        xn = [S.sb([128, 1024], BF16, f"xn{i}") for i in range(2)]
        ss1 = S.sb([128, 8], F32, "ss1")
        xT = [S.sb([128, 8, 512], BF16, f"xT{i}") for i in range(1)]
        pre = [S.sb([128, 515], F32, f"pre{i}") for i in range(2)]
        cacc = [S.sb([128, 512], F32, f"cacc{i}") for i in range(2)]
        sg = [S.sb([128, 512], F32, f"sg{i}") for i in range(2)]
        qkT = [S.sb([128, 8, 512], BF16, f"qkT{i}") for i in range(1)]
        ktm = S.sb([128, 4, 128], BF16, "ktm")
        gsb = S.sb([128, 4, 8], F32, "gsb")
        spl = S.sb([128, 4, 4], F32, "spl")
        a_tm = S.sb([128, 4, 4], F32, "a_tm")
        cum_sb = S.sb([128, 4, 4], F32, "cum_sb")
        e_tm = S.sb([128, 4, 4], F32, "e_tm")
        dn_tm = S.sb([128, 4, 4], F32, "dn_tm")
        gb_sb = S.sb([128, 4, 8], F32, "gb_sb")
        rw = S.sb([4, 20], F32, "rw")
        rhs8 = S.sb([4, 4, 8], F32, "rhs8")
        lnc0_t = S.sb([128, 1], F32, "lnc0_t")
        S.op("pool", lambda e: e.memset(lnc0_t[:], -float(np.log(128 ** -0.5))), writes=["lnc0"])
        ve = [S.sb([128, 4, 129], BF16, f"ve{i}") for i in range(2)]
        smask = [S.sb([128, 4, 128], BF16, f"smask{i}") for i in range(2)]
        og = S.sb([128, 512], F32, "og")
        hs = S.sb([128, 4, 128], F32, "hs")
        nqa = S.sb([128, 4], F32, "nqa")
        sm8 = S.sb([128, 16], F32, "sm8")
        tmpa = S.sb([128, 1024], F32, "tmpa")
        ux = S.sb([128, 1024], F32, "ux")
        t1 = S.sb([128, 1024], F32, "t1")
        vn = S.sb([128, 512], BF16, "vn")
        gt = S.sb([128, 8, 64], F32, "gt")
        ybuf = [S.sb([128, 1024], BF16, f"ybuf{i}") for i in range(2)]
        yT = [S.sb([128, 8, 128], BF16, f"yT{i}") for i in range(2)]
        h1b = [S.sb([128, 1024], F32, f"h1b{i}") for i in range(2)]
        g2_b = bload("g2_b", norm2_g, 1024)
        br_b = bload("br_b", b_r, 36)
        wr_f = S.sb([128, 8, 36], F32, "wr_f")
        wr_b = S.sb([128, 8, 36], BF16, "wr_b")
        S.op("sp", lambda e: e.dma_start(out=wr_f[:], in_=w_r.ap().rearrange("(c p) n -> p c n", p=128)), writes=["wr_f"], dma="wr_f")
        S.op("dve", lambda e: e.tensor_copy(out=wr_b[:], in_=wr_f[:]), reads=["wr_f"], writes=["wr_b"])
        lstr_b = S.sb([128, 128], BF16, "lstr_b")
        ones_b = S.sb([128, 128], BF16, "ones_b")
        lstr_f = S.sb([128, 128], F32, "lstr_f")
        S.op("pool", lambda e: e.affine_select(out=lstr_f[:], in_=ones_f[:], pattern=[[1, 128]], compare_op=ALU.is_gt, fill=0.0, base=0, channel_multiplier=-1),
             reads=["ones_f"], writes=["lstr_f"])
        S.op("dve", lambda e: e.tensor_copy(out=lstr_b[:], in_=lstr_f[:]), reads=["lstr_f"], writes=["lstr_b"])
        S.op("dve", lambda e: e.tensor_copy(out=ones_b[:], in_=ones_f[:]), reads=["ones_f"], writes=["ones_b"])
        xn2t = [S.sb([128, 1024], BF16, f"xn2t{i}") for i in range(2)]
        xn2T = [S.sb([128, 8, 128], BF16, f"xn2T{i}") for i in range(1)]
        lg = S.sb([128, 36], F32, "lg")
        rt = S.sb([128, 64], F32, "rt")
        t32 = S.sb([128, 4, 8], F32, "t32")
        le8 = S.sb([128, 16], F32, "le8")
        ind_b = S.sb([128, 32], BF16, "ind_b")
        posf = S.sb([128, 32], F32, "posf")
        S.op("pool", lambda e: e.memset(base_b[:], 0.0), writes=["base_b"])
        xn2lin = nc.dram_tensor("xn2lin", [TOK, D], BF16)
        cnt = {"tile": 0, "st": 0, "ch": 0, "tl2": 0, "y": 0}

        LN_C0 = float(np.log(128 ** -0.5))

        def dump(name, ap_fn, reads, row0=0):
            if name in dbg_t:
                t = dbg_t[name]
                finals.append(S.op("sp", lambda e: e.dma_start(out=t.ap()[row0:row0 + 128, :], in_=ap_fn()), reads=reads, dma=("dbg", name)))

        def supertile(xsrc, st_i, mode, gi):
            main = mode == "main"
            sl = cnt["st"] % 2
            cnt["st"] += 1
            xb = xbuf[sl]
            xt = xT[0]
            qk = qkT[0]
            S.op("sp", lambda e: e.dma_start(out=xb[:], in_=xsrc.ap()[st_i * 512:(st_i + 1) * 512, :].rearrange("(j p) d -> p j d", p=128)),
                 writes=[("x", sl)], dma=("x", sl))
            conv_some(gi, sl)
            for j in range(4):
                tl = cnt["tile"] % 2
                cnt["tile"] += 1
                xnj = xn[tl]
                S.op("dve", lambda e, j=j: e.memset(ss1[:, j:j + 1], 0.0), writes=[("ss1", j)])
                S.op("act", lambda e, j=j: e.activation(out=junk[:], in_=xb[:, j, :], func=AF.Square, accum_out=ss1[:, j:j + 1]),
                     reads=[("x", sl), ("ss1", j)], writes=["junk", ("ss1", j)])
                S.op("act", lambda e, j=j: e.activation(out=ss1[:, 4 + j:5 + j], in_=ss1[:, j:j + 1], func=AF.Ln, scale=1.0 / D, bias=EPS),
                     reads=[("ss1", j)], writes=[("ss1", 4 + j)])
                S.op("act", lambda e, j=j: e.activation(out=ss1[:, 4 + j:5 + j], in_=ss1[:, 4 + j:5 + j], func=AF.Exp, scale=-0.5),
                     reads=[("ss1", 4 + j)], writes=[("ss1", 4 + j)])
                S.op("act", lambda e, j=j, xnj=xnj: e.mul(xnj[:], xb[:, j, :], ss1[:, 4 + j:5 + j]),
                     reads=[("x", sl), ("ss1", 4 + j)], writes=[("xn", tl)])
                pb, pt = tbank()
                for c in range(8):
                    S.op("pe", lambda e, c=c, pb=pb, xnj=xnj: e.transpose(out=pb[:, c * 128:(c + 1) * 128], in_=xnj[:, c * 128:(c + 1) * 128], identity=ident_b[:]),
                         reads=[("xn", tl), "ident_b"], accw=[pt])
                S.op("dve" if j % 2 == 0 else "act",
                     (lambda e, pb=pb, j=j: e.tensor_copy(out=xt[:, :, j * 128:(j + 1) * 128], in_=pb[:].rearrange("p (c t) -> p c t", c=8))) if j % 2 == 0 else
                     (lambda e, pb=pb, j=j: e.copy(out=xt[:, :, j * 128:(j + 1) * 128], in_=pb[:].rearrange("p (c t) -> p c t", c=8))),
                     reads=[pt], accw=[("xT", 0)])
            chunks = range(8) if mode != "pre" else range(4, 8)
            def chunk_s1(ch):
                pb, pt = fbank()
                for kc in range(8):
                    S.op("pe", lambda e, kc=kc, ch=ch, pb=pb: e.matmul(out=pb[:], lhsT=wqk[:, kc, ch * 128:(ch + 1) * 128], rhs=xt[:, kc, :], start=(kc == 0), stop=(kc == 7)),
                         reads=[("xT", 0), "wqk"], accw=[pt])
                ps_ = cnt["ch"] % 2
                cnt["ch"] += 1
                pr = pre[ps_]
                ca = cacc[ps_]
                s_ = sg[ps_]
                S.op("dve", lambda e, pr=pr, ch=ch: e.tensor_copy(out=pr[:, 0:3], in_=halo[:, ch, :]), reads=[("halo", ch)], writes=[("pre", ps_, "h")])
                S.op("act", lambda e, pr=pr, pb=pb: e.copy(out=pr[:, 3:515], in_=pb[:]), reads=[pt], writes=[("pre", ps_)])
                S.op("dve", lambda e, pr=pr, ch=ch: e.tensor_copy(out=halo[:, ch, :], in_=pr[:, 512:515]), reads=[("pre", ps_), ("pre", ps_, "h")], writes=[("halo", ch)])
                S.op("dve", lambda e, pr=pr, ca=ca, ch=ch: e.tensor_scalar(out=ca[:], in0=pr[:, 0:512], scalar1=cw[:, 0, ch:ch + 1], scalar2=None, op0=ALU.mult),
                     reads=[("pre", ps_), ("pre", ps_, "h"), "cw"], writes=[("cacc", ps_)])
                for i in range(1, 4):
                    S.op("dve", lambda e, pr=pr, ca=ca, ch=ch, i=i: e.scalar_tensor_tensor(out=ca[:], in0=pr[:, i:i + 512], scalar=cw[:, i, ch:ch + 1], in1=ca[:], op0=ALU.mult, op1=ALU.add),
                         reads=[("pre", ps_), ("pre", ps_, "h"), ("cacc", ps_), "cw"], writes=[("cacc", ps_)])
                return (ch, ps_, ca, s_)

            def chunk_s2(args):
                ch, ps_, ca, s_ = args
                S.op("act", lambda e, ca=ca, s_=s_: e.activation(out=s_[:], in_=ca[:], func=AF.Exp, scale=-1.0), reads=[("cacc", ps_)], writes=[("sg", ps_)])
                S.op("act", lambda e, s_=s_: e.activation(out=s_[:], in_=s_[:], func=AF.Ln, bias=1.0), reads=[("sg", ps_)], writes=[("sg", ps_)])
                S.op("act", lambda e, s_=s_: e.activation(out=s_[:], in_=s_[:], func=AF.Exp, scale=-1.0), reads=[("sg", ps_)], writes=[("sg", ps_)])
                S.op("dve", lambda e, s_=s_, ca=ca, ch=ch: e.tensor_tensor(out=qk[:, ch, :], in0=s_[:], in1=ca[:], op=ALU.mult),
                     reads=[("sg", ps_), ("cacc", ps_)], accw=[("qkT", 0)])


            pend = None
            for ch in chunks:
                cur = chunk_s1(ch)
                if pend is not None:
                    chunk_s2(pend)
                pend = cur
            chunk_s2(pend)
            pg, pgt = fbank()
            for j in range(4):
                for kc in range(8):
                    S.op("pe", lambda e, j=j, kc=kc: e.matmul(out=pg[:, j * 8:(j + 1) * 8], lhsT=xt[:, kc, j * 128:(j + 1) * 128], rhs=wif[:, kc, :], start=(kc == 0), stop=(kc == 7)),
                         reads=[("xT", 0), "wif"], accw=[pgt])
            S.op("dve", lambda e: e.tensor_tensor(out=gsb[:], in0=pg[:, 0:32].rearrange("p (c g) -> p c g", c=4), in1=bif_b[:].unsqueeze(1).broadcast_to([128, 4, 8]), op=ALU.add),
                 reads=[pgt, "bif_b"], writes=["gsb"])
            S.op("act", lambda e: e.activation(out=spl[:], in_=gsb[:, :, 4:8], func=AF.Exp, scale=-1.0), reads=["gsb"], writes=["spl"])
            S.op("act", lambda e: e.activation(out=spl[:], in_=spl[:], func=AF.Ln, bias=1.0), reads=["spl"], writes=["spl"])
            pc, pct = fbank()
            prw, prt = fbank()
            for c in range(4):
                S.op("pe", lambda e, c=c: e.matmul(out=pc[:, c * 4:(c + 1) * 4], lhsT=triu_f[:], rhs=spl[:, c, :], start=True, stop=True),
                     reads=["spl", "triu_f"], accw=[pct])
                S.op("pe", lambda e, c=c: e.matmul(out=pc[0:4, 16 + c:17 + c], lhsT=spl[:, c, :], rhs=ones_f[:, 0:1], start=True, stop=True),
                     reads=["spl", "ones_f"], accw=[pct])
                S.op("pe", lambda e, c=c: e.matmul(out=prw[0:4, c * 128:(c + 1) * 128], lhsT=gsb[:, c, 0:4], rhs=ident_f[:], start=True, stop=False),
                     reads=["gsb", "ident_f"], accw=[prt])
                S.op("pe", lambda e, c=c: e.matmul(out=prw[0:4, c * 128:(c + 1) * 128], lhsT=spl[:, c, :], rhs=triu_f[:], start=False, stop=True),
                     reads=["spl", "triu_f"], accw=[prt])
            S.op("dve", lambda e: e.tensor_tensor(out=a_tm[:], in0=gsb[:, :, 0:4], in1=pc[:, 0:16].rearrange("p (c h) -> p c h", c=4), op=ALU.add),
                 reads=["gsb", pct], writes=["a_tm"])
            S.op("dve", lambda e: e.tensor_copy(out=cum_sb[:], in_=pc[:, 0:16].rearrange("p (c h) -> p c h", c=4)), reads=[pct], writes=["cum_sb"])
            S.op("dve", lambda e: e.tensor_copy(out=rw[:, 4:8], in_=pc[0:4, 16:20]), reads=[pct], writes=["rw_tot"])
            S.op("dve", lambda e: e.tensor_reduce(out=rw[:, 0:4], in_=prw[0:4, :].rearrange("p (c l) -> p c l", c=4), axis=AX.X, op=ALU.max),
                 reads=[prt], writes=["rw_A"])
            for c in range(4):
                S.op("dve", lambda e, c=c: e.tensor_copy(out=rw[:, 12 + c:13 + c], in_=m_st[:]), reads=["m_st"], writes=[("rw_mp", c)])
                S.op("dve", lambda e, c=c: e.tensor_tensor(out=rw[:, 8 + c:9 + c], in0=m_st[:], in1=rw[:, c:c + 1], op=ALU.max), reads=["m_st", "rw_A"], writes=[("rw_G", c)])
                S.op("dve", lambda e, c=c: e.tensor_tensor(out=m_st[:], in0=rw[:, 8 + c:9 + c], in1=rw[:, 4 + c:5 + c], op=ALU.subtract), reads=[("rw_G", c), "rw_tot"], writes=["m_st"])
            S.op("dve", lambda e: e.tensor_tensor(out=rw[:, 16:20], in0=rw[:, 12:16], in1=rw[:, 8:12], op=ALU.subtract),
                 reads=[("rw_G", c) for c in range(4)] + [("rw_mp", c) for c in range(4)], writes=["rw_D"])
            S.op("act", lambda e: e.activation(out=rw[:, 16:20], in_=rw[:, 16:20], func=AF.Exp), reads=["rw_D"], writes=["rw_D"])
            S.op("dve", lambda e: e.tensor_tensor(out=rhs8[:, :, 0:4], in0=ident_f[0:4, 0:4].unsqueeze(1).broadcast_to([4, 4, 4]), in1=rw[:, 8:12].unsqueeze(2).broadcast_to([4, 4, 4]), op=ALU.mult),
                 reads=[("rw_G", c) for c in range(4)] + ["ident_f"], writes=["rhs8a"])
            S.op("dve", lambda e: e.tensor_tensor(out=rhs8[:, :, 4:8], in0=ident_f[0:4, 0:4].unsqueeze(1).broadcast_to([4, 4, 4]), in1=rw[:, 16:20].unsqueeze(2).broadcast_to([4, 4, 4]), op=ALU.mult),
                 reads=["rw_D", "ident_f"], writes=["rhs8b"])
            S.op("pe", lambda e: e.matmul(out=pc[:, 32:64], lhsT=ones_f[0:4, :], rhs=rhs8[:].rearrange("p c g -> p (c g)"), start=True, stop=True),
                 reads=["rhs8a", "rhs8b", "ones_f"], accw=[pct])
            S.op("dve", lambda e: e.tensor_copy(out=gb_sb[:], in_=pc[:, 32:64].rearrange("p (c g) -> p c g", c=4)), reads=[pct], writes=["gb_sb"])
            S.op("dve", lambda e: e.tensor_tensor(out=e_tm[:], in0=a_tm[:], in1=gb_sb[:, :, 0:4], op=ALU.subtract), reads=["a_tm", "gb_sb"], writes=["e_tm"])
            S.op("act", lambda e: e.activation(out=e_tm[:], in_=e_tm[:], func=AF.Exp), reads=["e_tm"], writes=["e_tm"])
            S.op("dve", lambda e: e.tensor_tensor(out=dn_tm[:], in0=cum_sb[:], in1=gb_sb[:, :, 0:4], op=ALU.subtract), reads=["cum_sb", "gb_sb"], writes=["dn_tm"])
            S.op("act", lambda e: e.activation(out=dn_tm[:], in_=dn_tm[:], func=AF.Exp, bias=lnc0_t[:, 0:1]), reads=["dn_tm", "lnc0"], writes=["dn_tm"])

            def tile_body(j):
                c = j
                csl = slice(c * 128, (c + 1) * 128)
                tsl = cnt["tl2"] % 2
                cnt["tl2"] += 1
                vea = ve[tsl]
                ysl = cnt["y"] % 2
                if main:
                    cnt["y"] += 1
                yt_ = ybuf[ysl]
                def chainA():
                    pv, pvt = fbankA()
                    for kc in range(8):
                        yield S.op("pe", lambda e, kc=kc, pv=pv: e.matmul(out=pv[:], lhsT=xt[:, kc, csl], rhs=wvo[:, kc, 0:512], start=(kc == 0), stop=(kc == 7)),
                             reads=[("xT", 0), "wvo"], accw=[pvt])
                    yield S.op("dve", lambda e, pv=pv, vea=vea: e.tensor_tensor(out=vea[:, :, 0:128], in0=pv[:].rearrange("p (h d) -> p h d", h=4), in1=e_tm[:, c, :].unsqueeze(2).broadcast_to([128, 4, 128]), op=ALU.mult),
                         reads=[pvt, "e_tm"], writes=[("ve", tsl)])
                    yield S.op("dve", lambda e, vea=vea: e.tensor_copy(out=vea[:, :, 128:129], in_=e_tm[:, c, :].unsqueeze(2)), reads=["e_tm"], writes=[("ve", tsl, "e")])
                    pb, pt = (ptr[0], ("ptr", 0))
                    for h in range(4):
                        yield S.op("pe", lambda e, h=h, pb=pb: e.transpose(out=pb[:, h * 128:(h + 1) * 128], in_=qk[:, 4 + h, csl], identity=ident_b[:]),
                             reads=[("qkT", 0), "ident_b"], accw=[pt])
                    yield S.op("act", lambda e, pb=pb: e.copy(out=ktm[:].rearrange("p h d -> p (h d)"), in_=pb[:, 0:512]), reads=[pt], writes=["ktm"])
                    if main:
                        po, pot = fbankA()
                        for kc in range(8):
                            yield S.op("pe", lambda e, kc=kc, po=po: e.matmul(out=po[:], lhsT=xt[:, kc, csl], rhs=wvo[:, kc, 512:1024], start=(kc == 0), stop=(kc == 7)),
                                 reads=[("xT", 0), "wvo"], accw=[pot])
                        yield S.op("act", lambda e, po=po: e.activation(out=og[:], in_=po[:], func=AF.Exp, scale=-1.0), reads=[pot], writes=["og"])
                        yield S.op("act", lambda e: e.activation(out=og[:], in_=og[:], func=AF.Ln, bias=1.0), reads=["og"], writes=["og"])
                        yield S.op("act", lambda e: e.activation(out=og[:], in_=og[:], func=AF.Exp, scale=-1.0), reads=["og"], writes=["og"])
                        pS, pSt = fbankA()
                        for h in range(4):
                            yield S.op("pe", lambda e, h=h, pS=pS: e.matmul(out=pS[:, h * 128:(h + 1) * 128], lhsT=qk[:, 4 + h, csl], rhs=qk[:, h, csl], start=True, stop=True),
                                 reads=[("qkT", 0)], accw=[pSt])
                        sm = smask[tsl]
                        yield S.op("dve", lambda e, pS=pS, sm=sm: e.tensor_tensor(out=sm[:], in0=pS[:].rearrange("p (h l) -> p h l", h=4), in1=triu_f[:].unsqueeze(1).broadcast_to([128, 4, 128]), op=ALU.mult),
                             reads=[pSt, "triu_f"], writes=[("smask", tsl)])
                        for h in range(4):
                            yield S.op("act", lambda e, h=h: e.mul(Csb[:, h, :], C32[:, h, :], gb_sb[:, c, 4 + h:5 + h]), reads=[("C32", h), "gb_sb"], writes=[("Csb", h)])
                        nbanks = []
                        for hp in range(2):
                            pn, pnt = fbankA()
                            nbanks.append((pn, pnt))
                            for hh in range(2):
                                h = hp * 2 + hh
                                yield S.op("pe", lambda e, h=h, hh=hh, pn=pn, sm=sm, vea=vea: e.matmul(out=pn[:, hh * 129:(hh + 1) * 129], lhsT=sm[:, h, :], rhs=vea[:, h, :], start=True, stop=False),
                                     reads=[("smask", tsl), ("ve", tsl), ("ve", tsl, "e")], accw=[pnt])
                                yield S.op("pe", lambda e, h=h, hh=hh, pn=pn: e.matmul(out=pn[:, hh * 129:(hh + 1) * 129], lhsT=qk[:, h, csl], rhs=Csb[:, h, :], start=False, stop=True),
                                     reads=[("qkT", 0), ("Csb", h)], accw=[pnt])
                        for hp in range(2):
                            pn, pnt = nbanks[hp]
                            yield S.op("act", lambda e, pn=pn, hp=hp: e.activation(out=nqa[:, hp * 2:hp * 2 + 2].unsqueeze(2), in_=pn[:, 0:258].rearrange("p (h d) -> p h d", h=2)[:, :, 128:129], func=AF.Abs),
                                 reads=[pnt], writes=["nqa"])
                        yield S.op("dve", lambda e: e.tensor_tensor(out=nqa[:], in0=nqa[:], in1=dn_tm[:, c, :], op=ALU.max), reads=["nqa", "dn_tm"], writes=["nqa"])
                        yield S.op("dve", lambda e: e.reciprocal(out=nqa[:], in_=nqa[:]), reads=["nqa"], writes=["nqa"])
                        for hp in range(2):
                            pn, pnt = nbanks[hp]
                            yield S.op("dve", lambda e, pn=pn, hp=hp: e.tensor_tensor(out=hs[:, hp * 2:hp * 2 + 2, :], in0=pn[:, 0:258].rearrange("p (h d) -> p h d", h=2)[:, :, 0:128], in1=nqa[:, hp * 2:hp * 2 + 2].unsqueeze(2).broadcast_to([128, 2, 128]), op=ALU.mult),
                                 reads=[pnt, "nqa"], writes=["hs"])
                        yield S.op("dve", lambda e: e.tensor_tensor(out=hs[:].rearrange("p h d -> p (h d)"), in0=hs[:].rearrange("p h d -> p (h d)"), in1=og[:], op=ALU.mult),
                             reads=["hs", "og"], writes=["hs"])
                        yield S.op("dve", lambda e: e.tensor_tensor(out=tmpa[:, 0:512], in0=hs[:].rearrange("p h d -> p (h d)"), in1=hs[:].rearrange("p h d -> p (h d)"), op=ALU.mult), reads=["hs"], writes=["tmpa"])
                        yield S.op("dve", lambda e: e.tensor_reduce(out=sm8[:, 0:4], in_=tmpa[:, 0:512].rearrange("p (h d) -> p h d", h=4), axis=AX.X, op=ALU.add), reads=["tmpa"], writes=["sm8a"])
                        yield S.op("act", lambda e: e.activation(out=sm8[:, 0:4], in_=sm8[:, 0:4], func=AF.Ln, scale=1.0 / 128, bias=EPS), reads=["sm8a"], writes=["sm8a"])
                        yield S.op("act", lambda e: e.activation(out=sm8[:, 0:4], in_=sm8[:, 0:4], func=AF.Exp, scale=-0.5), reads=["sm8a"], writes=["sm8a"])
                        yield S.op("dve", lambda e: e.tensor_tensor(out=hs[:], in0=hs[:], in1=sm8[:, 0:4].unsqueeze(2).broadcast_to([128, 4, 128]), op=ALU.mult), reads=["hs", "sm8a"], writes=["hs"])
                        yield S.op("dve", lambda e, yt_=yt_: e.tensor_tensor(out=yt_[:, 0:512], in0=hs[:].rearrange("p h d -> p (h d)"), in1=gml_b[:], op=ALU.mult), reads=["hs", "gml_b"], writes=[("y", ysl, 0)])
                    for hp in range(2):
                        pu_, put = fbankA()
                        for hh in range(2):
                            h = hp * 2 + hh
                            yield S.op("pe", lambda e, h=h, hh=hh, pu_=pu_, vea=vea: e.matmul(out=pu_[:, hh * 129:(hh + 1) * 129], lhsT=ktm[:, h, :], rhs=vea[:, h, :], start=True, stop=True),
                                 reads=["ktm", ("ve", tsl), ("ve", tsl, "e")], accw=[put])
                        for hh in range(2):
                            h = hp * 2 + hh
                            yield S.op("dve", lambda e, h=h, hh=hh, pu_=pu_: e.scalar_tensor_tensor(out=C32[:, h, :], in0=C32[:, h, :], scalar=gb_sb[:, c, 4 + h:5 + h], in1=pu_[:, hh * 129:(hh + 1) * 129], op0=ALU.mult, op1=ALU.add),
                                 reads=[put, "gb_sb", ("C32", h)], writes=[("C32", h)])

                    yield None
                def chainB():
                    pU, pUt = fbankB()
                    pV, pVt = fbankB()
                    for kc in range(8):
                        yield S.op("pe", lambda e, kc=kc, pU=pU: e.matmul(out=pU[:], lhsT=xt[:, kc, csl], rhs=wuv[:, kc, 0:512], start=(kc == 0), stop=(kc == 7)),
                             reads=[("xT", 0), "wuv"], accw=[pUt])
                    for kc in range(8):
                        yield S.op("pe", lambda e, kc=kc, pV=pV: e.matmul(out=pV[:], lhsT=xt[:, kc, csl], rhs=wuv[:, kc, 512:1024], start=(kc == 0), stop=(kc == 7)),
                             reads=[("xT", 0), "wuv"], accw=[pVt])
                    yield S.op("act", lambda e, pU=pU: e.copy(out=ux[:, 0:512], in_=pU[:]), reads=[pUt], writes=["ux"])
                    yield S.op("act", lambda e, pV=pV: e.copy(out=ux[:, 512:1024], in_=pV[:]), reads=[pVt], writes=["ux"])
                    yield S.op("act", lambda e: e.activation(out=t1[:], in_=ux[:], func=AF.Square), reads=["ux"], writes=["t1"])
                    yield S.op("dve", lambda e: e.tensor_scalar(out=t1[:], in0=t1[:], scalar1=0.044715, scalar2=1.0, op0=ALU.mult, op1=ALU.add), reads=["t1"], writes=["t1"])
                    yield S.op("dve", lambda e: e.tensor_tensor(out=t1[:], in0=t1[:], in1=ux[:], op=ALU.mult), reads=["t1", "ux"], writes=["t1"])
                    yield S.op("act", lambda e: e.activation(out=t1[:], in_=t1[:], func=AF.Exp, scale=-2.0 * 0.7978845608028654), reads=["t1"], writes=["t1"])
                    yield S.op("act", lambda e: e.activation(out=t1[:], in_=t1[:], func=AF.Ln, bias=1.0), reads=["t1"], writes=["t1"])
                    yield S.op("act", lambda e: e.activation(out=t1[:], in_=t1[:], func=AF.Exp, scale=-1.0), reads=["t1"], writes=["t1"])
                    yield S.op("dve", lambda e: e.tensor_tensor(out=ux[:], in0=t1[:], in1=ux[:], op=ALU.mult), reads=["t1", "ux"], writes=["ux"])
                    yield S.op("dve", lambda e: e.memset(sm8[:, 4:5], 0.0), writes=["sm8b"])
                    yield S.op("act", lambda e: e.activation(out=junk[:, 0:512], in_=ux[:, 512:1024], func=AF.Square, accum_out=sm8[:, 4:5]), reads=["ux", "sm8b"], writes=["junk", "sm8b"])
                    yield S.op("act", lambda e: e.activation(out=sm8[:, 4:5], in_=sm8[:, 4:5], func=AF.Ln, scale=1.0 / 512, bias=EPS), reads=["sm8b"], writes=["sm8b"])
                    yield S.op("act", lambda e: e.activation(out=sm8[:, 4:5], in_=sm8[:, 4:5], func=AF.Exp, scale=-0.5), reads=["sm8b"], writes=["sm8b"])
                    yield S.op("dve", lambda e: e.scalar_tensor_tensor(out=vn[:], in0=ux[:, 512:1024], scalar=sm8[:, 4:5], in1=ggv_b[:], op0=ALU.mult, op1=ALU.mult), reads=["ux", "sm8b", "ggv_b"], writes=["vn"])
                    pM, pMt = fbankB()
                    for g in range(8):
                        yield S.op("pe", lambda e, g=g, pM=pM: e.matmul(out=pM[:, g * 64:(g + 1) * 64], lhsT=wspT[:, g, :], rhs=vn[:, g * 64:(g + 1) * 64], start=True, stop=True),
                             reads=["vn", "wspT"], accw=[pMt])
                    yield S.op("dve", lambda e, pM=pM: e.tensor_tensor(out=gt[:], in0=pM[:].rearrange("p (g c) -> p g c", g=8), in1=bsp[:].unsqueeze(2).broadcast_to([128, 8, 64]), op=ALU.add),
                         reads=[pMt, "bsp"], writes=["gt"])
                    yield S.op("dve", lambda e: e.tensor_tensor(out=gt[:].rearrange("p g c -> p (g c)"), in0=gt[:].rearrange("p g c -> p (g c)"), in1=ux[:, 0:512], op=ALU.mult), reads=["gt", "ux"], writes=["gt"])
                    yield S.op("dve", lambda e: e.tensor_tensor(out=tmpa[:, 512:1024], in0=gt[:].rearrange("p g c -> p (g c)"), in1=gt[:].rearrange("p g c -> p (g c)"), op=ALU.mult), reads=["gt"], writes=["tmpb"])
                    yield S.op("dve", lambda e: e.tensor_reduce(out=sm8[:, 8:16], in_=tmpa[:, 512:1024].rearrange("p (g c) -> p g c", g=8), axis=AX.X, op=ALU.add), reads=["tmpb"], writes=["sm8c"])
                    yield S.op("act", lambda e: e.activation(out=sm8[:, 8:16], in_=sm8[:, 8:16], func=AF.Ln, scale=1.0 / 64, bias=EPS), reads=["sm8c"], writes=["sm8c"])
                    yield S.op("act", lambda e: e.activation(out=sm8[:, 8:16], in_=sm8[:, 8:16], func=AF.Exp, scale=-0.5), reads=["sm8c"], writes=["sm8c"])
                    yield S.op("dve", lambda e: e.tensor_tensor(out=gt[:], in0=gt[:], in1=sm8[:, 8:16].unsqueeze(2).broadcast_to([128, 8, 64]), op=ALU.mult), reads=["gt", "sm8c"], writes=["gt"])
                    yield S.op("dve", lambda e, yt_=yt_: e.tensor_tensor(out=yt_[:, 512:1024], in0=gt[:].rearrange("p g c -> p (g c)"), in1=ggo_b[:], op=ALU.mult), reads=["gt", "ggo_b"], writes=[("y", ysl, 1)])

                    yield None
                def chainC():
                    if "y" in dbg_t:
                        row0d = st_i * 512 + j * 128
                        finals.append(S.op("pool", lambda e, yt_=yt_, row0d=row0d: e.dma_start(out=dbg_t["y"].ap()[row0d:row0d + 128, :], in_=yt_[:]), reads=[("y", ysl, 0), ("y", ysl, 1)], dma=("dbgy", ysl)))
                    pb, pt = (ptr[1], ("ptr", 1))
                    for ec in range(8):
                        yield S.op("pe", lambda e, ec=ec, pb=pb, yt_=yt_: e.transpose(out=pb[:, ec * 128:(ec + 1) * 128], in_=yt_[:, ec * 128:(ec + 1) * 128], identity=ident_b[:]),
                             reads=[("y", ysl, 0), ("y", ysl, 1), "ident_b"], accw=[pt])
                    yT_ = yT[ysl]
                    yield S.op("act", lambda e, pb=pb, yT_=yT_: e.copy(out=yT_[:].rearrange("p c t -> p (c t)"), in_=pb[:]), reads=[pt], writes=[("yT", ysl)])
                    h1t = h1b[ysl]
                    for hf in range(2):
                        ph, pht = fbankC()
                        for ec in range(8):
                            yield S.op("pe", lambda e, ec=ec, ph=ph, hf=hf, yT_=yT_: e.matmul(out=ph[:], lhsT=yT_[:, ec, :], rhs=wout[:, ec, hf * 512:(hf + 1) * 512], start=(ec == 0), stop=(ec == 7)),
                                 reads=[("yT", ysl), "wout"], accw=[pht])
                        yield S.op("dve", lambda e, ph=ph, hf=hf, h1t=h1t: e.tensor_tensor(out=h1t[:, hf * 512:(hf + 1) * 512], in0=ph[:], in1=xb[:, j, hf * 512:(hf + 1) * 512], op=ALU.add),
                             reads=[pht, ("x", sl)], writes=[("h1", ysl, hf)])
                    row0 = st_i * 512 + j * 128
                    yield S.op("sp", lambda e, h1t=h1t, row0=row0: e.dma_start(out=h1buf.ap()[row0:row0 + 128, :], in_=h1t[:]), reads=[("h1", ysl, 0), ("h1", ysl, 1)], accw=["h1buf"], dma=("h1st", ysl))
                    if "h1" in dbg_t:
                        finals.append(S.op("sp", lambda e, h1t=h1t, row0=row0: e.dma_start(out=dbg_t["h1"].ap()[row0:row0 + 128, :], in_=h1t[:]), reads=[("h1", ysl, 0), ("h1", ysl, 1)], dma=("dbgh1", ysl)))
                    ti = st_i * 4 + j
                    x2 = xn2t[ysl]
                    yield S.op("dve", lambda e: e.memset(sm8[:, 5:6], 0.0), writes=["sm8d"])
                    yield S.op("act", lambda e: e.activation(out=junk[:], in_=h1t[:], func=AF.Square, accum_out=sm8[:, 5:6]), reads=[("h1", ysl, 0), ("h1", ysl, 1), "sm8d"], writes=["junk", "sm8d"])
                    yield S.op("act", lambda e: e.activation(out=sm8[:, 5:6], in_=sm8[:, 5:6], func=AF.Ln, scale=1.0 / D, bias=EPS), reads=["sm8d"], writes=["sm8d"])
                    yield S.op("act", lambda e: e.activation(out=sm8[:, 5:6], in_=sm8[:, 5:6], func=AF.Exp, scale=-0.5), reads=["sm8d"], writes=["sm8d"])
                    yield S.op("dve", lambda e: e.scalar_tensor_tensor(out=x2[:], in0=h1t[:], scalar=sm8[:, 5:6], in1=g2_b[:], op0=ALU.mult, op1=ALU.mult),
                         reads=[("h1", ysl, 0), ("h1", ysl, 1), "sm8d", "g2_b"], writes=[("xn2", ysl)])
                    yield S.op("sp", lambda e: e.dma_start(out=xn2lin.ap()[row0:row0 + 128, :], in_=x2[:]), reads=[("xn2", ysl)], accw=["xn2lin"], dma=("xn2st", ysl))
                    pb2, pt2 = (ptr[1], ("ptr", 1))
                    for kc in range(8):
                        yield S.op("pe", lambda e, kc=kc: e.transpose(out=pb2[:, kc * 128:(kc + 1) * 128], in_=x2[:, kc * 128:(kc + 1) * 128], identity=ident_b[:]),
                             reads=[("xn2", ysl), "ident_b"], accw=[pt2])
                    x2T = xn2T[0]
                    yield S.op("act", lambda e: e.copy(out=x2T[:].rearrange("p c t -> p (c t)"), in_=pb2[:]), reads=[pt2], writes=[("xn2T", 0)])
                    pl, plt = fbankC()
                    for kc in range(8):
                        yield S.op("pe", lambda e, kc=kc: e.matmul(out=pl[:, 0:36], lhsT=x2T[:, kc, :], rhs=wr_b[:, kc, :], start=(kc == 0), stop=(kc == 7)),
                             reads=[("xn2T", 0), "wr_b"], accw=[plt])
                    yield S.op("dve", lambda e: e.tensor_tensor(out=lg[:], in0=pl[:, 0:36], in1=br_b[:], op=ALU.add), reads=[plt, "br_b"], writes=["lg"])
                    R_ = lambda a, b: rt[:, a:b]
                    yield S.op("dve", lambda e: e.tensor_reduce(out=R_(0, 1), in_=lg[:, 0:4], axis=AX.X, op=ALU.max), reads=["lg"], writes=["rt"])
                    yield S.op("dve", lambda e: e.tensor_scalar(out=R_(12, 16), in0=lg[:, 0:4], scalar1=R_(0, 1), scalar2=None, op0=ALU.is_equal), reads=["lg", "rt"], writes=["rt"])
                    yield S.op("dve", lambda e: e.tensor_scalar(out=R_(1, 2), in0=R_(0, 1), scalar1=-1.0, scalar2=None, op0=ALU.mult), reads=["rt"], writes=["rt"])
                    yield S.op("dve", lambda e: e.memset(R_(2, 3), 0.0), reads=["rt"], writes=["rt"])
                    yield S.op("act", lambda e: e.activation(out=le8[:, 8:12], in_=lg[:, 0:4], func=AF.Exp, bias=R_(1, 2), accum_out=R_(2, 3)), reads=["lg", "rt"], writes=["rt", "le8x"])
                    yield S.op("dve", lambda e: e.reciprocal(out=R_(3, 4), in_=R_(2, 3)), reads=["rt"], writes=["rt"])
                    yield S.op("dve", lambda e: e.tensor_tensor(out=t32[:], in0=lg[:, 4:36].rearrange("p (g j) -> p g j", g=4), in1=R_(12, 16).unsqueeze(2).broadcast_to([128, 4, 8]), op=ALU.mult), reads=["lg", "rt"], writes=["t32"])
                    yield S.op("dve", lambda e: e.tensor_reduce(out=le8[:, 0:8], in_=t32[:].rearrange("p g j -> p j g"), axis=AX.X, op=ALU.add), reads=["t32"], writes=["le8"])
                    yield S.op("dve", lambda e: e.tensor_reduce(out=R_(4, 5), in_=le8[:, 0:8], axis=AX.X, op=ALU.max), reads=["le8", "rt"], writes=["rt"])
                    yield S.op("dve", lambda e: e.tensor_scalar(out=R_(16, 24), in0=le8[:, 0:8], scalar1=R_(4, 5), scalar2=None, op0=ALU.is_equal), reads=["le8", "rt"], writes=["rt"])
                    yield S.op("dve", lambda e: e.scalar_tensor_tensor(out=le8[:, 0:8], in0=R_(16, 24), scalar=-1e30, in1=le8[:, 0:8], op0=ALU.mult, op1=ALU.add), reads=["le8", "rt"], writes=["le8"])
                    yield S.op("dve", lambda e: e.tensor_reduce(out=R_(5, 6), in_=le8[:, 0:8], axis=AX.X, op=ALU.max), reads=["le8", "rt"], writes=["rt"])
                    yield S.op("dve", lambda e: e.tensor_scalar(out=R_(24, 32), in0=le8[:, 0:8], scalar1=R_(5, 6), scalar2=None, op0=ALU.is_equal), reads=["le8", "rt"], writes=["rt"])
                    yield S.op("dve", lambda e: e.tensor_tensor(out=R_(6, 7), in0=R_(5, 6), in1=R_(4, 5), op=ALU.subtract), reads=["rt"], writes=["rt"])
                    yield S.op("act", lambda e: e.activation(out=R_(6, 7), in_=R_(6, 7), func=AF.Exp), reads=["rt"], writes=["rt"])
                    yield S.op("dve", lambda e: e.tensor_scalar(out=R_(7, 8), in0=R_(6, 7), scalar1=1.0, scalar2=None, op0=ALU.add), reads=["rt"], writes=["rt"])
                    yield S.op("dve", lambda e: e.reciprocal(out=R_(7, 8), in_=R_(7, 8)), reads=["rt"], writes=["rt"])
                    yield S.op("dve", lambda e: e.tensor_tensor(out=R_(8, 9), in0=R_(6, 7), in1=R_(7, 8), op=ALU.mult), reads=["rt"], writes=["rt"])
                    yield S.op("dve", lambda e: e.tensor_scalar(out=pw[:, ti, 2:4], in0=R_(7, 9), scalar1=R_(3, 4), scalar2=None, op0=ALU.mult), reads=["rt"], accw=["pw"])
                    E1 = E1s[:, ti, :]
                    E2 = E2s[:, ti, :]
                    yield S.op("dve", lambda e: e.tensor_tensor(out=E1.rearrange("p (g j) -> p g j", g=4), in0=R_(12, 16).unsqueeze(2).broadcast_to([128, 4, 8]), in1=R_(16, 24).unsqueeze(1).broadcast_to([128, 4, 8]), op=ALU.mult), reads=["rt"], accw=["E1s"])
                    yield S.op("dve", lambda e: e.tensor_tensor(out=E2.rearrange("p (g j) -> p g j", g=4), in0=R_(12, 16).unsqueeze(2).broadcast_to([128, 4, 8]), in1=R_(24, 32).unsqueeze(1).broadcast_to([128, 4, 8]), op=ALU.mult), reads=["rt"], accw=["E2s"])
                    yield S.op("dve", lambda e: e.tensor_tensor(out=ind_b[:], in0=E1, in1=E2, op=ALU.add), reads=["E1s", "E2s"], writes=["ind_b"])
                    pp, ppt = fbankC()
                    yield S.op("pe", lambda e: e.matmul(out=pp[:, 0:32], lhsT=lstr_b[:], rhs=ind_b[:], start=True, stop=True), reads=["ind_b", "lstr_b"], accw=[ppt])
                    yield S.op("pe", lambda e: e.matmul(out=pp[:, 32:64], lhsT=ones_b[:], rhs=ind_b[:], start=True, stop=True), reads=["ind_b", "ones_b"], accw=[ppt])
                    yield S.op("dve", lambda e: e.tensor_tensor(out=posf[:], in0=pp[:, 0:32], in1=base_b[:], op=ALU.add), reads=[ppt, "base_b"], writes=["posf"])
                    yield S.op("dve", lambda e: e.tensor_tensor(out=base_b[:], in0=pp[:, 32:64], in1=base_b[:], op=ALU.add), reads=[ppt, "base_b"], writes=["base_b"])
                    yield S.op("dve", lambda e: e.tensor_tensor(out=t32[:].rearrange("p g j -> p (g j)"), in0=E1, in1=posf[:], op=ALU.mult), reads=["E1s", "posf"], writes=["t32"])
                    yield S.op("dve", lambda e: e.tensor_reduce(out=pw[:, ti, 0:1], in_=t32[:].rearrange("p g j -> p (g j)"), axis=AX.X, op=ALU.add), reads=["t32"], accw=["pw"])
                    yield S.op("dve", lambda e: e.tensor_tensor(out=t32[:].rearrange("p g j -> p (g j)"), in0=E2, in1=posf[:], op=ALU.mult), reads=["E2s", "posf", "pw"], writes=["t32"])
                    yield S.op("dve", lambda e: e.tensor_reduce(out=pw[:, ti, 1:2], in_=t32[:].rearrange("p g j -> p (g j)"), axis=AX.X, op=ALU.add), reads=["t32"], accw=["pw"])
                    yield None
                return chainA(), (chainB() if main else None), (chainC() if main else None)

            def run_chains(gens):
                act = [g for g in gens if g is not None]
                while act:
                    for g in list(act):
                        try:
                            next(g)
                        except StopIteration:
                            act.remove(g)

            cprev = None
            for j in range(4):
                ca_, cb_, cc_ = tile_body(j)
                run_chains([ca_, cb_, cprev])
                cprev = cc_
            run_chains([cprev])
            if gi == 0 and "qkT" in dbg_t:
                tmpd = S.sb([128, 8, 512], F32, "sdbg_qk")
                S.op("dve", lambda e: e.tensor_copy(out=tmpd[:], in_=qk[:]), reads=[("qkT", 0)], writes=["dbg_qk"])
                finals.append(S.op("sp", lambda e: e.dma_start(out=dbg_t["qkT"].ap(), in_=tmpd[:].rearrange("p a b -> p (a b)")), reads=["dbg_qk"], dma=("dbg", "qkT")))
            if gi == 0 and "xT" in dbg_t:
                tmpx = S.sb([128, 8, 512], F32, "sdbg_xT")
                S.op("dve", lambda e: e.tensor_copy(out=tmpx[:], in_=xt[:]), reads=[("xT", 0)], writes=["dbg_xT"])
                finals.append(S.op("sp", lambda e: e.dma_start(out=dbg_t["xT"].ap(), in_=tmpx[:].rearrange("p a b -> p (a b)")), reads=["dbg_xT"], dma=("dbg", "xT")))

        wcat_bf = nc.dram_tensor("wcat_bf", [NE * 128, 12288], BF16)
        NSUP = NPRE + NST
        conv_rows = NE * 128
        conv_state = {"r": 0, "i": 0}

        def conv_some(gidx, sl):
            tgt = conv_rows * (gidx + 1) // NSUP
            while conv_state["r"] < tgt:
                r0 = conv_state["r"]
                r1 = min(r0 + 32, tgt)
                k = conv_state["i"] % 2
                conv_state["i"] += 1
                conv_state["r"] = r1
                S.op("pool", lambda e, r0=r0, r1=r1: e.dma_start(out=wcat_bf.ap()[r0:r1, :], in_=wcat.ap()[r0:r1, :]),
                     reads=[("x", sl)], writes=[("wck", k)], dma=("wck", k))

        gi = 0
        for s_i in range(NPRE):
            supertile(x_pre, s_i, "prelast" if s_i == NPRE - 1 else "pre", gi)
            gi += 1
        for s_i in range(NST):
            supertile(x_main, s_i, "main", gi)
            gi += 1


        S.flush()
        stA.close()
        stB = contextlib.ExitStack()
        S.cur = stB
        NBLK = NT * 2 + NE
        NSL = NBLK * 128
        xslots = nc.dram_tensor("xslots", [NSL, D], BF16)
        yslots = nc.dram_tensor("yslots", [NSL, D], F32)
        pl_ = S.sb([128, 6, 32], F32, "plan")
        pl_i = S.sb([128, 32], I32, "plan_i")
        p128 = S.sb([128, 1], F32, "p128")
        woff_f = S.sb([128, NBLK], F32, "woff_f")
        woff_i = S.sb([128, NBLK], I32, "woff_i")
        bval = S.sb([128, NBLK], F32, "bval")
        neq = S.sb([128, NBLK], F32, "neq")
        cmpb = S.sb([128, NBLK, 32], F32, "cmpb")
        dst_f = S.sb([128, 2, NT], F32, "dst_f")
        dst_i = S.sb([128, 2, NT], I32, "dst_i")
        big = S.sb([128, NT, 32], F32, "bigtmp")
        S.op("pool", lambda e: e.iota(p128[:], [[0, 1]], base=0, channel_multiplier=1, allow_small_or_imprecise_dtypes=True), writes=["p128"])
        S.op("dve", lambda e: e.tensor_scalar(out=pl_[:, 3, :], in0=base_b[:], scalar1=1.0 / 128, scalar2=63.5 / 128, op0=ALU.mult, op1=ALU.add), reads=["base_b"], writes=["plan"])
        S.op("dve", lambda e: e.tensor_copy(out=pl_i[:], in_=pl_[:, 3, :]), reads=["plan"], writes=["plan_i"])
        S.op("dve", lambda e: e.tensor_copy(out=pl_[:, 0, :], in_=pl_i[:]), reads=["plan_i", "plan"], writes=["plan"])
        S.op("dve", lambda e: e.tensor_scalar(out=pl_[:, 0, :], in0=pl_[:, 0, :], scalar1=128.0, scalar2=None, op0=ALU.mult), reads=["plan"], writes=["plan"])
        S.op("dve", lambda e: e.tensor_tensor_scan(out=pl_[:, 1, :], data0=pl_[:, 0, :], data1=pl_[:, 0, :], initial=0.0, op0=ALU.add, op1=ALU.bypass), reads=["plan"], writes=["plan"])
        S.op("dve", lambda e: e.tensor_tensor(out=pl_[:, 2, :], in0=pl_[:, 1, :], in1=pl_[:, 0, :], op=ALU.subtract), reads=["plan"], writes=["plan"])
        S.op("pool", lambda e: e.iota(bval[:], [[128, NBLK]], base=0, channel_multiplier=0, allow_small_or_imprecise_dtypes=True), writes=["bval"])
        S.op("dve", lambda e: e.tensor_tensor(out=cmpb[:], in0=pl_[:, 1, :].unsqueeze(1).broadcast_to([128, NBLK, 32]), in1=bval[:].unsqueeze(2).broadcast_to([128, NBLK, 32]), op=ALU.is_le), reads=["plan", "bval"], writes=["cmpb"])
        S.op("dve", lambda e: e.tensor_reduce(out=woff_f[:], in_=cmpb[:], axis=AX.X, op=ALU.add), reads=["cmpb"], writes=["woff_f"])
        BIGI = 1000000.0
        S.op("dve", lambda e: e.tensor_scalar(out=woff_f[:], in0=woff_f[:], scalar1=31.0, scalar2=None, op0=ALU.min), reads=["woff_f"], writes=["woff_f"])
        S.op("dve", lambda e: e.memset(neq[:, 0:2], 1.0), writes=["neq0"])
        S.op("dve", lambda e: e.tensor_tensor(out=neq[:, 2:NBLK], in0=woff_f[:, 2:NBLK], in1=woff_f[:, 0:NBLK - 2], op=ALU.not_equal), reads=["woff_f"], writes=["neq"])
        S.op("dve", lambda e: e.tensor_scalar(out=woff_f[:], in0=woff_f[:], scalar1=128.0, scalar2=-BIGI, op0=ALU.mult, op1=ALU.add), reads=["woff_f", "neq"], writes=["woff_f"])
        S.op("dve", lambda e: e.tensor_scalar(out=woff_f[:], in0=woff_f[:], scalar1=p128[:, 0:1], scalar2=None, op0=ALU.add), reads=["woff_f", "p128"], writes=["woff_f"])
        S.op("dve", lambda e: e.tensor_tensor(out=woff_f[:], in0=woff_f[:], in1=neq[:], op=ALU.mult), reads=["woff_f", "neq", "neq0"], writes=["woff_f"])
        S.op("dve", lambda e: e.tensor_scalar(out=woff_f[:], in0=woff_f[:], scalar1=BIGI, scalar2=None, op0=ALU.add), reads=["woff_f"], writes=["woff_f"])
        S.op("dve", lambda e: e.tensor_copy(out=woff_i[:], in_=woff_f[:]), reads=["woff_f"], writes=["woff_i"])
        for k_, Es in ((0, E1s), (1, E2s)):
            S.op("dve", lambda e, Es=Es: e.tensor_tensor(out=big[:], in0=Es[:], in1=pl_[:, 2, :].unsqueeze(1).broadcast_to([128, NT, 32]), op=ALU.mult), reads=["E1s", "E2s", "plan"], writes=["big"])
            S.op("dve", lambda e, k_=k_: e.tensor_reduce(out=dst_f[:, k_, :], in_=big[:], axis=AX.X, op=ALU.add), reads=["big"], writes=[("dst_f", k_)])
            S.op("dve", lambda e, k_=k_: e.tensor_tensor(out=dst_f[:, k_, :], in0=dst_f[:, k_, :], in1=pw[:, :, k_], op=ALU.add), reads=[("dst_f", k_), "pw"], writes=[("dst_f", k_)])
        S.op("dve", lambda e: e.tensor_copy(out=dst_i[:], in_=dst_f[:]), reads=[("dst_f", 0), ("dst_f", 1)], writes=["dst_i"])
        if "plan" in dbg_t:
            finals.append(S.op("sp", lambda e: e.dma_start(out=dbg_t["plan"].ap()[:, 0:192], in_=pl_[:].rearrange("p a b -> p (a b)")), reads=["plan"], dma=("dbg", "plan")))
            finals.append(S.op("sp", lambda e: e.dma_start(out=dbg_t["plan"].ap()[:, 768:768 + NBLK], in_=woff_f[:]), reads=["woff_f"], dma=("dbg", "plan2")))
            finals.append(S.op("sp", lambda e: e.dma_start(out=dbg_t["plan"].ap()[:, 256:256 + 2 * NT], in_=dst_f[:].rearrange("p a b -> p (a b)")), reads=[("dst_f", 0), ("dst_f", 1)], dma=("dbg", "plan3")))
            finals.append(S.op("sp", lambda e: e.dma_start(out=dbg_t["plan"].ap()[:, 512:512 + 4 * NT], in_=pw[:].rearrange("p a b -> p (a b)")), reads=["pw"], dma=("dbg", "plan4")))
        xsc = [S.sb([128, 1024], BF16, f"xsc{i}") for i in range(2)]
        for ti in range(NT):
            bsl = ti % 2
            S.op("sp", lambda e, ti=ti, bsl=bsl: e.dma_start(out=xsc[bsl][:], in_=xn2lin.ap()[ti * 128:(ti + 1) * 128, :]), reads=["xn2lin"], writes=[("xsc", bsl)], dma=("xsc", bsl))
            for k_ in range(2):
                S.op("pool", lambda e, ti=ti, bsl=bsl, k_=k_: e.indirect_dma_start(out=xslots.ap(), out_offset=bass.IndirectOffsetOnAxis(ap=dst_i[:, k_, ti:ti + 1], axis=0), in_=xsc[bsl][:], in_offset=None),
                     reads=[("xsc", bsl), "dst_i"], accw=["xslots"], dma=("scat", bsl, k_))

        wbuf = [S.sb([128, 12288], BF16, f"wbuf{i}") for i in range(2)]
        xs_b = [S.sb([128, 1024], BF16, f"xs_b{i}") for i in range(4)]
        xsT = [S.sb([128, 8, 128], BF16, f"xsT{i}") for i in range(2)]
        eg = [S.sb([128, 512], F32, f"eg{i}") for i in range(2)]
        hid = [S.sb([128, 512], BF16, f"hid{i}") for i in range(2)]
        hidT = [S.sb([128, 4, 128], BF16, f"hidT{i}") for i in range(2)]
        ysb = [S.sb([128, 1024], F32, f"ysb{i}") for i in range(2)]
        regs = {}

        def wgather(e, b, ws):
            if "bnd" not in regs:
                regs["bnd"] = st.enter_context(e.register("wbnd"))
                e.reg_mov(regs["bnd"], NE * 128 - 1)
            return e.indirect_dma_start(out=wbuf[ws][:], out_offset=None, in_=wcat_bf.ap(), in_offset=bass.IndirectOffsetOnAxis(ap=woff_i[:, b:b + 1], axis=0),
                                        bounds_check=regs["bnd"], oob_is_err=False)

        mrr = [0, 0]

        def fbankM(p):
            i = 3 * p + mrr[p] % 3
            mrr[p] += 1
            return pfb[i], ("pf", i)

        def blk(b):
            ws = b % 2
            yield S.op("pool", lambda e, b=b, ws=ws: wgather(e, b, ws), reads=["woff_i"], writes=[("wb", ws)], dma=("wb", ws))
            if b < 2:
                yield S.op("sp", lambda e, b=b: e.dma_start(out=xs_b[b % 4][:], in_=xslots.ap()[b * 128:(b + 1) * 128, :]), reads=["xslots"], writes=[("xs", b % 4)], dma=("xs", b % 4))
            if b + 2 < NBLK:
                yield S.op("sp", lambda e, b=b: e.dma_start(out=xs_b[(b + 2) % 4][:], in_=xslots.ap()[(b + 2) * 128:(b + 3) * 128, :]), reads=["xslots"], writes=[("xs", (b + 2) % 4)], dma=("xs", (b + 2) % 4))
            xq = b % 4
            pb, pt = (ptr[ws], ("ptr", ws))
            for kc in range(8):
                yield S.op("pe", lambda e, kc=kc, pb=pb, xq=xq: e.transpose(out=pb[:, kc * 128:(kc + 1) * 128], in_=xs_b[xq][:, kc * 128:(kc + 1) * 128], identity=ident_b[:]),
                     reads=[("xs", xq), "ident_b"], accw=[pt])
            yield S.op("act", lambda e, pb=pb, ws=ws: e.copy(out=xsT[ws][:].rearrange("p c t -> p (c t)"), in_=pb[:]), reads=[pt], writes=[("xsT", ws)])
            pG, pGt = fbankM(ws)
            pU2, pU2t = fbankM(ws)
            for kc in range(8):
                yield S.op("pe", lambda e, kc=kc, pG=pG, ws=ws: e.matmul(out=pG[:], lhsT=xsT[ws][:, kc, :], rhs=wbuf[ws][:, kc * 1024:kc * 1024 + 512], start=(kc == 0), stop=(kc == 7)),
                     reads=[("xsT", ws), ("wb", ws)], accw=[pGt])
            for kc in range(8):
                yield S.op("pe", lambda e, kc=kc, pU2=pU2, ws=ws: e.matmul(out=pU2[:], lhsT=xsT[ws][:, kc, :], rhs=wbuf[ws][:, kc * 1024 + 512:(kc + 1) * 1024], start=(kc == 0), stop=(kc == 7)),
                     reads=[("xsT", ws), ("wb", ws)], accw=[pU2t])
            yield S.op("act", lambda e, pG=pG, ws=ws: e.activation(out=eg[ws][:], in_=pG[:], func=AF.Exp, scale=-1.0), reads=[pGt], writes=[("eg", ws)])
            yield S.op("act", lambda e, ws=ws: e.activation(out=eg[ws][:], in_=eg[ws][:], func=AF.Ln, bias=1.0), reads=[("eg", ws)], writes=[("eg", ws)])
            yield S.op("act", lambda e, ws=ws: e.activation(out=eg[ws][:], in_=eg[ws][:], func=AF.Exp, scale=-1.0), reads=[("eg", ws)], writes=[("eg", ws)])
            yield S.op("dve", lambda e, pG=pG, ws=ws: e.tensor_tensor(out=eg[ws][:], in0=eg[ws][:], in1=pG[:], op=ALU.mult), reads=[("eg", ws), pGt], writes=[("eg", ws)])
            yield S.op("dve", lambda e, pU2=pU2, ws=ws: e.tensor_tensor(out=hid[ws][:], in0=eg[ws][:], in1=pU2[:], op=ALU.mult), reads=[("eg", ws), pU2t], writes=[("hid", ws)])
            pb, pt = (ptr[ws], ("ptr", ws))
            for fc in range(4):
                yield S.op("pe", lambda e, fc=fc, pb=pb, ws=ws: e.transpose(out=pb[:, fc * 128:(fc + 1) * 128], in_=hid[ws][:, fc * 128:(fc + 1) * 128], identity=ident_b[:]),
                     reads=[("hid", ws), "ident_b"], accw=[pt])
            yield S.op("act", lambda e, pb=pb, ws=ws: e.copy(out=hidT[ws][:].rearrange("p c t -> p (c t)"), in_=pb[:, 0:512]), reads=[pt], writes=[("hidT", ws)])
            for hf in range(2):
                pY, pYt = fbankM(ws)
                for fc in range(4):
                    yield S.op("pe", lambda e, fc=fc, pY=pY, ws=ws, hf=hf: e.matmul(out=pY[:], lhsT=hidT[ws][:, fc, :], rhs=wbuf[ws][:, 8192 + fc * 1024 + hf * 512:8192 + fc * 1024 + (hf + 1) * 512], start=(fc == 0), stop=(fc == 3)),
                         reads=[("hidT", ws), ("wb", ws)], accw=[pYt])
                if hf == 0:
                    yield S.op("act", lambda e, pY=pY, ws=ws: e.copy(out=ysb[ws][:, 0:512], in_=pY[:]), reads=[pYt], writes=[("ysb", ws, 0)])
                else:
                    yield S.op("dve", lambda e, pY=pY, ws=ws: e.tensor_copy(out=ysb[ws][:, 512:1024], in_=pY[:]), reads=[pYt], writes=[("ysb", ws, 1)])
            yield S.op("sp", lambda e, b=b, ws=ws: e.dma_start(out=yslots.ap()[b * 128:(b + 1) * 128, :], in_=ysb[ws][:]), reads=[("ysb", ws, 0), ("ysb", ws, 1)], accw=["yslots"], dma=("yst", ws))


            yield None

        def slot_seq(p):
            for b in range(p, NBLK, 2):
                yield from blk(b)

        seqs = [slot_seq(0), slot_seq(1)]
        while seqs:
            for g in list(seqs):
                try:
                    next(g)
                except StopIteration:
                    seqs.remove(g)

        fg_b = bload("fg_b", final_g, 1024)
        NCB = 3
        hc = [S.sb([128, 1024], F32, f"hc{i}") for i in range(NCB)]
        y1 = [S.sb([128, 1024], F32, f"y1_{i}") for i in range(NCB)]
        y2 = [S.sb([128, 1024], F32, f"y2_{i}") for i in range(NCB)]
        fs = S.sb([128, 2], F32, "fs")

        def cloads(ti):
            cs = ti % NCB
            S.op("sp", lambda e, ti=ti, cs=cs: e.dma_start(out=hc[cs][:], in_=h1buf.ap()[ti * 128:(ti + 1) * 128, :]), reads=["h1buf"], writes=[("hc", cs)], dma=("hc", cs))
            S.op("pool", lambda e, ti=ti, cs=cs: e.indirect_dma_start(out=y1[cs][:], out_offset=None, in_=yslots.ap(), in_offset=bass.IndirectOffsetOnAxis(ap=dst_i[:, 0, ti:ti + 1], axis=0)),
                 reads=["yslots", "dst_i"], writes=[("y1", cs)], dma=("y1", cs))
            S.op("pool", lambda e, ti=ti, cs=cs: e.indirect_dma_start(out=y2[cs][:], out_offset=None, in_=yslots.ap(), in_offset=bass.IndirectOffsetOnAxis(ap=dst_i[:, 1, ti:ti + 1], axis=0)),
                 reads=["yslots", "dst_i"], writes=[("y2", cs)], dma=("y2", cs))

        for ti in range(min(NCB - 1, NT)):
            cloads(ti)
        for ti in range(NT):
            cs = ti % NCB
            if ti + NCB - 1 < NT:
                cloads(ti + NCB - 1)
            S.op("dve", lambda e, ti=ti, cs=cs: e.scalar_tensor_tensor(out=hc[cs][:], in0=y1[cs][:], scalar=pw[:, ti, 2:3], in1=hc[cs][:], op0=ALU.mult, op1=ALU.add), reads=[("hc", cs), ("y1", cs), "pw"], writes=[("hc", cs)])
            S.op("dve", lambda e, ti=ti, cs=cs: e.scalar_tensor_tensor(out=hc[cs][:], in0=y2[cs][:], scalar=pw[:, ti, 3:4], in1=hc[cs][:], op0=ALU.mult, op1=ALU.add), reads=[("hc", cs), ("y2", cs), "pw"], writes=[("hc", cs)])
            S.op("dve", lambda e, ti=ti: e.memset(fs[:, (ti % 2):(ti % 2) + 1], 0.0), writes=[("fs", ti % 2)])
            S.op("act", lambda e, ti=ti, cs=cs: e.activation(out=junk[:], in_=hc[cs][:], func=AF.Square, accum_out=fs[:, (ti % 2):(ti % 2) + 1]), reads=[("hc", cs), ("fs", ti % 2)], writes=["junk", ("fs", ti % 2)])
            S.op("act", lambda e, ti=ti: e.activation(out=fs[:, (ti % 2):(ti % 2) + 1], in_=fs[:, (ti % 2):(ti % 2) + 1], func=AF.Ln, scale=1.0 / D, bias=EPS), reads=[("fs", ti % 2)], writes=[("fs", ti % 2)])
            S.op("act", lambda e, ti=ti: e.activation(out=fs[:, (ti % 2):(ti % 2) + 1], in_=fs[:, (ti % 2):(ti % 2) + 1], func=AF.Exp, scale=-0.5), reads=[("fs", ti % 2)], writes=[("fs", ti % 2)])
            S.op("dve", lambda e, ti=ti, cs=cs: e.scalar_tensor_tensor(out=y1[cs][:], in0=hc[cs][:], scalar=fs[:, (ti % 2):(ti % 2) + 1], in1=fg_b[:], op0=ALU.mult, op1=ALU.mult), reads=[("hc", cs), ("fs", ti % 2), "fg_b", ("y1", cs)], writes=[("y1", cs)])
            finals.append(S.op("sp", lambda e, ti=ti, cs=cs: e.dma_start(out=out.ap()[ti * 128:(ti + 1) * 128, :], in_=y1[cs][:]), reads=[("y1", cs)], dma=("ost", cs)))

        S.flush()
        stB.close()
    return nc


def make_wcat(w_gate, w_up, w_down):
    g = w_gate.reshape(NE, 8, 128, 512).transpose(0, 2, 1, 3)
    u = w_up.reshape(NE, 8, 128, 512).transpose(0, 2, 1, 3)
    gu = np.concatenate([g, u], axis=3).reshape(NE, 128, 8192)
    dn = w_down.reshape(NE, 4, 128, 1024).transpose(0, 2, 1, 3).reshape(NE, 128, 4096)
    return np.ascontiguousarray(np.concatenate([gu, dn], axis=2).reshape(NE * 128, 12288))


def kernel(**inputs):
    f = lambda k: np.ascontiguousarray(np.asarray(inputs[k], dtype=np.float32))
    x = f("x")
    com = {
        "norm1_g": f("norm1_g")[0], "w_in": f("w_in")[0], "conv_qk": f("conv_qk")[0],
        "b_if": np.concatenate([f("b_igate")[0], f("b_fgate")[0]]), "g_mlstm_out": f("g_mlstm_out")[0],
        "g_gmlp_v": f("g_gmlp_v")[0], "w_spatial": f("w_spatial")[0], "b_spatial": f("b_spatial")[0],
        "g_gmlp_out": f("g_gmlp_out")[0], "w_out": f("w_out")[0], "norm2_g": f("norm2_g")[0],
        "w_router": np.ascontiguousarray(np.concatenate([f("w_router_group")[0], f("w_router_expert")[0]], axis=1)),
        "b_router": np.concatenate([f("b_router_group")[0], f("b_router_expert")[0]]),
        "wcat": make_wcat(f("w_gate")[0], f("w_up")[0], f("w_down")[0]), "final_g": f("final_g"),
    }
    in_maps = []
    for c in range(8):
        b, half = c // 2, c % 2
        m = dict(com)
        m["x_main"] = np.ascontiguousarray(x[b, half * 4096:(half + 1) * 4096])
        m["x_pre"] = np.ascontiguousarray(x[b, 0:4096]) if half == 1 else np.zeros((4096, D), np.float32)
        in_maps.append(m)
    nc = build(8, 8)
    res = run_bass_kernel_spmd(nc, in_maps, core_ids=list(range(8)))
    out = np.empty((4, 8192, D), np.float32)
    for c in range(8):
        out[c // 2, (c % 2) * 4096:(c % 2 + 1) * 4096] = res.results[c]["out"]
    return out
```

```python
import contextlib
import numpy as np
import concourse.bass as bass
import concourse.mybir as mybir
from concourse.bass_utils import run_bass_kernel_spmd

F32 = mybir.dt.float32
BF16 = mybir.dt.bfloat16
I32 = mybir.dt.int32
AF = mybir.ActivationFunctionType
ALU = mybir.AluOpType
AX = mybir.AxisListType

ENGS = ("pe", "act", "dve", "pool", "sp")
SEM_CH = 8000
D = 1024
EPS = 1e-6
NE = 32
NBLK_MAX = 96
NSLOT = NBLK_MAX * 128


class Sched:
    def __init__(self, nc, stack):
        self.nc = nc
        self.stack = stack
        self.ops = []
        self.last_w = {}
        self.readers = {}
        self.dma_count = {}
        self.nbuf = 0
        self.flushed = 0
        self.sems = {}
        self.eng_seq = {e: 0 for e in ENGS}
        self.waited = {e: {} for e in ENGS}
        self.cur = stack

    def sb(self, shape, dt, name=None, persist=False):
        self.nbuf += 1
        return (self.stack if persist else self.cur).enter_context(self.nc.sbuf_tensor(name or f"sb{self.nbuf}", list(shape), dt))

    def ps(self, shape, dt, name=None):
        self.nbuf += 1
        return self.stack.enter_context(self.nc.psum_tensor(name or f"ps{self.nbuf}", list(shape), dt))

    def op(self, eng, fn, reads=(), writes=(), accw=(), dma=None):
        i = len(self.ops)
        deps = set()
        for t in reads:
            deps.update(self.last_w.get(t, ()))
        for t in writes:
            deps.update(self.last_w.get(t, ()))
            deps.update(self.readers.get(t, ()))
        for t in accw:
            deps.update(self.readers.get(t, ()))
        for t in reads:
            self.readers.setdefault(t, []).append(i)
        for t in writes:
            self.last_w[t] = [i]
            self.readers[t] = []
        for t in accw:
            if self.readers.get(t):
                self.last_w[t] = []
                self.readers[t] = []
            self.last_w.setdefault(t, []).append(i)
        deps.discard(i)
        self.ops.append(dict(eng=eng, fn=fn, deps=sorted(deps), dma=dma, sig=None))
        return i

    def flush(self):
        nc = self.nc
        ops = self.ops
        lo = self.flushed
        last = {}
        for i in range(lo, len(ops)):
            o = ops[i]
            key = ("dma", o["dma"]) if o["dma"] is not None else ("eng", o["eng"])
            last[key] = i
        bdeps = sorted(last.values())
        for en in ENGS:
            self.ops.append(dict(eng=en, fn=lambda e: e.nop(), deps=list(bdeps), dma=None, sig=None, barrier=True))
        hi = len(ops)

        def pe_pair(a, b):
            return (a["eng"] == "pe" and b["eng"] == "pe" and a["dma"] is None and b["dma"] is None
                    and not b.get("barrier"))

        needed = set()
        for i in range(lo, hi):
            o = ops[i]
            for d in o["deps"]:
                if pe_pair(ops[d], o):
                    continue
                needed.add(d)
        for i in range(lo, hi):
            o = ops[i]
            if o["dma"] is not None:
                k = ("dma", o["dma"])
                self.dma_count[k] = self.dma_count.get(k, 0) + 1
                o["sig"] = (k, 16 * self.dma_count[k])
                self.get_sem(k)
            elif i in needed:
                e = o["eng"]
                n = self.eng_seq[e]
                self.eng_seq[e] += 1
                k = ("eng", e, n // SEM_CH)
                o["sig"] = (k, n % SEM_CH + 1)
                self.get_sem(k)
        sems = self.sems
        waited_all = self.waited

        def run(engname):
            def body(e):
                waited = waited_all[engname]
                for i in range(lo, hi):
                    o = ops[i]
                    if o["eng"] != engname:
                        continue
                    for d in o["deps"]:
                        od = ops[d]
                        if od["sig"] is None or pe_pair(od, o):
                            continue
                        k, v = od["sig"]
                        if waited.get(k, 0) >= v:
                            continue
                        e.wait_ge(sems[k], v)
                        waited[k] = v
                    ins = o["fn"](e)
                    if o["sig"] is not None:
                        k, v = o["sig"]
                        ins.then_inc(sems[k], 16 if o["dma"] is not None else 1)
            return body

        with nc.Block() as block:
            block.tensor(run("pe"))
            block.scalar(run("act"))
            block.vector(run("dve"))
            block.gpsimd(run("pool"))
            block.sync(run("sp"))
        self.flushed = hi
        self.last_w = {}
        self.readers = {}

    def get_sem(self, key):
        if key not in self.sems:
            self.sems[key] = self.stack.enter_context(self.nc.semaphore(f"s_{len(self.sems)}"))
        return self.sems[key]


def build(NST=8, NPRE=8, dbg=None):
    nc = bass.Bass("TRN2", target_bir_lowering=False)
    NT = NST * 4
    TOK = NST * 512

    def din(name, shape, dt=F32):
        return nc.dram_tensor(name, list(shape), dt, kind="ExternalInput")

    x_main = din("x_main", [TOK, D])
    x_pre = din("x_pre", [max(NPRE, 1) * 512, D])
    norm1_g = din("norm1_g", [D])
    w_in = din("w_in", [D, 3080])
    conv_qk = din("conv_qk", [4, 1024])
    b_if = din("b_if", [8])
    g_mlstm = din("g_mlstm_out", [512])
    g_gv = din("g_gmlp_v", [512])
    w_sp = din("w_spatial", [8, 128, 128])
    b_sp = din("b_spatial", [8, 128])
    g_go = din("g_gmlp_out", [512])
    w_out = din("w_out", [D, D])
    norm2_g = din("norm2_g", [D])
    w_r = din("w_router", [D, 36])
    b_r = din("b_router", [36])
    wcat = din("wcat", [NE * 128, 12288])
    final_g = din("final_g", [D])
    out = nc.dram_tensor("out", [TOK, D], F32, kind="ExternalOutput")
    h1buf = nc.dram_tensor("h1buf", [TOK, D], F32)
    dbg_t = {}
    if dbg:
        for k, shp in dbg.items():
            dbg_t[k] = nc.dram_tensor("dbg_" + k, list(shp), F32, kind="ExternalOutput")

    with contextlib.ExitStack() as st:
        S = Sched(nc, st)
        finals = []

        ident_f = S.sb([128, 128], F32, "ident_f")
        ident_b = S.sb([128, 128], BF16, "ident_b")
        triu_f = S.sb([128, 128], F32, "triu_f")
        triu_b = S.sb([128, 128], BF16, "triu_b")
        tril_f = S.sb([128, 128], F32, "tril_f")
        ones_f = S.sb([128, 128], F32, "ones_f")
        S.op("pool", lambda e: e.memset(ident_f[:], 0.0), writes=["ident_f"])
        S.op("pool", lambda e: e.affine_select(out=ident_f[:], in_=ident_f[:], pattern=[[-1, 128]],
                                               compare_op=ALU.not_equal, fill=1.0, base=0, channel_multiplier=1),
             reads=["ident_f"], writes=["ident_f"])
        S.op("pool", lambda e: e.memset(ones_f[:], 1.0), writes=["ones_f"])
        S.op("pool", lambda e: e.affine_select(out=triu_f[:], in_=ones_f[:], pattern=[[1, 128]],
                                               compare_op=ALU.is_ge, fill=0.0, base=0, channel_multiplier=-1),
             reads=["ones_f"], writes=["triu_f"])
        S.op("pool", lambda e: e.affine_select(out=tril_f[:], in_=ones_f[:], pattern=[[-1, 128]],
                                               compare_op=ALU.is_ge, fill=0.0, base=0, channel_multiplier=1),
             reads=["ones_f"], writes=["tril_f"])
        S.op("dve", lambda e: e.tensor_copy(out=ident_b[:], in_=ident_f[:]), reads=["ident_f"], writes=["ident_b"])
        S.op("dve", lambda e: e.tensor_copy(out=triu_b[:], in_=triu_f[:]), reads=["triu_f"], writes=["triu_b"])

        def bload(name, src, n, eng="sp"):
            t = S.sb([128, n], F32, name)
            S.op(eng, lambda e: e.dma_start(out=t[:], in_=bass.AP(src, 0, [[0, 128], [1, n]])),
                 writes=[name], dma=name)
            return t

        gml_b = bload("gml_b", g_mlstm, 512)
        ggv_b = bload("ggv_b", g_gv, 512)
        ggo_b = bload("ggo_b", g_go, 512)
        bif_b = bload("bif_b", b_if, 8)

        g1col = S.sb([128, 8], F32, "g1col")
        cw = S.sb([128, 4, 8], F32, "cw")
        bsp = S.sb([128, 8], F32, "bsp")
        S.op("sp", lambda e: e.dma_start(out=g1col[:], in_=norm1_g.ap().rearrange("(c p) -> p c", p=128),
                                         allow_slow_non_contiguous=True), writes=["g1col"], dma="g1col")
        for i in range(4):
            S.op("sp", lambda e, i=i: e.dma_start(out=cw[:, i, :], in_=conv_qk.ap()[i, :].rearrange("(c p) -> p c", p=128),
                                                  allow_slow_non_contiguous=True), accw=["cw"], dma=("cw", i))
        S.op("sp", lambda e: e.dma_start(out=bsp[:], in_=b_sp.ap().rearrange("g t -> t g"),
                                         allow_slow_non_contiguous=True), writes=["bsp"], dma="bsp")

        ptr = [S.ps([128, 1024], BF16, f"ptr{i}") for i in range(2)]
        pfb = [S.ps([128, 512], F32, f"pf{i}") for i in range(6)]
        rr = {"t": 0, "f": 0, "A": 0, "B": 0}

        def tbank():
            i = rr["t"] % 2
            rr["t"] += 1
            return ptr[i], ("ptr", i)

        def fbank():
            i = rr["f"] % 6
            rr["f"] += 1
            return pfb[i], ("pf", i)

        def fbankA():
            i = rr["A"] % 3
            rr["A"] += 1
            return pfb[i], ("pf", i)

        def fbankB():
            i = 3 + rr["B"] % 2
            rr["B"] += 1
            return pfb[i], ("pf", i)

        def fbankC():
            return pfb[5], ("pf", 5)

        junk = S.sb([128, 1024], BF16, "junk", persist=True)
        base_b = S.sb([128, 32], F32, "base_b", persist=True)
        E1s = S.sb([128, NT, 32], BF16, "E1s", persist=True)
        E2s = S.sb([128, NT, 32], BF16, "E2s", persist=True)
        pw = S.sb([128, NT, 4], F32, "pw", persist=True)
        stA = contextlib.ExitStack()
        S.cur = stA
        xbuf = [S.sb([128, 4, 1024], F32, f"xbuf{i}") for i in range(2)]
        wqk = S.sb([128, 8, 1024], BF16, "wqk")
        wvo = S.sb([128, 8, 1024], BF16, "wvo")
        wuv = S.sb([128, 8, 1024], BF16, "wuv")
        wif = S.sb([128, 8, 8], BF16, "wif")
        wout = S.sb([128, 8, 1024], BF16, "wout")
        for kc in range(8):
            sl = kc % 2
            stg = xbuf[sl][:].rearrange("p a b -> p (a b)")
            S.op("sp", lambda e, stg=stg, kc=kc: e.dma_start(out=stg[:, 0:3080], in_=w_in.ap()[kc * 128:(kc + 1) * 128, :]),
                 writes=[("x", sl)], dma=("x", sl))
            for (dst, c0, n, tok) in ((wqk, 0, 1024, "wqk"), (wvo, 1024, 1024, "wvo"), (wif, 2048, 8, "wif"), (wuv, 2056, 1024, "wuv")):
                S.op("act", lambda e, dst=dst, c0=c0, n=n, kc=kc, stg=stg: e.mul(dst[:, kc, 0:n], stg[:, c0:c0 + n], g1col[:, kc:kc + 1]),
                     reads=[("x", sl), "g1col"], accw=[tok])
        S.op("pool", lambda e: e.dma_start(out=wout[:], in_=w_out.ap().rearrange("(c p) n -> p c n", p=128)),
             writes=["wout"], dma="wout")

        wsp_f = xbuf[1][:, 3, :].rearrange("p (g s) -> p g s", g=8)
        wspT = S.sb([128, 8, 128], BF16, "wspT")
        S.op("sp", lambda e: e.dma_start(out=wsp_f, in_=w_sp.ap().rearrange("g t s -> t g s")), writes=[("x", 1)], dma=("x", 1))
        S.op("dve", lambda e: e.tensor_tensor(out=wsp_f, in0=wsp_f, in1=tril_f[:].unsqueeze(1).broadcast_to([128, 8, 128]), op=ALU.mult),
             reads=[("x", 1), "tril_f"], writes=[("x", 1)])
        for half in range(2):
            pb, pt = fbank()
            for g4 in range(4):
                g = half * 4 + g4
                S.op("pe", lambda e, pb=pb, g=g, g4=g4: e.transpose(out=pb[:, g4 * 128:(g4 + 1) * 128], in_=wsp_f[:, g, :], identity=ident_f[:]),
                     reads=[("x", 1), "ident_f"], accw=[pt])
            S.op("dve", lambda e, pb=pb, half=half: e.tensor_copy(out=wspT[:, half * 4:(half + 1) * 4, :].rearrange("p a b -> p (a b)"), in_=pb[:]),
                 reads=[pt], accw=["wspT"])

        C32 = S.sb([128, 4, 129], F32, "C32")
        Csb = S.sb([128, 4, 129], BF16, "Csb")
        m_st = S.sb([4, 1], F32, "m_st")
        halo = S.sb([128, 8, 3], F32, "halo")
        S.op("pool", lambda e: e.memset(C32[:], 0.0), writes=[("C32", h) for h in range(4)])
        S.op("pool", lambda e: e.memset(m_st[:], 0.0), writes=["m_st"])
        S.op("pool", lambda e: e.memset(halo[:], 0.0), writes=[("halo", c) for c in range(8)])

        xn = [S.sb([128, 1024], BF16, f"xn{i}") for i in range(2)]
        ss1 = S.sb([128, 8], F32, "ss1")
        xT = [S.sb([128, 8, 512], BF16, f"xT{i}") for i in range(1)]
        pre = [S.sb([128, 515], F32, f"pre{i}") for i in range(2)]
        cacc = [S.sb([128, 512], F32, f"cacc{i}") for i in range(2)]
        sg = [S.sb([128, 512], F32, f"sg{i}") for i in range(2)]
        qkT = [S.sb([128, 8, 512], BF16, f"qkT{i}") for i in range(1)]
        ktm = S.sb([128, 4, 128], BF16, "ktm")
        gsb = S.sb([128, 4, 8], F32, "gsb")
        spl = S.sb([128, 4, 4], F32, "spl")
        a_tm = S.sb([128, 4, 4], F32, "a_tm")
        cum_sb = S.sb([128, 4, 4], F32, "cum_sb")
        e_tm = S.sb([128, 4, 4], F32, "e_tm")
        dn_tm = S.sb([128, 4, 4], F32, "dn_tm")
        gb_sb = S.sb([128, 4, 8], F32, "gb_sb")
        rw = S.sb([4, 20], F32, "rw")
        rhs8 = S.sb([4, 4, 8], F32, "rhs8")
        lnc0_t = S.sb([128, 1], F32, "lnc0_t")
        S.op("pool", lambda e: e.memset(lnc0_t[:], -float(np.log(128 ** -0.5))), writes=["lnc0"])
        ve = [S.sb([128, 4, 129], BF16, f"ve{i}") for i in range(2)]
        smask = [S.sb([128, 4, 128], BF16, f"smask{i}") for i in range(2)]
        og = S.sb([128, 512], F32, "og")
        hs = S.sb([128, 4, 128], F32, "hs")
        nqa = S.sb([128, 4], F32, "nqa")
        sm8 = S.sb([128, 16], F32, "sm8")
        tmpa = S.sb([128, 1024], F32, "tmpa")
        ux = S.sb([128, 1024], F32, "ux")
        t1 = S.sb([128, 1024], F32, "t1")
        vn = S.sb([128, 512], BF16, "vn")
        gt = S.sb([128, 8, 64], F32, "gt")
        ybuf = [S.sb([128, 1024], BF16, f"ybuf{i}") for i in range(2)]
        yT = [S.sb([128, 8, 128], BF16, f"yT{i}") for i in range(2)]
        h1b = [S.sb([128, 1024], F32, f"h1b{i}") for i in range(2)]
        g2_b = bload("g2_b", norm2_g, 1024)
        br_b = bload("br_b", b_r, 36)
        wr_f = S.sb([128, 8, 36], F32, "wr_f")
        wr_b = S.sb([128, 8, 36], BF16, "wr_b")
        S.op("sp", lambda e: e.dma_start(out=wr_f[:], in_=w_r.ap().rearrange("(c p) n -> p c n", p=128)), writes=["wr_f"], dma="wr_f")
        S.op("dve", lambda e: e.tensor_copy(out=wr_b[:], in_=wr_f[:]), reads=["wr_f"], writes=["wr_b"])
        lstr_b = S.sb([128, 128], BF16, "lstr_b")
        ones_b = S.sb([128, 128], BF16, "ones_b")
        lstr_f = S.sb([128, 128], F32, "lstr_f")
        S.op("pool", lambda e: e.affine_select(out=lstr_f[:], in_=ones_f[:], pattern=[[1, 128]], compare_op=ALU.is_gt, fill=0.0, base=0, channel_multiplier=-1),
             reads=["ones_f"], writes=["lstr_f"])
        S.op("dve", lambda e: e.tensor_copy(out=lstr_b[:], in_=lstr_f[:]), reads=["lstr_f"], writes=["lstr_b"])
        S.op("dve", lambda e: e.tensor_copy(out=ones_b[:], in_=ones_f[:]), reads=["ones_f"], writes=["ones_b"])
        xn2t = [S.sb([128, 1024], BF16, f"xn2t{i}") for i in range(2)]
        xn2T = [S.sb([128, 8, 128], BF16, f"xn2T{i}") for i in range(1)]
        lg = S.sb([128, 36], F32, "lg")
        rt = S.sb([128, 64], F32, "rt")
        t32 = S.sb([128, 4, 8], F32, "t32")
        le8 = S.sb([128, 16], F32, "le8")
        ind_b = S.sb([128, 32], BF16, "ind_b")
        posf = S.sb([128, 32], F32, "posf")
        S.op("pool", lambda e: e.memset(base_b[:], 0.0), writes=["base_b"])
        xn2lin = nc.dram_tensor("xn2lin", [TOK, D], BF16)
        cnt = {"tile": 0, "st": 0, "ch": 0, "tl2": 0, "y": 0}

        LN_C0 = float(np.log(128 ** -0.5))

        def dump(name, ap_fn, reads, row0=0):
            if name in dbg_t:
                t = dbg_t[name]
                finals.append(S.op("sp", lambda e: e.dma_start(out=t.ap()[row0:row0 + 128, :], in_=ap_fn()), reads=reads, dma=("dbg", name)))

        def supertile(xsrc, st_i, mode, gi):
            main = mode == "main"
            sl = cnt["st"] % 2
            cnt["st"] += 1
            xb = xbuf[sl]
            xt = xT[0]
            qk = qkT[0]
            S.op("sp", lambda e: e.dma_start(out=xb[:], in_=xsrc.ap()[st_i * 512:(st_i + 1) * 512, :].rearrange("(j p) d -> p j d", p=128)),
                 writes=[("x", sl)], dma=("x", sl))
            conv_some(gi, sl)
            for j in range(4):
                tl = cnt["tile"] % 2
                cnt["tile"] += 1
                xnj = xn[tl]
                S.op("dve", lambda e, j=j: e.memset(ss1[:, j:j + 1], 0.0), writes=[("ss1", j)])
                S.op("act", lambda e, j=j: e.activation(out=junk[:], in_=xb[:, j, :], func=AF.Square, accum_out=ss1[:, j:j + 1]),
                     reads=[("x", sl), ("ss1", j)], writes=["junk", ("ss1", j)])
                S.op("act", lambda e, j=j: e.activation(out=ss1[:, 4 + j:5 + j], in_=ss1[:, j:j + 1], func=AF.Ln, scale=1.0 / D, bias=EPS),
                     reads=[("ss1", j)], writes=[("ss1", 4 + j)])
                S.op("act", lambda e, j=j: e.activation(out=ss1[:, 4 + j:5 + j], in_=ss1[:, 4 + j:5 + j], func=AF.Exp, scale=-0.5),
                     reads=[("ss1", 4 + j)], writes=[("ss1", 4 + j)])
                S.op("act", lambda e, j=j, xnj=xnj: e.mul(xnj[:], xb[:, j, :], ss1[:, 4 + j:5 + j]),
                     reads=[("x", sl), ("ss1", 4 + j)], writes=[("xn", tl)])
                pb, pt = tbank()
                for c in range(8):
                    S.op("pe", lambda e, c=c, pb=pb, xnj=xnj: e.transpose(out=pb[:, c * 128:(c + 1) * 128], in_=xnj[:, c * 128:(c + 1) * 128], identity=ident_b[:]),
                         reads=[("xn", tl), "ident_b"], accw=[pt])
                S.op("dve" if j % 2 == 0 else "act",
                     (lambda e, pb=pb, j=j: e.tensor_copy(out=xt[:, :, j * 128:(j + 1) * 128], in_=pb[:].rearrange("p (c t) -> p c t", c=8))) if j % 2 == 0 else
                     (lambda e, pb=pb, j=j: e.copy(out=xt[:, :, j * 128:(j + 1) * 128], in_=pb[:].rearrange("p (c t) -> p c t", c=8))),
                     reads=[pt], accw=[("xT", 0)])
            chunks = range(8) if mode != "pre" else range(4, 8)
            def chunk_s1(ch):
                pb, pt = fbank()
                for kc in range(8):
                    S.op("pe", lambda e, kc=kc, ch=ch, pb=pb: e.matmul(out=pb[:], lhsT=wqk[:, kc, ch * 128:(ch + 1) * 128], rhs=xt[:, kc, :], start=(kc == 0), stop=(kc == 7)),
                         reads=[("xT", 0), "wqk"], accw=[pt])
                ps_ = cnt["ch"] % 2
                cnt["ch"] += 1
                pr = pre[ps_]
                ca = cacc[ps_]
                s_ = sg[ps_]
                S.op("dve", lambda e, pr=pr, ch=ch: e.tensor_copy(out=pr[:, 0:3], in_=halo[:, ch, :]), reads=[("halo", ch)], writes=[("pre", ps_, "h")])
                S.op("act", lambda e, pr=pr, pb=pb: e.copy(out=pr[:, 3:515], in_=pb[:]), reads=[pt], writes=[("pre", ps_)])
                S.op("dve", lambda e, pr=pr, ch=ch: e.tensor_copy(out=halo[:, ch, :], in_=pr[:, 512:515]), reads=[("pre", ps_), ("pre", ps_, "h")], writes=[("halo", ch)])
                S.op("dve", lambda e, pr=pr, ca=ca, ch=ch: e.tensor_scalar(out=ca[:], in0=pr[:, 0:512], scalar1=cw[:, 0, ch:ch + 1], scalar2=None, op0=ALU.mult),
                     reads=[("pre", ps_), ("pre", ps_, "h"), "cw"], writes=[("cacc", ps_)])
                for i in range(1, 4):
                    S.op("dve", lambda e, pr=pr, ca=ca, ch=ch, i=i: e.scalar_tensor_tensor(out=ca[:], in0=pr[:, i:i + 512], scalar=cw[:, i, ch:ch + 1], in1=ca[:], op0=ALU.mult, op1=ALU.add),
                         reads=[("pre", ps_), ("pre", ps_, "h"), ("cacc", ps_), "cw"], writes=[("cacc", ps_)])
                return (ch, ps_, ca, s_)

            def chunk_s2(args):
                ch, ps_, ca, s_ = args
                S.op("act", lambda e, ca=ca, s_=s_: e.activation(out=s_[:], in_=ca[:], func=AF.Exp, scale=-1.0), reads=[("cacc", ps_)], writes=[("sg", ps_)])
                S.op("act", lambda e, s_=s_: e.activation(out=s_[:], in_=s_[:], func=AF.Ln, bias=1.0), reads=[("sg", ps_)], writes=[("sg", ps_)])
                S.op("act", lambda e, s_=s_: e.activation(out=s_[:], in_=s_[:], func=AF.Exp, scale=-1.0), reads=[("sg", ps_)], writes=[("sg", ps_)])
                S.op("dve", lambda e, s_=s_, ca=ca, ch=ch: e.tensor_tensor(out=qk[:, ch, :], in0=s_[:], in1=ca[:], op=ALU.mult),
                     reads=[("sg", ps_), ("cacc", ps_)], accw=[("qkT", 0)])


            pend = None
            for ch in chunks:
                cur = chunk_s1(ch)
                if pend is not None:
                    chunk_s2(pend)
                pend = cur
            chunk_s2(pend)
            pg, pgt = fbank()
            for j in range(4):
                for kc in range(8):
                    S.op("pe", lambda e, j=j, kc=kc: e.matmul(out=pg[:, j * 8:(j + 1) * 8], lhsT=xt[:, kc, j * 128:(j + 1) * 128], rhs=wif[:, kc, :], start=(kc == 0), stop=(kc == 7)),
                         reads=[("xT", 0), "wif"], accw=[pgt])
            S.op("dve", lambda e: e.tensor_tensor(out=gsb[:], in0=pg[:, 0:32].rearrange("p (c g) -> p c g", c=4), in1=bif_b[:].unsqueeze(1).broadcast_to([128, 4, 8]), op=ALU.add),
                 reads=[pgt, "bif_b"], writes=["gsb"])
            S.op("act", lambda e: e.activation(out=spl[:], in_=gsb[:, :, 4:8], func=AF.Exp, scale=-1.0), reads=["gsb"], writes=["spl"])
            S.op("act", lambda e: e.activation(out=spl[:], in_=spl[:], func=AF.Ln, bias=1.0), reads=["spl"], writes=["spl"])
            pc, pct = fbank()
            prw, prt = fbank()
            for c in range(4):
                S.op("pe", lambda e, c=c: e.matmul(out=pc[:, c * 4:(c + 1) * 4], lhsT=triu_f[:], rhs=spl[:, c, :], start=True, stop=True),
                     reads=["spl", "triu_f"], accw=[pct])
                S.op("pe", lambda e, c=c: e.matmul(out=pc[0:4, 16 + c:17 + c], lhsT=spl[:, c, :], rhs=ones_f[:, 0:1], start=True, stop=True),
                     reads=["spl", "ones_f"], accw=[pct])
                S.op("pe", lambda e, c=c: e.matmul(out=prw[0:4, c * 128:(c + 1) * 128], lhsT=gsb[:, c, 0:4], rhs=ident_f[:], start=True, stop=False),
                     reads=["gsb", "ident_f"], accw=[prt])
                S.op("pe", lambda e, c=c: e.matmul(out=prw[0:4, c * 128:(c + 1) * 128], lhsT=spl[:, c, :], rhs=triu_f[:], start=False, stop=True),
                     reads=["spl", "triu_f"], accw=[prt])
            S.op("dve", lambda e: e.tensor_tensor(out=a_tm[:], in0=gsb[:, :, 0:4], in1=pc[:, 0:16].rearrange("p (c h) -> p c h", c=4), op=ALU.add),
                 reads=["gsb", pct], writes=["a_tm"])
            S.op("dve", lambda e: e.tensor_copy(out=cum_sb[:], in_=pc[:, 0:16].rearrange("p (c h) -> p c h", c=4)), reads=[pct], writes=["cum_sb"])
            S.op("dve", lambda e: e.tensor_copy(out=rw[:, 4:8], in_=pc[0:4, 16:20]), reads=[pct], writes=["rw_tot"])
            S.op("dve", lambda e: e.tensor_reduce(out=rw[:, 0:4], in_=prw[0:4, :].rearrange("p (c l) -> p c l", c=4), axis=AX.X, op=ALU.max),
                 reads=[prt], writes=["rw_A"])
            for c in range(4):
                S.op("dve", lambda e, c=c: e.tensor_copy(out=rw[:, 12 + c:13 + c], in_=m_st[:]), reads=["m_st"], writes=[("rw_mp", c)])
                S.op("dve", lambda e, c=c: e.tensor_tensor(out=rw[:, 8 + c:9 + c], in0=m_st[:], in1=rw[:, c:c + 1], op=ALU.max), reads=["m_st", "rw_A"], writes=[("rw_G", c)])
                S.op("dve", lambda e, c=c: e.tensor_tensor(out=m_st[:], in0=rw[:, 8 + c:9 + c], in1=rw[:, 4 + c:5 + c], op=ALU.subtract), reads=[("rw_G", c), "rw_tot"], writes=["m_st"])
            S.op("dve", lambda e: e.tensor_tensor(out=rw[:, 16:20], in0=rw[:, 12:16], in1=rw[:, 8:12], op=ALU.subtract),
                 reads=[("rw_G", c) for c in range(4)] + [("rw_mp", c) for c in range(4)], writes=["rw_D"])
            S.op("act", lambda e: e.activation(out=rw[:, 16:20], in_=rw[:, 16:20], func=AF.Exp), reads=["rw_D"], writes=["rw_D"])
            S.op("dve", lambda e: e.tensor_tensor(out=rhs8[:, :, 0:4], in0=ident_f[0:4, 0:4].unsqueeze(1).broadcast_to([4, 4, 4]), in1=rw[:, 8:12].unsqueeze(2).broadcast_to([4, 4, 4]), op=ALU.mult),
                 reads=[("rw_G", c) for c in range(4)] + ["ident_f"], writes=["rhs8a"])
            S.op("dve", lambda e: e.tensor_tensor(out=rhs8[:, :, 4:8], in0=ident_f[0:4, 0:4].unsqueeze(1).broadcast_to([4, 4, 4]), in1=rw[:, 16:20].unsqueeze(2).broadcast_to([4, 4, 4]), op=ALU.mult),
                 reads=["rw_D", "ident_f"], writes=["rhs8b"])
            S.op("pe", lambda e: e.matmul(out=pc[:, 32:64], lhsT=ones_f[0:4, :], rhs=rhs8[:].rearrange("p c g -> p (c g)"), start=True, stop=True),
                 reads=["rhs8a", "rhs8b", "ones_f"], accw=[pct])
            S.op("dve", lambda e: e.tensor_copy(out=gb_sb[:], in_=pc[:, 32:64].rearrange("p (c g) -> p c g", c=4)), reads=[pct], writes=["gb_sb"])
            S.op("dve", lambda e: e.tensor_tensor(out=e_tm[:], in0=a_tm[:], in1=gb_sb[:, :, 0:4], op=ALU.subtract), reads=["a_tm", "gb_sb"], writes=["e_tm"])
            S.op("act", lambda e: e.activation(out=e_tm[:], in_=e_tm[:], func=AF.Exp), reads=["e_tm"], writes=["e_tm"])
            S.op("dve", lambda e: e.tensor_tensor(out=dn_tm[:], in0=cum_sb[:], in1=gb_sb[:, :, 0:4], op=ALU.subtract), reads=["cum_sb", "gb_sb"], writes=["dn_tm"])
            S.op("act", lambda e: e.activation(out=dn_tm[:], in_=dn_tm[:], func=AF.Exp, bias=lnc0_t[:, 0:1]), reads=["dn_tm", "lnc0"], writes=["dn_tm"])

            def tile_body(j):
                c = j
                csl = slice(c * 128, (c + 1) * 128)
                tsl = cnt["tl2"] % 2
                cnt["tl2"] += 1
                vea = ve[tsl]
                ysl = cnt["y"] % 2
                if main:
                    cnt["y"] += 1
                yt_ = ybuf[ysl]
                def chainA():
                    pv, pvt = fbankA()
                    for kc in range(8):
                        yield S.op("pe", lambda e, kc=kc, pv=pv: e.matmul(out=pv[:], lhsT=xt[:, kc, csl], rhs=wvo[:, kc, 0:512], start=(kc == 0), stop=(kc == 7)),
                             reads=[("xT", 0), "wvo"], accw=[pvt])
                    yield S.op("dve", lambda e, pv=pv, vea=vea: e.tensor_tensor(out=vea[:, :, 0:128], in0=pv[:].rearrange("p (h d) -> p h d", h=4), in1=e_tm[:, c, :].unsqueeze(2).broadcast_to([128, 4, 128]), op=ALU.mult),
                         reads=[pvt, "e_tm"], writes=[("ve", tsl)])
                    yield S.op("dve", lambda e, vea=vea: e.tensor_copy(out=vea[:, :, 128:129], in_=e_tm[:, c, :].unsqueeze(2)), reads=["e_tm"], writes=[("ve", tsl, "e")])
                    pb, pt = (ptr[0], ("ptr", 0))
                    for h in range(4):
                        yield S.op("pe", lambda e, h=h, pb=pb: e.transpose(out=pb[:, h * 128:(h + 1) * 128], in_=qk[:, 4 + h, csl], identity=ident_b[:]),
                             reads=[("qkT", 0), "ident_b"], accw=[pt])
                    yield S.op("act", lambda e, pb=pb: e.copy(out=ktm[:].rearrange("p h d -> p (h d)"), in_=pb[:, 0:512]), reads=[pt], writes=["ktm"])
                    if main:
                        po, pot = fbankA()
                        for kc in range(8):
                            yield S.op("pe", lambda e, kc=kc, po=po: e.matmul(out=po[:], lhsT=xt[:, kc, csl], rhs=wvo[:, kc, 512:1024], start=(kc == 0), stop=(kc == 7)),
                                 reads=[("xT", 0), "wvo"], accw=[pot])
                        yield S.op("act", lambda e, po=po: e.activation(out=og[:], in_=po[:], func=AF.Exp, scale=-1.0), reads=[pot], writes=["og"])
                        yield S.op("act", lambda e: e.activation(out=og[:], in_=og[:], func=AF.Ln, bias=1.0), reads=["og"], writes=["og"])
                        yield S.op("act", lambda e: e.activation(out=og[:], in_=og[:], func=AF.Exp, scale=-1.0), reads=["og"], writes=["og"])
                        pS, pSt = fbankA()
                        for h in range(4):
                            yield S.op("pe", lambda e, h=h, pS=pS: e.matmul(out=pS[:, h * 128:(h + 1) * 128], lhsT=qk[:, 4 + h, csl], rhs=qk[:, h, csl], start=True, stop=True),
                                 reads=[("qkT", 0)], accw=[pSt])
                        sm = smask[tsl]
                        yield S.op("dve", lambda e, pS=pS, sm=sm: e.tensor_tensor(out=sm[:], in0=pS[:].rearrange("p (h l) -> p h l", h=4), in1=triu_f[:].unsqueeze(1).broadcast_to([128, 4, 128]), op=ALU.mult),
                             reads=[pSt, "triu_f"], writes=[("smask", tsl)])
                        for h in range(4):
                            yield S.op("act", lambda e, h=h: e.mul(Csb[:, h, :], C32[:, h, :], gb_sb[:, c, 4 + h:5 + h]), reads=[("C32", h), "gb_sb"], writes=[("Csb", h)])
                        nbanks = []
                        for hp in range(2):
                            pn, pnt = fbankA()
                            nbanks.append((pn, pnt))
                            for hh in range(2):
                                h = hp * 2 + hh
                                yield S.op("pe", lambda e, h=h, hh=hh, pn=pn, sm=sm, vea=vea: e.matmul(out=pn[:, hh * 129:(hh + 1) * 129], lhsT=sm[:, h, :], rhs=vea[:, h, :], start=True, stop=False),
                                     reads=[("smask", tsl), ("ve", tsl), ("ve", tsl, "e")], accw=[pnt])
                                yield S.op("pe", lambda e, h=h, hh=hh, pn=pn: e.matmul(out=pn[:, hh * 129:(hh + 1) * 129], lhsT=qk[:, h, csl], rhs=Csb[:, h, :], start=False, stop=True),
                                     reads=[("qkT", 0), ("Csb", h)], accw=[pnt])
                        for hp in range(2):
                            pn, pnt = nbanks[hp]
                            yield S.op("act", lambda e, pn=pn, hp=hp: e.activation(out=nqa[:, hp * 2:hp * 2 + 2].unsqueeze(2), in_=pn[:, 0:258].rearrange("p (h d) -> p h d", h=2)[:, :, 128:129], func=AF.Abs),
                                 reads=[pnt], writes=["nqa"])
                        yield S.op("dve", lambda e: e.tensor_tensor(out=nqa[:], in0=nqa[:], in1=dn_tm[:, c, :], op=ALU.max), reads=["nqa", "dn_tm"], writes=["nqa"])
                        yield S.op("dve", lambda e: e.reciprocal(out=nqa[:], in_=nqa[:]), reads=["nqa"], writes=["nqa"])
                        for hp in range(2):
                            pn, pnt = nbanks[hp]
                            yield S.op("dve", lambda e, pn=pn, hp=hp: e.tensor_tensor(out=hs[:, hp * 2:hp * 2 + 2, :], in0=pn[:, 0:258].rearrange("p (h d) -> p h d", h=2)[:, :, 0:128], in1=nqa[:, hp * 2:hp * 2 + 2].unsqueeze(2).broadcast_to([128, 2, 128]), op=ALU.mult),
                                 reads=[pnt, "nqa"], writes=["hs"])
                        yield S.op("dve", lambda e: e.tensor_tensor(out=hs[:].rearrange("p h d -> p (h d)"), in0=hs[:].rearrange("p h d -> p (h d)"), in1=og[:], op=ALU.mult),
                             reads=["hs", "og"], writes=["hs"])
                        yield S.op("dve", lambda e: e.tensor_tensor(out=tmpa[:, 0:512], in0=hs[:].rearrange("p h d -> p (h d)"), in1=hs[:].rearrange("p h d -> p (h d)"), op=ALU.mult), reads=["hs"], writes=["tmpa"])
                        yield S.op("dve", lambda e: e.tensor_reduce(out=sm8[:, 0:4], in_=tmpa[:, 0:512].rearrange("p (h d) -> p h d", h=4), axis=AX.X, op=ALU.add), reads=["tmpa"], writes=["sm8a"])
                        yield S.op("act", lambda e: e.activation(out=sm8[:, 0:4], in_=sm8[:, 0:4], func=AF.Ln, scale=1.0 / 128, bias=EPS), reads=["sm8a"], writes=["sm8a"])
                        yield S.op("act", lambda e: e.activation(out=sm8[:, 0:4], in_=sm8[:, 0:4], func=AF.Exp, scale=-0.5), reads=["sm8a"], writes=["sm8a"])
                        yield S.op("dve", lambda e: e.tensor_tensor(out=hs[:], in0=hs[:], in1=sm8[:, 0:4].unsqueeze(2).broadcast_to([128, 4, 128]), op=ALU.mult), reads=["hs", "sm8a"], writes=["hs"])
                        yield S.op("dve", lambda e, yt_=yt_: e.tensor_tensor(out=yt_[:, 0:512], in0=hs[:].rearrange("p h d -> p (h d)"), in1=gml_b[:], op=ALU.mult), reads=["hs", "gml_b"], writes=[("y", ysl, 0)])
                    for hp in range(2):
                        pu_, put = fbankA()
                        for hh in range(2):
                            h = hp * 2 + hh
                            yield S.op("pe", lambda e, h=h, hh=hh, pu_=pu_, vea=vea: e.matmul(out=pu_[:, hh * 129:(hh + 1) * 129], lhsT=ktm[:, h, :], rhs=vea[:, h, :], start=True, stop=True),
                                 reads=["ktm", ("ve", tsl), ("ve", tsl, "e")], accw=[put])
                        for hh in range(2):
                            h = hp * 2 + hh
                            yield S.op("dve", lambda e, h=h, hh=hh, pu_=pu_: e.scalar_tensor_tensor(out=C32[:, h, :], in0=C32[:, h, :], scalar=gb_sb[:, c, 4 + h:5 + h], in1=pu_[:, hh * 129:(hh + 1) * 129], op0=ALU.mult, op1=ALU.add),
                                 reads=[put, "gb_sb", ("C32", h)], writes=[("C32", h)])

                    yield None
                def chainB():
                    pU, pUt = fbankB()
                    pV, pVt = fbankB()
                    for kc in range(8):
                        yield S.op("pe", lambda e, kc=kc, pU=pU: e.matmul(out=pU[:], lhsT=xt[:, kc, csl], rhs=wuv[:, kc, 0:512], start=(kc == 0), stop=(kc == 7)),
                             reads=[("xT", 0), "wuv"], accw=[pUt])
                    for kc in range(8):
                        yield S.op("pe", lambda e, kc=kc, pV=pV: e.matmul(out=pV[:], lhsT=xt[:, kc, csl], rhs=wuv[:, kc, 512:1024], start=(kc == 0), stop=(kc == 7)),
                             reads=[("xT", 0), "wuv"], accw=[pVt])
                    yield S.op("act", lambda e, pU=pU: e.copy(out=ux[:, 0:512], in_=pU[:]), reads=[pUt], writes=["ux"])
                    yield S.op("act", lambda e, pV=pV: e.copy(out=ux[:, 512:1024], in_=pV[:]), reads=[pVt], writes=["ux"])
                    yield S.op("act", lambda e: e.activation(out=t1[:], in_=ux[:], func=AF.Square), reads=["ux"], writes=["t1"])
                    yield S.op("dve", lambda e: e.tensor_scalar(out=t1[:], in0=t1[:], scalar1=0.044715, scalar2=1.0, op0=ALU.mult, op1=ALU.add), reads=["t1"], writes=["t1"])
                    yield S.op("dve", lambda e: e.tensor_tensor(out=t1[:], in0=t1[:], in1=ux[:], op=ALU.mult), reads=["t1", "ux"], writes=["t1"])
                    yield S.op("act", lambda e: e.activation(out=t1[:], in_=t1[:], func=AF.Exp, scale=-2.0 * 0.7978845608028654), reads=["t1"], writes=["t1"])
                    yield S.op("act", lambda e: e.activation(out=t1[:], in_=t1[:], func=AF.Ln, bias=1.0), reads=["t1"], writes=["t1"])
                    yield S.op("act", lambda e: e.activation(out=t1[:], in_=t1[:], func=AF.Exp, scale=-1.0), reads=["t1"], writes=["t1"])
                    yield S.op("dve", lambda e: e.tensor_tensor(out=ux[:], in0=t1[:], in1=ux[:], op=ALU.mult), reads=["t1", "ux"], writes=["ux"])
                    yield S.op("dve", lambda e: e.memset(sm8[:, 4:5], 0.0), writes=["sm8b"])
                    yield S.op("act", lambda e: e.activation(out=junk[:, 0:512], in_=ux[:, 512:1024], func=AF.Square, accum_out=sm8[:, 4:5]), reads=["ux", "sm8b"], writes=["junk", "sm8b"])
                    yield S.op("act", lambda e: e.activation(out=sm8[:, 4:5], in_=sm8[:, 4:5], func=AF.Ln, scale=1.0 / 512, bias=EPS), reads=["sm8b"], writes=["sm8b"])
                    yield S.op("act", lambda e: e.activation(out=sm8[:, 4:5], in_=sm8[:, 4:5], func=AF.Exp, scale=-0.5), reads=["sm8b"], writes=["sm8b"])
                    yield S.op("dve", lambda e: e.scalar_tensor_tensor(out=vn[:], in0=ux[:, 512:1024], scalar=sm8[:, 4:5], in1=ggv_b[:], op0=ALU.mult, op1=ALU.mult), reads=["ux", "sm8b", "ggv_b"], writes=["vn"])
                    pM, pMt = fbankB()
                    for g in range(8):
                        yield S.op("pe", lambda e, g=g, pM=pM: e.matmul(out=pM[:, g * 64:(g + 1) * 64], lhsT=wspT[:, g, :], rhs=vn[:, g * 64:(g + 1) * 64], start=True, stop=True),
                             reads=["vn", "wspT"], accw=[pMt])
                    yield S.op("dve", lambda e, pM=pM: e.tensor_tensor(out=gt[:], in0=pM[:].rearrange("p (g c) -> p g c", g=8), in1=bsp[:].unsqueeze(2).broadcast_to([128, 8, 64]), op=ALU.add),
                         reads=[pMt, "bsp"], writes=["gt"])
                    yield S.op("dve", lambda e: e.tensor_tensor(out=gt[:].rearrange("p g c -> p (g c)"), in0=gt[:].rearrange("p g c -> p (g c)"), in1=ux[:, 0:512], op=ALU.mult), reads=["gt", "ux"], writes=["gt"])
                    yield S.op("dve", lambda e: e.tensor_tensor(out=tmpa[:, 512:1024], in0=gt[:].rearrange("p g c -> p (g c)"), in1=gt[:].rearrange("p g c -> p (g c)"), op=ALU.mult), reads=["gt"], writes=["tmpb"])
                    yield S.op("dve", lambda e: e.tensor_reduce(out=sm8[:, 8:16], in_=tmpa[:, 512:1024].rearrange("p (g c) -> p g c", g=8), axis=AX.X, op=ALU.add), reads=["tmpb"], writes=["sm8c"])
                    yield S.op("act", lambda e: e.activation(out=sm8[:, 8:16], in_=sm8[:, 8:16], func=AF.Ln, scale=1.0 / 64, bias=EPS), reads=["sm8c"], writes=["sm8c"])
                    yield S.op("act", lambda e: e.activation(out=sm8[:, 8:16], in_=sm8[:, 8:16], func=AF.Exp, scale=-0.5), reads=["sm8c"], writes=["sm8c"])
                    yield S.op("dve", lambda e: e.tensor_tensor(out=gt[:], in0=gt[:], in1=sm8[:, 8:16].unsqueeze(2).broadcast_to([128, 8, 64]), op=ALU.mult), reads=["gt", "sm8c"], writes=["gt"])
                    yield S.op("dve", lambda e, yt_=yt_: e.tensor_tensor(out=yt_[:, 512:1024], in0=gt[:].rearrange("p g c -> p (g c)"), in1=ggo_b[:], op=ALU.mult), reads=["gt", "ggo_b"], writes=[("y", ysl, 1)])

                    yield None
                def chainC():
                    if "y" in dbg_t:
                        row0d = st_i * 512 + j * 128
                        finals.append(S.op("pool", lambda e, yt_=yt_, row0d=row0d: e.dma_start(out=dbg_t["y"].ap()[row0d:row0d + 128, :], in_=yt_[:]), reads=[("y", ysl, 0), ("y", ysl, 1)], dma=("dbgy", ysl)))
                    pb, pt = (ptr[1], ("ptr", 1))
                    for ec in range(8):
                        yield S.op("pe", lambda e, ec=ec, pb=pb, yt_=yt_: e.transpose(out=pb[:, ec * 128:(ec + 1) * 128], in_=yt_[:, ec * 128:(ec + 1) * 128], identity=ident_b[:]),
                             reads=[("y", ysl, 0), ("y", ysl, 1), "ident_b"], accw=[pt])
                    yT_ = yT[ysl]
                    yield S.op("act", lambda e, pb=pb, yT_=yT_: e.copy(out=yT_[:].rearrange("p c t -> p (c t)"), in_=pb[:]), reads=[pt], writes=[("yT", ysl)])
                    h1t = h1b[ysl]
                    for hf in range(2):
                        ph, pht = fbankC()
                        for ec in range(8):
                            yield S.op("pe", lambda e, ec=ec, ph=ph, hf=hf, yT_=yT_: e.matmul(out=ph[:], lhsT=yT_[:, ec, :], rhs=wout[:, ec, hf * 512:(hf + 1) * 512], start=(ec == 0), stop=(ec == 7)),
                                 reads=[("yT", ysl), "wout"], accw=[pht])
                        yield S.op("dve", lambda e, ph=ph, hf=hf, h1t=h1t: e.tensor_tensor(out=h1t[:, hf * 512:(hf + 1) * 512], in0=ph[:], in1=xb[:, j, hf * 512:(hf + 1) * 512], op=ALU.add),
                             reads=[pht, ("x", sl)], writes=[("h1", ysl, hf)])
                    row0 = st_i * 512 + j * 128
                    yield S.op("sp", lambda e, h1t=h1t, row0=row0: e.dma_start(out=h1buf.ap()[row0:row0 + 128, :], in_=h1t[:]), reads=[("h1", ysl, 0), ("h1", ysl, 1)], accw=["h1buf"], dma=("h1st", ysl))
                    if "h1" in dbg_t:
                        finals.append(S.op("sp", lambda e, h1t=h1t, row0=row0: e.dma_start(out=dbg_t["h1"].ap()[row0:row0 + 128, :], in_=h1t[:]), reads=[("h1", ysl, 0), ("h1", ysl, 1)], dma=("dbgh1", ysl)))
                    ti = st_i * 4 + j
                    x2 = xn2t[ysl]
                    yield S.op("dve", lambda e: e.memset(sm8[:, 5:6], 0.0), writes=["sm8d"])
                    yield S.op("act", lambda e: e.activation(out=junk[:], in_=h1t[:], func=AF.Square, accum_out=sm8[:, 5:6]), reads=[("h1", ysl, 0), ("h1", ysl, 1), "sm8d"], writes=["junk", "sm8d"])
                    yield S.op("act", lambda e: e.activation(out=sm8[:, 5:6], in_=sm8[:, 5:6], func=AF.Ln, scale=1.0 / D, bias=EPS), reads=["sm8d"], writes=["sm8d"])
                    yield S.op("act", lambda e: e.activation(out=sm8[:, 5:6], in_=sm8[:, 5:6], func=AF.Exp, scale=-0.5), reads=["sm8d"], writes=["sm8d"])
                    yield S.op("dve", lambda e: e.scalar_tensor_tensor(out=x2[:], in0=h1t[:], scalar=sm8[:, 5:6], in1=g2_b[:], op0=ALU.mult, op1=ALU.mult),
                         reads=[("h1", ysl, 0), ("h1", ysl, 1), "sm8d", "g2_b"], writes=[("xn2", ysl)])
                    yield S.op("sp", lambda e: e.dma_start(out=xn2lin.ap()[row0:row0 + 128, :], in_=x2[:]), reads=[("xn2", ysl)], accw=["xn2lin"], dma=("xn2st", ysl))
                    pb2, pt2 = (ptr[1], ("ptr", 1))
                    for kc in range(8):
                        yield S.op("pe", lambda e, kc=kc: e.transpose(out=pb2[:, kc * 128:(kc + 1) * 128], in_=x2[:, kc * 128:(kc + 1) * 128], identity=ident_b[:]),
                             reads=[("xn2", ysl), "ident_b"], accw=[pt2])
                    x2T = xn2T[0]
                    yield S.op("act", lambda e: e.copy(out=x2T[:].rearrange("p c t -> p (c t)"), in_=pb2[:]), reads=[pt2], writes=[("xn2T", 0)])
                    pl, plt = fbankC()
                    for kc in range(8):
                        yield S.op("pe", lambda e, kc=kc: e.matmul(out=pl[:, 0:36], lhsT=x2T[:, kc, :], rhs=wr_b[:, kc, :], start=(kc == 0), stop=(kc == 7)),
                             reads=[("xn2T", 0), "wr_b"], accw=[plt])
                    yield S.op("dve", lambda e: e.tensor_tensor(out=lg[:], in0=pl[:, 0:36], in1=br_b[:], op=ALU.add), reads=[plt, "br_b"], writes=["lg"])
                    R_ = lambda a, b: rt[:, a:b]
                    yield S.op("dve", lambda e: e.tensor_reduce(out=R_(0, 1), in_=lg[:, 0:4], axis=AX.X, op=ALU.max), reads=["lg"], writes=["rt"])
                    yield S.op("dve", lambda e: e.tensor_scalar(out=R_(12, 16), in0=lg[:, 0:4], scalar1=R_(0, 1), scalar2=None, op0=ALU.is_equal), reads=["lg", "rt"], writes=["rt"])
                    yield S.op("dve", lambda e: e.tensor_scalar(out=R_(1, 2), in0=R_(0, 1), scalar1=-1.0, scalar2=None, op0=ALU.mult), reads=["rt"], writes=["rt"])
                    yield S.op("dve", lambda e: e.memset(R_(2, 3), 0.0), reads=["rt"], writes=["rt"])
                    yield S.op("act", lambda e: e.activation(out=le8[:, 8:12], in_=lg[:, 0:4], func=AF.Exp, bias=R_(1, 2), accum_out=R_(2, 3)), reads=["lg", "rt"], writes=["rt", "le8x"])
                    yield S.op("dve", lambda e: e.reciprocal(out=R_(3, 4), in_=R_(2, 3)), reads=["rt"], writes=["rt"])
                    yield S.op("dve", lambda e: e.tensor_tensor(out=t32[:], in0=lg[:, 4:36].rearrange("p (g j) -> p g j", g=4), in1=R_(12, 16).unsqueeze(2).broadcast_to([128, 4, 8]), op=ALU.mult), reads=["lg", "rt"], writes=["t32"])
                    yield S.op("dve", lambda e: e.tensor_reduce(out=le8[:, 0:8], in_=t32[:].rearrange("p g j -> p j g"), axis=AX.X, op=ALU.add), reads=["t32"], writes=["le8"])
                    yield S.op("dve", lambda e: e.tensor_reduce(out=R_(4, 5), in_=le8[:, 0:8], axis=AX.X, op=ALU.max), reads=["le8", "rt"], writes=["rt"])
                    yield S.op("dve", lambda e: e.tensor_scalar(out=R_(16, 24), in0=le8[:, 0:8], scalar1=R_(4, 5), scalar2=None, op0=ALU.is_equal), reads=["le8", "rt"], writes=["rt"])
                    yield S.op("dve", lambda e: e.scalar_tensor_tensor(out=le8[:, 0:8], in0=R_(16, 24), scalar=-1e30, in1=le8[:, 0:8], op0=ALU.mult, op1=ALU.add), reads=["le8", "rt"], writes=["le8"])
                    yield S.op("dve", lambda e: e.tensor_reduce(out=R_(5, 6), in_=le8[:, 0:8], axis=AX.X, op=ALU.max), reads=["le8", "rt"], writes=["rt"])
                    yield S.op("dve", lambda e: e.tensor_scalar(out=R_(24, 32), in0=le8[:, 0:8], scalar1=R_(5, 6), scalar2=None, op0=ALU.is_equal), reads=["le8", "rt"], writes=["rt"])
                    yield S.op("dve", lambda e: e.tensor_tensor(out=R_(6, 7), in0=R_(5, 6), in1=R_(4, 5), op=ALU.subtract), reads=["rt"], writes=["rt"])
                    yield S.op("act", lambda e: e.activation(out=R_(6, 7), in_=R_(6, 7), func=AF.Exp), reads=["rt"], writes=["rt"])
                    yield S.op("dve", lambda e: e.tensor_scalar(out=R_(7, 8), in0=R_(6, 7), scalar1=1.0, scalar2=None, op0=ALU.add), reads=["rt"], writes=["rt"])
                    yield S.op("dve", lambda e: e.reciprocal(out=R_(7, 8), in_=R_(7, 8)), reads=["rt"], writes=["rt"])
                    yield S.op("dve", lambda e: e.tensor_tensor(out=R_(8, 9), in0=R_(6, 7), in1=R_(7, 8), op=ALU.mult), reads=["rt"], writes=["rt"])
                    yield S.op("dve", lambda e: e.tensor_scalar(out=pw[:, ti, 2:4], in0=R_(7, 9), scalar1=R_(3, 4), scalar2=None, op0=ALU.mult), reads=["rt"], accw=["pw"])
                    E1 = E1s[:, ti, :]
                    E2 = E2s[:, ti, :]
                    yield S.op("dve", lambda e: e.tensor_tensor(out=E1.rearrange("p (g j) -> p g j", g=4), in0=R_(12, 16).unsqueeze(2).broadcast_to([128, 4, 8]), in1=R_(16, 24).unsqueeze(1).broadcast_to([128, 4, 8]), op=ALU.mult), reads=["rt"], accw=["E1s"])
                    yield S.op("dve", lambda e: e.tensor_tensor(out=E2.rearrange("p (g j) -> p g j", g=4), in0=R_(12, 16).unsqueeze(2).broadcast_to([128, 4, 8]), in1=R_(24, 32).unsqueeze(1).broadcast_to([128, 4, 8]), op=ALU.mult), reads=["rt"], accw=["E2s"])
                    yield S.op("dve", lambda e: e.tensor_tensor(out=ind_b[:], in0=E1, in1=E2, op=ALU.add), reads=["E1s", "E2s"], writes=["ind_b"])
                    pp, ppt = fbankC()
                    yield S.op("pe", lambda e: e.matmul(out=pp[:, 0:32], lhsT=lstr_b[:], rhs=ind_b[:], start=True, stop=True), reads=["ind_b", "lstr_b"], accw=[ppt])
                    yield S.op("pe", lambda e: e.matmul(out=pp[:, 32:64], lhsT=ones_b[:], rhs=ind_b[:], start=True, stop=True), reads=["ind_b", "ones_b"], accw=[ppt])
                    yield S.op("dve", lambda e: e.tensor_tensor(out=posf[:], in0=pp[:, 0:32], in1=base_b[:], op=ALU.add), reads=[ppt, "base_b"], writes=["posf"])
                    yield S.op("dve", lambda e: e.tensor_tensor(out=base_b[:], in0=pp[:, 32:64], in1=base_b[:], op=ALU.add), reads=[ppt, "base_b"], writes=["base_b"])
                    yield S.op("dve", lambda e: e.tensor_tensor(out=t32[:].rearrange("p g j -> p (g j)"), in0=E1, in1=posf[:], op=ALU.mult), reads=["E1s", "posf"], writes=["t32"])
                    yield S.op("dve", lambda e: e.tensor_reduce(out=pw[:, ti, 0:1], in_=t32[:].rearrange("p g j -> p (g j)"), axis=AX.X, op=ALU.add), reads=["t32"], accw=["pw"])
                    yield S.op("dve", lambda e: e.tensor_tensor(out=t32[:].rearrange("p g j -> p (g j)"), in0=E2, in1=posf[:], op=ALU.mult), reads=["E2s", "posf", "pw"], writes=["t32"])
                    yield S.op("dve", lambda e: e.tensor_reduce(out=pw[:, ti, 1:2], in_=t32[:].rearrange("p g j -> p (g j)"), axis=AX.X, op=ALU.add), reads=["t32"], accw=["pw"])
                    yield None
                return chainA(), (chainB() if main else None), (chainC() if main else None)

            def run_chains(gens):
                act = [g for g in gens if g is not None]
                while act:
                    for g in list(act):
                        try:
                            next(g)
                        except StopIteration:
                            act.remove(g)

            cprev = None
            for j in range(4):
                ca_, cb_, cc_ = tile_body(j)
                run_chains([ca_, cb_, cprev])
                cprev = cc_
            run_chains([cprev])
            if gi == 0 and "qkT" in dbg_t:
                tmpd = S.sb([128, 8, 512], F32, "sdbg_qk")
                S.op("dve", lambda e: e.tensor_copy(out=tmpd[:], in_=qk[:]), reads=[("qkT", 0)], writes=["dbg_qk"])
                finals.append(S.op("sp", lambda e: e.dma_start(out=dbg_t["qkT"].ap(), in_=tmpd[:].rearrange("p a b -> p (a b)")), reads=["dbg_qk"], dma=("dbg", "qkT")))
            if gi == 0 and "xT" in dbg_t:
                tmpx = S.sb([128, 8, 512], F32, "sdbg_xT")
                S.op("dve", lambda e: e.tensor_copy(out=tmpx[:], in_=xt[:]), reads=[("xT", 0)], writes=["dbg_xT"])
                finals.append(S.op("sp", lambda e: e.dma_start(out=dbg_t["xT"].ap(), in_=tmpx[:].rearrange("p a b -> p (a b)")), reads=["dbg_xT"], dma=("dbg", "xT")))

        wcat_bf = nc.dram_tensor("wcat_bf", [NE * 128, 12288], BF16)
        NSUP = NPRE + NST
        conv_rows = NE * 128
        conv_state = {"r": 0, "i": 0}

        def conv_some(gidx, sl):
            tgt = conv_rows * (gidx + 1) // NSUP
            while conv_state["r"] < tgt:
                r0 = conv_state["r"]
                r1 = min(r0 + 32, tgt)
                k = conv_state["i"] % 2
                conv_state["i"] += 1
                conv_state["r"] = r1
                S.op("pool", lambda e, r0=r0, r1=r1: e.dma_start(out=wcat_bf.ap()[r0:r1, :], in_=wcat.ap()[r0:r1, :]),
                     reads=[("x", sl)], writes=[("wck", k)], dma=("wck", k))

        NBLK0 = NT * 2 + NE
        xslots = nc.dram_tensor("xslots", [NBLK0 * 128, D], BF16)
        ztile = S.sb([128, 1024], BF16, "ztile")
        S.op("dve", lambda e: e.memset(ztile[:], 0.0), writes=["ztile"])
        for zb in range(0, NBLK0, 8):
            nb_ = min(8, NBLK0 - zb)
            S.op("sp", lambda e, zb=zb, nb_=nb_: e.dma_start(out=xslots.ap()[zb * 128:(zb + nb_) * 128, :].rearrange("(j p) d -> p j d", p=128),
                                                             in_=ztile[:].unsqueeze(1).broadcast_to([128, nb_, 1024])),
                 reads=["ztile"], accw=["xslots"], dma=("zinit", (zb // 8) % 4))
        gi = 0
        for s_i in range(NPRE):
            supertile(x_pre, s_i, "prelast" if s_i == NPRE - 1 else "pre", gi)
            gi += 1
        for s_i in range(NST):
            supertile(x_main, s_i, "main", gi)
            gi += 1


        S.flush()
        stA.close()
        stB = contextlib.ExitStack()
        S.cur = stB
        NBLK = NT * 2 + NE
        NSL = NBLK * 128
        yslots = nc.dram_tensor("yslots", [NSL, D], F32)
        pl_ = S.sb([128, 6, 32], F32, "plan")
        pl_i = S.sb([128, 32], I32, "plan_i")
        p128 = S.sb([128, 1], F32, "p128")
        woff_f = S.sb([128, NBLK], F32, "woff_f")
        woff_i = S.sb([128, NBLK], I32, "woff_i")
        bval = S.sb([128, NBLK], F32, "bval")
        neq = S.sb([128, NBLK], F32, "neq")
        cmpb = S.sb([128, NBLK, 32], F32, "cmpb")
        dst_f = S.sb([128, 2, NT], F32, "dst_f")
        dst_i = S.sb([128, 2, NT], I32, "dst_i")
        big = S.sb([128, NT, 32], F32, "bigtmp")
        S.op("pool", lambda e: e.iota(p128[:], [[0, 1]], base=0, channel_multiplier=1, allow_small_or_imprecise_dtypes=True), writes=["p128"])
        S.op("dve", lambda e: e.tensor_scalar(out=pl_[:, 3, :], in0=base_b[:], scalar1=1.0 / 128, scalar2=63.5 / 128, op0=ALU.mult, op1=ALU.add), reads=["base_b"], writes=["plan"])
        S.op("dve", lambda e: e.tensor_copy(out=pl_i[:], in_=pl_[:, 3, :]), reads=["plan"], writes=["plan_i"])
        S.op("dve", lambda e: e.tensor_copy(out=pl_[:, 0, :], in_=pl_i[:]), reads=["plan_i", "plan"], writes=["plan"])
        S.op("dve", lambda e: e.tensor_scalar(out=pl_[:, 0, :], in0=pl_[:, 0, :], scalar1=128.0, scalar2=None, op0=ALU.mult), reads=["plan"], writes=["plan"])
        S.op("dve", lambda e: e.tensor_tensor_scan(out=pl_[:, 1, :], data0=pl_[:, 0, :], data1=pl_[:, 0, :], initial=0.0, op0=ALU.add, op1=ALU.bypass), reads=["plan"], writes=["plan"])
        S.op("dve", lambda e: e.tensor_tensor(out=pl_[:, 2, :], in0=pl_[:, 1, :], in1=pl_[:, 0, :], op=ALU.subtract), reads=["plan"], writes=["plan"])
        S.op("pool", lambda e: e.iota(bval[:], [[128, NBLK]], base=0, channel_multiplier=0, allow_small_or_imprecise_dtypes=True), writes=["bval"])
        S.op("dve", lambda e: e.tensor_tensor(out=cmpb[:], in0=pl_[:, 1, :].unsqueeze(1).broadcast_to([128, NBLK, 32]), in1=bval[:].unsqueeze(2).broadcast_to([128, NBLK, 32]), op=ALU.is_le), reads=["plan", "bval"], writes=["cmpb"])
        S.op("dve", lambda e: e.tensor_reduce(out=woff_f[:], in_=cmpb[:], axis=AX.X, op=ALU.add), reads=["cmpb"], writes=["woff_f"])
        BIGI = 1000000.0
        S.op("dve", lambda e: e.tensor_scalar(out=woff_f[:], in0=woff_f[:], scalar1=31.0, scalar2=None, op0=ALU.min), reads=["woff_f"], writes=["woff_f"])
        S.op("dve", lambda e: e.memset(neq[:, 0:2], 1.0), writes=["neq0"])
        S.op("dve", lambda e: e.tensor_tensor(out=neq[:, 2:NBLK], in0=woff_f[:, 2:NBLK], in1=woff_f[:, 0:NBLK - 2], op=ALU.not_equal), reads=["woff_f"], writes=["neq"])
        S.op("dve", lambda e: e.tensor_scalar(out=woff_f[:], in0=woff_f[:], scalar1=128.0, scalar2=-BIGI, op0=ALU.mult, op1=ALU.add), reads=["woff_f", "neq"], writes=["woff_f"])
        S.op("dve", lambda e: e.tensor_scalar(out=woff_f[:], in0=woff_f[:], scalar1=p128[:, 0:1], scalar2=None, op0=ALU.add), reads=["woff_f", "p128"], writes=["woff_f"])
        S.op("dve", lambda e: e.tensor_tensor(out=woff_f[:], in0=woff_f[:], in1=neq[:], op=ALU.mult), reads=["woff_f", "neq", "neq0"], writes=["woff_f"])
        S.op("dve", lambda e: e.tensor_scalar(out=woff_f[:], in0=woff_f[:], scalar1=BIGI, scalar2=None, op0=ALU.add), reads=["woff_f"], writes=["woff_f"])
        S.op("dve", lambda e: e.tensor_copy(out=woff_i[:], in_=woff_f[:]), reads=["woff_f"], writes=["woff_i"])
        for k_, Es in ((0, E1s), (1, E2s)):
            S.op("dve", lambda e, Es=Es: e.tensor_tensor(out=big[:], in0=Es[:], in1=pl_[:, 2, :].unsqueeze(1).broadcast_to([128, NT, 32]), op=ALU.mult), reads=["E1s", "E2s", "plan"], writes=["big"])
            S.op("dve", lambda e, k_=k_: e.tensor_reduce(out=dst_f[:, k_, :], in_=big[:], axis=AX.X, op=ALU.add), reads=["big"], writes=[("dst_f", k_)])
            S.op("dve", lambda e, k_=k_: e.tensor_tensor(out=dst_f[:, k_, :], in0=dst_f[:, k_, :], in1=pw[:, :, k_], op=ALU.add), reads=[("dst_f", k_), "pw"], writes=[("dst_f", k_)])
        S.op("dve", lambda e: e.tensor_copy(out=dst_i[:], in_=dst_f[:]), reads=[("dst_f", 0), ("dst_f", 1)], writes=["dst_i"])
        if "plan" in dbg_t:
            finals.append(S.op("sp", lambda e: e.dma_start(out=dbg_t["plan"].ap()[:, 0:192], in_=pl_[:].rearrange("p a b -> p (a b)")), reads=["plan"], dma=("dbg", "plan")))
            finals.append(S.op("sp", lambda e: e.dma_start(out=dbg_t["plan"].ap()[:, 768:768 + NBLK], in_=woff_f[:]), reads=["woff_f"], dma=("dbg", "plan2")))
            finals.append(S.op("sp", lambda e: e.dma_start(out=dbg_t["plan"].ap()[:, 256:256 + 2 * NT], in_=dst_f[:].rearrange("p a b -> p (a b)")), reads=[("dst_f", 0), ("dst_f", 1)], dma=("dbg", "plan3")))
            finals.append(S.op("sp", lambda e: e.dma_start(out=dbg_t["plan"].ap()[:, 512:512 + 4 * NT], in_=pw[:].rearrange("p a b -> p (a b)")), reads=["pw"], dma=("dbg", "plan4")))
        xsc = [S.sb([128, 1024], BF16, f"xsc{i}") for i in range(2)]
        for ti in range(NT):
            bsl = ti % 2
            S.op("sp", lambda e, ti=ti, bsl=bsl: e.dma_start(out=xsc[bsl][:], in_=xn2lin.ap()[ti * 128:(ti + 1) * 128, :]), reads=["xn2lin"], writes=[("xsc", bsl)], dma=("xsc", bsl))
            for k_ in range(2):
                S.op("pool", lambda e, ti=ti, bsl=bsl, k_=k_: e.indirect_dma_start(out=xslots.ap(), out_offset=bass.IndirectOffsetOnAxis(ap=dst_i[:, k_, ti:ti + 1], axis=0), in_=xsc[bsl][:], in_offset=None),
                     reads=[("xsc", bsl), "dst_i"], accw=["xslots"], dma=("scat", bsl, k_))

        wbuf = [S.sb([128, 12288], BF16, f"wbuf{i}") for i in range(2)]
        xs_b = [S.sb([128, 1024], BF16, f"xs_b{i}") for i in range(4)]
        xsT = [S.sb([128, 8, 128], BF16, f"xsT{i}") for i in range(2)]
        eg = [S.sb([128, 512], F32, f"eg{i}") for i in range(2)]
        hid = [S.sb([128, 512], BF16, f"hid{i}") for i in range(2)]
        hidT = [S.sb([128, 4, 128], BF16, f"hidT{i}") for i in range(2)]
        ysb = [S.sb([128, 1024], F32, f"ysb{i}") for i in range(2)]
        regs = {}

        def wgather(e, b, ws):
            if "bnd" not in regs:
                regs["bnd"] = st.enter_context(e.register("wbnd"))
                e.reg_mov(regs["bnd"], NE * 128 - 1)
            return e.indirect_dma_start(out=wbuf[ws][:], out_offset=None, in_=wcat_bf.ap(), in_offset=bass.IndirectOffsetOnAxis(ap=woff_i[:, b:b + 1], axis=0),
                                        bounds_check=regs["bnd"], oob_is_err=False)

        mrr = [0, 0]

        def fbankM(p):
            i = 3 * p + mrr[p] % 3
            mrr[p] += 1
            return pfb[i], ("pf", i)

        def blk(b):
            ws = b % 2
            yield S.op("pool", lambda e, b=b, ws=ws: wgather(e, b, ws), reads=["woff_i"], writes=[("wb", ws)], dma=("wb", ws))
            if b < 2:
                yield S.op("sp", lambda e, b=b: e.dma_start(out=xs_b[b % 4][:], in_=xslots.ap()[b * 128:(b + 1) * 128, :]), reads=["xslots"], writes=[("xs", b % 4)], dma=("xs", b % 4))
            if b + 2 < NBLK:
                yield S.op("sp", lambda e, b=b: e.dma_start(out=xs_b[(b + 2) % 4][:], in_=xslots.ap()[(b + 2) * 128:(b + 3) * 128, :]), reads=["xslots"], writes=[("xs", (b + 2) % 4)], dma=("xs", (b + 2) % 4))
            xq = b % 4
            pb, pt = (ptr[ws], ("ptr", ws))
            for kc in range(8):
                yield S.op("pe", lambda e, kc=kc, pb=pb, xq=xq: e.transpose(out=pb[:, kc * 128:(kc + 1) * 128], in_=xs_b[xq][:, kc * 128:(kc + 1) * 128], identity=ident_b[:]),
                     reads=[("xs", xq), "ident_b"], accw=[pt])
            yield S.op("act", lambda e, pb=pb, ws=ws: e.copy(out=xsT[ws][:].rearrange("p c t -> p (c t)"), in_=pb[:]), reads=[pt], writes=[("xsT", ws)])
            pG, pGt = fbankM(ws)
            pU2, pU2t = fbankM(ws)
            for kc in range(8):
                yield S.op("pe", lambda e, kc=kc, pG=pG, ws=ws: e.matmul(out=pG[:], lhsT=xsT[ws][:, kc, :], rhs=wbuf[ws][:, kc * 1024:kc * 1024 + 512], start=(kc == 0), stop=(kc == 7)),
                     reads=[("xsT", ws), ("wb", ws)], accw=[pGt])
            for kc in range(8):
                yield S.op("pe", lambda e, kc=kc, pU2=pU2, ws=ws: e.matmul(out=pU2[:], lhsT=xsT[ws][:, kc, :], rhs=wbuf[ws][:, kc * 1024 + 512:(kc + 1) * 1024], start=(kc == 0), stop=(kc == 7)),
                     reads=[("xsT", ws), ("wb", ws)], accw=[pU2t])
            yield S.op("act", lambda e, pG=pG, ws=ws: e.activation(out=eg[ws][:], in_=pG[:], func=AF.Exp, scale=-1.0), reads=[pGt], writes=[("eg", ws)])
            yield S.op("act", lambda e, ws=ws: e.activation(out=eg[ws][:], in_=eg[ws][:], func=AF.Ln, bias=1.0), reads=[("eg", ws)], writes=[("eg", ws)])
            yield S.op("act", lambda e, ws=ws: e.activation(out=eg[ws][:], in_=eg[ws][:], func=AF.Exp, scale=-1.0), reads=[("eg", ws)], writes=[("eg", ws)])
            yield S.op("dve", lambda e, pG=pG, ws=ws: e.tensor_tensor(out=eg[ws][:], in0=eg[ws][:], in1=pG[:], op=ALU.mult), reads=[("eg", ws), pGt], writes=[("eg", ws)])
            yield S.op("dve", lambda e, pU2=pU2, ws=ws: e.tensor_tensor(out=hid[ws][:], in0=eg[ws][:], in1=pU2[:], op=ALU.mult), reads=[("eg", ws), pU2t], writes=[("hid", ws)])
            pb, pt = (ptr[ws], ("ptr", ws))
            for fc in range(4):
                yield S.op("pe", lambda e, fc=fc, pb=pb, ws=ws: e.transpose(out=pb[:, fc * 128:(fc + 1) * 128], in_=hid[ws][:, fc * 128:(fc + 1) * 128], identity=ident_b[:]),
                     reads=[("hid", ws), "ident_b"], accw=[pt])
            yield S.op("act", lambda e, pb=pb, ws=ws: e.copy(out=hidT[ws][:].rearrange("p c t -> p (c t)"), in_=pb[:, 0:512]), reads=[pt], writes=[("hidT", ws)])
            for hf in range(2):
                pY, pYt = fbankM(ws)
                for fc in range(4):
                    yield S.op("pe", lambda e, fc=fc, pY=pY, ws=ws, hf=hf: e.matmul(out=pY[:], lhsT=hidT[ws][:, fc, :], rhs=wbuf[ws][:, 8192 + fc * 1024 + hf * 512:8192 + fc * 1024 + (hf + 1) * 512], start=(fc == 0), stop=(fc == 3)),
                         reads=[("hidT", ws), ("wb", ws)], accw=[pYt])
                if hf == 0:
                    yield S.op("act", lambda e, pY=pY, ws=ws: e.copy(out=ysb[ws][:, 0:512], in_=pY[:]), reads=[pYt], writes=[("ysb", ws, 0)])
                else:
                    yield S.op("dve", lambda e, pY=pY, ws=ws: e.tensor_copy(out=ysb[ws][:, 512:1024], in_=pY[:]), reads=[pYt], writes=[("ysb", ws, 1)])
            yield S.op("sp", lambda e, b=b, ws=ws: e.dma_start(out=yslots.ap()[b * 128:(b + 1) * 128, :], in_=ysb[ws][:]), reads=[("ysb", ws, 0), ("ysb", ws, 1)], accw=["yslots"], dma=("yst", ws))


            yield None

        for b in range(NBLK):
            for _ in blk(b):
                pass

        fg_b = bload("fg_b", final_g, 1024)
        NCB = 3
        hc = [S.sb([128, 1024], F32, f"hc{i}") for i in range(NCB)]
        y1 = [S.sb([128, 1024], F32, f"y1_{i}") for i in range(NCB)]
        y2 = [S.sb([128, 1024], F32, f"y2_{i}") for i in range(NCB)]
        fs = S.sb([128, 2], F32, "fs")

        def cloads(ti):
            cs = ti % NCB
            S.op("sp", lambda e, ti=ti, cs=cs: e.dma_start(out=hc[cs][:], in_=h1buf.ap()[ti * 128:(ti + 1) * 128, :]), reads=["h1buf"], writes=[("hc", cs)], dma=("hc", cs))
            S.op("pool", lambda e, ti=ti, cs=cs: e.indirect_dma_start(out=y1[cs][:], out_offset=None, in_=yslots.ap(), in_offset=bass.IndirectOffsetOnAxis(ap=dst_i[:, 0, ti:ti + 1], axis=0)),
                 reads=["yslots", "dst_i"], writes=[("y1", cs)], dma=("y1", cs))
            S.op("pool", lambda e, ti=ti, cs=cs: e.indirect_dma_start(out=y2[cs][:], out_offset=None, in_=yslots.ap(), in_offset=bass.IndirectOffsetOnAxis(ap=dst_i[:, 1, ti:ti + 1], axis=0)),
                 reads=["yslots", "dst_i"], writes=[("y2", cs)], dma=("y2", cs))

        for ti in range(min(NCB - 1, NT)):
            cloads(ti)
        for ti in range(NT):
            cs = ti % NCB
            if ti + NCB - 1 < NT:
                cloads(ti + NCB - 1)
            S.op("dve", lambda e, ti=ti, cs=cs: e.scalar_tensor_tensor(out=hc[cs][:], in0=y1[cs][:], scalar=pw[:, ti, 2:3], in1=hc[cs][:], op0=ALU.mult, op1=ALU.add), reads=[("hc", cs), ("y1", cs), "pw"], writes=[("hc", cs)])
            S.op("dve", lambda e, ti=ti, cs=cs: e.scalar_tensor_tensor(out=hc[cs][:], in0=y2[cs][:], scalar=pw[:, ti, 3:4], in1=hc[cs][:], op0=ALU.mult, op1=ALU.add), reads=[("hc", cs), ("y2", cs), "pw"], writes=[("hc", cs)])
            S.op("dve", lambda e, ti=ti: e.memset(fs[:, (ti % 2):(ti % 2) + 1], 0.0), writes=[("fs", ti % 2)])
            S.op("act", lambda e, ti=ti, cs=cs: e.activation(out=junk[:], in_=hc[cs][:], func=AF.Square, accum_out=fs[:, (ti % 2):(ti % 2) + 1]), reads=[("hc", cs), ("fs", ti % 2)], writes=["junk", ("fs", ti % 2)])
            S.op("act", lambda e, ti=ti: e.activation(out=fs[:, (ti % 2):(ti % 2) + 1], in_=fs[:, (ti % 2):(ti % 2) + 1], func=AF.Ln, scale=1.0 / D, bias=EPS), reads=[("fs", ti % 2)], writes=[("fs", ti % 2)])
            S.op("act", lambda e, ti=ti: e.activation(out=fs[:, (ti % 2):(ti % 2) + 1], in_=fs[:, (ti % 2):(ti % 2) + 1], func=AF.Exp, scale=-0.5), reads=[("fs", ti % 2)], writes=[("fs", ti % 2)])
            S.op("dve", lambda e, ti=ti, cs=cs: e.scalar_tensor_tensor(out=y1[cs][:], in0=hc[cs][:], scalar=fs[:, (ti % 2):(ti % 2) + 1], in1=fg_b[:], op0=ALU.mult, op1=ALU.mult), reads=[("hc", cs), ("fs", ti % 2), "fg_b", ("y1", cs)], writes=[("y1", cs)])
            finals.append(S.op("sp", lambda e, ti=ti, cs=cs: e.dma_start(out=out.ap()[ti * 128:(ti + 1) * 128, :], in_=y1[cs][:]), reads=[("y1", cs)], dma=("ost", cs)))

        S.flush()
        stB.close()
    return nc


def make_wcat(w_gate, w_up, w_down):
    g = w_gate.reshape(NE, 8, 128, 512).transpose(0, 2, 1, 3)
    u = w_up.reshape(NE, 8, 128, 512).transpose(0, 2, 1, 3)
    gu = np.concatenate([g, u], axis=3).reshape(NE, 128, 8192)
    dn = w_down.reshape(NE, 4, 128, 1024).transpose(0, 2, 1, 3).reshape(NE, 128, 4096)
    return np.ascontiguousarray(np.concatenate([gu, dn], axis=2).reshape(NE * 128, 12288))


def kernel(**inputs):
    f = lambda k: np.ascontiguousarray(np.asarray(inputs[k], dtype=np.float32))
    x = f("x")
    com = {
        "norm1_g": f("norm1_g")[0], "w_in": f("w_in")[0], "conv_qk": f("conv_qk")[0],
        "b_if": np.concatenate([f("b_igate")[0], f("b_fgate")[0]]), "g_mlstm_out": f("g_mlstm_out")[0],
        "g_gmlp_v": f("g_gmlp_v")[0], "w_spatial": f("w_spatial")[0], "b_spatial": f("b_spatial")[0],
        "g_gmlp_out": f("g_gmlp_out")[0], "w_out": f("w_out")[0], "norm2_g": f("norm2_g")[0],
        "w_router": np.ascontiguousarray(np.concatenate([f("w_router_group")[0], f("w_router_expert")[0]], axis=1)),
        "b_router": np.concatenate([f("b_router_group")[0], f("b_router_expert")[0]]),
        "wcat": make_wcat(f("w_gate")[0], f("w_up")[0], f("w_down")[0]), "final_g": f("final_g"),
    }
    in_maps = []
    for c in range(8):
        b, half = c // 2, c % 2
        m = dict(com)
        m["x_main"] = np.ascontiguousarray(x[b, half * 4096:(half + 1) * 4096])
        m["x_pre"] = np.ascontiguousarray(x[b, 0:4096]) if half == 1 else np.zeros((4096, D), np.float32)
        in_maps.append(m)
    nc = build(8, 8)
    res = run_bass_kernel_spmd(nc, in_maps, core_ids=list(range(8)))
    out = np.empty((4, 8192, D), np.float32)
    for c in range(8):
        out[c // 2, (c % 2) * 4096:(c % 2 + 1) * 4096] = res.results[c]["out"]
    return out
```

```python
import contextlib
import numpy as np
import concourse.bass as bass
import concourse.mybir as mybir
from concourse.bass_utils import run_bass_kernel_spmd

F32 = mybir.dt.float32
BF16 = mybir.dt.bfloat16
I32 = mybir.dt.int32
AF = mybir.ActivationFunctionType
ALU = mybir.AluOpType
AX = mybir.AxisListType

ENGS = ("pe", "act", "dve", "pool", "sp")
SEM_CH = 8000
D = 1024
EPS = 1e-6
NE = 32
NBLK_MAX = 96
NSLOT = NBLK_MAX * 128


class Sched:
    def __init__(self, nc, stack):
        self.nc = nc
        self.stack = stack
        self.ops = []
        self.last_w = {}
        self.readers = {}
        self.dma_count = {}
        self.nbuf = 0
        self.flushed = 0
        self.sems = {}
        self.eng_seq = {e: 0 for e in ENGS}
        self.waited = {e: {} for e in ENGS}
        self.cur = stack

    def sb(self, shape, dt, name=None, persist=False):
        self.nbuf += 1
        return (self.stack if persist else self.cur).enter_context(self.nc.sbuf_tensor(name or f"sb{self.nbuf}", list(shape), dt))

    def ps(self, shape, dt, name=None):
        self.nbuf += 1
        return self.stack.enter_context(self.nc.psum_tensor(name or f"ps{self.nbuf}", list(shape), dt))

    def op(self, eng, fn, reads=(), writes=(), accw=(), dma=None):
        i = len(self.ops)
        deps = set()
        for t in reads:
            deps.update(self.last_w.get(t, ()))
        for t in writes:
            deps.update(self.last_w.get(t, ()))
            deps.update(self.readers.get(t, ()))
        for t in accw:
            deps.update(self.readers.get(t, ()))
        for t in reads:
            self.readers.setdefault(t, []).append(i)
        for t in writes:
            self.last_w[t] = [i]
            self.readers[t] = []
        for t in accw:
            if self.readers.get(t):
                self.last_w[t] = []
                self.readers[t] = []
            self.last_w.setdefault(t, []).append(i)
        deps.discard(i)
        self.ops.append(dict(eng=eng, fn=fn, deps=sorted(deps), dma=dma, sig=None))
        return i

    def flush(self):
        nc = self.nc
        ops = self.ops
        lo = self.flushed
        last = {}
        for i in range(lo, len(ops)):
            o = ops[i]
            key = ("dma", o["dma"]) if o["dma"] is not None else ("eng", o["eng"])
            last[key] = i
        bdeps = sorted(last.values())
        for en in ENGS:
            self.ops.append(dict(eng=en, fn=lambda e: e.nop(), deps=list(bdeps), dma=None, sig=None, barrier=True))
        hi = len(ops)

        def pe_pair(a, b):
            return (a["eng"] == "pe" and b["eng"] == "pe" and a["dma"] is None and b["dma"] is None
                    and not b.get("barrier"))

        needed = set()
        for i in range(lo, hi):
            o = ops[i]
            for d in o["deps"]:
                if pe_pair(ops[d], o):
                    continue
                needed.add(d)
        for i in range(lo, hi):
            o = ops[i]
            if o["dma"] is not None:
                k = ("dma", o["dma"])
                self.dma_count[k] = self.dma_count.get(k, 0) + 1
                o["sig"] = (k, 16 * self.dma_count[k])
                self.get_sem(k)
            elif i in needed:
                e = o["eng"]
                n = self.eng_seq[e]
                self.eng_seq[e] += 1
                k = ("eng", e, n // SEM_CH)
                o["sig"] = (k, n % SEM_CH + 1)
                self.get_sem(k)
        sems = self.sems
        waited_all = self.waited

        def run(engname):
            def body(e):
                waited = waited_all[engname]
                for i in range(lo, hi):
                    o = ops[i]
                    if o["eng"] != engname:
                        continue
                    for d in o["deps"]:
                        od = ops[d]
                        if od["sig"] is None or pe_pair(od, o):
                            continue
                        k, v = od["sig"]
                        if waited.get(k, 0) >= v:
                            continue
                        e.wait_ge(sems[k], v)
                        waited[k] = v
                    ins = o["fn"](e)
                    if o["sig"] is not None:
                        k, v = o["sig"]
                        ins.then_inc(sems[k], 16 if o["dma"] is not None else 1)
            return body

        with nc.Block() as block:
            block.tensor(run("pe"))
            block.scalar(run("act"))
            block.vector(run("dve"))
            block.gpsimd(run("pool"))
            block.sync(run("sp"))
        self.flushed = hi
        self.last_w = {}
        self.readers = {}

    def get_sem(self, key):
        if key not in self.sems:
            self.sems[key] = self.stack.enter_context(self.nc.semaphore(f"s_{len(self.sems)}"))
        return self.sems[key]


def build(NST=8, NPRE=8, dbg=None):
    nc = bass.Bass("TRN2", target_bir_lowering=False)
    NT = NST * 4
    TOK = NST * 512

    def din(name, shape, dt=F32):
        return nc.dram_tensor(name, list(shape), dt, kind="ExternalInput")

    x_main = din("x_main", [TOK, D])
    x_pre = din("x_pre", [max(NPRE, 1) * 512, D])
    norm1_g = din("norm1_g", [D])
    w_in = din("w_in", [D, 3080])
    conv_qk = din("conv_qk", [4, 1024])
    b_if = din("b_if", [8])
    g_mlstm = din("g_mlstm_out", [512])
    g_gv = din("g_gmlp_v", [512])
    w_sp = din("w_spatial", [8, 128, 128])
    b_sp = din("b_spatial", [8, 128])
    g_go = din("g_gmlp_out", [512])
    w_out = din("w_out", [D, D])
    norm2_g = din("norm2_g", [D])
    w_r = din("w_router", [D, 36])
    b_r = din("b_router", [36])
    wcat = din("wcat", [NE * 128, 12288])
    final_g = din("final_g", [D])
    out = nc.dram_tensor("out", [TOK, D], F32, kind="ExternalOutput")
    h1buf = nc.dram_tensor("h1buf", [TOK, D], F32)
    dbg_t = {}
    if dbg:
        for k, shp in dbg.items():
            dbg_t[k] = nc.dram_tensor("dbg_" + k, list(shp), F32, kind="ExternalOutput")

    with contextlib.ExitStack() as st:
        S = Sched(nc, st)
        finals = []

        ident_f = S.sb([128, 128], F32, "ident_f")
        ident_b = S.sb([128, 128], BF16, "ident_b")
        triu_f = S.sb([128, 128], F32, "triu_f")
        triu_b = S.sb([128, 128], BF16, "triu_b")
        tril_f = S.sb([128, 128], F32, "tril_f")
        ones_f = S.sb([128, 128], F32, "ones_f")
        S.op("pool", lambda e: e.memset(ident_f[:], 0.0), writes=["ident_f"])
        S.op("pool", lambda e: e.affine_select(out=ident_f[:], in_=ident_f[:], pattern=[[-1, 128]],
                                               compare_op=ALU.not_equal, fill=1.0, base=0, channel_multiplier=1),
             reads=["ident_f"], writes=["ident_f"])
        S.op("pool", lambda e: e.memset(ones_f[:], 1.0), writes=["ones_f"])
        S.op("pool", lambda e: e.affine_select(out=triu_f[:], in_=ones_f[:], pattern=[[1, 128]],
                                               compare_op=ALU.is_ge, fill=0.0, base=0, channel_multiplier=-1),
             reads=["ones_f"], writes=["triu_f"])
        S.op("pool", lambda e: e.affine_select(out=tril_f[:], in_=ones_f[:], pattern=[[-1, 128]],
                                               compare_op=ALU.is_ge, fill=0.0, base=0, channel_multiplier=1),
             reads=["ones_f"], writes=["tril_f"])
        S.op("dve", lambda e: e.tensor_copy(out=ident_b[:], in_=ident_f[:]), reads=["ident_f"], writes=["ident_b"])
        S.op("dve", lambda e: e.tensor_copy(out=triu_b[:], in_=triu_f[:]), reads=["triu_f"], writes=["triu_b"])

        def bload(name, src, n, eng="sp"):
            t = S.sb([128, n], F32, name)
            S.op(eng, lambda e: e.dma_start(out=t[:], in_=bass.AP(src, 0, [[0, 128], [1, n]])),
                 writes=[name], dma=name)
            return t

        gml_b = bload("gml_b", g_mlstm, 512)
        ggv_b = bload("ggv_b", g_gv, 512)
        ggo_b = bload("ggo_b", g_go, 512)
        bif_b = bload("bif_b", b_if, 8)

        g1col = S.sb([128, 8], F32, "g1col")
        cw = S.sb([128, 4, 8], F32, "cw")
        bsp = S.sb([128, 8], F32, "bsp")
        S.op("sp", lambda e: e.dma_start(out=g1col[:], in_=norm1_g.ap().rearrange("(c p) -> p c", p=128),
                                         allow_slow_non_contiguous=True), writes=["g1col"], dma="g1col")
        for i in range(4):
            S.op("sp", lambda e, i=i: e.dma_start(out=cw[:, i, :], in_=conv_qk.ap()[i, :].rearrange("(c p) -> p c", p=128),
                                                  allow_slow_non_contiguous=True), accw=["cw"], dma=("cw", i))
        S.op("sp", lambda e: e.dma_start(out=bsp[:], in_=b_sp.ap().rearrange("g t -> t g"),
                                         allow_slow_non_contiguous=True), writes=["bsp"], dma="bsp")

        ptr = [S.ps([128, 1024], BF16, f"ptr{i}") for i in range(2)]
        pfb = [S.ps([128, 512], F32, f"pf{i}") for i in range(6)]
        rr = {"t": 0, "f": 0, "A": 0, "B": 0, "A2": 0}

        def tbank():
            i = rr["t"] % 2
            rr["t"] += 1
            return ptr[i], ("ptr", i)

        def fbank():
            i = rr["f"] % 6
            rr["f"] += 1
            return pfb[i], ("pf", i)

        def fbankA():
            return pfb[0], ("pf", 0)

        def fbankA2():
            i = 1 + rr["A2"] % 2
            rr["A2"] += 1
            return pfb[i], ("pf", i)

        def fbankB():
            i = 3 + rr["B"] % 2
            rr["B"] += 1
            return pfb[i], ("pf", i)

        def fbankC():
            return pfb[5], ("pf", 5)

        junk = S.sb([128, 1024], BF16, "junk", persist=True)
        base_b = S.sb([128, 32], F32, "base_b", persist=True)
        E1s = S.sb([128, NT, 32], BF16, "E1s", persist=True)
        E2s = S.sb([128, NT, 32], BF16, "E2s", persist=True)
        pw = S.sb([128, NT, 4], F32, "pw", persist=True)
        stA = contextlib.ExitStack()
        S.cur = stA
        xbuf = [S.sb([128, 4, 1024], F32, f"xbuf{i}") for i in range(2)]
        wqk = S.sb([128, 8, 1024], BF16, "wqk")
        wvo = S.sb([128, 8, 1024], BF16, "wvo")
        wuv = S.sb([128, 8, 1024], BF16, "wuv")
        wif = S.sb([128, 8, 8], BF16, "wif")
        wout = S.sb([128, 8, 1024], BF16, "wout")
        for kc in range(8):
            sl = kc % 2
            stg = xbuf[sl][:].rearrange("p a b -> p (a b)")
            S.op("sp", lambda e, stg=stg, kc=kc: e.dma_start(out=stg[:, 0:3080], in_=w_in.ap()[kc * 128:(kc + 1) * 128, :]),
                 writes=[("x", sl)], dma=("x", sl))
            for (dst, c0, n, tok) in ((wqk, 0, 1024, "wqk"), (wvo, 1024, 1024, "wvo"), (wif, 2048, 8, "wif"), (wuv, 2056, 1024, "wuv")):
                S.op("act", lambda e, dst=dst, c0=c0, n=n, kc=kc, stg=stg: e.mul(dst[:, kc, 0:n], stg[:, c0:c0 + n], g1col[:, kc:kc + 1]),
                     reads=[("x", sl), "g1col"], accw=[tok])
        S.op("pool", lambda e: e.dma_start(out=wout[:], in_=w_out.ap().rearrange("(c p) n -> p c n", p=128)),
             writes=["wout"], dma="wout")

        wsp_f = xbuf[1][:, 3, :].rearrange("p (g s) -> p g s", g=8)
        wspT = S.sb([128, 8, 128], BF16, "wspT")
        S.op("sp", lambda e: e.dma_start(out=wsp_f, in_=w_sp.ap().rearrange("g t s -> t g s")), writes=[("x", 1)], dma=("x", 1))
        S.op("dve", lambda e: e.tensor_tensor(out=wsp_f, in0=wsp_f, in1=tril_f[:].unsqueeze(1).broadcast_to([128, 8, 128]), op=ALU.mult),
             reads=[("x", 1), "tril_f"], writes=[("x", 1)])
        for half in range(2):
            pb, pt = fbank()
            for g4 in range(4):
                g = half * 4 + g4
                S.op("pe", lambda e, pb=pb, g=g, g4=g4: e.transpose(out=pb[:, g4 * 128:(g4 + 1) * 128], in_=wsp_f[:, g, :], identity=ident_f[:]),
                     reads=[("x", 1), "ident_f"], accw=[pt])
            S.op("dve", lambda e, pb=pb, half=half: e.tensor_copy(out=wspT[:, half * 4:(half + 1) * 4, :].rearrange("p a b -> p (a b)"), in_=pb[:]),
                 reads=[pt], accw=["wspT"])

        C32 = S.sb([128, 4, 129], F32, "C32")
        Csb = S.sb([128, 4, 129], BF16, "Csb")
        m_st = S.sb([4, 1], F32, "m_st")
        halo = S.sb([128, 8, 3], F32, "halo")
        S.op("pool", lambda e: e.memset(C32[:], 0.0), writes=[("C32", h) for h in range(4)])
        S.op("pool", lambda e: e.memset(m_st[:], 0.0), writes=["m_st"])
        S.op("pool", lambda e: e.memset(halo[:], 0.0), writes=[("halo", c) for c in range(8)])

        xn = [S.sb([128, 1024], BF16, f"xn{i}") for i in range(2)]
        ss1 = S.sb([128, 8], F32, "ss1")
        xT = [S.sb([128, 8, 512], BF16, f"xT{i}") for i in range(1)]
        pre = [S.sb([128, 515], F32, f"pre{i}") for i in range(2)]
        cacc = [S.sb([128, 512], F32, f"cacc{i}") for i in range(2)]
        sg = [S.sb([128, 512], F32, f"sg{i}") for i in range(2)]
        qkT = [S.sb([128, 8, 512], BF16, f"qkT{i}") for i in range(1)]
        ktm2 = [S.sb([128, 4, 128], BF16, f"ktm{i}") for i in range(2)]
        gsb = S.sb([128, 4, 8], F32, "gsb")
        spl = S.sb([128, 4, 4], F32, "spl")
        a_tm = S.sb([128, 4, 4], F32, "a_tm")
        cum_sb = S.sb([128, 4, 4], F32, "cum_sb")
        e_tm = S.sb([128, 4, 4], F32, "e_tm")
        dn_tm = S.sb([128, 4, 4], F32, "dn_tm")
        gb_sb = S.sb([128, 4, 8], F32, "gb_sb")
        rw = S.sb([4, 20], F32, "rw")
        rhs8 = S.sb([4, 4, 8], F32, "rhs8")
        lnc0_t = S.sb([128, 1], F32, "lnc0_t")
        S.op("pool", lambda e: e.memset(lnc0_t[:], -float(np.log(128 ** -0.5))), writes=["lnc0"])
        ve = [S.sb([128, 4, 129], BF16, f"ve{i}") for i in range(2)]
        smask = [S.sb([128, 4, 128], BF16, f"smask{i}") for i in range(2)]
        og2 = [S.sb([128, 512], F32, f"og{i}") for i in range(2)]
        hs = S.sb([128, 4, 128], F32, "hs")
        nqa = S.sb([128, 4], F32, "nqa")
        sm8 = S.sb([128, 16], F32, "sm8")
        tmpa = S.sb([128, 1024], F32, "tmpa")
        ux = S.sb([128, 1024], F32, "ux")
        t1 = S.sb([128, 1024], F32, "t1")
        vn = S.sb([128, 512], BF16, "vn")
        gt = S.sb([128, 8, 64], F32, "gt")
        ybuf = [S.sb([128, 1024], BF16, f"ybuf{i}") for i in range(2)]
        yT = [S.sb([128, 8, 128], BF16, f"yT{i}") for i in range(2)]
        h1b = [S.sb([128, 1024], F32, f"h1b{i}") for i in range(2)]
        g2_b = bload("g2_b", norm2_g, 1024)
        br_b = bload("br_b", b_r, 36)
        wr_f = S.sb([128, 8, 36], F32, "wr_f")
        wr_b = S.sb([128, 8, 36], BF16, "wr_b")
        S.op("sp", lambda e: e.dma_start(out=wr_f[:], in_=w_r.ap().rearrange("(c p) n -> p c n", p=128)), writes=["wr_f"], dma="wr_f")
        S.op("dve", lambda e: e.tensor_copy(out=wr_b[:], in_=wr_f[:]), reads=["wr_f"], writes=["wr_b"])
        lstr_b = S.sb([128, 128], BF16, "lstr_b")
        ones_b = S.sb([128, 128], BF16, "ones_b")
        lstr_f = S.sb([128, 128], F32, "lstr_f")
        S.op("pool", lambda e: e.affine_select(out=lstr_f[:], in_=ones_f[:], pattern=[[1, 128]], compare_op=ALU.is_gt, fill=0.0, base=0, channel_multiplier=-1),
             reads=["ones_f"], writes=["lstr_f"])
        S.op("dve", lambda e: e.tensor_copy(out=lstr_b[:], in_=lstr_f[:]), reads=["lstr_f"], writes=["lstr_b"])
        S.op("dve", lambda e: e.tensor_copy(out=ones_b[:], in_=ones_f[:]), reads=["ones_f"], writes=["ones_b"])
        xn2t = [S.sb([128, 1024], BF16, f"xn2t{i}") for i in range(2)]
        xn2T = [S.sb([128, 8, 128], BF16, f"xn2T{i}") for i in range(1)]
        lg = S.sb([128, 36], F32, "lg")
        rt = S.sb([128, 64], F32, "rt")
        t32 = S.sb([128, 4, 8], F32, "t32")
        le8 = S.sb([128, 16], F32, "le8")
        ind_b = S.sb([128, 32], BF16, "ind_b")
        posf = S.sb([128, 32], F32, "posf")
        S.op("pool", lambda e: e.memset(base_b[:], 0.0), writes=["base_b"])
        xn2lin = nc.dram_tensor("xn2lin", [TOK, D], BF16)
        cnt = {"tile": 0, "st": 0, "ch": 0, "tl2": 0, "y": 0}

        LN_C0 = float(np.log(128 ** -0.5))

        def dump(name, ap_fn, reads, row0=0):
            if name in dbg_t:
                t = dbg_t[name]
                finals.append(S.op("sp", lambda e: e.dma_start(out=t.ap()[row0:row0 + 128, :], in_=ap_fn()), reads=reads, dma=("dbg", name)))

        plan_x = [(x_pre, i) for i in range(NPRE)] + [(x_main, i) for i in range(NST)]

        def xload(g):
            src_, i_ = plan_x[g]
            sl_ = g % 2
            S.op("sp", lambda e: e.dma_start(out=xbuf[sl_][:], in_=src_.ap()[i_ * 512:(i_ + 1) * 512, :].rearrange("(j p) d -> p j d", p=128)),
                 writes=[("x", sl_)], dma=("x", sl_))
            conv_some(g, sl_)

        def supertile(xsrc, st_i, mode, gi):
            main = mode == "main"
            sl = cnt["st"] % 2
            cnt["st"] += 1
            xb = xbuf[sl]
            xt = xT[0]
            qk = qkT[0]
            if gi == 0:
                xload(0)
            for j in range(4):
                tl = cnt["tile"] % 2
                cnt["tile"] += 1
                xnj = xn[tl]
                S.op("dve", lambda e, j=j: e.memset(ss1[:, j:j + 1], 0.0), writes=[("ss1", j)])
                S.op("act", lambda e, j=j: e.activation(out=junk[:], in_=xb[:, j, :], func=AF.Square, accum_out=ss1[:, j:j + 1]),
                     reads=[("x", sl), ("ss1", j)], writes=["junk", ("ss1", j)])
                S.op("act", lambda e, j=j: e.activation(out=ss1[:, 4 + j:5 + j], in_=ss1[:, j:j + 1], func=AF.Ln, scale=1.0 / D, bias=EPS),
                     reads=[("ss1", j)], writes=[("ss1", 4 + j)])
                S.op("act", lambda e, j=j: e.activation(out=ss1[:, 4 + j:5 + j], in_=ss1[:, 4 + j:5 + j], func=AF.Exp, scale=-0.5),
                     reads=[("ss1", 4 + j)], writes=[("ss1", 4 + j)])
                S.op("act", lambda e, j=j, xnj=xnj: e.mul(xnj[:], xb[:, j, :], ss1[:, 4 + j:5 + j]),
                     reads=[("x", sl), ("ss1", 4 + j)], writes=[("xn", tl)])
                pb, pt = tbank()
                for c in range(8):
                    S.op("pe", lambda e, c=c, pb=pb, xnj=xnj: e.transpose(out=pb[:, c * 128:(c + 1) * 128], in_=xnj[:, c * 128:(c + 1) * 128], identity=ident_b[:]),
                         reads=[("xn", tl), "ident_b"], accw=[pt])
                S.op("dve" if j % 2 == 0 else "act",
                     (lambda e, pb=pb, j=j: e.tensor_copy(out=xt[:, :, j * 128:(j + 1) * 128], in_=pb[:].rearrange("p (c t) -> p c t", c=8))) if j % 2 == 0 else
                     (lambda e, pb=pb, j=j: e.copy(out=xt[:, :, j * 128:(j + 1) * 128], in_=pb[:].rearrange("p (c t) -> p c t", c=8))),
                     reads=[pt], accw=[("xT", 0)])
            chunks = range(8) if mode != "pre" else range(4, 8)
            def chunk_s1(ch):
                pb, pt = fbank()
                for kc in range(8):
                    S.op("pe", lambda e, kc=kc, ch=ch, pb=pb: e.matmul(out=pb[:], lhsT=wqk[:, kc, ch * 128:(ch + 1) * 128], rhs=xt[:, kc, :], start=(kc == 0), stop=(kc == 7)),
                         reads=[("xT", 0), "wqk"], accw=[pt])
                ps_ = cnt["ch"] % 2
                cnt["ch"] += 1
                pr = pre[ps_]
                ca = cacc[ps_]
                s_ = sg[ps_]
                S.op("dve", lambda e, pr=pr, ch=ch: e.tensor_copy(out=pr[:, 0:3], in_=halo[:, ch, :]), reads=[("halo", ch)], writes=[("pre", ps_, "h")])
                S.op("act", lambda e, pr=pr, pb=pb: e.copy(out=pr[:, 3:515], in_=pb[:]), reads=[pt], writes=[("pre", ps_)])
                S.op("dve", lambda e, pr=pr, ch=ch: e.tensor_copy(out=halo[:, ch, :], in_=pr[:, 512:515]), reads=[("pre", ps_), ("pre", ps_, "h")], writes=[("halo", ch)])
                S.op("dve", lambda e, pr=pr, ca=ca, ch=ch: e.tensor_scalar(out=ca[:], in0=pr[:, 0:512], scalar1=cw[:, 0, ch:ch + 1], scalar2=None, op0=ALU.mult),
                     reads=[("pre", ps_), ("pre", ps_, "h"), "cw"], writes=[("cacc", ps_)])
                for i in range(1, 4):
                    S.op("dve", lambda e, pr=pr, ca=ca, ch=ch, i=i: e.scalar_tensor_tensor(out=ca[:], in0=pr[:, i:i + 512], scalar=cw[:, i, ch:ch + 1], in1=ca[:], op0=ALU.mult, op1=ALU.add),
                         reads=[("pre", ps_), ("pre", ps_, "h"), ("cacc", ps_), "cw"], writes=[("cacc", ps_)])
                return (ch, ps_, ca, s_)

            def chunk_s2(args):
                ch, ps_, ca, s_ = args
                S.op("act", lambda e, ca=ca, s_=s_: e.activation(out=s_[:], in_=ca[:], func=AF.Exp, scale=-1.0), reads=[("cacc", ps_)], writes=[("sg", ps_)])
                S.op("act", lambda e, s_=s_: e.activation(out=s_[:], in_=s_[:], func=AF.Ln, bias=1.0), reads=[("sg", ps_)], writes=[("sg", ps_)])
                S.op("act", lambda e, s_=s_: e.activation(out=s_[:], in_=s_[:], func=AF.Exp, scale=-1.0), reads=[("sg", ps_)], writes=[("sg", ps_)])
                S.op("dve", lambda e, s_=s_, ca=ca, ch=ch: e.tensor_tensor(out=qk[:, ch, :], in0=s_[:], in1=ca[:], op=ALU.mult),
                     reads=[("sg", ps_), ("cacc", ps_)], accw=[("qkT", 0)])


            pend = None
            for ch in chunks:
                cur = chunk_s1(ch)
                if pend is not None:
                    chunk_s2(pend)
                pend = cur
            chunk_s2(pend)
            pg, pgt = fbank()
            for j in range(4):
                for kc in range(8):
                    S.op("pe", lambda e, j=j, kc=kc: e.matmul(out=pg[:, j * 8:(j + 1) * 8], lhsT=xt[:, kc, j * 128:(j + 1) * 128], rhs=wif[:, kc, :], start=(kc == 0), stop=(kc == 7)),
                         reads=[("xT", 0), "wif"], accw=[pgt])
            S.op("dve", lambda e: e.tensor_tensor(out=gsb[:], in0=pg[:, 0:32].rearrange("p (c g) -> p c g", c=4), in1=bif_b[:].unsqueeze(1).broadcast_to([128, 4, 8]), op=ALU.add),
                 reads=[pgt, "bif_b"], writes=["gsb"])
            S.op("act", lambda e: e.activation(out=spl[:], in_=gsb[:, :, 4:8], func=AF.Exp, scale=-1.0), reads=["gsb"], writes=["spl"])
            S.op("act", lambda e: e.activation(out=spl[:], in_=spl[:], func=AF.Ln, bias=1.0), reads=["spl"], writes=["spl"])
            pc, pct = fbank()
            prw, prt = fbank()
            for c in range(4):
                S.op("pe", lambda e, c=c: e.matmul(out=pc[:, c * 4:(c + 1) * 4], lhsT=triu_f[:], rhs=spl[:, c, :], start=True, stop=True),
                     reads=["spl", "triu_f"], accw=[pct])
                S.op("pe", lambda e, c=c: e.matmul(out=pc[0:4, 16 + c:17 + c], lhsT=spl[:, c, :], rhs=ones_f[:, 0:1], start=True, stop=True),
                     reads=["spl", "ones_f"], accw=[pct])
                S.op("pe", lambda e, c=c: e.matmul(out=prw[0:4, c * 128:(c + 1) * 128], lhsT=gsb[:, c, 0:4], rhs=ident_f[:], start=True, stop=False),
                     reads=["gsb", "ident_f"], accw=[prt])
                S.op("pe", lambda e, c=c: e.matmul(out=prw[0:4, c * 128:(c + 1) * 128], lhsT=spl[:, c, :], rhs=triu_f[:], start=False, stop=True),
                     reads=["spl", "triu_f"], accw=[prt])
            S.op("dve", lambda e: e.tensor_tensor(out=a_tm[:], in0=gsb[:, :, 0:4], in1=pc[:, 0:16].rearrange("p (c h) -> p c h", c=4), op=ALU.add),
                 reads=["gsb", pct], writes=["a_tm"])
            S.op("dve", lambda e: e.tensor_copy(out=cum_sb[:], in_=pc[:, 0:16].rearrange("p (c h) -> p c h", c=4)), reads=[pct], writes=["cum_sb"])
            S.op("dve", lambda e: e.tensor_copy(out=rw[:, 4:8], in_=pc[0:4, 16:20]), reads=[pct], writes=["rw_tot"])
            S.op("dve", lambda e: e.tensor_reduce(out=rw[:, 0:4], in_=prw[0:4, :].rearrange("p (c l) -> p c l", c=4), axis=AX.X, op=ALU.max),
                 reads=[prt], writes=["rw_A"])
            for c in range(4):
                S.op("dve", lambda e, c=c: e.tensor_copy(out=rw[:, 12 + c:13 + c], in_=m_st[:]), reads=["m_st"], writes=[("rw_mp", c)])
                S.op("dve", lambda e, c=c: e.tensor_tensor(out=rw[:, 8 + c:9 + c], in0=m_st[:], in1=rw[:, c:c + 1], op=ALU.max), reads=["m_st", "rw_A"], writes=[("rw_G", c)])
                S.op("dve", lambda e, c=c: e.tensor_tensor(out=m_st[:], in0=rw[:, 8 + c:9 + c], in1=rw[:, 4 + c:5 + c], op=ALU.subtract), reads=[("rw_G", c), "rw_tot"], writes=["m_st"])
            S.op("dve", lambda e: e.tensor_tensor(out=rw[:, 16:20], in0=rw[:, 12:16], in1=rw[:, 8:12], op=ALU.subtract),
                 reads=[("rw_G", c) for c in range(4)] + [("rw_mp", c) for c in range(4)], writes=["rw_D"])
            S.op("act", lambda e: e.activation(out=rw[:, 16:20], in_=rw[:, 16:20], func=AF.Exp), reads=["rw_D"], writes=["rw_D"])
            S.op("dve", lambda e: e.tensor_tensor(out=rhs8[:, :, 0:4], in0=ident_f[0:4, 0:4].unsqueeze(1).broadcast_to([4, 4, 4]), in1=rw[:, 8:12].unsqueeze(2).broadcast_to([4, 4, 4]), op=ALU.mult),
                 reads=[("rw_G", c) for c in range(4)] + ["ident_f"], writes=["rhs8a"])
            S.op("dve", lambda e: e.tensor_tensor(out=rhs8[:, :, 4:8], in0=ident_f[0:4, 0:4].unsqueeze(1).broadcast_to([4, 4, 4]), in1=rw[:, 16:20].unsqueeze(2).broadcast_to([4, 4, 4]), op=ALU.mult),
                 reads=["rw_D", "ident_f"], writes=["rhs8b"])
            S.op("pe", lambda e: e.matmul(out=pc[:, 32:64], lhsT=ones_f[0:4, :], rhs=rhs8[:].rearrange("p c g -> p (c g)"), start=True, stop=True),
                 reads=["rhs8a", "rhs8b", "ones_f"], accw=[pct])
            S.op("dve", lambda e: e.tensor_copy(out=gb_sb[:], in_=pc[:, 32:64].rearrange("p (c g) -> p c g", c=4)), reads=[pct], writes=["gb_sb"])
            S.op("dve", lambda e: e.tensor_tensor(out=e_tm[:], in0=a_tm[:], in1=gb_sb[:, :, 0:4], op=ALU.subtract), reads=["a_tm", "gb_sb"], writes=["e_tm"])
            S.op("act", lambda e: e.activation(out=e_tm[:], in_=e_tm[:], func=AF.Exp), reads=["e_tm"], writes=["e_tm"])
            S.op("dve", lambda e: e.tensor_tensor(out=dn_tm[:], in0=cum_sb[:], in1=gb_sb[:, :, 0:4], op=ALU.subtract), reads=["cum_sb", "gb_sb"], writes=["dn_tm"])
            S.op("act", lambda e: e.activation(out=dn_tm[:], in_=dn_tm[:], func=AF.Exp, bias=lnc0_t[:, 0:1]), reads=["dn_tm", "lnc0"], writes=["dn_tm"])

            if gi + 1 < NPRE + NST:
                xload(gi + 1)
            def tile_body(j):
                c = j
                csl = slice(c * 128, (c + 1) * 128)
                tsl = cnt["tl2"] % 2
                cnt["tl2"] += 1
                vea = ve[tsl]
                sm = smask[tsl]
                ktm = ktm2[tsl]
                og = og2[tsl]
                ysl = cnt["y"] % 2
                if main:
                    cnt["y"] += 1
                yt_ = ybuf[ysl]
                def chainA1():
                    pv, pvt = fbankA()
                    for kc in range(8):
                        yield S.op("pe", lambda e, kc=kc, pv=pv: e.matmul(out=pv[:], lhsT=xt[:, kc, csl], rhs=wvo[:, kc, 0:512], start=(kc == 0), stop=(kc == 7)),
                             reads=[("xT", 0), "wvo"], accw=[pvt])
                    yield S.op("dve", lambda e, pv=pv, vea=vea: e.tensor_tensor(out=vea[:, :, 0:128], in0=pv[:].rearrange("p (h d) -> p h d", h=4), in1=e_tm[:, c, :].unsqueeze(2).broadcast_to([128, 4, 128]), op=ALU.mult),
                         reads=[pvt, "e_tm"], writes=[("ve", tsl)])
                    yield S.op("dve", lambda e, vea=vea: e.tensor_copy(out=vea[:, :, 128:129], in_=e_tm[:, c, :].unsqueeze(2)), reads=["e_tm"], writes=[("ve", tsl, "e")])
                    pb, pt = (ptr[0], ("ptr", 0))
                    for h in range(4):
                        yield S.op("pe", lambda e, h=h, pb=pb: e.transpose(out=pb[:, h * 128:(h + 1) * 128], in_=qk[:, 4 + h, csl], identity=ident_b[:]),
                             reads=[("qkT", 0), "ident_b"], accw=[pt])
                    yield S.op("act", lambda e, pb=pb: e.copy(out=ktm[:].rearrange("p h d -> p (h d)"), in_=pb[:, 0:512]), reads=[pt], writes=[("ktm", tsl)])
                    if main:
                        po, pot = fbankA()
                        for kc in range(8):
                            yield S.op("pe", lambda e, kc=kc, po=po: e.matmul(out=po[:], lhsT=xt[:, kc, csl], rhs=wvo[:, kc, 512:1024], start=(kc == 0), stop=(kc == 7)),
                                 reads=[("xT", 0), "wvo"], accw=[pot])
                        yield S.op("act", lambda e, po=po: e.activation(out=og[:], in_=po[:], func=AF.Exp, scale=-1.0), reads=[pot], writes=[("og", tsl)])
                        yield S.op("act", lambda e: e.activation(out=og[:], in_=og[:], func=AF.Ln, bias=1.0), reads=[("og", tsl)], writes=[("og", tsl)])
                        yield S.op("act", lambda e: e.activation(out=og[:], in_=og[:], func=AF.Exp, scale=-1.0), reads=[("og", tsl)], writes=[("og", tsl)])
                        pS, pSt = fbankA()
                        for h in range(4):
                            yield S.op("pe", lambda e, h=h, pS=pS: e.matmul(out=pS[:, h * 128:(h + 1) * 128], lhsT=qk[:, 4 + h, csl], rhs=qk[:, h, csl], start=True, stop=True),
                                 reads=[("qkT", 0)], accw=[pSt])
                        sm = smask[tsl]
                        yield S.op("dve", lambda e, pS=pS, sm=sm: e.tensor_tensor(out=sm[:], in0=pS[:].rearrange("p (h l) -> p h l", h=4), in1=triu_f[:].unsqueeze(1).broadcast_to([128, 4, 128]), op=ALU.mult),
                             reads=[pSt, "triu_f"], writes=[("smask", tsl)])
                    yield None
                def chainA2():
                    if main:
                        for h in range(4):
                            yield S.op("act", lambda e, h=h: e.mul(Csb[:, h, :], C32[:, h, :], gb_sb[:, c, 4 + h:5 + h]), reads=[("C32", h), "gb_sb"], writes=[("Csb", h)])
                        nbanks = []
                        for hp in range(2):
                            pn, pnt = fbankA2()
                            nbanks.append((pn, pnt))
                            for hh in range(2):
                                h = hp * 2 + hh
                                yield S.op("pe", lambda e, h=h, hh=hh, pn=pn, sm=sm, vea=vea: e.matmul(out=pn[:, hh * 129:(hh + 1) * 129], lhsT=sm[:, h, :], rhs=vea[:, h, :], start=True, stop=False),
                                     reads=[("smask", tsl), ("ve", tsl), ("ve", tsl, "e")], accw=[pnt])
                                yield S.op("pe", lambda e, h=h, hh=hh, pn=pn: e.matmul(out=pn[:, hh * 129:(hh + 1) * 129], lhsT=qk[:, h, csl], rhs=Csb[:, h, :], start=False, stop=True),
                                     reads=[("qkT", 0), ("Csb", h)], accw=[pnt])
                        for hp in range(2):
                            pn, pnt = nbanks[hp]
                            yield S.op("act", lambda e, pn=pn, hp=hp: e.activation(out=nqa[:, hp * 2:hp * 2 + 2].unsqueeze(2), in_=pn[:, 0:258].rearrange("p (h d) -> p h d", h=2)[:, :, 128:129], func=AF.Abs),
                                 reads=[pnt], writes=["nqa"])
                        yield S.op("dve", lambda e: e.tensor_tensor(out=nqa[:], in0=nqa[:], in1=dn_tm[:, c, :], op=ALU.max), reads=["nqa", "dn_tm"], writes=["nqa"])
                        yield S.op("dve", lambda e: e.reciprocal(out=nqa[:], in_=nqa[:]), reads=["nqa"], writes=["nqa"])
                        for hp in range(2):
                            pn, pnt = nbanks[hp]
                            yield S.op("dve", lambda e, pn=pn, hp=hp: e.tensor_tensor(out=hs[:, hp * 2:hp * 2 + 2, :], in0=pn[:, 0:258].rearrange("p (h d) -> p h d", h=2)[:, :, 0:128], in1=nqa[:, hp * 2:hp * 2 + 2].unsqueeze(2).broadcast_to([128, 2, 128]), op=ALU.mult),
                                 reads=[pnt, "nqa"], writes=["hs"])
                        yield S.op("dve", lambda e: e.tensor_tensor(out=hs[:].rearrange("p h d -> p (h d)"), in0=hs[:].rearrange("p h d -> p (h d)"), in1=og[:], op=ALU.mult),
                             reads=["hs", ("og", tsl)], writes=["hs"])
                        yield S.op("dve", lambda e: e.tensor_tensor(out=tmpa[:, 0:512], in0=hs[:].rearrange("p h d -> p (h d)"), in1=hs[:].rearrange("p h d -> p (h d)"), op=ALU.mult), reads=["hs"], writes=["tmpa"])
                        yield S.op("dve", lambda e: e.tensor_reduce(out=sm8[:, 0:4], in_=tmpa[:, 0:512].rearrange("p (h d) -> p h d", h=4), axis=AX.X, op=ALU.add), reads=["tmpa"], writes=["sm8a"])
                        yield S.op("act", lambda e: e.activation(out=sm8[:, 0:4], in_=sm8[:, 0:4], func=AF.Ln, scale=1.0 / 128, bias=EPS), reads=["sm8a"], writes=["sm8a"])
                        yield S.op("act", lambda e: e.activation(out=sm8[:, 0:4], in_=sm8[:, 0:4], func=AF.Exp, scale=-0.5), reads=["sm8a"], writes=["sm8a"])
                        yield S.op("dve", lambda e: e.tensor_tensor(out=hs[:], in0=hs[:], in1=sm8[:, 0:4].unsqueeze(2).broadcast_to([128, 4, 128]), op=ALU.mult), reads=["hs", "sm8a"], writes=["hs"])
                        yield S.op("dve", lambda e, yt_=yt_: e.tensor_tensor(out=yt_[:, 0:512], in0=hs[:].rearrange("p h d -> p (h d)"), in1=gml_b[:], op=ALU.mult), reads=["hs", "gml_b"], writes=[("y", ysl, 0)])
                    for hp in range(2):
                        pu_, put = fbankA2()
                        for hh in range(2):
                            h = hp * 2 + hh
                            yield S.op("pe", lambda e, h=h, hh=hh, pu_=pu_, vea=vea: e.matmul(out=pu_[:, hh * 129:(hh + 1) * 129], lhsT=ktm[:, h, :], rhs=vea[:, h, :], start=True, stop=True),
                                 reads=[("ktm", tsl), ("ve", tsl), ("ve", tsl, "e")], accw=[put])
                        for hh in range(2):
                            h = hp * 2 + hh
                            yield S.op("dve", lambda e, h=h, hh=hh, pu_=pu_: e.scalar_tensor_tensor(out=C32[:, h, :], in0=C32[:, h, :], scalar=gb_sb[:, c, 4 + h:5 + h], in1=pu_[:, hh * 129:(hh + 1) * 129], op0=ALU.mult, op1=ALU.add),
                                 reads=[put, "gb_sb", ("C32", h)], writes=[("C32", h)])

                    yield None
                def chainB():
                    pU, pUt = fbankB()
                    pV, pVt = fbankB()
                    for kc in range(8):
                        yield S.op("pe", lambda e, kc=kc, pU=pU: e.matmul(out=pU[:], lhsT=xt[:, kc, csl], rhs=wuv[:, kc, 0:512], start=(kc == 0), stop=(kc == 7)),
                             reads=[("xT", 0), "wuv"], accw=[pUt])
                    for kc in range(8):
                        yield S.op("pe", lambda e, kc=kc, pV=pV: e.matmul(out=pV[:], lhsT=xt[:, kc, csl], rhs=wuv[:, kc, 512:1024], start=(kc == 0), stop=(kc == 7)),
                             reads=[("xT", 0), "wuv"], accw=[pVt])
                    yield S.op("act", lambda e, pU=pU: e.copy(out=ux[:, 0:512], in_=pU[:]), reads=[pUt], writes=["ux"])
                    yield S.op("act", lambda e, pV=pV: e.copy(out=ux[:, 512:1024], in_=pV[:]), reads=[pVt], writes=["ux"])
                    yield S.op("act", lambda e: e.activation(out=t1[:], in_=ux[:], func=AF.Square), reads=["ux"], writes=["t1"])
                    yield S.op("dve", lambda e: e.tensor_scalar(out=t1[:], in0=t1[:], scalar1=0.044715, scalar2=1.0, op0=ALU.mult, op1=ALU.add), reads=["t1"], writes=["t1"])
                    yield S.op("dve", lambda e: e.tensor_tensor(out=t1[:], in0=t1[:], in1=ux[:], op=ALU.mult), reads=["t1", "ux"], writes=["t1"])
                    yield S.op("act", lambda e: e.activation(out=t1[:], in_=t1[:], func=AF.Exp, scale=-2.0 * 0.7978845608028654), reads=["t1"], writes=["t1"])
                    yield S.op("act", lambda e: e.activation(out=t1[:], in_=t1[:], func=AF.Ln, bias=1.0), reads=["t1"], writes=["t1"])
                    yield S.op("act", lambda e: e.activation(out=t1[:], in_=t1[:], func=AF.Exp, scale=-1.0), reads=["t1"], writes=["t1"])
                    yield S.op("dve", lambda e: e.tensor_tensor(out=ux[:], in0=t1[:], in1=ux[:], op=ALU.mult), reads=["t1", "ux"], writes=["ux"])
                    yield S.op("dve", lambda e: e.memset(sm8[:, 4:5], 0.0), writes=["sm8b"])
                    yield S.op("act", lambda e: e.activation(out=junk[:, 0:512], in_=ux[:, 512:1024], func=AF.Square, accum_out=sm8[:, 4:5]), reads=["ux", "sm8b"], writes=["junk", "sm8b"])
                    yield S.op("act", lambda e: e.activation(out=sm8[:, 4:5], in_=sm8[:, 4:5], func=AF.Ln, scale=1.0 / 512, bias=EPS), reads=["sm8b"], writes=["sm8b"])
                    yield S.op("act", lambda e: e.activation(out=sm8[:, 4:5], in_=sm8[:, 4:5], func=AF.Exp, scale=-0.5), reads=["sm8b"], writes=["sm8b"])
                    yield S.op("dve", lambda e: e.scalar_tensor_tensor(out=vn[:], in0=ux[:, 512:1024], scalar=sm8[:, 4:5], in1=ggv_b[:], op0=ALU.mult, op1=ALU.mult), reads=["ux", "sm8b", "ggv_b"], writes=["vn"])
                    pM, pMt = fbankB()
                    for g in range(8):
                        yield S.op("pe", lambda e, g=g, pM=pM: e.matmul(out=pM[:, g * 64:(g + 1) * 64], lhsT=wspT[:, g, :], rhs=vn[:, g * 64:(g + 1) * 64], start=True, stop=True),
                             reads=["vn", "wspT"], accw=[pMt])
                    yield S.op("dve", lambda e, pM=pM: e.tensor_tensor(out=gt[:], in0=pM[:].rearrange("p (g c) -> p g c", g=8), in1=bsp[:].unsqueeze(2).broadcast_to([128, 8, 64]), op=ALU.add),
                         reads=[pMt, "bsp"], writes=["gt"])
                    yield S.op("dve", lambda e: e.tensor_tensor(out=gt[:].rearrange("p g c -> p (g c)"), in0=gt[:].rearrange("p g c -> p (g c)"), in1=ux[:, 0:512], op=ALU.mult), reads=["gt", "ux"], writes=["gt"])
                    yield S.op("dve", lambda e: e.tensor_tensor(out=tmpa[:, 512:1024], in0=gt[:].rearrange("p g c -> p (g c)"), in1=gt[:].rearrange("p g c -> p (g c)"), op=ALU.mult), reads=["gt"], writes=["tmpb"])
                    yield S.op("dve", lambda e: e.tensor_reduce(out=sm8[:, 8:16], in_=tmpa[:, 512:1024].rearrange("p (g c) -> p g c", g=8), axis=AX.X, op=ALU.add), reads=["tmpb"], writes=["sm8c"])
                    yield S.op("act", lambda e: e.activation(out=sm8[:, 8:16], in_=sm8[:, 8:16], func=AF.Ln, scale=1.0 / 64, bias=EPS), reads=["sm8c"], writes=["sm8c"])
                    yield S.op("act", lambda e: e.activation(out=sm8[:, 8:16], in_=sm8[:, 8:16], func=AF.Exp, scale=-0.5), reads=["sm8c"], writes=["sm8c"])
                    yield S.op("dve", lambda e: e.tensor_tensor(out=gt[:], in0=gt[:], in1=sm8[:, 8:16].unsqueeze(2).broadcast_to([128, 8, 64]), op=ALU.mult), reads=["gt", "sm8c"], writes=["gt"])
                    yield S.op("dve", lambda e, yt_=yt_: e.tensor_tensor(out=yt_[:, 512:1024], in0=gt[:].rearrange("p g c -> p (g c)"), in1=ggo_b[:], op=ALU.mult), reads=["gt", "ggo_b"], writes=[("y", ysl, 1)])

                    yield None
                def chainC():
                    if "y" in dbg_t:
                        row0d = st_i * 512 + j * 128
                        finals.append(S.op("pool", lambda e, yt_=yt_, row0d=row0d: e.dma_start(out=dbg_t["y"].ap()[row0d:row0d + 128, :], in_=yt_[:]), reads=[("y", ysl, 0), ("y", ysl, 1)], dma=("dbgy", ysl)))
                    pb, pt = (ptr[1], ("ptr", 1))
                    for ec in range(8):
                        yield S.op("pe", lambda e, ec=ec, pb=pb, yt_=yt_: e.transpose(out=pb[:, ec * 128:(ec + 1) * 128], in_=yt_[:, ec * 128:(ec + 1) * 128], identity=ident_b[:]),
                             reads=[("y", ysl, 0), ("y", ysl, 1), "ident_b"], accw=[pt])
                    yT_ = yT[ysl]
                    yield S.op("act", lambda e, pb=pb, yT_=yT_: e.copy(out=yT_[:].rearrange("p c t -> p (c t)"), in_=pb[:]), reads=[pt], writes=[("yT", ysl)])
                    h1t = h1b[ysl]
                    for hf in range(2):
                        ph, pht = fbankC()
                        for ec in range(8):
                            yield S.op("pe", lambda e, ec=ec, ph=ph, hf=hf, yT_=yT_: e.matmul(out=ph[:], lhsT=yT_[:, ec, :], rhs=wout[:, ec, hf * 512:(hf + 1) * 512], start=(ec == 0), stop=(ec == 7)),
                                 reads=[("yT", ysl), "wout"], accw=[pht])
                        yield S.op("dve", lambda e, ph=ph, hf=hf, h1t=h1t: e.tensor_tensor(out=h1t[:, hf * 512:(hf + 1) * 512], in0=ph[:], in1=xb[:, j, hf * 512:(hf + 1) * 512], op=ALU.add),
                             reads=[pht, ("x", sl)], writes=[("h1", ysl, hf)])
                    row0 = st_i * 512 + j * 128
                    yield S.op("sp", lambda e, h1t=h1t, row0=row0: e.dma_start(out=h1buf.ap()[row0:row0 + 128, :], in_=h1t[:]), reads=[("h1", ysl, 0), ("h1", ysl, 1)], accw=["h1buf"], dma=("h1st", ysl))
                    if "h1" in dbg_t:
                        finals.append(S.op("sp", lambda e, h1t=h1t, row0=row0: e.dma_start(out=dbg_t["h1"].ap()[row0:row0 + 128, :], in_=h1t[:]), reads=[("h1", ysl, 0), ("h1", ysl, 1)], dma=("dbgh1", ysl)))
                    ti = st_i * 4 + j
                    x2 = xn2t[ysl]
                    yield S.op("dve", lambda e: e.memset(sm8[:, 5:6], 0.0), writes=["sm8d"])
                    yield S.op("act", lambda e: e.activation(out=junk[:], in_=h1t[:], func=AF.Square, accum_out=sm8[:, 5:6]), reads=[("h1", ysl, 0), ("h1", ysl, 1), "sm8d"], writes=["junk", "sm8d"])
                    yield S.op("act", lambda e: e.activation(out=sm8[:, 5:6], in_=sm8[:, 5:6], func=AF.Ln, scale=1.0 / D, bias=EPS), reads=["sm8d"], writes=["sm8d"])
                    yield S.op("act", lambda e: e.activation(out=sm8[:, 5:6], in_=sm8[:, 5:6], func=AF.Exp, scale=-0.5), reads=["sm8d"], writes=["sm8d"])
                    yield S.op("dve", lambda e: e.scalar_tensor_tensor(out=x2[:], in0=h1t[:], scalar=sm8[:, 5:6], in1=g2_b[:], op0=ALU.mult, op1=ALU.mult),
                         reads=[("h1", ysl, 0), ("h1", ysl, 1), "sm8d", "g2_b"], writes=[("xn2", ysl)])
                    yield S.op("sp", lambda e: e.dma_start(out=xn2lin.ap()[row0:row0 + 128, :], in_=x2[:]), reads=[("xn2", ysl)], accw=["xn2lin"], dma=("xn2st", ysl))
                    pb2, pt2 = (ptr[1], ("ptr", 1))
                    for kc in range(8):
                        yield S.op("pe", lambda e, kc=kc: e.transpose(out=pb2[:, kc * 128:(kc + 1) * 128], in_=x2[:, kc * 128:(kc + 1) * 128], identity=ident_b[:]),
                             reads=[("xn2", ysl), "ident_b"], accw=[pt2])
                    x2T = xn2T[0]
                    yield S.op("act", lambda e: e.copy(out=x2T[:].rearrange("p c t -> p (c t)"), in_=pb2[:]), reads=[pt2], writes=[("xn2T", 0)])
                    pl, plt = fbankC()
                    for kc in range(8):
                        yield S.op("pe", lambda e, kc=kc: e.matmul(out=pl[:, 0:36], lhsT=x2T[:, kc, :], rhs=wr_b[:, kc, :], start=(kc == 0), stop=(kc == 7)),
                             reads=[("xn2T", 0), "wr_b"], accw=[plt])
                    yield S.op("dve", lambda e: e.tensor_tensor(out=lg[:], in0=pl[:, 0:36], in1=br_b[:], op=ALU.add), reads=[plt, "br_b"], writes=["lg"])
                    R_ = lambda a, b: rt[:, a:b]
                    yield S.op("dve", lambda e: e.tensor_reduce(out=R_(0, 1), in_=lg[:, 0:4], axis=AX.X, op=ALU.max), reads=["lg"], writes=["rt"])
                    yield S.op("dve", lambda e: e.tensor_scalar(out=R_(12, 16), in0=lg[:, 0:4], scalar1=R_(0, 1), scalar2=None, op0=ALU.is_equal), reads=["lg", "rt"], writes=["rt"])
                    yield S.op("dve", lambda e: e.tensor_scalar(out=R_(1, 2), in0=R_(0, 1), scalar1=-1.0, scalar2=None, op0=ALU.mult), reads=["rt"], writes=["rt"])
                    yield S.op("dve", lambda e: e.memset(R_(2, 3), 0.0), reads=["rt"], writes=["rt"])
                    yield S.op("act", lambda e: e.activation(out=le8[:, 8:12], in_=lg[:, 0:4], func=AF.Exp, bias=R_(1, 2), accum_out=R_(2, 3)), reads=["lg", "rt"], writes=["rt", "le8x"])
                    yield S.op("dve", lambda e: e.reciprocal(out=R_(3, 4), in_=R_(2, 3)), reads=["rt"], writes=["rt"])
                    yield S.op("dve", lambda e: e.tensor_tensor(out=t32[:], in0=lg[:, 4:36].rearrange("p (g j) -> p g j", g=4), in1=R_(12, 16).unsqueeze(2).broadcast_to([128, 4, 8]), op=ALU.mult), reads=["lg", "rt"], writes=["t32"])
                    yield S.op("dve", lambda e: e.tensor_reduce(out=le8[:, 0:8], in_=t32[:].rearrange("p g j -> p j g"), axis=AX.X, op=ALU.add), reads=["t32"], writes=["le8"])
                    yield S.op("dve", lambda e: e.tensor_reduce(out=R_(4, 5), in_=le8[:, 0:8], axis=AX.X, op=ALU.max), reads=["le8", "rt"], writes=["rt"])
                    yield S.op("dve", lambda e: e.tensor_scalar(out=R_(16, 24), in0=le8[:, 0:8], scalar1=R_(4, 5), scalar2=None, op0=ALU.is_equal), reads=["le8", "rt"], writes=["rt"])
                    yield S.op("dve", lambda e: e.scalar_tensor_tensor(out=le8[:, 0:8], in0=R_(16, 24), scalar=-1e30, in1=le8[:, 0:8], op0=ALU.mult, op1=ALU.add), reads=["le8", "rt"], writes=["le8"])
                    yield S.op("dve", lambda e: e.tensor_reduce(out=R_(5, 6), in_=le8[:, 0:8], axis=AX.X, op=ALU.max), reads=["le8", "rt"], writes=["rt"])
                    yield S.op("dve", lambda e: e.tensor_scalar(out=R_(24, 32), in0=le8[:, 0:8], scalar1=R_(5, 6), scalar2=None, op0=ALU.is_equal), reads=["le8", "rt"], writes=["rt"])
                    yield S.op("dve", lambda e: e.tensor_tensor(out=R_(6, 7), in0=R_(5, 6), in1=R_(4, 5), op=ALU.subtract), reads=["rt"], writes=["rt"])
                    yield S.op("act", lambda e: e.activation(out=R_(6, 7), in_=R_(6, 7), func=AF.Exp), reads=["rt"], writes=["rt"])
                    yield S.op("dve", lambda e: e.tensor_scalar(out=R_(7, 8), in0=R_(6, 7), scalar1=1.0, scalar2=None, op0=ALU.add), reads=["rt"], writes=["rt"])
                    yield S.op("dve", lambda e: e.reciprocal(out=R_(7, 8), in_=R_(7, 8)), reads=["rt"], writes=["rt"])
                    yield S.op("dve", lambda e: e.tensor_tensor(out=R_(8, 9), in0=R_(6, 7), in1=R_(7, 8), op=ALU.mult), reads=["rt"], writes=["rt"])
                    yield S.op("dve", lambda e: e.tensor_scalar(out=pw[:, ti, 2:4], in0=R_(7, 9), scalar1=R_(3, 4), scalar2=None, op0=ALU.mult), reads=["rt"], accw=["pw"])
                    E1 = E1s[:, ti, :]
                    E2 = E2s[:, ti, :]
                    yield S.op("dve", lambda e: e.tensor_tensor(out=E1.rearrange("p (g j) -> p g j", g=4), in0=R_(12, 16).unsqueeze(2).broadcast_to([128, 4, 8]), in1=R_(16, 24).unsqueeze(1).broadcast_to([128, 4, 8]), op=ALU.mult), reads=["rt"], accw=["E1s"])
                    yield S.op("dve", lambda e: e.tensor_tensor(out=E2.rearrange("p (g j) -> p g j", g=4), in0=R_(12, 16).unsqueeze(2).broadcast_to([128, 4, 8]), in1=R_(24, 32).unsqueeze(1).broadcast_to([128, 4, 8]), op=ALU.mult), reads=["rt"], accw=["E2s"])
                    yield S.op("dve", lambda e: e.tensor_tensor(out=ind_b[:], in0=E1, in1=E2, op=ALU.add), reads=["E1s", "E2s"], writes=["ind_b"])
                    pp, ppt = fbankC()
                    yield S.op("pe", lambda e: e.matmul(out=pp[:, 0:32], lhsT=lstr_b[:], rhs=ind_b[:], start=True, stop=True), reads=["ind_b", "lstr_b"], accw=[ppt])
                    yield S.op("pe", lambda e: e.matmul(out=pp[:, 32:64], lhsT=ones_b[:], rhs=ind_b[:], start=True, stop=True), reads=["ind_b", "ones_b"], accw=[ppt])
                    yield S.op("dve", lambda e: e.tensor_tensor(out=posf[:], in0=pp[:, 0:32], in1=base_b[:], op=ALU.add), reads=[ppt, "base_b"], writes=["posf"])
                    yield S.op("dve", lambda e: e.tensor_tensor(out=base_b[:], in0=pp[:, 32:64], in1=base_b[:], op=ALU.add), reads=[ppt, "base_b"], writes=["base_b"])
                    yield S.op("dve", lambda e: e.tensor_tensor(out=t32[:].rearrange("p g j -> p (g j)"), in0=E1, in1=posf[:], op=ALU.mult), reads=["E1s", "posf"], writes=["t32"])
                    yield S.op("dve", lambda e: e.tensor_reduce(out=pw[:, ti, 0:1], in_=t32[:].rearrange("p g j -> p (g j)"), axis=AX.X, op=ALU.add), reads=["t32"], accw=["pw"])
                    yield S.op("dve", lambda e: e.tensor_tensor(out=t32[:].rearrange("p g j -> p (g j)"), in0=E2, in1=posf[:], op=ALU.mult), reads=["E2s", "posf", "pw"], writes=["t32"])
                    yield S.op("dve", lambda e: e.tensor_reduce(out=pw[:, ti, 1:2], in_=t32[:].rearrange("p g j -> p (g j)"), axis=AX.X, op=ALU.add), reads=["t32"], accw=["pw"])
                    yield None
                return chainA1(), chainA2(), (chainB() if main else None), (chainC() if main else None)

            def run_chains(gens):
                act = [g for g in gens if g is not None]
                while act:
                    for g in list(act):
                        try:
                            next(g)
                        except StopIteration:
                            act.remove(g)

            made = {}

            def get(j):
                if j not in made:
                    made[j] = list(tile_body(j))
                return made[j]

            act = {}
            done = set()
            nxt = {"A1": 0, "A2": 0, "B": 0, "C": 0}
            IDX = {"A1": 0, "A2": 1, "B": 2, "C": 3}

            def fin(kind, j):
                return j < 0 or (kind, j) in done

            def start(kind, j):
                g = get(j)[IDX[kind]]
                if g is None:
                    done.add((kind, j))
                else:
                    act[(kind, j)] = g
                nxt[kind] += 1

            def try_start():
                j = nxt["A1"]
                if j < 4 and fin("A1", j - 1) and fin("A2", j - 2):
                    start("A1", j)
                j = nxt["A2"]
                if j < 4 and fin("A1", j) and j < nxt["A1"] and fin("A2", j - 1) and fin("C", j - 2):
                    start("A2", j)
                j = nxt["B"]
                if j < 4 and j < nxt["A1"] and fin("B", j - 1) and fin("C", j - 2):
                    start("B", j)
                j = nxt["C"]
                if j < 4 and j < nxt["A1"] and fin("A2", j) and fin("B", j) and fin("C", j - 1) and j < nxt["A2"] and j < nxt["B"]:
                    start("C", j)

            while len(done) < 16:
                try_start()
                for k_ in list(act.keys()):
                    try:
                        next(act[k_])
                    except StopIteration:
                        del act[k_]
                        done.add(k_)
            if gi == 0 and "qkT" in dbg_t:
                tmpd = S.sb([128, 8, 512], F32, "sdbg_qk")
                S.op("dve", lambda e: e.tensor_copy(out=tmpd[:], in_=qk[:]), reads=[("qkT", 0)], writes=["dbg_qk"])
                finals.append(S.op("sp", lambda e: e.dma_start(out=dbg_t["qkT"].ap(), in_=tmpd[:].rearrange("p a b -> p (a b)")), reads=["dbg_qk"], dma=("dbg", "qkT")))
            if gi == 0 and "xT" in dbg_t:
                tmpx = S.sb([128, 8, 512], F32, "sdbg_xT")
                S.op("dve", lambda e: e.tensor_copy(out=tmpx[:], in_=xt[:]), reads=[("xT", 0)], writes=["dbg_xT"])
                finals.append(S.op("sp", lambda e: e.dma_start(out=dbg_t["xT"].ap(), in_=tmpx[:].rearrange("p a b -> p (a b)")), reads=["dbg_xT"], dma=("dbg", "xT")))

        wcat_bf = nc.dram_tensor("wcat_bf", [NE * 128, 12288], BF16)
        NSUP = NPRE + NST
        conv_rows = NE * 128
        conv_state = {"r": 0, "i": 0}

        def conv_some(gidx, sl):
            tgt = conv_rows * (gidx + 1) // NSUP
            while conv_state["r"] < tgt:
                r0 = conv_state["r"]
                r1 = min(r0 + 32, tgt)
                k = conv_state["i"] % 2
                conv_state["i"] += 1
                conv_state["r"] = r1
                S.op("pool", lambda e, r0=r0, r1=r1: e.dma_start(out=wcat_bf.ap()[r0:r1, :], in_=wcat.ap()[r0:r1, :]),
                     reads=[("x", sl)], writes=[("wck", k)], dma=("wck", k))

        NBLK0 = NT * 2 + NE
        xslots = nc.dram_tensor("xslots", [NBLK0 * 128, D], BF16)
        ztile = junk
        S.op("dve", lambda e: e.memset(ztile[:], 0.0), writes=["junk"])
        for zb in range(0, NBLK0, 8):
            nb_ = min(8, NBLK0 - zb)
            S.op("sp", lambda e, zb=zb, nb_=nb_: e.dma_start(out=xslots.ap()[zb * 128:(zb + nb_) * 128, :].rearrange("(j p) d -> p j d", p=128),
                                                             in_=ztile[:].unsqueeze(1).broadcast_to([128, nb_, 1024])),
                 reads=["junk"], accw=["xslots"], dma=("zinit", (zb // 8) % 4))
        gi = 0
        for s_i in range(NPRE):
            supertile(x_pre, s_i, "prelast" if s_i == NPRE - 1 else "pre", gi)
            gi += 1
        for s_i in range(NST):
            supertile(x_main, s_i, "main", gi)
            gi += 1


        S.flush()
        stA.close()
        stB = contextlib.ExitStack()
        S.cur = stB
        NBLK = NT * 2 + NE
        NSL = NBLK * 128
        yslots = nc.dram_tensor("yslots", [NSL, D], F32)
        pl_ = S.sb([128, 6, 32], F32, "plan")
        pl_i = S.sb([128, 32], I32, "plan_i")
        p128 = S.sb([128, 1], F32, "p128")
        woff_f = S.sb([128, NBLK], F32, "woff_f")
        woff_i = S.sb([128, NBLK], I32, "woff_i")
        bval = S.sb([128, NBLK], F32, "bval")
        neq = S.sb([128, NBLK], F32, "neq")
        cmpb = S.sb([128, NBLK, 32], F32, "cmpb")
        dst_f = S.sb([128, 2, NT], F32, "dst_f")
        dst_i = S.sb([128, 2, NT], I32, "dst_i")
        big = S.sb([128, NT, 32], F32, "bigtmp")
        S.op("pool", lambda e: e.iota(p128[:], [[0, 1]], base=0, channel_multiplier=1, allow_small_or_imprecise_dtypes=True), writes=["p128"])
        S.op("dve", lambda e: e.tensor_scalar(out=pl_[:, 3, :], in0=base_b[:], scalar1=1.0 / 128, scalar2=63.5 / 128, op0=ALU.mult, op1=ALU.add), reads=["base_b"], writes=["plan"])
        S.op("dve", lambda e: e.tensor_copy(out=pl_i[:], in_=pl_[:, 3, :]), reads=["plan"], writes=["plan_i"])
        S.op("dve", lambda e: e.tensor_copy(out=pl_[:, 0, :], in_=pl_i[:]), reads=["plan_i", "plan"], writes=["plan"])
        S.op("dve", lambda e: e.tensor_scalar(out=pl_[:, 0, :], in0=pl_[:, 0, :], scalar1=128.0, scalar2=None, op0=ALU.mult), reads=["plan"], writes=["plan"])
        S.op("dve", lambda e: e.tensor_tensor_scan(out=pl_[:, 1, :], data0=pl_[:, 0, :], data1=pl_[:, 0, :], initial=0.0, op0=ALU.add, op1=ALU.bypass), reads=["plan"], writes=["plan"])
        S.op("dve", lambda e: e.tensor_tensor(out=pl_[:, 2, :], in0=pl_[:, 1, :], in1=pl_[:, 0, :], op=ALU.subtract), reads=["plan"], writes=["plan"])
        S.op("pool", lambda e: e.iota(bval[:], [[128, NBLK]], base=0, channel_multiplier=0, allow_small_or_imprecise_dtypes=True), writes=["bval"])
        S.op("dve", lambda e: e.tensor_tensor(out=cmpb[:], in0=pl_[:, 1, :].unsqueeze(1).broadcast_to([128, NBLK, 32]), in1=bval[:].unsqueeze(2).broadcast_to([128, NBLK, 32]), op=ALU.is_le), reads=["plan", "bval"], writes=["cmpb"])
        S.op("dve", lambda e: e.tensor_reduce(out=woff_f[:], in_=cmpb[:], axis=AX.X, op=ALU.add), reads=["cmpb"], writes=["woff_f"])
        BIGI = 1000000.0
        S.op("dve", lambda e: e.tensor_scalar(out=woff_f[:], in0=woff_f[:], scalar1=31.0, scalar2=None, op0=ALU.min), reads=["woff_f"], writes=["woff_f"])
        S.op("dve", lambda e: e.memset(neq[:, 0:2], 1.0), writes=["neq0"])
        S.op("dve", lambda e: e.tensor_tensor(out=neq[:, 2:NBLK], in0=woff_f[:, 2:NBLK], in1=woff_f[:, 0:NBLK - 2], op=ALU.not_equal), reads=["woff_f"], writes=["neq"])
        S.op("dve", lambda e: e.tensor_scalar(out=woff_f[:], in0=woff_f[:], scalar1=128.0, scalar2=-BIGI, op0=ALU.mult, op1=ALU.add), reads=["woff_f", "neq"], writes=["woff_f"])
        S.op("dve", lambda e: e.tensor_scalar(out=woff_f[:], in0=woff_f[:], scalar1=p128[:, 0:1], scalar2=None, op0=ALU.add), reads=["woff_f", "p128"], writes=["woff_f"])
        S.op("dve", lambda e: e.tensor_tensor(out=woff_f[:], in0=woff_f[:], in1=neq[:], op=ALU.mult), reads=["woff_f", "neq", "neq0"], writes=["woff_f"])
        S.op("dve", lambda e: e.tensor_scalar(out=woff_f[:], in0=woff_f[:], scalar1=BIGI, scalar2=None, op0=ALU.add), reads=["woff_f"], writes=["woff_f"])
        S.op("dve", lambda e: e.tensor_copy(out=woff_i[:], in_=woff_f[:]), reads=["woff_f"], writes=["woff_i"])
        for k_, Es in ((0, E1s), (1, E2s)):
            S.op("dve", lambda e, Es=Es: e.tensor_tensor(out=big[:], in0=Es[:], in1=pl_[:, 2, :].unsqueeze(1).broadcast_to([128, NT, 32]), op=ALU.mult), reads=["E1s", "E2s", "plan"], writes=["big"])
            S.op("dve", lambda e, k_=k_: e.tensor_reduce(out=dst_f[:, k_, :], in_=big[:], axis=AX.X, op=ALU.add), reads=["big"], writes=[("dst_f", k_)])
            S.op("dve", lambda e, k_=k_: e.tensor_tensor(out=dst_f[:, k_, :], in0=dst_f[:, k_, :], in1=pw[:, :, k_], op=ALU.add), reads=[("dst_f", k_), "pw"], writes=[("dst_f", k_)])
        S.op("dve", lambda e: e.tensor_copy(out=dst_i[:], in_=dst_f[:]), reads=[("dst_f", 0), ("dst_f", 1)], writes=["dst_i"])
        if "plan" in dbg_t:
            finals.append(S.op("sp", lambda e: e.dma_start(out=dbg_t["plan"].ap()[:, 0:192], in_=pl_[:].rearrange("p a b -> p (a b)")), reads=["plan"], dma=("dbg", "plan")))
            finals.append(S.op("sp", lambda e: e.dma_start(out=dbg_t["plan"].ap()[:, 768:768 + NBLK], in_=woff_f[:]), reads=["woff_f"], dma=("dbg", "plan2")))
            finals.append(S.op("sp", lambda e: e.dma_start(out=dbg_t["plan"].ap()[:, 256:256 + 2 * NT], in_=dst_f[:].rearrange("p a b -> p (a b)")), reads=[("dst_f", 0), ("dst_f", 1)], dma=("dbg", "plan3")))
            finals.append(S.op("sp", lambda e: e.dma_start(out=dbg_t["plan"].ap()[:, 512:512 + 4 * NT], in_=pw[:].rearrange("p a b -> p (a b)")), reads=["pw"], dma=("dbg", "plan4")))
        xsc = [S.sb([128, 1024], BF16, f"xsc{i}") for i in range(2)]
        for ti in range(NT):
            bsl = ti % 2
            S.op("sp", lambda e, ti=ti, bsl=bsl: e.dma_start(out=xsc[bsl][:], in_=xn2lin.ap()[ti * 128:(ti + 1) * 128, :]), reads=["xn2lin"], writes=[("xsc", bsl)], dma=("xsc", bsl))
            for k_ in range(2):
                S.op("pool", lambda e, ti=ti, bsl=bsl, k_=k_: e.indirect_dma_start(out=xslots.ap(), out_offset=bass.IndirectOffsetOnAxis(ap=dst_i[:, k_, ti:ti + 1], axis=0), in_=xsc[bsl][:], in_offset=None),
                     reads=[("xsc", bsl), "dst_i"], accw=["xslots"], dma=("scat", bsl, k_))

        wbuf = [S.sb([128, 12288], BF16, f"wbuf{i}") for i in range(2)]
        xs_b = [S.sb([128, 1024], BF16, f"xs_b{i}") for i in range(4)]
        xsT = [S.sb([128, 8, 128], BF16, f"xsT{i}") for i in range(2)]
        eg = [S.sb([128, 512], F32, f"eg{i}") for i in range(2)]
        hid = [S.sb([128, 512], BF16, f"hid{i}") for i in range(2)]
        hidT = [S.sb([128, 4, 128], BF16, f"hidT{i}") for i in range(2)]
        ysb = [S.sb([128, 1024], F32, f"ysb{i}") for i in range(2)]
        regs = {}

        def wgather(e, b, ws):
            if "bnd" not in regs:
                regs["bnd"] = st.enter_context(e.register("wbnd"))
                e.reg_mov(regs["bnd"], NE * 128 - 1)
            return e.indirect_dma_start(out=wbuf[ws][:], out_offset=None, in_=wcat_bf.ap(), in_offset=bass.IndirectOffsetOnAxis(ap=woff_i[:, b:b + 1], axis=0),
                                        bounds_check=regs["bnd"], oob_is_err=False)

        mrr = [0, 0]

        def fbankM(p):
            i = 3 * p + mrr[p] % 3
            mrr[p] += 1
            return pfb[i], ("pf", i)

        def blk(b):
            ws = b % 2
            yield S.op("pool", lambda e, b=b, ws=ws: wgather(e, b, ws), reads=["woff_i"], writes=[("wb", ws)], dma=("wb", ws))
            if b < 2:
                yield S.op("sp", lambda e, b=b: e.dma_start(out=xs_b[b % 4][:], in_=xslots.ap()[b * 128:(b + 1) * 128, :]), reads=["xslots"], writes=[("xs", b % 4)], dma=("xs", b % 4))
            if b + 2 < NBLK:
                yield S.op("sp", lambda e, b=b: e.dma_start(out=xs_b[(b + 2) % 4][:], in_=xslots.ap()[(b + 2) * 128:(b + 3) * 128, :]), reads=["xslots"], writes=[("xs", (b + 2) % 4)], dma=("xs", (b + 2) % 4))
            xq = b % 4
            pb, pt = (ptr[ws], ("ptr", ws))
            for kc in range(8):
                yield S.op("pe", lambda e, kc=kc, pb=pb, xq=xq: e.transpose(out=pb[:, kc * 128:(kc + 1) * 128], in_=xs_b[xq][:, kc * 128:(kc + 1) * 128], identity=ident_b[:]),
                     reads=[("xs", xq), "ident_b"], accw=[pt])
            yield S.op("act", lambda e, pb=pb, ws=ws: e.copy(out=xsT[ws][:].rearrange("p c t -> p (c t)"), in_=pb[:]), reads=[pt], writes=[("xsT", ws)])
            pG, pGt = fbankM(ws)
            pU2, pU2t = fbankM(ws)
            for kc in range(8):
                yield S.op("pe", lambda e, kc=kc, pG=pG, ws=ws: e.matmul(out=pG[:], lhsT=xsT[ws][:, kc, :], rhs=wbuf[ws][:, kc * 1024:kc * 1024 + 512], start=(kc == 0), stop=(kc == 7)),
                     reads=[("xsT", ws), ("wb", ws)], accw=[pGt])
            for kc in range(8):
                yield S.op("pe", lambda e, kc=kc, pU2=pU2, ws=ws: e.matmul(out=pU2[:], lhsT=xsT[ws][:, kc, :], rhs=wbuf[ws][:, kc * 1024 + 512:(kc + 1) * 1024], start=(kc == 0), stop=(kc == 7)),
                     reads=[("xsT", ws), ("wb", ws)], accw=[pU2t])
            yield S.op("act", lambda e, pG=pG, ws=ws: e.activation(out=eg[ws][:], in_=pG[:], func=AF.Exp, scale=-1.0), reads=[pGt], writes=[("eg", ws)])
            yield S.op("act", lambda e, ws=ws: e.activation(out=eg[ws][:], in_=eg[ws][:], func=AF.Ln, bias=1.0), reads=[("eg", ws)], writes=[("eg", ws)])
            yield S.op("act", lambda e, ws=ws: e.activation(out=eg[ws][:], in_=eg[ws][:], func=AF.Exp, scale=-1.0), reads=[("eg", ws)], writes=[("eg", ws)])
            yield S.op("dve", lambda e, pG=pG, ws=ws: e.tensor_tensor(out=eg[ws][:], in0=eg[ws][:], in1=pG[:], op=ALU.mult), reads=[("eg", ws), pGt], writes=[("eg", ws)])
            yield S.op("dve", lambda e, pU2=pU2, ws=ws: e.tensor_tensor(out=hid[ws][:], in0=eg[ws][:], in1=pU2[:], op=ALU.mult), reads=[("eg", ws), pU2t], writes=[("hid", ws)])
            pb, pt = (ptr[ws], ("ptr", ws))
            for fc in range(4):
                yield S.op("pe", lambda e, fc=fc, pb=pb, ws=ws: e.transpose(out=pb[:, fc * 128:(fc + 1) * 128], in_=hid[ws][:, fc * 128:(fc + 1) * 128], identity=ident_b[:]),
                     reads=[("hid", ws), "ident_b"], accw=[pt])
            yield S.op("act", lambda e, pb=pb, ws=ws: e.copy(out=hidT[ws][:].rearrange("p c t -> p (c t)"), in_=pb[:, 0:512]), reads=[pt], writes=[("hidT", ws)])
            for hf in range(2):
                pY, pYt = fbankM(ws)
                for fc in range(4):
                    yield S.op("pe", lambda e, fc=fc, pY=pY, ws=ws, hf=hf: e.matmul(out=pY[:], lhsT=hidT[ws][:, fc, :], rhs=wbuf[ws][:, 8192 + fc * 1024 + hf * 512:8192 + fc * 1024 + (hf + 1) * 512], start=(fc == 0), stop=(fc == 3)),
                         reads=[("hidT", ws), ("wb", ws)], accw=[pYt])
                if hf == 0:
                    yield S.op("act", lambda e, pY=pY, ws=ws: e.copy(out=ysb[ws][:, 0:512], in_=pY[:]), reads=[pYt], writes=[("ysb", ws, 0)])
                else:
                    yield S.op("dve", lambda e, pY=pY, ws=ws: e.tensor_copy(out=ysb[ws][:, 512:1024], in_=pY[:]), reads=[pYt], writes=[("ysb", ws, 1)])
            yield S.op("sp", lambda e, b=b, ws=ws: e.dma_start(out=yslots.ap()[b * 128:(b + 1) * 128, :], in_=ysb[ws][:]), reads=[("ysb", ws, 0), ("ysb", ws, 1)], accw=["yslots"], dma=("yst", ws))


            yield None

        for b in range(NBLK):
            for _ in blk(b):
                pass

        fg_b = bload("fg_b", final_g, 1024)
        NCB = 3
        hc = [S.sb([128, 1024], F32, f"hc{i}") for i in range(NCB)]
        y1 = [S.sb([128, 1024], F32, f"y1_{i}") for i in range(NCB)]
        y2 = [S.sb([128, 1024], F32, f"y2_{i}") for i in range(NCB)]
        fs = S.sb([128, 2], F32, "fs")

        def cloads(ti):
            cs = ti % NCB
            S.op("sp", lambda e, ti=ti, cs=cs: e.dma_start(out=hc[cs][:], in_=h1buf.ap()[ti * 128:(ti + 1) * 128, :]), reads=["h1buf"], writes=[("hc", cs)], dma=("hc", cs))
            S.op("pool", lambda e, ti=ti, cs=cs: e.indirect_dma_start(out=y1[cs][:], out_offset=None, in_=yslots.ap(), in_offset=bass.IndirectOffsetOnAxis(ap=dst_i[:, 0, ti:ti + 1], axis=0)),
                 reads=["yslots", "dst_i"], writes=[("y1", cs)], dma=("y1", cs))
            S.op("pool", lambda e, ti=ti, cs=cs: e.indirect_dma_start(out=y2[cs][:], out_offset=None, in_=yslots.ap(), in_offset=bass.IndirectOffsetOnAxis(ap=dst_i[:, 1, ti:ti + 1], axis=0)),
                 reads=["yslots", "dst_i"], writes=[("y2", cs)], dma=("y2", cs))

        for ti in range(min(NCB - 1, NT)):
            cloads(ti)
        for ti in range(NT):
            cs = ti % NCB
            if ti + NCB - 1 < NT:
                cloads(ti + NCB - 1)
            S.op("dve", lambda e, ti=ti, cs=cs: e.scalar_tensor_tensor(out=hc[cs][:], in0=y1[cs][:], scalar=pw[:, ti, 2:3], in1=hc[cs][:], op0=ALU.mult, op1=ALU.add), reads=[("hc", cs), ("y1", cs), "pw"], writes=[("hc", cs)])
            S.op("dve", lambda e, ti=ti, cs=cs: e.scalar_tensor_tensor(out=hc[cs][:], in0=y2[cs][:], scalar=pw[:, ti, 3:4], in1=hc[cs][:], op0=ALU.mult, op1=ALU.add), reads=[("hc", cs), ("y2", cs), "pw"], writes=[("hc", cs)])
            S.op("dve", lambda e, ti=ti: e.memset(fs[:, (ti % 2):(ti % 2) + 1], 0.0), writes=[("fs", ti % 2)])
            S.op("act", lambda e, ti=ti, cs=cs: e.activation(out=junk[:], in_=hc[cs][:], func=AF.Square, accum_out=fs[:, (ti % 2):(ti % 2) + 1]), reads=[("hc", cs), ("fs", ti % 2)], writes=["junk", ("fs", ti % 2)])
            S.op("act", lambda e, ti=ti: e.activation(out=fs[:, (ti % 2):(ti % 2) + 1], in_=fs[:, (ti % 2):(ti % 2) + 1], func=AF.Ln, scale=1.0 / D, bias=EPS), reads=[("fs", ti % 2)], writes=[("fs", ti % 2)])
            S.op("act", lambda e, ti=ti: e.activation(out=fs[:, (ti % 2):(ti % 2) + 1], in_=fs[:, (ti % 2):(ti % 2) + 1], func=AF.Exp, scale=-0.5), reads=[("fs", ti % 2)], writes=[("fs", ti % 2)])
            S.op("dve", lambda e, ti=ti, cs=cs: e.scalar_tensor_tensor(out=y1[cs][:], in0=hc[cs][:], scalar=fs[:, (ti % 2):(ti % 2) + 1], in1=fg_b[:], op0=ALU.mult, op1=ALU.mult), reads=[("hc", cs), ("fs", ti % 2), "fg_b", ("y1", cs)], writes=[("y1", cs)])
            finals.append(S.op("sp", lambda e, ti=ti, cs=cs: e.dma_start(out=out.ap()[ti * 128:(ti + 1) * 128, :], in_=y1[cs][:]), reads=[("y1", cs)], dma=("ost", cs)))

        S.flush()
        stB.close()
    return nc


def make_wcat(w_gate, w_up, w_down):
    g = w_gate.reshape(NE, 8, 128, 512).transpose(0, 2, 1, 3)
    u = w_up.reshape(NE, 8, 128, 512).transpose(0, 2, 1, 3)
    gu = np.concatenate([g, u], axis=3).reshape(NE, 128, 8192)
    dn = w_down.reshape(NE, 4, 128, 1024).transpose(0, 2, 1, 3).reshape(NE, 128, 4096)
    return np.ascontiguousarray(np.concatenate([gu, dn], axis=2).reshape(NE * 128, 12288))


def kernel(**inputs):
    f = lambda k: np.ascontiguousarray(np.asarray(inputs[k], dtype=np.float32))
    x = f("x")
    com = {
        "norm1_g": f("norm1_g")[0], "w_in": f("w_in")[0], "conv_qk": f("conv_qk")[0],
        "b_if": np.concatenate([f("b_igate")[0], f("b_fgate")[0]]), "g_mlstm_out": f("g_mlstm_out")[0],
        "g_gmlp_v": f("g_gmlp_v")[0], "w_spatial": f("w_spatial")[0], "b_spatial": f("b_spatial")[0],
        "g_gmlp_out": f("g_gmlp_out")[0], "w_out": f("w_out")[0], "norm2_g": f("norm2_g")[0],
        "w_router": np.ascontiguousarray(np.concatenate([f("w_router_group")[0], f("w_router_expert")[0]], axis=1)),
        "b_router": np.concatenate([f("b_router_group")[0], f("b_router_expert")[0]]),
        "wcat": make_wcat(f("w_gate")[0], f("w_up")[0], f("w_down")[0]), "final_g": f("final_g"),
    }
    in_maps = []
    for c in range(8):
        b, half = c // 2, c % 2
        m = dict(com)
        m["x_main"] = np.ascontiguousarray(x[b, half * 4096:(half + 1) * 4096])
        m["x_pre"] = np.ascontiguousarray(x[b, 0:4096]) if half == 1 else np.zeros((4096, D), np.float32)
        in_maps.append(m)
    nc = build(8, 8)
    res = run_bass_kernel_spmd(nc, in_maps, core_ids=list(range(8)))
    out = np.empty((4, 8192, D), np.float32)
    for c in range(8):
        out[c // 2, (c % 2) * 4096:(c % 2 + 1) * 4096] = res.results[c]["out"]
    return out
```

```python
import contextlib
import numpy as np
import concourse.bass as bass
import concourse.mybir as mybir
from concourse.bass_utils import run_bass_kernel_spmd

F32 = mybir.dt.float32
BF16 = mybir.dt.bfloat16
I32 = mybir.dt.int32
AF = mybir.ActivationFunctionType
ALU = mybir.AluOpType
AX = mybir.AxisListType

ENGS = ("pe", "act", "dve", "pool", "sp")
SEM_CH = 8000
D = 1024
EPS = 1e-6
NE = 32
NBLK_MAX = 96
NSLOT = NBLK_MAX * 128


class Sched:
    def __init__(self, nc, stack):
        self.nc = nc
        self.stack = stack
        self.ops = []
        self.last_w = {}
        self.readers = {}
        self.dma_count = {}
        self.nbuf = 0
        self.flushed = 0
        self.sems = {}
        self.eng_seq = {e: 0 for e in ENGS}
        self.waited = {e: {} for e in ENGS}
        self.cur = stack
        self.est_end = []
        self.eng_free = {e: 0.0 for e in ENGS}
        self.last_est = 0.0

    def sb(self, shape, dt, name=None, persist=False):
        self.nbuf += 1
        return (self.stack if persist else self.cur).enter_context(self.nc.sbuf_tensor(name or f"sb{self.nbuf}", list(shape), dt))

    def ps(self, shape, dt, name=None):
        self.nbuf += 1
        return self.stack.enter_context(self.nc.psum_tensor(name or f"ps{self.nbuf}", list(shape), dt))

    def op(self, eng, fn, reads=(), writes=(), accw=(), dma=None):
        i = len(self.ops)
        deps = set()
        for t in reads:
            deps.update(self.last_w.get(t, ()))
        for t in writes:
            deps.update(self.last_w.get(t, ()))
            deps.update(self.readers.get(t, ()))
        for t in accw:
            deps.update(self.readers.get(t, ()))
        for t in reads:
            self.readers.setdefault(t, []).append(i)
        for t in writes:
            self.last_w[t] = [i]
            self.readers[t] = []
        for t in accw:
            if self.readers.get(t):
                self.last_w[t] = []
                self.readers[t] = []
            self.last_w.setdefault(t, []).append(i)
        deps.discard(i)
        self.ops.append(dict(eng=eng, fn=fn, deps=sorted(deps), dma=dma, sig=None))
        cost = 2.5 if dma is not None else {"pe": 0.25, "act": 0.7, "dve": 0.6, "pool": 1.0, "sp": 0.1}[eng]
        t0 = max([self.eng_free[eng]] + [self.est_end[d] for d in deps if d < len(self.est_end)])
        if dma is not None:
            self.eng_free[eng] = t0 + 0.1
        else:
            self.eng_free[eng] = t0 + cost
        self.est_end.append(t0 + cost)
        self.last_est = t0 + cost
        return i

    def flush(self):
        nc = self.nc
        ops = self.ops
        lo = self.flushed
        last = {}
        for i in range(lo, len(ops)):
            o = ops[i]
            key = ("dma", o["dma"]) if o["dma"] is not None else ("eng", o["eng"])
            last[key] = i
        bdeps = sorted(last.values())
        for en in ENGS:
            self.ops.append(dict(eng=en, fn=lambda e: e.nop(), deps=list(bdeps), dma=None, sig=None, barrier=True))
            self.est_end.append(max(self.est_end) if self.est_end else 0.0)
        hi = len(ops)

        def pe_pair(a, b):
            return (a["eng"] == "pe" and b["eng"] == "pe" and a["dma"] is None and b["dma"] is None
                    and not b.get("barrier"))

        needed = set()
        for i in range(lo, hi):
            o = ops[i]
            for d in o["deps"]:
                if pe_pair(ops[d], o):
                    continue
                needed.add(d)
        for i in range(lo, hi):
            o = ops[i]
            if o["dma"] is not None:
                k = ("dma", o["dma"])
                self.dma_count[k] = self.dma_count.get(k, 0) + 1
                o["sig"] = (k, 16 * self.dma_count[k])
                self.get_sem(k)
            elif i in needed:
                e = o["eng"]
                n = self.eng_seq[e]
                self.eng_seq[e] += 1
                k = ("eng", e, n // SEM_CH)
                o["sig"] = (k, n % SEM_CH + 1)
                self.get_sem(k)
        sems = self.sems
        waited_all = self.waited

        def run(engname):
            def body(e):
                waited = waited_all[engname]
                for i in range(lo, hi):
                    o = ops[i]
                    if o["eng"] != engname:
                        continue
                    for d in o["deps"]:
                        od = ops[d]
                        if od["sig"] is None or pe_pair(od, o):
                            continue
                        k, v = od["sig"]
                        if waited.get(k, 0) >= v:
                            continue
                        e.wait_ge(sems[k], v)
                        waited[k] = v
                    ins = o["fn"](e)
                    if o["sig"] is not None:
                        k, v = o["sig"]
                        ins.then_inc(sems[k], 16 if o["dma"] is not None else 1)
            return body

        with nc.Block() as block:
            block.tensor(run("pe"))
            block.scalar(run("act"))
            block.vector(run("dve"))
            block.gpsimd(run("pool"))
            block.sync(run("sp"))
        self.flushed = hi
        self.last_w = {}
        self.readers = {}

    def get_sem(self, key):
        if key not in self.sems:
            self.sems[key] = self.stack.enter_context(self.nc.semaphore(f"s_{len(self.sems)}"))
        return self.sems[key]


def build(NST=8, NPRE=8, dbg=None):
    nc = bass.Bass("TRN2", target_bir_lowering=False)
    NT = NST * 4
    TOK = NST * 512

    def din(name, shape, dt=F32):
        return nc.dram_tensor(name, list(shape), dt, kind="ExternalInput")

    x_main = din("x_main", [TOK, D])
    x_pre = din("x_pre", [max(NPRE, 1) * 512, D])
    norm1_g = din("norm1_g", [D])
    w_in = din("w_in", [D, 3080])
    conv_qk = din("conv_qk", [4, 1024])
    b_if = din("b_if", [8])
    g_mlstm = din("g_mlstm_out", [512])
    g_gv = din("g_gmlp_v", [512])
    w_sp = din("w_spatial", [8, 128, 128])
    b_sp = din("b_spatial", [8, 128])
    g_go = din("g_gmlp_out", [512])
    w_out = din("w_out", [D, D])
    norm2_g = din("norm2_g", [D])
    w_r = din("w_router", [D, 36])
    b_r = din("b_router", [36])
    wcat = din("wcat", [NE * 128, 12288])
    final_g = din("final_g", [D])
    out = nc.dram_tensor("out", [TOK, D], F32, kind="ExternalOutput")
    h1buf = nc.dram_tensor("h1buf", [TOK, D], F32)
    dbg_t = {}
    if dbg:
        for k, shp in dbg.items():
            dbg_t[k] = nc.dram_tensor("dbg_" + k, list(shp), F32, kind="ExternalOutput")

    with contextlib.ExitStack() as st:
        S = Sched(nc, st)
        finals = []

        ident_f = S.sb([128, 128], F32, "ident_f")
        ident_b = S.sb([128, 128], BF16, "ident_b")
        triu_f = S.sb([128, 128], F32, "triu_f")
        triu_b = S.sb([128, 128], BF16, "triu_b")
        tril_f = S.sb([128, 128], F32, "tril_f")
        ones_f = S.sb([128, 128], F32, "ones_f")
        S.op("pool", lambda e: e.memset(ident_f[:], 0.0), writes=["ident_f"])
        S.op("pool", lambda e: e.affine_select(out=ident_f[:], in_=ident_f[:], pattern=[[-1, 128]],
                                               compare_op=ALU.not_equal, fill=1.0, base=0, channel_multiplier=1),
             reads=["ident_f"], writes=["ident_f"])
        S.op("pool", lambda e: e.memset(ones_f[:], 1.0), writes=["ones_f"])
        S.op("pool", lambda e: e.affine_select(out=triu_f[:], in_=ones_f[:], pattern=[[1, 128]],
                                               compare_op=ALU.is_ge, fill=0.0, base=0, channel_multiplier=-1),
             reads=["ones_f"], writes=["triu_f"])
        S.op("pool", lambda e: e.affine_select(out=tril_f[:], in_=ones_f[:], pattern=[[-1, 128]],
                                               compare_op=ALU.is_ge, fill=0.0, base=0, channel_multiplier=1),
             reads=["ones_f"], writes=["tril_f"])
        S.op("dve", lambda e: e.tensor_copy(out=ident_b[:], in_=ident_f[:]), reads=["ident_f"], writes=["ident_b"])
        S.op("dve", lambda e: e.tensor_copy(out=triu_b[:], in_=triu_f[:]), reads=["triu_f"], writes=["triu_b"])

        def bload(name, src, n, eng="sp"):
            t = S.sb([128, n], F32, name)
            S.op(eng, lambda e: e.dma_start(out=t[:], in_=bass.AP(src, 0, [[0, 128], [1, n]])),
                 writes=[name], dma=name)
            return t

        gml_b = bload("gml_b", g_mlstm, 512)
        ggv_b = bload("ggv_b", g_gv, 512)
        ggo_b = bload("ggo_b", g_go, 512)
        bif_b = bload("bif_b", b_if, 8)

        g1col = S.sb([128, 8], F32, "g1col")
        cw = S.sb([128, 4, 8], F32, "cw")
        bsp = S.sb([128, 8], F32, "bsp")
        S.op("sp", lambda e: e.dma_start(out=g1col[:], in_=norm1_g.ap().rearrange("(c p) -> p c", p=128),
                                         allow_slow_non_contiguous=True), writes=["g1col"], dma="g1col")
        for i in range(4):
            S.op("sp", lambda e, i=i: e.dma_start(out=cw[:, i, :], in_=conv_qk.ap()[i, :].rearrange("(c p) -> p c", p=128),
                                                  allow_slow_non_contiguous=True), accw=["cw"], dma=("cw", i))
        S.op("sp", lambda e: e.dma_start(out=bsp[:], in_=b_sp.ap().rearrange("g t -> t g"),
                                         allow_slow_non_contiguous=True), writes=["bsp"], dma="bsp")

        ptr = [S.ps([128, 1024], BF16, f"ptr{i}") for i in range(2)]
        pfb = [S.ps([128, 512], F32, f"pf{i}") for i in range(6)]
        rr = {"t": 0, "f": 0, "A": 0, "B": 0, "A2": 0}

        def tbank():
            i = rr["t"] % 2
            rr["t"] += 1
            return ptr[i], ("ptr", i)

        def fbank():
            i = rr["f"] % 6
            rr["f"] += 1
            return pfb[i], ("pf", i)

        def fbankA():
            return pfb[0], ("pf", 0)

        def fbankA2():
            i = 1 + rr["A2"] % 2
            rr["A2"] += 1
            return pfb[i], ("pf", i)

        def fbankB():
            i = 3 + rr["B"] % 2
            rr["B"] += 1
            return pfb[i], ("pf", i)

        def fbankC():
            return pfb[5], ("pf", 5)

        junk = S.sb([128, 1024], BF16, "junk", persist=True)
        base_b = S.sb([128, 32], F32, "base_b", persist=True)
        E1s = S.sb([128, NT, 32], BF16, "E1s", persist=True)
        E2s = S.sb([128, NT, 32], BF16, "E2s", persist=True)
        pw = S.sb([128, NT, 4], F32, "pw", persist=True)
        stA = contextlib.ExitStack()
        S.cur = stA
        xbuf = [S.sb([128, 4, 1024], F32, f"xbuf{i}") for i in range(2)]
        wqk = S.sb([128, 8, 1024], BF16, "wqk")
        wvo = S.sb([128, 8, 1024], BF16, "wvo")
        wuv = S.sb([128, 8, 1024], BF16, "wuv")
        wif = S.sb([128, 8, 8], BF16, "wif")
        wout = S.sb([128, 8, 1024], BF16, "wout")
        for kc in range(8):
            sl = kc % 2
            stg = xbuf[sl][:].rearrange("p a b -> p (a b)")
            S.op("sp", lambda e, stg=stg, kc=kc: e.dma_start(out=stg[:, 0:3080], in_=w_in.ap()[kc * 128:(kc + 1) * 128, :]),
                 writes=[("x", sl)], dma=("x", sl))
            for (dst, c0, n, tok) in ((wqk, 0, 1024, "wqk"), (wvo, 1024, 1024, "wvo"), (wif, 2048, 8, "wif"), (wuv, 2056, 1024, "wuv")):
                S.op("act", lambda e, dst=dst, c0=c0, n=n, kc=kc, stg=stg: e.mul(dst[:, kc, 0:n], stg[:, c0:c0 + n], g1col[:, kc:kc + 1]),
                     reads=[("x", sl), "g1col"], accw=[tok])
        S.op("pool", lambda e: e.dma_start(out=wout[:], in_=w_out.ap().rearrange("(c p) n -> p c n", p=128)),
             writes=["wout"], dma="wout")

        wsp_f = xbuf[1][:, 3, :].rearrange("p (g s) -> p g s", g=8)
        wspT = S.sb([128, 8, 128], BF16, "wspT")
        S.op("sp", lambda e: e.dma_start(out=wsp_f, in_=w_sp.ap().rearrange("g t s -> t g s")), writes=[("x", 1)], dma=("x", 1))
        S.op("dve", lambda e: e.tensor_tensor(out=wsp_f, in0=wsp_f, in1=tril_f[:].unsqueeze(1).broadcast_to([128, 8, 128]), op=ALU.mult),
             reads=[("x", 1), "tril_f"], writes=[("x", 1)])
        for half in range(2):
            pb, pt = fbank()
            for g4 in range(4):
                g = half * 4 + g4
                S.op("pe", lambda e, pb=pb, g=g, g4=g4: e.transpose(out=pb[:, g4 * 128:(g4 + 1) * 128], in_=wsp_f[:, g, :], identity=ident_f[:]),
                     reads=[("x", 1), "ident_f"], accw=[pt])
            S.op("dve", lambda e, pb=pb, half=half: e.tensor_copy(out=wspT[:, half * 4:(half + 1) * 4, :].rearrange("p a b -> p (a b)"), in_=pb[:]),
                 reads=[pt], accw=["wspT"])

        C32 = S.sb([128, 4, 129], F32, "C32")
        Csb = S.sb([128, 4, 129], BF16, "Csb")
        m_st = S.sb([4, 1], F32, "m_st")
        halo = S.sb([128, 8, 3], F32, "halo")
        S.op("pool", lambda e: e.memset(C32[:], 0.0), writes=[("C32", h) for h in range(4)])
        S.op("pool", lambda e: e.memset(m_st[:], 0.0), writes=["m_st"])
        S.op("pool", lambda e: e.memset(halo[:], 0.0), writes=[("halo", c) for c in range(8)])

        xn = [S.sb([128, 1024], BF16, f"xn{i}") for i in range(2)]
        ss1 = S.sb([128, 8], F32, "ss1")
        xT = [S.sb([128, 8, 512], BF16, f"xT{i}") for i in range(1)]
        pre = [S.sb([128, 515], F32, f"pre{i}") for i in range(2)]
        cacc = [S.sb([128, 512], F32, f"cacc{i}") for i in range(2)]
        sg = [S.sb([128, 512], F32, f"sg{i}") for i in range(2)]
        qkT = [S.sb([128, 8, 512], BF16, f"qkT{i}") for i in range(1)]
        ktm2 = [S.sb([128, 4, 128], BF16, f"ktm{i}") for i in range(2)]
        gsb = S.sb([128, 4, 8], F32, "gsb")
        spl = S.sb([128, 4, 4], F32, "spl")
        a_tm = S.sb([128, 4, 4], F32, "a_tm")
        cum_sb = S.sb([128, 4, 4], F32, "cum_sb")
        e_tm = S.sb([128, 4, 4], F32, "e_tm")
        dn_tm = S.sb([128, 4, 4], F32, "dn_tm")
        gb_sb = S.sb([128, 4, 8], F32, "gb_sb")
        rw = S.sb([4, 20], F32, "rw")
        rhs8 = S.sb([4, 4, 8], F32, "rhs8")
        lnc0_t = S.sb([128, 1], F32, "lnc0_t")
        S.op("pool", lambda e: e.memset(lnc0_t[:], -float(np.log(128 ** -0.5))), writes=["lnc0"])
        ve = [S.sb([128, 4, 129], BF16, f"ve{i}") for i in range(2)]
        smask = [S.sb([128, 4, 128], BF16, f"smask{i}") for i in range(2)]
        og2 = [S.sb([128, 512], F32, f"og{i}") for i in range(2)]
        hs = S.sb([128, 4, 128], F32, "hs")
        nqa = S.sb([128, 4], F32, "nqa")
        sm8 = S.sb([128, 16], F32, "sm8")
        tmpa = S.sb([128, 1024], F32, "tmpa")
        ux = S.sb([128, 1024], F32, "ux")
        t1 = S.sb([128, 1024], F32, "t1")
        vn = S.sb([128, 512], BF16, "vn")
        gt = S.sb([128, 8, 64], F32, "gt")
        ybuf = [S.sb([128, 1024], BF16, f"ybuf{i}") for i in range(2)]
        yT = [S.sb([128, 8, 128], BF16, f"yT{i}") for i in range(2)]
        h1b = [S.sb([128, 1024], F32, f"h1b{i}") for i in range(2)]
        g2_b = bload("g2_b", norm2_g, 1024)
        br_b = bload("br_b", b_r, 36)
        wr_f = S.sb([128, 8, 36], F32, "wr_f")
        wr_b = S.sb([128, 8, 36], BF16, "wr_b")
        S.op("sp", lambda e: e.dma_start(out=wr_f[:], in_=w_r.ap().rearrange("(c p) n -> p c n", p=128)), writes=["wr_f"], dma="wr_f")
        S.op("dve", lambda e: e.tensor_copy(out=wr_b[:], in_=wr_f[:]), reads=["wr_f"], writes=["wr_b"])
        lstr_b = S.sb([128, 128], BF16, "lstr_b")
        ones_b = S.sb([128, 128], BF16, "ones_b")
        lstr_f = S.sb([128, 128], F32, "lstr_f")
        S.op("pool", lambda e: e.affine_select(out=lstr_f[:], in_=ones_f[:], pattern=[[1, 128]], compare_op=ALU.is_gt, fill=0.0, base=0, channel_multiplier=-1),
             reads=["ones_f"], writes=["lstr_f"])
        S.op("dve", lambda e: e.tensor_copy(out=lstr_b[:], in_=lstr_f[:]), reads=["lstr_f"], writes=["lstr_b"])
        S.op("dve", lambda e: e.tensor_copy(out=ones_b[:], in_=ones_f[:]), reads=["ones_f"], writes=["ones_b"])
        xn2t = [S.sb([128, 1024], BF16, f"xn2t{i}") for i in range(2)]
        xn2T = [S.sb([128, 8, 128], BF16, f"xn2T{i}") for i in range(1)]
        lg = S.sb([128, 36], F32, "lg")
        rt = S.sb([128, 64], F32, "rt")
        t32 = S.sb([128, 4, 8], F32, "t32")
        le8 = S.sb([128, 16], F32, "le8")
        ind_b = S.sb([128, 32], BF16, "ind_b")
        posf = S.sb([128, 32], F32, "posf")
        S.op("pool", lambda e: e.memset(base_b[:], 0.0), writes=["base_b"])
        xn2lin = nc.dram_tensor("xn2lin", [TOK, D], BF16)
        cnt = {"tile": 0, "st": 0, "ch": 0, "tl2": 0, "y": 0}

        LN_C0 = float(np.log(128 ** -0.5))

        def dump(name, ap_fn, reads, row0=0):
            if name in dbg_t:
                t = dbg_t[name]
                finals.append(S.op("sp", lambda e: e.dma_start(out=t.ap()[row0:row0 + 128, :], in_=ap_fn()), reads=reads, dma=("dbg", name)))

        plan_x = [(x_pre, i) for i in range(NPRE)] + [(x_main, i) for i in range(NST)]

        def xload(g):
            src_, i_ = plan_x[g]
            sl_ = g % 2
            S.op("sp", lambda e: e.dma_start(out=xbuf[sl_][:], in_=src_.ap()[i_ * 512:(i_ + 1) * 512, :].rearrange("(j p) d -> p j d", p=128)),
                 writes=[("x", sl_)], dma=("x", sl_))
            conv_some(g, sl_)

        def supertile(xsrc, st_i, mode, gi):
            main = mode == "main"
            sl = cnt["st"] % 2
            cnt["st"] += 1
            xb = xbuf[sl]
            xt = xT[0]
            qk = qkT[0]
            if gi == 0:
                xload(0)
            for j in range(4):
                tl = cnt["tile"] % 2
                cnt["tile"] += 1
                xnj = xn[tl]
                S.op("dve", lambda e, j=j: e.memset(ss1[:, j:j + 1], 0.0), writes=[("ss1", j)])
                S.op("act", lambda e, j=j: e.activation(out=junk[:], in_=xb[:, j, :], func=AF.Square, accum_out=ss1[:, j:j + 1]),
                     reads=[("x", sl), ("ss1", j)], writes=["junk", ("ss1", j)])
                S.op("act", lambda e, j=j: e.activation(out=ss1[:, 4 + j:5 + j], in_=ss1[:, j:j + 1], func=AF.Ln, scale=1.0 / D, bias=EPS),
                     reads=[("ss1", j)], writes=[("ss1", 4 + j)])
                S.op("act", lambda e, j=j: e.activation(out=ss1[:, 4 + j:5 + j], in_=ss1[:, 4 + j:5 + j], func=AF.Exp, scale=-0.5),
                     reads=[("ss1", 4 + j)], writes=[("ss1", 4 + j)])
                S.op("act", lambda e, j=j, xnj=xnj: e.mul(xnj[:], xb[:, j, :], ss1[:, 4 + j:5 + j]),
                     reads=[("x", sl), ("ss1", 4 + j)], writes=[("xn", tl)])
                pb, pt = tbank()
                for c in range(8):
                    S.op("pe", lambda e, c=c, pb=pb, xnj=xnj: e.transpose(out=pb[:, c * 128:(c + 1) * 128], in_=xnj[:, c * 128:(c + 1) * 128], identity=ident_b[:]),
                         reads=[("xn", tl), "ident_b"], accw=[pt])
                S.op("dve" if j % 2 == 0 else "act",
                     (lambda e, pb=pb, j=j: e.tensor_copy(out=xt[:, :, j * 128:(j + 1) * 128], in_=pb[:].rearrange("p (c t) -> p c t", c=8))) if j % 2 == 0 else
                     (lambda e, pb=pb, j=j: e.copy(out=xt[:, :, j * 128:(j + 1) * 128], in_=pb[:].rearrange("p (c t) -> p c t", c=8))),
                     reads=[pt], accw=[("xT", 0)])
            chunks = range(8) if mode != "pre" else range(4, 8)
            def chunk_s1(ch):
                pb, pt = fbank()
                for kc in range(8):
                    S.op("pe", lambda e, kc=kc, ch=ch, pb=pb: e.matmul(out=pb[:], lhsT=wqk[:, kc, ch * 128:(ch + 1) * 128], rhs=xt[:, kc, :], start=(kc == 0), stop=(kc == 7)),
                         reads=[("xT", 0), "wqk"], accw=[pt])
                ps_ = cnt["ch"] % 2
                cnt["ch"] += 1
                pr = pre[ps_]
                ca = cacc[ps_]
                s_ = sg[ps_]
                S.op("dve", lambda e, pr=pr, ch=ch: e.tensor_copy(out=pr[:, 0:3], in_=halo[:, ch, :]), reads=[("halo", ch)], writes=[("pre", ps_, "h")])
                S.op("act", lambda e, pr=pr, pb=pb: e.copy(out=pr[:, 3:515], in_=pb[:]), reads=[pt], writes=[("pre", ps_)])
                S.op("dve", lambda e, pr=pr, ch=ch: e.tensor_copy(out=halo[:, ch, :], in_=pr[:, 512:515]), reads=[("pre", ps_), ("pre", ps_, "h")], writes=[("halo", ch)])
                S.op("dve", lambda e, pr=pr, ca=ca, ch=ch: e.tensor_scalar(out=ca[:], in0=pr[:, 0:512], scalar1=cw[:, 0, ch:ch + 1], scalar2=None, op0=ALU.mult),
                     reads=[("pre", ps_), ("pre", ps_, "h"), "cw"], writes=[("cacc", ps_)])
                for i in range(1, 4):
                    S.op("dve", lambda e, pr=pr, ca=ca, ch=ch, i=i: e.scalar_tensor_tensor(out=ca[:], in0=pr[:, i:i + 512], scalar=cw[:, i, ch:ch + 1], in1=ca[:], op0=ALU.mult, op1=ALU.add),
                         reads=[("pre", ps_), ("pre", ps_, "h"), ("cacc", ps_), "cw"], writes=[("cacc", ps_)])
                return (ch, ps_, ca, s_)

            def chunk_s2(args):
                ch, ps_, ca, s_ = args
                S.op("act", lambda e, ca=ca, s_=s_: e.activation(out=s_[:], in_=ca[:], func=AF.Exp, scale=-1.0), reads=[("cacc", ps_)], writes=[("sg", ps_)])
                S.op("act", lambda e, s_=s_: e.activation(out=s_[:], in_=s_[:], func=AF.Ln, bias=1.0), reads=[("sg", ps_)], writes=[("sg", ps_)])
                S.op("act", lambda e, s_=s_: e.activation(out=s_[:], in_=s_[:], func=AF.Exp, scale=-1.0), reads=[("sg", ps_)], writes=[("sg", ps_)])
                S.op("dve", lambda e, s_=s_, ca=ca, ch=ch: e.tensor_tensor(out=qk[:, ch, :], in0=s_[:], in1=ca[:], op=ALU.mult),
                     reads=[("sg", ps_), ("cacc", ps_)], accw=[("qkT", 0)])


            pend = None
            for ch in chunks:
                cur = chunk_s1(ch)
                if pend is not None:
                    chunk_s2(pend)
                pend = cur
            chunk_s2(pend)
            pg, pgt = fbank()
            for j in range(4):
                for kc in range(8):
                    S.op("pe", lambda e, j=j, kc=kc: e.matmul(out=pg[:, j * 8:(j + 1) * 8], lhsT=xt[:, kc, j * 128:(j + 1) * 128], rhs=wif[:, kc, :], start=(kc == 0), stop=(kc == 7)),
                         reads=[("xT", 0), "wif"], accw=[pgt])
            S.op("dve", lambda e: e.tensor_tensor(out=gsb[:], in0=pg[:, 0:32].rearrange("p (c g) -> p c g", c=4), in1=bif_b[:].unsqueeze(1).broadcast_to([128, 4, 8]), op=ALU.add),
                 reads=[pgt, "bif_b"], writes=["gsb"])
            S.op("act", lambda e: e.activation(out=spl[:], in_=gsb[:, :, 4:8], func=AF.Exp, scale=-1.0), reads=["gsb"], writes=["spl"])
            S.op("act", lambda e: e.activation(out=spl[:], in_=spl[:], func=AF.Ln, bias=1.0), reads=["spl"], writes=["spl"])
            pc, pct = fbank()
            prw, prt = fbank()
            for c in range(4):
                S.op("pe", lambda e, c=c: e.matmul(out=pc[:, c * 4:(c + 1) * 4], lhsT=triu_f[:], rhs=spl[:, c, :], start=True, stop=True),
                     reads=["spl", "triu_f"], accw=[pct])
                S.op("pe", lambda e, c=c: e.matmul(out=pc[0:4, 16 + c:17 + c], lhsT=spl[:, c, :], rhs=ones_f[:, 0:1], start=True, stop=True),
                     reads=["spl", "ones_f"], accw=[pct])
                S.op("pe", lambda e, c=c: e.matmul(out=prw[0:4, c * 128:(c + 1) * 128], lhsT=gsb[:, c, 0:4], rhs=ident_f[:], start=True, stop=False),
                     reads=["gsb", "ident_f"], accw=[prt])
                S.op("pe", lambda e, c=c: e.matmul(out=prw[0:4, c * 128:(c + 1) * 128], lhsT=spl[:, c, :], rhs=triu_f[:], start=False, stop=True),
                     reads=["spl", "triu_f"], accw=[prt])
            S.op("dve", lambda e: e.tensor_tensor(out=a_tm[:], in0=gsb[:, :, 0:4], in1=pc[:, 0:16].rearrange("p (c h) -> p c h", c=4), op=ALU.add),
                 reads=["gsb", pct], writes=["a_tm"])
            S.op("dve", lambda e: e.tensor_copy(out=cum_sb[:], in_=pc[:, 0:16].rearrange("p (c h) -> p c h", c=4)), reads=[pct], writes=["cum_sb"])
            S.op("dve", lambda e: e.tensor_copy(out=rw[:, 4:8], in_=pc[0:4, 16:20]), reads=[pct], writes=["rw_tot"])
            S.op("dve", lambda e: e.tensor_reduce(out=rw[:, 0:4], in_=prw[0:4, :].rearrange("p (c l) -> p c l", c=4), axis=AX.X, op=ALU.max),
                 reads=[prt], writes=["rw_A"])
            for c in range(4):
                S.op("dve", lambda e, c=c: e.tensor_copy(out=rw[:, 12 + c:13 + c], in_=m_st[:]), reads=["m_st"], writes=[("rw_mp", c)])
                S.op("dve", lambda e, c=c: e.tensor_tensor(out=rw[:, 8 + c:9 + c], in0=m_st[:], in1=rw[:, c:c + 1], op=ALU.max), reads=["m_st", "rw_A"], writes=[("rw_G", c)])
                S.op("dve", lambda e, c=c: e.tensor_tensor(out=m_st[:], in0=rw[:, 8 + c:9 + c], in1=rw[:, 4 + c:5 + c], op=ALU.subtract), reads=[("rw_G", c), "rw_tot"], writes=["m_st"])
            S.op("dve", lambda e: e.tensor_tensor(out=rw[:, 16:20], in0=rw[:, 12:16], in1=rw[:, 8:12], op=ALU.subtract),
                 reads=[("rw_G", c) for c in range(4)] + [("rw_mp", c) for c in range(4)], writes=["rw_D"])
            S.op("act", lambda e: e.activation(out=rw[:, 16:20], in_=rw[:, 16:20], func=AF.Exp), reads=["rw_D"], writes=["rw_D"])
            S.op("dve", lambda e: e.tensor_tensor(out=rhs8[:, :, 0:4], in0=ident_f[0:4, 0:4].unsqueeze(1).broadcast_to([4, 4, 4]), in1=rw[:, 8:12].unsqueeze(2).broadcast_to([4, 4, 4]), op=ALU.mult),
                 reads=[("rw_G", c) for c in range(4)] + ["ident_f"], writes=["rhs8a"])
            S.op("dve", lambda e: e.tensor_tensor(out=rhs8[:, :, 4:8], in0=ident_f[0:4, 0:4].unsqueeze(1).broadcast_to([4, 4, 4]), in1=rw[:, 16:20].unsqueeze(2).broadcast_to([4, 4, 4]), op=ALU.mult),
                 reads=["rw_D", "ident_f"], writes=["rhs8b"])
            S.op("pe", lambda e: e.matmul(out=pc[:, 32:64], lhsT=ones_f[0:4, :], rhs=rhs8[:].rearrange("p c g -> p (c g)"), start=True, stop=True),
                 reads=["rhs8a", "rhs8b", "ones_f"], accw=[pct])
            S.op("dve", lambda e: e.tensor_copy(out=gb_sb[:], in_=pc[:, 32:64].rearrange("p (c g) -> p c g", c=4)), reads=[pct], writes=["gb_sb"])
            S.op("dve", lambda e: e.tensor_tensor(out=e_tm[:], in0=a_tm[:], in1=gb_sb[:, :, 0:4], op=ALU.subtract), reads=["a_tm", "gb_sb"], writes=["e_tm"])
            S.op("act", lambda e: e.activation(out=e_tm[:], in_=e_tm[:], func=AF.Exp), reads=["e_tm"], writes=["e_tm"])
            S.op("dve", lambda e: e.tensor_tensor(out=dn_tm[:], in0=cum_sb[:], in1=gb_sb[:, :, 0:4], op=ALU.subtract), reads=["cum_sb", "gb_sb"], writes=["dn_tm"])
            S.op("act", lambda e: e.activation(out=dn_tm[:], in_=dn_tm[:], func=AF.Exp, bias=lnc0_t[:, 0:1]), reads=["dn_tm", "lnc0"], writes=["dn_tm"])

            if gi + 1 < NPRE + NST:
                xload(gi + 1)
            def tile_body(j):
                c = j
                csl = slice(c * 128, (c + 1) * 128)
                tsl = cnt["tl2"] % 2
                cnt["tl2"] += 1
                vea = ve[tsl]
                sm = smask[tsl]
                ktm = ktm2[tsl]
                og = og2[tsl]
                ysl = cnt["y"] % 2
                if main:
                    cnt["y"] += 1
                yt_ = ybuf[ysl]
                def chainA1():
                    pv, pvt = fbankA()
                    for kc in range(8):
                        yield S.op("pe", lambda e, kc=kc, pv=pv: e.matmul(out=pv[:], lhsT=xt[:, kc, csl], rhs=wvo[:, kc, 0:512], start=(kc == 0), stop=(kc == 7)),
                             reads=[("xT", 0), "wvo"], accw=[pvt])
                    yield S.op("dve", lambda e, pv=pv, vea=vea: e.tensor_tensor(out=vea[:, :, 0:128], in0=pv[:].rearrange("p (h d) -> p h d", h=4), in1=e_tm[:, c, :].unsqueeze(2).broadcast_to([128, 4, 128]), op=ALU.mult),
                         reads=[pvt, "e_tm"], writes=[("ve", tsl)])
                    yield S.op("dve", lambda e, vea=vea: e.tensor_copy(out=vea[:, :, 128:129], in_=e_tm[:, c, :].unsqueeze(2)), reads=["e_tm"], writes=[("ve", tsl, "e")])
                    pb, pt = (ptr[0], ("ptr", 0))
                    for h in range(4):
                        yield S.op("pe", lambda e, h=h, pb=pb: e.transpose(out=pb[:, h * 128:(h + 1) * 128], in_=qk[:, 4 + h, csl], identity=ident_b[:]),
                             reads=[("qkT", 0), "ident_b"], accw=[pt])
                    yield S.op("act", lambda e, pb=pb: e.copy(out=ktm[:].rearrange("p h d -> p (h d)"), in_=pb[:, 0:512]), reads=[pt], writes=[("ktm", tsl)])
                    if main:
                        po, pot = fbankA()
                        for kc in range(8):
                            yield S.op("pe", lambda e, kc=kc, po=po: e.matmul(out=po[:], lhsT=xt[:, kc, csl], rhs=wvo[:, kc, 512:1024], start=(kc == 0), stop=(kc == 7)),
                                 reads=[("xT", 0), "wvo"], accw=[pot])
                        yield S.op("act", lambda e, po=po: e.activation(out=og[:], in_=po[:], func=AF.Exp, scale=-1.0), reads=[pot], writes=[("og", tsl)])
                        yield S.op("act", lambda e: e.activation(out=og[:], in_=og[:], func=AF.Ln, bias=1.0), reads=[("og", tsl)], writes=[("og", tsl)])
                        yield S.op("act", lambda e: e.activation(out=og[:], in_=og[:], func=AF.Exp, scale=-1.0), reads=[("og", tsl)], writes=[("og", tsl)])
                        pS, pSt = fbankA()
                        for h in range(4):
                            yield S.op("pe", lambda e, h=h, pS=pS: e.matmul(out=pS[:, h * 128:(h + 1) * 128], lhsT=qk[:, 4 + h, csl], rhs=qk[:, h, csl], start=True, stop=True),
                                 reads=[("qkT", 0)], accw=[pSt])
                        sm = smask[tsl]
                        yield S.op("dve", lambda e, pS=pS, sm=sm: e.tensor_tensor(out=sm[:], in0=pS[:].rearrange("p (h l) -> p h l", h=4), in1=triu_f[:].unsqueeze(1).broadcast_to([128, 4, 128]), op=ALU.mult),
                             reads=[pSt, "triu_f"], writes=[("smask", tsl)])
                    yield None
                def chainA2():
                    if main:
                        for h in range(4):
                            yield S.op("act", lambda e, h=h: e.mul(Csb[:, h, :], C32[:, h, :], gb_sb[:, c, 4 + h:5 + h]), reads=[("C32", h), "gb_sb"], writes=[("Csb", h)])
                        nbanks = []
                        for hp in range(2):
                            pn, pnt = fbankA2()
                            nbanks.append((pn, pnt))
                            for hh in range(2):
                                h = hp * 2 + hh
                                yield S.op("pe", lambda e, h=h, hh=hh, pn=pn, sm=sm, vea=vea: e.matmul(out=pn[:, hh * 129:(hh + 1) * 129], lhsT=sm[:, h, :], rhs=vea[:, h, :], start=True, stop=False),
                                     reads=[("smask", tsl), ("ve", tsl), ("ve", tsl, "e")], accw=[pnt])
                                yield S.op("pe", lambda e, h=h, hh=hh, pn=pn: e.matmul(out=pn[:, hh * 129:(hh + 1) * 129], lhsT=qk[:, h, csl], rhs=Csb[:, h, :], start=False, stop=True),
                                     reads=[("qkT", 0), ("Csb", h)], accw=[pnt])
                        for hp in range(2):
                            pn, pnt = nbanks[hp]
                            yield S.op("act", lambda e, pn=pn, hp=hp: e.activation(out=nqa[:, hp * 2:hp * 2 + 2].unsqueeze(2), in_=pn[:, 0:258].rearrange("p (h d) -> p h d", h=2)[:, :, 128:129], func=AF.Abs),
                                 reads=[pnt], writes=["nqa"])
                        yield S.op("dve", lambda e: e.tensor_tensor(out=nqa[:], in0=nqa[:], in1=dn_tm[:, c, :], op=ALU.max), reads=["nqa", "dn_tm"], writes=["nqa"])
                        yield S.op("dve", lambda e: e.reciprocal(out=nqa[:], in_=nqa[:]), reads=["nqa"], writes=["nqa"])
                        for hp in range(2):
                            pn, pnt = nbanks[hp]
                            yield S.op("dve", lambda e, pn=pn, hp=hp: e.tensor_tensor(out=hs[:, hp * 2:hp * 2 + 2, :], in0=pn[:, 0:258].rearrange("p (h d) -> p h d", h=2)[:, :, 0:128], in1=nqa[:, hp * 2:hp * 2 + 2].unsqueeze(2).broadcast_to([128, 2, 128]), op=ALU.mult),
                                 reads=[pnt, "nqa"], writes=["hs"])
                        yield S.op("dve", lambda e: e.tensor_tensor(out=hs[:].rearrange("p h d -> p (h d)"), in0=hs[:].rearrange("p h d -> p (h d)"), in1=og[:], op=ALU.mult),
                             reads=["hs", ("og", tsl)], writes=["hs"])
                        yield S.op("dve", lambda e: e.tensor_tensor(out=tmpa[:, 0:512], in0=hs[:].rearrange("p h d -> p (h d)"), in1=hs[:].rearrange("p h d -> p (h d)"), op=ALU.mult), reads=["hs"], writes=["tmpa"])
                        yield S.op("dve", lambda e: e.tensor_reduce(out=sm8[:, 0:4], in_=tmpa[:, 0:512].rearrange("p (h d) -> p h d", h=4), axis=AX.X, op=ALU.add), reads=["tmpa"], writes=["sm8a"])
                        yield S.op("act", lambda e: e.activation(out=sm8[:, 0:4], in_=sm8[:, 0:4], func=AF.Ln, scale=1.0 / 128, bias=EPS), reads=["sm8a"], writes=["sm8a"])
                        yield S.op("act", lambda e: e.activation(out=sm8[:, 0:4], in_=sm8[:, 0:4], func=AF.Exp, scale=-0.5), reads=["sm8a"], writes=["sm8a"])
                        yield S.op("dve", lambda e: e.tensor_tensor(out=hs[:], in0=hs[:], in1=sm8[:, 0:4].unsqueeze(2).broadcast_to([128, 4, 128]), op=ALU.mult), reads=["hs", "sm8a"], writes=["hs"])
                        yield S.op("dve", lambda e, yt_=yt_: e.tensor_tensor(out=yt_[:, 0:512], in0=hs[:].rearrange("p h d -> p (h d)"), in1=gml_b[:], op=ALU.mult), reads=["hs", "gml_b"], writes=[("y", ysl, 0)])
                    for hp in range(2):
                        pu_, put = fbankA2()
                        for hh in range(2):
                            h = hp * 2 + hh
                            yield S.op("pe", lambda e, h=h, hh=hh, pu_=pu_, vea=vea: e.matmul(out=pu_[:, hh * 129:(hh + 1) * 129], lhsT=ktm[:, h, :], rhs=vea[:, h, :], start=True, stop=True),
                                 reads=[("ktm", tsl), ("ve", tsl), ("ve", tsl, "e")], accw=[put])
                        for hh in range(2):
                            h = hp * 2 + hh
                            yield S.op("dve", lambda e, h=h, hh=hh, pu_=pu_: e.scalar_tensor_tensor(out=C32[:, h, :], in0=C32[:, h, :], scalar=gb_sb[:, c, 4 + h:5 + h], in1=pu_[:, hh * 129:(hh + 1) * 129], op0=ALU.mult, op1=ALU.add),
                                 reads=[put, "gb_sb", ("C32", h)], writes=[("C32", h)])

                    yield None
                def chainB():
                    pU, pUt = fbankB()
                    pV, pVt = fbankB()
                    for kc in range(8):
                        yield S.op("pe", lambda e, kc=kc, pU=pU: e.matmul(out=pU[:], lhsT=xt[:, kc, csl], rhs=wuv[:, kc, 0:512], start=(kc == 0), stop=(kc == 7)),
                             reads=[("xT", 0), "wuv"], accw=[pUt])
                    for kc in range(8):
                        yield S.op("pe", lambda e, kc=kc, pV=pV: e.matmul(out=pV[:], lhsT=xt[:, kc, csl], rhs=wuv[:, kc, 512:1024], start=(kc == 0), stop=(kc == 7)),
                             reads=[("xT", 0), "wuv"], accw=[pVt])
                    yield S.op("act", lambda e, pU=pU: e.copy(out=ux[:, 0:512], in_=pU[:]), reads=[pUt], writes=["ux"])
                    yield S.op("act", lambda e, pV=pV: e.copy(out=ux[:, 512:1024], in_=pV[:]), reads=[pVt], writes=["ux"])
                    yield S.op("act", lambda e: e.activation(out=t1[:], in_=ux[:], func=AF.Square), reads=["ux"], writes=["t1"])
                    yield S.op("dve", lambda e: e.tensor_scalar(out=t1[:], in0=t1[:], scalar1=0.044715, scalar2=1.0, op0=ALU.mult, op1=ALU.add), reads=["t1"], writes=["t1"])
                    yield S.op("dve", lambda e: e.tensor_tensor(out=t1[:], in0=t1[:], in1=ux[:], op=ALU.mult), reads=["t1", "ux"], writes=["t1"])
                    yield S.op("act", lambda e: e.activation(out=t1[:], in_=t1[:], func=AF.Exp, scale=-2.0 * 0.7978845608028654), reads=["t1"], writes=["t1"])
                    yield S.op("act", lambda e: e.activation(out=t1[:], in_=t1[:], func=AF.Ln, bias=1.0), reads=["t1"], writes=["t1"])
                    yield S.op("act", lambda e: e.activation(out=t1[:], in_=t1[:], func=AF.Exp, scale=-1.0), reads=["t1"], writes=["t1"])
                    yield S.op("dve", lambda e: e.tensor_tensor(out=ux[:], in0=t1[:], in1=ux[:], op=ALU.mult), reads=["t1", "ux"], writes=["ux"])
                    yield S.op("dve", lambda e: e.memset(sm8[:, 4:5], 0.0), writes=["sm8b"])
                    yield S.op("act", lambda e: e.activation(out=junk[:, 0:512], in_=ux[:, 512:1024], func=AF.Square, accum_out=sm8[:, 4:5]), reads=["ux", "sm8b"], writes=["junk", "sm8b"])
                    yield S.op("act", lambda e: e.activation(out=sm8[:, 4:5], in_=sm8[:, 4:5], func=AF.Ln, scale=1.0 / 512, bias=EPS), reads=["sm8b"], writes=["sm8b"])
                    yield S.op("act", lambda e: e.activation(out=sm8[:, 4:5], in_=sm8[:, 4:5], func=AF.Exp, scale=-0.5), reads=["sm8b"], writes=["sm8b"])
                    yield S.op("dve", lambda e: e.scalar_tensor_tensor(out=vn[:], in0=ux[:, 512:1024], scalar=sm8[:, 4:5], in1=ggv_b[:], op0=ALU.mult, op1=ALU.mult), reads=["ux", "sm8b", "ggv_b"], writes=["vn"])
                    pM, pMt = fbankB()
                    for g in range(8):
                        yield S.op("pe", lambda e, g=g, pM=pM: e.matmul(out=pM[:, g * 64:(g + 1) * 64], lhsT=wspT[:, g, :], rhs=vn[:, g * 64:(g + 1) * 64], start=True, stop=True),
                             reads=["vn", "wspT"], accw=[pMt])
                    yield S.op("dve", lambda e, pM=pM: e.tensor_tensor(out=gt[:], in0=pM[:].rearrange("p (g c) -> p g c", g=8), in1=bsp[:].unsqueeze(2).broadcast_to([128, 8, 64]), op=ALU.add),
                         reads=[pMt, "bsp"], writes=["gt"])
                    yield S.op("dve", lambda e: e.tensor_tensor(out=gt[:].rearrange("p g c -> p (g c)"), in0=gt[:].rearrange("p g c -> p (g c)"), in1=ux[:, 0:512], op=ALU.mult), reads=["gt", "ux"], writes=["gt"])
                    yield S.op("dve", lambda e: e.tensor_tensor(out=tmpa[:, 512:1024], in0=gt[:].rearrange("p g c -> p (g c)"), in1=gt[:].rearrange("p g c -> p (g c)"), op=ALU.mult), reads=["gt"], writes=["tmpb"])
                    yield S.op("dve", lambda e: e.tensor_reduce(out=sm8[:, 8:16], in_=tmpa[:, 512:1024].rearrange("p (g c) -> p g c", g=8), axis=AX.X, op=ALU.add), reads=["tmpb"], writes=["sm8c"])
                    yield S.op("act", lambda e: e.activation(out=sm8[:, 8:16], in_=sm8[:, 8:16], func=AF.Ln, scale=1.0 / 64, bias=EPS), reads=["sm8c"], writes=["sm8c"])
                    yield S.op("act", lambda e: e.activation(out=sm8[:, 8:16], in_=sm8[:, 8:16], func=AF.Exp, scale=-0.5), reads=["sm8c"], writes=["sm8c"])
                    yield S.op("dve", lambda e: e.tensor_tensor(out=gt[:], in0=gt[:], in1=sm8[:, 8:16].unsqueeze(2).broadcast_to([128, 8, 64]), op=ALU.mult), reads=["gt", "sm8c"], writes=["gt"])
                    yield S.op("dve", lambda e, yt_=yt_: e.tensor_tensor(out=yt_[:, 512:1024], in0=gt[:].rearrange("p g c -> p (g c)"), in1=ggo_b[:], op=ALU.mult), reads=["gt", "ggo_b"], writes=[("y", ysl, 1)])

                    yield None
                def chainC():
                    if "y" in dbg_t:
                        row0d = st_i * 512 + j * 128
                        finals.append(S.op("pool", lambda e, yt_=yt_, row0d=row0d: e.dma_start(out=dbg_t["y"].ap()[row0d:row0d + 128, :], in_=yt_[:]), reads=[("y", ysl, 0), ("y", ysl, 1)], dma=("dbgy", ysl)))
                    pb, pt = (ptr[1], ("ptr", 1))
                    for ec in range(8):
                        yield S.op("pe", lambda e, ec=ec, pb=pb, yt_=yt_: e.transpose(out=pb[:, ec * 128:(ec + 1) * 128], in_=yt_[:, ec * 128:(ec + 1) * 128], identity=ident_b[:]),
                             reads=[("y", ysl, 0), ("y", ysl, 1), "ident_b"], accw=[pt])
                    yT_ = yT[ysl]
                    yield S.op("act", lambda e, pb=pb, yT_=yT_: e.copy(out=yT_[:].rearrange("p c t -> p (c t)"), in_=pb[:]), reads=[pt], writes=[("yT", ysl)])
                    h1t = h1b[ysl]
                    for hf in range(2):
                        ph, pht = fbankC()
                        for ec in range(8):
                            yield S.op("pe", lambda e, ec=ec, ph=ph, hf=hf, yT_=yT_: e.matmul(out=ph[:], lhsT=yT_[:, ec, :], rhs=wout[:, ec, hf * 512:(hf + 1) * 512], start=(ec == 0), stop=(ec == 7)),
                                 reads=[("yT", ysl), "wout"], accw=[pht])
                        yield S.op("dve", lambda e, ph=ph, hf=hf, h1t=h1t: e.tensor_tensor(out=h1t[:, hf * 512:(hf + 1) * 512], in0=ph[:], in1=xb[:, j, hf * 512:(hf + 1) * 512], op=ALU.add),
                             reads=[pht, ("x", sl)], writes=[("h1", ysl, hf)])
                    row0 = st_i * 512 + j * 128
                    yield S.op("sp", lambda e, h1t=h1t, row0=row0: e.dma_start(out=h1buf.ap()[row0:row0 + 128, :], in_=h1t[:]), reads=[("h1", ysl, 0), ("h1", ysl, 1)], accw=["h1buf"], dma=("h1st", ysl))
                    if "h1" in dbg_t:
                        finals.append(S.op("sp", lambda e, h1t=h1t, row0=row0: e.dma_start(out=dbg_t["h1"].ap()[row0:row0 + 128, :], in_=h1t[:]), reads=[("h1", ysl, 0), ("h1", ysl, 1)], dma=("dbgh1", ysl)))
                    ti = st_i * 4 + j
                    x2 = xn2t[ysl]
                    yield S.op("dve", lambda e: e.memset(sm8[:, 5:6], 0.0), writes=["sm8d"])
                    yield S.op("act", lambda e: e.activation(out=junk[:], in_=h1t[:], func=AF.Square, accum_out=sm8[:, 5:6]), reads=[("h1", ysl, 0), ("h1", ysl, 1), "sm8d"], writes=["junk", "sm8d"])
                    yield S.op("act", lambda e: e.activation(out=sm8[:, 5:6], in_=sm8[:, 5:6], func=AF.Ln, scale=1.0 / D, bias=EPS), reads=["sm8d"], writes=["sm8d"])
                    yield S.op("act", lambda e: e.activation(out=sm8[:, 5:6], in_=sm8[:, 5:6], func=AF.Exp, scale=-0.5), reads=["sm8d"], writes=["sm8d"])
                    yield S.op("dve", lambda e: e.scalar_tensor_tensor(out=x2[:], in0=h1t[:], scalar=sm8[:, 5:6], in1=g2_b[:], op0=ALU.mult, op1=ALU.mult),
                         reads=[("h1", ysl, 0), ("h1", ysl, 1), "sm8d", "g2_b"], writes=[("xn2", ysl)])
                    yield S.op("sp", lambda e: e.dma_start(out=xn2lin.ap()[row0:row0 + 128, :], in_=x2[:]), reads=[("xn2", ysl)], accw=["xn2lin"], dma=("xn2st", ysl))
                    pb2, pt2 = (ptr[1], ("ptr", 1))
                    for kc in range(8):
                        yield S.op("pe", lambda e, kc=kc: e.transpose(out=pb2[:, kc * 128:(kc + 1) * 128], in_=x2[:, kc * 128:(kc + 1) * 128], identity=ident_b[:]),
                             reads=[("xn2", ysl), "ident_b"], accw=[pt2])
                    x2T = xn2T[0]
                    yield S.op("act", lambda e: e.copy(out=x2T[:].rearrange("p c t -> p (c t)"), in_=pb2[:]), reads=[pt2], writes=[("xn2T", 0)])
                    pl, plt = fbankC()
                    for kc in range(8):
                        yield S.op("pe", lambda e, kc=kc: e.matmul(out=pl[:, 0:36], lhsT=x2T[:, kc, :], rhs=wr_b[:, kc, :], start=(kc == 0), stop=(kc == 7)),
                             reads=[("xn2T", 0), "wr_b"], accw=[plt])
                    yield S.op("dve", lambda e: e.tensor_tensor(out=lg[:], in0=pl[:, 0:36], in1=br_b[:], op=ALU.add), reads=[plt, "br_b"], writes=["lg"])
                    R_ = lambda a, b: rt[:, a:b]
                    yield S.op("dve", lambda e: e.tensor_reduce(out=R_(0, 1), in_=lg[:, 0:4], axis=AX.X, op=ALU.max), reads=["lg"], writes=["rt"])
                    yield S.op("dve", lambda e: e.tensor_scalar(out=R_(12, 16), in0=lg[:, 0:4], scalar1=R_(0, 1), scalar2=None, op0=ALU.is_equal), reads=["lg", "rt"], writes=["rt"])
                    yield S.op("dve", lambda e: e.tensor_scalar(out=R_(1, 2), in0=R_(0, 1), scalar1=-1.0, scalar2=None, op0=ALU.mult), reads=["rt"], writes=["rt"])
                    yield S.op("dve", lambda e: e.memset(R_(2, 3), 0.0), reads=["rt"], writes=["rt"])
                    yield S.op("act", lambda e: e.activation(out=le8[:, 8:12], in_=lg[:, 0:4], func=AF.Exp, bias=R_(1, 2), accum_out=R_(2, 3)), reads=["lg", "rt"], writes=["rt", "le8x"])
                    yield S.op("dve", lambda e: e.reciprocal(out=R_(3, 4), in_=R_(2, 3)), reads=["rt"], writes=["rt"])
                    yield S.op("dve", lambda e: e.tensor_tensor(out=t32[:], in0=lg[:, 4:36].rearrange("p (g j) -> p g j", g=4), in1=R_(12, 16).unsqueeze(2).broadcast_to([128, 4, 8]), op=ALU.mult), reads=["lg", "rt"], writes=["t32"])
                    yield S.op("dve", lambda e: e.tensor_reduce(out=le8[:, 0:8], in_=t32[:].rearrange("p g j -> p j g"), axis=AX.X, op=ALU.add), reads=["t32"], writes=["le8"])
                    yield S.op("dve", lambda e: e.tensor_reduce(out=R_(4, 5), in_=le8[:, 0:8], axis=AX.X, op=ALU.max), reads=["le8", "rt"], writes=["rt"])
                    yield S.op("dve", lambda e: e.tensor_scalar(out=R_(16, 24), in0=le8[:, 0:8], scalar1=R_(4, 5), scalar2=None, op0=ALU.is_equal), reads=["le8", "rt"], writes=["rt"])
                    yield S.op("dve", lambda e: e.scalar_tensor_tensor(out=le8[:, 0:8], in0=R_(16, 24), scalar=-1e30, in1=le8[:, 0:8], op0=ALU.mult, op1=ALU.add), reads=["le8", "rt"], writes=["le8"])
                    yield S.op("dve", lambda e: e.tensor_reduce(out=R_(5, 6), in_=le8[:, 0:8], axis=AX.X, op=ALU.max), reads=["le8", "rt"], writes=["rt"])
                    yield S.op("dve", lambda e: e.tensor_scalar(out=R_(24, 32), in0=le8[:, 0:8], scalar1=R_(5, 6), scalar2=None, op0=ALU.is_equal), reads=["le8", "rt"], writes=["rt"])
                    yield S.op("dve", lambda e: e.tensor_tensor(out=R_(6, 7), in0=R_(5, 6), in1=R_(4, 5), op=ALU.subtract), reads=["rt"], writes=["rt"])
                    yield S.op("act", lambda e: e.activation(out=R_(6, 7), in_=R_(6, 7), func=AF.Exp), reads=["rt"], writes=["rt"])
                    yield S.op("dve", lambda e: e.tensor_scalar(out=R_(7, 8), in0=R_(6, 7), scalar1=1.0, scalar2=None, op0=ALU.add), reads=["rt"], writes=["rt"])
                    yield S.op("dve", lambda e: e.reciprocal(out=R_(7, 8), in_=R_(7, 8)), reads=["rt"], writes=["rt"])
                    yield S.op("dve", lambda e: e.tensor_tensor(out=R_(8, 9), in0=R_(6, 7), in1=R_(7, 8), op=ALU.mult), reads=["rt"], writes=["rt"])
                    yield S.op("dve", lambda e: e.tensor_scalar(out=pw[:, ti, 2:4], in0=R_(7, 9), scalar1=R_(3, 4), scalar2=None, op0=ALU.mult), reads=["rt"], accw=["pw"])
                    E1 = E1s[:, ti, :]
                    E2 = E2s[:, ti, :]
                    yield S.op("dve", lambda e: e.tensor_tensor(out=E1.rearrange("p (g j) -> p g j", g=4), in0=R_(12, 16).unsqueeze(2).broadcast_to([128, 4, 8]), in1=R_(16, 24).unsqueeze(1).broadcast_to([128, 4, 8]), op=ALU.mult), reads=["rt"], accw=["E1s"])
                    yield S.op("dve", lambda e: e.tensor_tensor(out=E2.rearrange("p (g j) -> p g j", g=4), in0=R_(12, 16).unsqueeze(2).broadcast_to([128, 4, 8]), in1=R_(24, 32).unsqueeze(1).broadcast_to([128, 4, 8]), op=ALU.mult), reads=["rt"], accw=["E2s"])
                    yield S.op("dve", lambda e: e.tensor_tensor(out=ind_b[:], in0=E1, in1=E2, op=ALU.add), reads=["E1s", "E2s"], writes=["ind_b"])
                    pp, ppt = fbankC()
                    yield S.op("pe", lambda e: e.matmul(out=pp[:, 0:32], lhsT=lstr_b[:], rhs=ind_b[:], start=True, stop=True), reads=["ind_b", "lstr_b"], accw=[ppt])
                    yield S.op("pe", lambda e: e.matmul(out=pp[:, 32:64], lhsT=ones_b[:], rhs=ind_b[:], start=True, stop=True), reads=["ind_b", "ones_b"], accw=[ppt])
                    yield S.op("dve", lambda e: e.tensor_tensor(out=posf[:], in0=pp[:, 0:32], in1=base_b[:], op=ALU.add), reads=[ppt, "base_b"], writes=["posf"])
                    yield S.op("dve", lambda e: e.tensor_tensor(out=base_b[:], in0=pp[:, 32:64], in1=base_b[:], op=ALU.add), reads=[ppt, "base_b"], writes=["base_b"])
                    yield S.op("dve", lambda e: e.tensor_tensor(out=t32[:].rearrange("p g j -> p (g j)"), in0=E1, in1=posf[:], op=ALU.mult), reads=["E1s", "posf"], writes=["t32"])
                    yield S.op("dve", lambda e: e.tensor_reduce(out=pw[:, ti, 0:1], in_=t32[:].rearrange("p g j -> p (g j)"), axis=AX.X, op=ALU.add), reads=["t32"], accw=["pw"])
                    yield S.op("dve", lambda e: e.tensor_tensor(out=t32[:].rearrange("p g j -> p (g j)"), in0=E2, in1=posf[:], op=ALU.mult), reads=["E2s", "posf", "pw"], writes=["t32"])
                    yield S.op("dve", lambda e: e.tensor_reduce(out=pw[:, ti, 1:2], in_=t32[:].rearrange("p g j -> p (g j)"), axis=AX.X, op=ALU.add), reads=["t32"], accw=["pw"])
                    yield None
                return chainA1(), chainA2(), (chainB() if main else None), (chainC() if main else None)

            def run_chains(gens):
                act = [g for g in gens if g is not None]
                while act:
                    for g in list(act):
                        try:
                            next(g)
                        except StopIteration:
                            act.remove(g)

            made = {}

            def get(j):
                if j not in made:
                    made[j] = list(tile_body(j))
                return made[j]

            act = {}
            done = set()
            nxt = {"A1": 0, "A2": 0, "B": 0, "C": 0}
            IDX = {"A1": 0, "A2": 1, "B": 2, "C": 3}

            def fin(kind, j):
                return j < 0 or (kind, j) in done

            def start(kind, j):
                g = get(j)[IDX[kind]]
                if g is None:
                    done.add((kind, j))
                else:
                    act[(kind, j)] = g
                nxt[kind] += 1

            def try_start():
                j = nxt["A1"]
                if j < 4 and fin("A1", j - 1) and fin("A2", j - 2):
                    start("A1", j)
                j = nxt["A2"]
                if j < 4 and fin("A1", j) and j < nxt["A1"] and fin("A2", j - 1) and fin("C", j - 2):
                    start("A2", j)
                j = nxt["B"]
                if j < 4 and j < nxt["A1"] and fin("B", j - 1) and fin("C", j - 2):
                    start("B", j)
                j = nxt["C"]
                if j < 4 and j < nxt["A1"] and fin("A2", j) and fin("B", j) and fin("C", j - 1) and j < nxt["A2"] and j < nxt["B"]:
                    start("C", j)

            ready = {}
            while len(done) < 16:
                try_start()
                if not act:
                    continue
                for k_ in act:
                    ready.setdefault(k_, 0.0)
                k_ = min(act.keys(), key=lambda q: ready[q])
                try:
                    next(act[k_])
                    ready[k_] = S.last_est
                except StopIteration:
                    del act[k_]
                    del ready[k_]
                    done.add(k_)
            if gi == 0 and "qkT" in dbg_t:
                tmpd = S.sb([128, 8, 512], F32, "sdbg_qk")
                S.op("dve", lambda e: e.tensor_copy(out=tmpd[:], in_=qk[:]), reads=[("qkT", 0)], writes=["dbg_qk"])
                finals.append(S.op("sp", lambda e: e.dma_start(out=dbg_t["qkT"].ap(), in_=tmpd[:].rearrange("p a b -> p (a b)")), reads=["dbg_qk"], dma=("dbg", "qkT")))
            if gi == 0 and "xT" in dbg_t:
                tmpx = S.sb([128, 8, 512], F32, "sdbg_xT")
                S.op("dve", lambda e: e.tensor_copy(out=tmpx[:], in_=xt[:]), reads=[("xT", 0)], writes=["dbg_xT"])
                finals.append(S.op("sp", lambda e: e.dma_start(out=dbg_t["xT"].ap(), in_=tmpx[:].rearrange("p a b -> p (a b)")), reads=["dbg_xT"], dma=("dbg", "xT")))

        wcat_bf = nc.dram_tensor("wcat_bf", [NE * 128, 12288], BF16)
        NSUP = NPRE + NST
        conv_rows = NE * 128
        conv_state = {"r": 0, "i": 0}

        def conv_some(gidx, sl):
            tgt = conv_rows * (gidx + 1) // NSUP
            while conv_state["r"] < tgt:
                r0 = conv_state["r"]
                r1 = min(r0 + 32, tgt)
                k = conv_state["i"] % 2
                conv_state["i"] += 1
                conv_state["r"] = r1
                S.op("pool", lambda e, r0=r0, r1=r1: e.dma_start(out=wcat_bf.ap()[r0:r1, :], in_=wcat.ap()[r0:r1, :]),
                     reads=[("x", sl)], writes=[("wck", k)], dma=("wck", k))

        NBLK0 = NT * 2 + NE
        xslots = nc.dram_tensor("xslots", [NBLK0 * 128, D], BF16)
        ztile = junk
        S.op("dve", lambda e: e.memset(ztile[:], 0.0), writes=["junk"])
        for zb in range(0, NBLK0, 8):
            nb_ = min(8, NBLK0 - zb)
            S.op("sp", lambda e, zb=zb, nb_=nb_: e.dma_start(out=xslots.ap()[zb * 128:(zb + nb_) * 128, :].rearrange("(j p) d -> p j d", p=128),
                                                             in_=ztile[:].unsqueeze(1).broadcast_to([128, nb_, 1024])),
                 reads=["junk"], accw=["xslots"], dma=("zinit", (zb // 8) % 4))
        gi = 0
        for s_i in range(NPRE):
            supertile(x_pre, s_i, "prelast" if s_i == NPRE - 1 else "pre", gi)
            gi += 1
        for s_i in range(NST):
            supertile(x_main, s_i, "main", gi)
            gi += 1


        S.flush()
        stA.close()
        stB = contextlib.ExitStack()
        S.cur = stB
        NBLK = NT * 2 + NE
        NSL = NBLK * 128
        yslots = nc.dram_tensor("yslots", [NSL, D], F32)
        pl_ = S.sb([128, 6, 32], F32, "plan")
        pl_i = S.sb([128, 32], I32, "plan_i")
        p128 = S.sb([128, 1], F32, "p128")
        woff_f = S.sb([128, NBLK], F32, "woff_f")
        woff_i = S.sb([128, NBLK], I32, "woff_i")
        bval = S.sb([128, NBLK], F32, "bval")
        neq = S.sb([128, NBLK], F32, "neq")
        cmpb = S.sb([128, NBLK, 32], F32, "cmpb")
        dst_f = S.sb([128, 2, NT], F32, "dst_f")
        dst_i = S.sb([128, 2, NT], I32, "dst_i")
        big = S.sb([128, NT, 32], F32, "bigtmp")
        S.op("pool", lambda e: e.iota(p128[:], [[0, 1]], base=0, channel_multiplier=1, allow_small_or_imprecise_dtypes=True), writes=["p128"])
        S.op("dve", lambda e: e.tensor_scalar(out=pl_[:, 3, :], in0=base_b[:], scalar1=1.0 / 128, scalar2=63.5 / 128, op0=ALU.mult, op1=ALU.add), reads=["base_b"], writes=["plan"])
        S.op("dve", lambda e: e.tensor_copy(out=pl_i[:], in_=pl_[:, 3, :]), reads=["plan"], writes=["plan_i"])
        S.op("dve", lambda e: e.tensor_copy(out=pl_[:, 0, :], in_=pl_i[:]), reads=["plan_i", "plan"], writes=["plan"])
        S.op("dve", lambda e: e.tensor_scalar(out=pl_[:, 0, :], in0=pl_[:, 0, :], scalar1=128.0, scalar2=None, op0=ALU.mult), reads=["plan"], writes=["plan"])
        S.op("dve", lambda e: e.tensor_tensor_scan(out=pl_[:, 1, :], data0=pl_[:, 0, :], data1=pl_[:, 0, :], initial=0.0, op0=ALU.add, op1=ALU.bypass), reads=["plan"], writes=["plan"])
        S.op("dve", lambda e: e.tensor_tensor(out=pl_[:, 2, :], in0=pl_[:, 1, :], in1=pl_[:, 0, :], op=ALU.subtract), reads=["plan"], writes=["plan"])
        S.op("pool", lambda e: e.iota(bval[:], [[128, NBLK]], base=0, channel_multiplier=0, allow_small_or_imprecise_dtypes=True), writes=["bval"])
        S.op("dve", lambda e: e.tensor_tensor(out=cmpb[:], in0=pl_[:, 1, :].unsqueeze(1).broadcast_to([128, NBLK, 32]), in1=bval[:].unsqueeze(2).broadcast_to([128, NBLK, 32]), op=ALU.is_le), reads=["plan", "bval"], writes=["cmpb"])
        S.op("dve", lambda e: e.tensor_reduce(out=woff_f[:], in_=cmpb[:], axis=AX.X, op=ALU.add), reads=["cmpb"], writes=["woff_f"])
        BIGI = 1000000.0
        S.op("dve", lambda e: e.tensor_scalar(out=woff_f[:], in0=woff_f[:], scalar1=31.0, scalar2=None, op0=ALU.min), reads=["woff_f"], writes=["woff_f"])
        S.op("dve", lambda e: e.memset(neq[:, 0:2], 1.0), writes=["neq0"])
        S.op("dve", lambda e: e.tensor_tensor(out=neq[:, 2:NBLK], in0=woff_f[:, 2:NBLK], in1=woff_f[:, 0:NBLK - 2], op=ALU.not_equal), reads=["woff_f"], writes=["neq"])
        S.op("dve", lambda e: e.tensor_scalar(out=woff_f[:], in0=woff_f[:], scalar1=128.0, scalar2=-BIGI, op0=ALU.mult, op1=ALU.add), reads=["woff_f", "neq"], writes=["woff_f"])
        S.op("dve", lambda e: e.tensor_scalar(out=woff_f[:], in0=woff_f[:], scalar1=p128[:, 0:1], scalar2=None, op0=ALU.add), reads=["woff_f", "p128"], writes=["woff_f"])
        S.op("dve", lambda e: e.tensor_tensor(out=woff_f[:], in0=woff_f[:], in1=neq[:], op=ALU.mult), reads=["woff_f", "neq", "neq0"], writes=["woff_f"])
        S.op("dve", lambda e: e.tensor_scalar(out=woff_f[:], in0=woff_f[:], scalar1=BIGI, scalar2=None, op0=ALU.add), reads=["woff_f"], writes=["woff_f"])
        S.op("dve", lambda e: e.tensor_copy(out=woff_i[:], in_=woff_f[:]), reads=["woff_f"], writes=["woff_i"])
        for k_, Es in ((0, E1s), (1, E2s)):
            S.op("dve", lambda e, Es=Es: e.tensor_tensor(out=big[:], in0=Es[:], in1=pl_[:, 2, :].unsqueeze(1).broadcast_to([128, NT, 32]), op=ALU.mult), reads=["E1s", "E2s", "plan"], writes=["big"])
            S.op("dve", lambda e, k_=k_: e.tensor_reduce(out=dst_f[:, k_, :], in_=big[:], axis=AX.X, op=ALU.add), reads=["big"], writes=[("dst_f", k_)])
            S.op("dve", lambda e, k_=k_: e.tensor_tensor(out=dst_f[:, k_, :], in0=dst_f[:, k_, :], in1=pw[:, :, k_], op=ALU.add), reads=[("dst_f", k_), "pw"], writes=[("dst_f", k_)])
        S.op("dve", lambda e: e.tensor_copy(out=dst_i[:], in_=dst_f[:]), reads=[("dst_f", 0), ("dst_f", 1)], writes=["dst_i"])
        if "plan" in dbg_t:
            finals.append(S.op("sp", lambda e: e.dma_start(out=dbg_t["plan"].ap()[:, 0:192], in_=pl_[:].rearrange("p a b -> p (a b)")), reads=["plan"], dma=("dbg", "plan")))
            finals.append(S.op("sp", lambda e: e.dma_start(out=dbg_t["plan"].ap()[:, 768:768 + NBLK], in_=woff_f[:]), reads=["woff_f"], dma=("dbg", "plan2")))
            finals.append(S.op("sp", lambda e: e.dma_start(out=dbg_t["plan"].ap()[:, 256:256 + 2 * NT], in_=dst_f[:].rearrange("p a b -> p (a b)")), reads=[("dst_f", 0), ("dst_f", 1)], dma=("dbg", "plan3")))
            finals.append(S.op("sp", lambda e: e.dma_start(out=dbg_t["plan"].ap()[:, 512:512 + 4 * NT], in_=pw[:].rearrange("p a b -> p (a b)")), reads=["pw"], dma=("dbg", "plan4")))
        xsc = [S.sb([128, 1024], BF16, f"xsc{i}") for i in range(2)]
        for ti in range(NT):
            bsl = ti % 2
            S.op("sp", lambda e, ti=ti, bsl=bsl: e.dma_start(out=xsc[bsl][:], in_=xn2lin.ap()[ti * 128:(ti + 1) * 128, :]), reads=["xn2lin"], writes=[("xsc", bsl)], dma=("xsc", bsl))
            for k_ in range(2):
                S.op("pool", lambda e, ti=ti, bsl=bsl, k_=k_: e.indirect_dma_start(out=xslots.ap(), out_offset=bass.IndirectOffsetOnAxis(ap=dst_i[:, k_, ti:ti + 1], axis=0), in_=xsc[bsl][:], in_offset=None),
                     reads=[("xsc", bsl), "dst_i"], accw=["xslots"], dma=("scat", bsl, k_))

        wbuf = [S.sb([128, 12288], BF16, f"wbuf{i}") for i in range(2)]
        xs_b = [S.sb([128, 1024], BF16, f"xs_b{i}") for i in range(4)]
        xsT = [S.sb([128, 8, 128], BF16, f"xsT{i}") for i in range(2)]
        eg = [S.sb([128, 512], F32, f"eg{i}") for i in range(2)]
        hid = [S.sb([128, 512], BF16, f"hid{i}") for i in range(2)]
        hidT = [S.sb([128, 4, 128], BF16, f"hidT{i}") for i in range(2)]
        ysb = [S.sb([128, 1024], F32, f"ysb{i}") for i in range(2)]
        regs = {}

        def wgather(e, b, ws):
            if "bnd" not in regs:
                regs["bnd"] = st.enter_context(e.register("wbnd"))
                e.reg_mov(regs["bnd"], NE * 128 - 1)
            return e.indirect_dma_start(out=wbuf[ws][:], out_offset=None, in_=wcat_bf.ap(), in_offset=bass.IndirectOffsetOnAxis(ap=woff_i[:, b:b + 1], axis=0),
                                        bounds_check=regs["bnd"], oob_is_err=False)

        mrr = [0, 0]

        def fbankM(p):
            i = 3 * p + mrr[p] % 3
            mrr[p] += 1
            return pfb[i], ("pf", i)

        def blk(b):
            ws = b % 2
            yield S.op("pool", lambda e, b=b, ws=ws: wgather(e, b, ws), reads=["woff_i"], writes=[("wb", ws)], dma=("wb", ws))
            if b < 2:
                yield S.op("sp", lambda e, b=b: e.dma_start(out=xs_b[b % 4][:], in_=xslots.ap()[b * 128:(b + 1) * 128, :]), reads=["xslots"], writes=[("xs", b % 4)], dma=("xs", b % 4))
            if b + 2 < NBLK:
                yield S.op("sp", lambda e, b=b: e.dma_start(out=xs_b[(b + 2) % 4][:], in_=xslots.ap()[(b + 2) * 128:(b + 3) * 128, :]), reads=["xslots"], writes=[("xs", (b + 2) % 4)], dma=("xs", (b + 2) % 4))
            xq = b % 4
            pb, pt = (ptr[ws], ("ptr", ws))
            for kc in range(8):
                yield S.op("pe", lambda e, kc=kc, pb=pb, xq=xq: e.transpose(out=pb[:, kc * 128:(kc + 1) * 128], in_=xs_b[xq][:, kc * 128:(kc + 1) * 128], identity=ident_b[:]),
                     reads=[("xs", xq), "ident_b"], accw=[pt])
            yield S.op("act", lambda e, pb=pb, ws=ws: e.copy(out=xsT[ws][:].rearrange("p c t -> p (c t)"), in_=pb[:]), reads=[pt], writes=[("xsT", ws)])
            pG, pGt = fbankM(ws)
            pU2, pU2t = fbankM(ws)
            for kc in range(8):
                yield S.op("pe", lambda e, kc=kc, pG=pG, ws=ws: e.matmul(out=pG[:], lhsT=xsT[ws][:, kc, :], rhs=wbuf[ws][:, kc * 1024:kc * 1024 + 512], start=(kc == 0), stop=(kc == 7)),
                     reads=[("xsT", ws), ("wb", ws)], accw=[pGt])
            for kc in range(8):
                yield S.op("pe", lambda e, kc=kc, pU2=pU2, ws=ws: e.matmul(out=pU2[:], lhsT=xsT[ws][:, kc, :], rhs=wbuf[ws][:, kc * 1024 + 512:(kc + 1) * 1024], start=(kc == 0), stop=(kc == 7)),
                     reads=[("xsT", ws), ("wb", ws)], accw=[pU2t])
            yield S.op("act", lambda e, pG=pG, ws=ws: e.activation(out=eg[ws][:], in_=pG[:], func=AF.Exp, scale=-1.0), reads=[pGt], writes=[("eg", ws)])
            yield S.op("act", lambda e, ws=ws: e.activation(out=eg[ws][:], in_=eg[ws][:], func=AF.Ln, bias=1.0), reads=[("eg", ws)], writes=[("eg", ws)])
            yield S.op("act", lambda e, ws=ws: e.activation(out=eg[ws][:], in_=eg[ws][:], func=AF.Exp, scale=-1.0), reads=[("eg", ws)], writes=[("eg", ws)])
            yield S.op("dve", lambda e, pG=pG, ws=ws: e.tensor_tensor(out=eg[ws][:], in0=eg[ws][:], in1=pG[:], op=ALU.mult), reads=[("eg", ws), pGt], writes=[("eg", ws)])
            yield S.op("dve", lambda e, pU2=pU2, ws=ws: e.tensor_tensor(out=hid[ws][:], in0=eg[ws][:], in1=pU2[:], op=ALU.mult), reads=[("eg", ws), pU2t], writes=[("hid", ws)])
            pb, pt = (ptr[ws], ("ptr", ws))
            for fc in range(4):
                yield S.op("pe", lambda e, fc=fc, pb=pb, ws=ws: e.transpose(out=pb[:, fc * 128:(fc + 1) * 128], in_=hid[ws][:, fc * 128:(fc + 1) * 128], identity=ident_b[:]),
                     reads=[("hid", ws), "ident_b"], accw=[pt])
            yield S.op("act", lambda e, pb=pb, ws=ws: e.copy(out=hidT[ws][:].rearrange("p c t -> p (c t)"), in_=pb[:, 0:512]), reads=[pt], writes=[("hidT", ws)])
            for hf in range(2):
                pY, pYt = fbankM(ws)
                for fc in range(4):
                    yield S.op("pe", lambda e, fc=fc, pY=pY, ws=ws, hf=hf: e.matmul(out=pY[:], lhsT=hidT[ws][:, fc, :], rhs=wbuf[ws][:, 8192 + fc * 1024 + hf * 512:8192 + fc * 1024 + (hf + 1) * 512], start=(fc == 0), stop=(fc == 3)),
                         reads=[("hidT", ws), ("wb", ws)], accw=[pYt])
                if hf == 0:
                    yield S.op("act", lambda e, pY=pY, ws=ws: e.copy(out=ysb[ws][:, 0:512], in_=pY[:]), reads=[pYt], writes=[("ysb", ws, 0)])
                else:
                    yield S.op("dve", lambda e, pY=pY, ws=ws: e.tensor_copy(out=ysb[ws][:, 512:1024], in_=pY[:]), reads=[pYt], writes=[("ysb", ws, 1)])
            yield S.op("sp", lambda e, b=b, ws=ws: e.dma_start(out=yslots.ap()[b * 128:(b + 1) * 128, :], in_=ysb[ws][:]), reads=[("ysb", ws, 0), ("ysb", ws, 1)], accw=["yslots"], dma=("yst", ws))


            yield None

        for b in range(NBLK):
            for _ in blk(b):
                pass

        fg_b = bload("fg_b", final_g, 1024)
        NCB = 3
        hc = [S.sb([128, 1024], F32, f"hc{i}") for i in range(NCB)]
        y1 = [S.sb([128, 1024], F32, f"y1_{i}") for i in range(NCB)]
        y2 = [S.sb([128, 1024], F32, f"y2_{i}") for i in range(NCB)]
        fs = S.sb([128, 2], F32, "fs")

        def cloads(ti):
            cs = ti % NCB
            S.op("sp", lambda e, ti=ti, cs=cs: e.dma_start(out=hc[cs][:], in_=h1buf.ap()[ti * 128:(ti + 1) * 128, :]), reads=["h1buf"], writes=[("hc", cs)], dma=("hc", cs))
            S.op("pool", lambda e, ti=ti, cs=cs: e.indirect_dma_start(out=y1[cs][:], out_offset=None, in_=yslots.ap(), in_offset=bass.IndirectOffsetOnAxis(ap=dst_i[:, 0, ti:ti + 1], axis=0)),
                 reads=["yslots", "dst_i"], writes=[("y1", cs)], dma=("y1", cs))
            S.op("pool", lambda e, ti=ti, cs=cs: e.indirect_dma_start(out=y2[cs][:], out_offset=None, in_=yslots.ap(), in_offset=bass.IndirectOffsetOnAxis(ap=dst_i[:, 1, ti:ti + 1], axis=0)),
                 reads=["yslots", "dst_i"], writes=[("y2", cs)], dma=("y2", cs))

        for ti in range(min(NCB - 1, NT)):
            cloads(ti)
        for ti in range(NT):
            cs = ti % NCB
            if ti + NCB - 1 < NT:
                cloads(ti + NCB - 1)
            S.op("dve", lambda e, ti=ti, cs=cs: e.scalar_tensor_tensor(out=hc[cs][:], in0=y1[cs][:], scalar=pw[:, ti, 2:3], in1=hc[cs][:], op0=ALU.mult, op1=ALU.add), reads=[("hc", cs), ("y1", cs), "pw"], writes=[("hc", cs)])
            S.op("dve", lambda e, ti=ti, cs=cs: e.scalar_tensor_tensor(out=hc[cs][:], in0=y2[cs][:], scalar=pw[:, ti, 3:4], in1=hc[cs][:], op0=ALU.mult, op1=ALU.add), reads=[("hc", cs), ("y2", cs), "pw"], writes=[("hc", cs)])
            S.op("dve", lambda e, ti=ti: e.memset(fs[:, (ti % 2):(ti % 2) + 1], 0.0), writes=[("fs", ti % 2)])
            S.op("act", lambda e, ti=ti, cs=cs: e.activation(out=junk[:], in_=hc[cs][:], func=AF.Square, accum_out=fs[:, (ti % 2):(ti % 2) + 1]), reads=[("hc", cs), ("fs", ti % 2)], writes=["junk", ("fs", ti % 2)])
            S.op("act", lambda e, ti=ti: e.activation(out=fs[:, (ti % 2):(ti % 2) + 1], in_=fs[:, (ti % 2):(ti % 2) + 1], func=AF.Ln, scale=1.0 / D, bias=EPS), reads=[("fs", ti % 2)], writes=[("fs", ti % 2)])
            S.op("act", lambda e, ti=ti: e.activation(out=fs[:, (ti % 2):(ti % 2) + 1], in_=fs[:, (ti % 2):(ti % 2) + 1], func=AF.Exp, scale=-0.5), reads=[("fs", ti % 2)], writes=[("fs", ti % 2)])
            S.op("dve", lambda e, ti=ti, cs=cs: e.scalar_tensor_tensor(out=y1[cs][:], in0=hc[cs][:], scalar=fs[:, (ti % 2):(ti % 2) + 1], in1=fg_b[:], op0=ALU.mult, op1=ALU.mult), reads=[("hc", cs), ("fs", ti % 2), "fg_b", ("y1", cs)], writes=[("y1", cs)])
            finals.append(S.op("sp", lambda e, ti=ti, cs=cs: e.dma_start(out=out.ap()[ti * 128:(ti + 1) * 128, :], in_=y1[cs][:]), reads=[("y1", cs)], dma=("ost", cs)))

        S.flush()
        stB.close()
    return nc


def make_wcat(w_gate, w_up, w_down):
    g = w_gate.reshape(NE, 8, 128, 512).transpose(0, 2, 1, 3)
    u = w_up.reshape(NE, 8, 128, 512).transpose(0, 2, 1, 3)
    gu = np.concatenate([g, u], axis=3).reshape(NE, 128, 8192)
    dn = w_down.reshape(NE, 4, 128, 1024).transpose(0, 2, 1, 3).reshape(NE, 128, 4096)
    return np.ascontiguousarray(np.concatenate([gu, dn], axis=2).reshape(NE * 128, 12288))


def kernel(**inputs):
    f = lambda k: np.ascontiguousarray(np.asarray(inputs[k], dtype=np.float32))
    x = f("x")
    com = {
        "norm1_g": f("norm1_g")[0], "w_in": f("w_in")[0], "conv_qk": f("conv_qk")[0],
        "b_if": np.concatenate([f("b_igate")[0], f("b_fgate")[0]]), "g_mlstm_out": f("g_mlstm_out")[0],
        "g_gmlp_v": f("g_gmlp_v")[0], "w_spatial": f("w_spatial")[0], "b_spatial": f("b_spatial")[0],
        "g_gmlp_out": f("g_gmlp_out")[0], "w_out": f("w_out")[0], "norm2_g": f("norm2_g")[0],
        "w_router": np.ascontiguousarray(np.concatenate([f("w_router_group")[0], f("w_router_expert")[0]], axis=1)),
        "b_router": np.concatenate([f("b_router_group")[0], f("b_router_expert")[0]]),
        "wcat": make_wcat(f("w_gate")[0], f("w_up")[0], f("w_down")[0]), "final_g": f("final_g"),
    }
    in_maps = []
    for c in range(8):
        b, half = c // 2, c % 2
        m = dict(com)
        m["x_main"] = np.ascontiguousarray(x[b, half * 4096:(half + 1) * 4096])
        m["x_pre"] = np.ascontiguousarray(x[b, 0:4096]) if half == 1 else np.zeros((4096, D), np.float32)
        in_maps.append(m)
    nc = build(8, 8)
    res = run_bass_kernel_spmd(nc, in_maps, core_ids=list(range(8)))
    out = np.empty((4, 8192, D), np.float32)
    for c in range(8):
        out[c // 2, (c % 2) * 4096:(c % 2 + 1) * 4096] = res.results[c]["out"]
    return out
```

```python
import contextlib
import numpy as np
import concourse.bass as bass
import concourse.mybir as mybir
from concourse.bass_utils import run_bass_kernel_spmd

F32 = mybir.dt.float32
BF16 = mybir.dt.bfloat16
I32 = mybir.dt.int32
AF = mybir.ActivationFunctionType
ALU = mybir.AluOpType
AX = mybir.AxisListType

ENGS = ("pe", "act", "dve", "pool", "sp")
SEM_CH = 8000
D = 1024
EPS = 1e-6
NE = 32
NBLK_MAX = 96
NSLOT = NBLK_MAX * 128


class Sched:
    def __init__(self, nc, stack):
        self.nc = nc
        self.stack = stack
        self.ops = []
        self.last_w = {}
        self.readers = {}
        self.dma_count = {}
        self.nbuf = 0
        self.flushed = 0
        self.sems = {}
        self.eng_seq = {e: 0 for e in ENGS}
        self.waited = {e: {} for e in ENGS}
        self.cur = stack
        self.est_end = []
        self.eng_free = {e: 0.0 for e in ENGS}
        self.last_est = 0.0

    def sb(self, shape, dt, name=None, persist=False):
        self.nbuf += 1
        return (self.stack if persist else self.cur).enter_context(self.nc.sbuf_tensor(name or f"sb{self.nbuf}", list(shape), dt))

    def ps(self, shape, dt, name=None):
        self.nbuf += 1
        return self.stack.enter_context(self.nc.psum_tensor(name or f"ps{self.nbuf}", list(shape), dt))

    def op(self, eng, fn, reads=(), writes=(), accw=(), dma=None):
        i = len(self.ops)
        deps = set()
        for t in reads:
            deps.update(self.last_w.get(t, ()))
        for t in writes:
            deps.update(self.last_w.get(t, ()))
            deps.update(self.readers.get(t, ()))
        for t in accw:
            deps.update(self.readers.get(t, ()))
        for t in reads:
            self.readers.setdefault(t, []).append(i)
        for t in writes:
            self.last_w[t] = [i]
            self.readers[t] = []
        for t in accw:
            if self.readers.get(t):
                self.last_w[t] = []
                self.readers[t] = []
            self.last_w.setdefault(t, []).append(i)
        deps.discard(i)
        self.ops.append(dict(eng=eng, fn=fn, deps=sorted(deps), dma=dma, sig=None))
        cost = 2.5 if dma is not None else {"pe": 0.25, "act": 0.7, "dve": 0.6, "pool": 1.0, "sp": 0.1}[eng]
        t0 = max([self.eng_free[eng]] + [self.est_end[d] for d in deps if d < len(self.est_end)])
        if dma is not None:
            self.eng_free[eng] = t0 + 0.1
        else:
            self.eng_free[eng] = t0 + cost
        self.est_end.append(t0 + cost)
        self.last_est = t0 + cost
        return i

    def flush(self):
        nc = self.nc
        ops = self.ops
        lo = self.flushed
        last = {}
        for i in range(lo, len(ops)):
            o = ops[i]
            key = ("dma", o["dma"]) if o["dma"] is not None else ("eng", o["eng"])
            last[key] = i
        bdeps = sorted(last.values())
        for en in ENGS:
            self.ops.append(dict(eng=en, fn=lambda e: e.nop(), deps=list(bdeps), dma=None, sig=None, barrier=True))
            self.est_end.append(max(self.est_end) if self.est_end else 0.0)
        hi = len(ops)

        def pe_pair(a, b):
            return (a["eng"] == "pe" and b["eng"] == "pe" and a["dma"] is None and b["dma"] is None
                    and not b.get("barrier"))

        needed = set()
        for i in range(lo, hi):
            o = ops[i]
            for d in o["deps"]:
                if pe_pair(ops[d], o):
                    continue
                needed.add(d)
        for i in range(lo, hi):
            o = ops[i]
            if o["dma"] is not None:
                k = ("dma", o["dma"])
                self.dma_count[k] = self.dma_count.get(k, 0) + 1
                o["sig"] = (k, 16 * self.dma_count[k])
                self.get_sem(k)
            elif i in needed:
                e = o["eng"]
                n = self.eng_seq[e]
                self.eng_seq[e] += 1
                k = ("eng", e, n // SEM_CH)
                o["sig"] = (k, n % SEM_CH + 1)
                self.get_sem(k)
        sems = self.sems
        waited_all = self.waited

        def run(engname):
            def body(e):
                waited = waited_all[engname]
                for i in range(lo, hi):
                    o = ops[i]
                    if o["eng"] != engname:
                        continue
                    for d in o["deps"]:
                        od = ops[d]
                        if od["sig"] is None or pe_pair(od, o):
                            continue
                        k, v = od["sig"]
                        if waited.get(k, 0) >= v:
                            continue
                        e.wait_ge(sems[k], v)
                        waited[k] = v
                    ins = o["fn"](e)
                    if o["sig"] is not None:
                        k, v = o["sig"]
                        ins.then_inc(sems[k], 16 if o["dma"] is not None else 1)
            return body

        with nc.Block() as block:
            block.tensor(run("pe"))
            block.scalar(run("act"))
            block.vector(run("dve"))
            block.gpsimd(run("pool"))
            block.sync(run("sp"))
        self.flushed = hi
        self.last_w = {}
        self.readers = {}

    def get_sem(self, key):
        if key not in self.sems:
            self.sems[key] = self.stack.enter_context(self.nc.semaphore(f"s_{len(self.sems)}"))
        return self.sems[key]


def build(NST=8, NPRE=8, dbg=None):
    nc = bass.Bass("TRN2", target_bir_lowering=False)
    NT = NST * 4
    TOK = NST * 512

    def din(name, shape, dt=F32):
        return nc.dram_tensor(name, list(shape), dt, kind="ExternalInput")

    x_main = din("x_main", [TOK, D])
    x_pre = din("x_pre", [max(NPRE, 1) * 512, D])
    norm1_g = din("norm1_g", [D])
    w_in = din("w_in", [D, 3080])
    conv_qk = din("conv_qk", [4, 1024])
    b_if = din("b_if", [8])
    g_mlstm = din("g_mlstm_out", [512])
    g_gv = din("g_gmlp_v", [512])
    w_sp = din("w_spatial", [8, 128, 128])
    b_sp = din("b_spatial", [8, 128])
    g_go = din("g_gmlp_out", [512])
    w_out = din("w_out", [D, D])
    norm2_g = din("norm2_g", [D])
    w_r = din("w_router", [D, 36])
    b_r = din("b_router", [36])
    wcat = din("wcat", [NE * 128, 12288])
    final_g = din("final_g", [D])
    out = nc.dram_tensor("out", [TOK, D], F32, kind="ExternalOutput")
    h1buf = nc.dram_tensor("h1buf", [TOK, D], F32)
    dbg_t = {}
    if dbg:
        for k, shp in dbg.items():
            dbg_t[k] = nc.dram_tensor("dbg_" + k, list(shp), F32, kind="ExternalOutput")

    with contextlib.ExitStack() as st:
        S = Sched(nc, st)
        finals = []

        ident_f = S.sb([128, 128], F32, "ident_f")
        ident_b = S.sb([128, 128], BF16, "ident_b")
        triu_f = S.sb([128, 128], F32, "triu_f")
        triu_b = S.sb([128, 128], BF16, "triu_b")
        tril_f = S.sb([128, 128], F32, "tril_f")
        ones_f = S.sb([128, 128], F32, "ones_f")
        S.op("pool", lambda e: e.memset(ident_f[:], 0.0), writes=["ident_f"])
        S.op("pool", lambda e: e.affine_select(out=ident_f[:], in_=ident_f[:], pattern=[[-1, 128]],
                                               compare_op=ALU.not_equal, fill=1.0, base=0, channel_multiplier=1),
             reads=["ident_f"], writes=["ident_f"])
        S.op("pool", lambda e: e.memset(ones_f[:], 1.0), writes=["ones_f"])
        S.op("pool", lambda e: e.affine_select(out=triu_f[:], in_=ones_f[:], pattern=[[1, 128]],
                                               compare_op=ALU.is_ge, fill=0.0, base=0, channel_multiplier=-1),
             reads=["ones_f"], writes=["triu_f"])
        S.op("pool", lambda e: e.affine_select(out=tril_f[:], in_=ones_f[:], pattern=[[-1, 128]],
                                               compare_op=ALU.is_ge, fill=0.0, base=0, channel_multiplier=1),
             reads=["ones_f"], writes=["tril_f"])
        S.op("dve", lambda e: e.tensor_copy(out=ident_b[:], in_=ident_f[:]), reads=["ident_f"], writes=["ident_b"])
        S.op("dve", lambda e: e.tensor_copy(out=triu_b[:], in_=triu_f[:]), reads=["triu_f"], writes=["triu_b"])

        def bload(name, src, n, eng="sp"):
            t = S.sb([128, n], F32, name)
            S.op(eng, lambda e: e.dma_start(out=t[:], in_=bass.AP(src, 0, [[0, 128], [1, n]])),
                 writes=[name], dma=name)
            return t

        gml_b = bload("gml_b", g_mlstm, 512)
        ggv_b = bload("ggv_b", g_gv, 512)
        ggo_b = bload("ggo_b", g_go, 512)
        bif_b = bload("bif_b", b_if, 8)

        g1col = S.sb([128, 8], F32, "g1col")
        cw = S.sb([128, 4, 8], F32, "cw")
        bsp = S.sb([128, 8], F32, "bsp")
        S.op("sp", lambda e: e.dma_start(out=g1col[:], in_=norm1_g.ap().rearrange("(c p) -> p c", p=128),
                                         allow_slow_non_contiguous=True), writes=["g1col"], dma="g1col")
        for i in range(4):
            S.op("sp", lambda e, i=i: e.dma_start(out=cw[:, i, :], in_=conv_qk.ap()[i, :].rearrange("(c p) -> p c", p=128),
                                                  allow_slow_non_contiguous=True), accw=["cw"], dma=("cw", i))
        S.op("sp", lambda e: e.dma_start(out=bsp[:], in_=b_sp.ap().rearrange("g t -> t g"),
                                         allow_slow_non_contiguous=True), writes=["bsp"], dma="bsp")

        ptr = [S.ps([128, 1024], BF16, f"ptr{i}") for i in range(2)]
        pfb = [S.ps([128, 512], F32, f"pf{i}") for i in range(6)]
        rr = {"t": 0, "f": 0, "A": 0, "B": 0, "A2": 0}

        def tbank():
            i = rr["t"] % 2
            rr["t"] += 1
            return ptr[i], ("ptr", i)

        def fbank():
            i = rr["f"] % 6
            rr["f"] += 1
            return pfb[i], ("pf", i)

        def fbankA():
            return pfb[0], ("pf", 0)

        def fbankA2():
            i = 1 + rr["A2"] % 2
            rr["A2"] += 1
            return pfb[i], ("pf", i)

        def fbankB():
            i = 3 + rr["B"] % 2
            rr["B"] += 1
            return pfb[i], ("pf", i)

        def fbankC():
            return pfb[5], ("pf", 5)

        junk = S.sb([128, 1024], BF16, "junk", persist=True)
        base_b = S.sb([128, 32], F32, "base_b", persist=True)
        E1s = S.sb([128, NT, 32], BF16, "E1s", persist=True)
        E2s = S.sb([128, NT, 32], BF16, "E2s", persist=True)
        pw = S.sb([128, NT, 4], F32, "pw", persist=True)
        stA = contextlib.ExitStack()
        S.cur = stA
        xbuf = [S.sb([128, 4, 1024], F32, f"xbuf{i}") for i in range(2)]
        wqk = S.sb([128, 8, 1024], BF16, "wqk")
        wvo = S.sb([128, 8, 1024], BF16, "wvo")
        wuv = S.sb([128, 8, 1024], BF16, "wuv")
        wif = S.sb([128, 8, 8], BF16, "wif")
        wout = S.sb([128, 8, 1024], BF16, "wout")
        for kc in range(8):
            sl = kc % 2
            stg = xbuf[sl][:].rearrange("p a b -> p (a b)")
            S.op("sp", lambda e, stg=stg, kc=kc: e.dma_start(out=stg[:, 0:3080], in_=w_in.ap()[kc * 128:(kc + 1) * 128, :]),
                 writes=[("x", sl)], dma=("x", sl))
            for (dst, c0, n, tok) in ((wqk, 0, 1024, "wqk"), (wvo, 1024, 1024, "wvo"), (wif, 2048, 8, "wif"), (wuv, 2056, 1024, "wuv")):
                S.op("act", lambda e, dst=dst, c0=c0, n=n, kc=kc, stg=stg: e.mul(dst[:, kc, 0:n], stg[:, c0:c0 + n], g1col[:, kc:kc + 1]),
                     reads=[("x", sl), "g1col"], accw=[tok])
        S.op("pool", lambda e: e.dma_start(out=wout[:], in_=w_out.ap().rearrange("(c p) n -> p c n", p=128)),
             writes=["wout"], dma="wout")

        wsp_f = xbuf[1][:, 3, :].rearrange("p (g s) -> p g s", g=8)
        wspT = S.sb([128, 8, 128], BF16, "wspT")
        S.op("sp", lambda e: e.dma_start(out=wsp_f, in_=w_sp.ap().rearrange("g t s -> t g s")), writes=[("x", 1)], dma=("x", 1))
        S.op("dve", lambda e: e.tensor_tensor(out=wsp_f, in0=wsp_f, in1=tril_f[:].unsqueeze(1).broadcast_to([128, 8, 128]), op=ALU.mult),
             reads=[("x", 1), "tril_f"], writes=[("x", 1)])
        for half in range(2):
            pb, pt = fbank()
            for g4 in range(4):
                g = half * 4 + g4
                S.op("pe", lambda e, pb=pb, g=g, g4=g4: e.transpose(out=pb[:, g4 * 128:(g4 + 1) * 128], in_=wsp_f[:, g, :], identity=ident_f[:]),
                     reads=[("x", 1), "ident_f"], accw=[pt])
            S.op("dve", lambda e, pb=pb, half=half: e.tensor_copy(out=wspT[:, half * 4:(half + 1) * 4, :].rearrange("p a b -> p (a b)"), in_=pb[:]),
                 reads=[pt], accw=["wspT"])

        C32 = S.sb([128, 4, 129], F32, "C32")
        Csb = S.sb([128, 4, 129], BF16, "Csb")
        m_st = S.sb([4, 1], F32, "m_st")
        halo = S.sb([128, 8, 3], F32, "halo")
        S.op("pool", lambda e: e.memset(C32[:], 0.0), writes=[("C32", h) for h in range(4)])
        S.op("pool", lambda e: e.memset(m_st[:], 0.0), writes=["m_st"])
        S.op("pool", lambda e: e.memset(halo[:], 0.0), writes=[("halo", c) for c in range(8)])

        xn = [S.sb([128, 1024], BF16, f"xn{i}") for i in range(2)]
        ss1 = S.sb([128, 8], F32, "ss1")
        xT = [S.sb([128, 8, 512], BF16, f"xT{i}") for i in range(1)]
        pre = [S.sb([128, 515], F32, f"pre{i}") for i in range(2)]
        cacc = [S.sb([128, 512], F32, f"cacc{i}") for i in range(2)]
        sg = [S.sb([128, 512], F32, f"sg{i}") for i in range(2)]
        qkT = [S.sb([128, 8, 512], BF16, f"qkT{i}") for i in range(1)]
        ktm2 = [S.sb([128, 4, 128], BF16, f"ktm{i}") for i in range(2)]
        gsb = S.sb([128, 4, 8], F32, "gsb")
        spl = S.sb([128, 4, 4], F32, "spl")
        a_tm = S.sb([128, 4, 4], F32, "a_tm")
        cum_sb = S.sb([128, 4, 4], F32, "cum_sb")
        e_tm = S.sb([128, 4, 4], F32, "e_tm")
        dn_tm = S.sb([128, 4, 4], F32, "dn_tm")
        gb_sb = S.sb([128, 4, 8], F32, "gb_sb")
        rw = S.sb([4, 20], F32, "rw")
        rhs8 = S.sb([4, 4, 8], F32, "rhs8")
        lnc0_t = S.sb([128, 1], F32, "lnc0_t")
        S.op("pool", lambda e: e.memset(lnc0_t[:], -float(np.log(128 ** -0.5))), writes=["lnc0"])
        ve = [S.sb([128, 4, 129], BF16, f"ve{i}") for i in range(2)]
        smask = [S.sb([128, 4, 128], BF16, f"smask{i}") for i in range(2)]
        og2 = [S.sb([128, 512], F32, f"og{i}") for i in range(2)]
        hs = S.sb([128, 4, 128], F32, "hs")
        nqa = S.sb([128, 4], F32, "nqa")
        sm8 = S.sb([128, 16], F32, "sm8")
        tmpa = S.sb([128, 1024], F32, "tmpa")
        ux = S.sb([128, 1024], F32, "ux")
        t1 = S.sb([128, 1024], F32, "t1")
        vn = S.sb([128, 512], BF16, "vn")
        gt = S.sb([128, 8, 64], F32, "gt")
        ybuf = [S.sb([128, 1024], BF16, f"ybuf{i}") for i in range(2)]
        yT = [S.sb([128, 8, 128], BF16, f"yT{i}") for i in range(2)]
        h1b = [S.sb([128, 1024], F32, f"h1b{i}") for i in range(2)]
        g2_b = bload("g2_b", norm2_g, 1024)
        br_b = bload("br_b", b_r, 36)
        wr_f = S.sb([128, 8, 36], F32, "wr_f")
        wr_b = S.sb([128, 8, 36], BF16, "wr_b")
        S.op("sp", lambda e: e.dma_start(out=wr_f[:], in_=w_r.ap().rearrange("(c p) n -> p c n", p=128)), writes=["wr_f"], dma="wr_f")
        S.op("dve", lambda e: e.tensor_copy(out=wr_b[:], in_=wr_f[:]), reads=["wr_f"], writes=["wr_b"])
        lstr_b = S.sb([128, 128], BF16, "lstr_b")
        ones_b = S.sb([128, 128], BF16, "ones_b")
        lstr_f = S.sb([128, 128], F32, "lstr_f")
        S.op("pool", lambda e: e.affine_select(out=lstr_f[:], in_=ones_f[:], pattern=[[1, 128]], compare_op=ALU.is_gt, fill=0.0, base=0, channel_multiplier=-1),
             reads=["ones_f"], writes=["lstr_f"])
        S.op("dve", lambda e: e.tensor_copy(out=lstr_b[:], in_=lstr_f[:]), reads=["lstr_f"], writes=["lstr_b"])
        S.op("dve", lambda e: e.tensor_copy(out=ones_b[:], in_=ones_f[:]), reads=["ones_f"], writes=["ones_b"])
        xn2t = [S.sb([128, 1024], BF16, f"xn2t{i}") for i in range(2)]
        xn2T = [S.sb([128, 8, 128], BF16, f"xn2T{i}") for i in range(1)]
        lg = S.sb([128, 36], F32, "lg")
        rt = S.sb([128, 64], F32, "rt")
        t32 = S.sb([128, 4, 8], F32, "t32")
        le8 = S.sb([128, 16], F32, "le8")
        ind_b = S.sb([128, 32], BF16, "ind_b")
        posf = S.sb([128, 32], F32, "posf")
        S.op("pool", lambda e: e.memset(base_b[:], 0.0), writes=["base_b"])
        xn2lin = nc.dram_tensor("xn2lin", [TOK, D], BF16)
        cnt = {"tile": 0, "st": 0, "ch": 0, "tl2": 0, "y": 0}

        LN_C0 = float(np.log(128 ** -0.5))

        def dump(name, ap_fn, reads, row0=0):
            if name in dbg_t:
                t = dbg_t[name]
                finals.append(S.op("sp", lambda e: e.dma_start(out=t.ap()[row0:row0 + 128, :], in_=ap_fn()), reads=reads, dma=("dbg", name)))

        plan_x = [(x_pre, i) for i in range(NPRE)] + [(x_main, i) for i in range(NST)]

        def xload(g):
            src_, i_ = plan_x[g]
            sl_ = g % 2
            S.op("sp", lambda e: e.dma_start(out=xbuf[sl_][:], in_=src_.ap()[i_ * 512:(i_ + 1) * 512, :].rearrange("(j p) d -> p j d", p=128)),
                 writes=[("x", sl_)], dma=("x", sl_))
            conv_some(g, sl_)

        def supertile(xsrc, st_i, mode, gi):
            main = mode == "main"
            sl = cnt["st"] % 2
            cnt["st"] += 1
            xb = xbuf[sl]
            xt = xT[0]
            qk = qkT[0]
            if gi == 0:
                xload(0)
            for j in range(4):
                tl = cnt["tile"] % 2
                cnt["tile"] += 1
                xnj = xn[tl]
                S.op("dve", lambda e, j=j: e.memset(ss1[:, j:j + 1], 0.0), writes=[("ss1", j)])
                S.op("act", lambda e, j=j: e.activation(out=junk[:], in_=xb[:, j, :], func=AF.Square, accum_out=ss1[:, j:j + 1]),
                     reads=[("x", sl), ("ss1", j)], writes=["junk", ("ss1", j)])
                S.op("act", lambda e, j=j: e.activation(out=ss1[:, 4 + j:5 + j], in_=ss1[:, j:j + 1], func=AF.Ln, scale=1.0 / D, bias=EPS),
                     reads=[("ss1", j)], writes=[("ss1", 4 + j)])
                S.op("act", lambda e, j=j: e.activation(out=ss1[:, 4 + j:5 + j], in_=ss1[:, 4 + j:5 + j], func=AF.Exp, scale=-0.5),
                     reads=[("ss1", 4 + j)], writes=[("ss1", 4 + j)])
                S.op("act", lambda e, j=j, xnj=xnj: e.mul(xnj[:], xb[:, j, :], ss1[:, 4 + j:5 + j]),
                     reads=[("x", sl), ("ss1", 4 + j)], writes=[("xn", tl)])
                pb, pt = tbank()
                for c in range(8):
                    S.op("pe", lambda e, c=c, pb=pb, xnj=xnj: e.transpose(out=pb[:, c * 128:(c + 1) * 128], in_=xnj[:, c * 128:(c + 1) * 128], identity=ident_b[:]),
                         reads=[("xn", tl), "ident_b"], accw=[pt])
                S.op("dve" if j % 2 == 0 else "act",
                     (lambda e, pb=pb, j=j: e.tensor_copy(out=xt[:, :, j * 128:(j + 1) * 128], in_=pb[:].rearrange("p (c t) -> p c t", c=8))) if j % 2 == 0 else
                     (lambda e, pb=pb, j=j: e.copy(out=xt[:, :, j * 128:(j + 1) * 128], in_=pb[:].rearrange("p (c t) -> p c t", c=8))),
                     reads=[pt], accw=[("xT", 0)])
            chunks = range(8) if mode != "pre" else range(4, 8)
            def chunk_s1(ch):
                pb, pt = fbank()
                for kc in range(8):
                    S.op("pe", lambda e, kc=kc, ch=ch, pb=pb: e.matmul(out=pb[:], lhsT=wqk[:, kc, ch * 128:(ch + 1) * 128], rhs=xt[:, kc, :], start=(kc == 0), stop=(kc == 7)),
                         reads=[("xT", 0), "wqk"], accw=[pt])
                ps_ = cnt["ch"] % 2
                cnt["ch"] += 1
                pr = pre[ps_]
                ca = cacc[ps_]
                s_ = sg[ps_]
                S.op("dve", lambda e, pr=pr, ch=ch: e.tensor_copy(out=pr[:, 0:3], in_=halo[:, ch, :]), reads=[("halo", ch)], writes=[("pre", ps_, "h")])
                S.op("act", lambda e, pr=pr, pb=pb: e.copy(out=pr[:, 3:515], in_=pb[:]), reads=[pt], writes=[("pre", ps_)])
                S.op("dve", lambda e, pr=pr, ch=ch: e.tensor_copy(out=halo[:, ch, :], in_=pr[:, 512:515]), reads=[("pre", ps_), ("pre", ps_, "h")], writes=[("halo", ch)])
                S.op("dve", lambda e, pr=pr, ca=ca, ch=ch: e.tensor_scalar(out=ca[:], in0=pr[:, 0:512], scalar1=cw[:, 0, ch:ch + 1], scalar2=None, op0=ALU.mult),
                     reads=[("pre", ps_), ("pre", ps_, "h"), "cw"], writes=[("cacc", ps_)])
                for i in range(1, 4):
                    S.op("dve", lambda e, pr=pr, ca=ca, ch=ch, i=i: e.scalar_tensor_tensor(out=ca[:], in0=pr[:, i:i + 512], scalar=cw[:, i, ch:ch + 1], in1=ca[:], op0=ALU.mult, op1=ALU.add),
                         reads=[("pre", ps_), ("pre", ps_, "h"), ("cacc", ps_), "cw"], writes=[("cacc", ps_)])
                return (ch, ps_, ca, s_)

            def chunk_s2(args):
                ch, ps_, ca, s_ = args
                S.op("act", lambda e, ca=ca, s_=s_: e.activation(out=s_[:], in_=ca[:], func=AF.Exp, scale=-1.0), reads=[("cacc", ps_)], writes=[("sg", ps_)])
                S.op("act", lambda e, s_=s_: e.activation(out=s_[:], in_=s_[:], func=AF.Ln, bias=1.0), reads=[("sg", ps_)], writes=[("sg", ps_)])
                S.op("act", lambda e, s_=s_: e.activation(out=s_[:], in_=s_[:], func=AF.Exp, scale=-1.0), reads=[("sg", ps_)], writes=[("sg", ps_)])
                S.op("dve", lambda e, s_=s_, ca=ca, ch=ch: e.tensor_tensor(out=qk[:, ch, :], in0=s_[:], in1=ca[:], op=ALU.mult),
                     reads=[("sg", ps_), ("cacc", ps_)], accw=[("qkT", 0)])


            pend = None
            for ch in chunks:
                cur = chunk_s1(ch)
                if pend is not None:
                    chunk_s2(pend)
                pend = cur
            chunk_s2(pend)
            pg, pgt = fbank()
            for j in range(4):
                for kc in range(8):
                    S.op("pe", lambda e, j=j, kc=kc: e.matmul(out=pg[:, j * 8:(j + 1) * 8], lhsT=xt[:, kc, j * 128:(j + 1) * 128], rhs=wif[:, kc, :], start=(kc == 0), stop=(kc == 7)),
                         reads=[("xT", 0), "wif"], accw=[pgt])
            S.op("dve", lambda e: e.tensor_tensor(out=gsb[:], in0=pg[:, 0:32].rearrange("p (c g) -> p c g", c=4), in1=bif_b[:].unsqueeze(1).broadcast_to([128, 4, 8]), op=ALU.add),
                 reads=[pgt, "bif_b"], writes=["gsb"])
            S.op("act", lambda e: e.activation(out=spl[:], in_=gsb[:, :, 4:8], func=AF.Exp, scale=-1.0), reads=["gsb"], writes=["spl"])
            S.op("act", lambda e: e.activation(out=spl[:], in_=spl[:], func=AF.Ln, bias=1.0), reads=["spl"], writes=["spl"])
            pc, pct = fbank()
            prw, prt = fbank()
            for c in range(4):
                S.op("pe", lambda e, c=c: e.matmul(out=pc[:, c * 4:(c + 1) * 4], lhsT=triu_f[:], rhs=spl[:, c, :], start=True, stop=True),
                     reads=["spl", "triu_f"], accw=[pct])
                S.op("pe", lambda e, c=c: e.matmul(out=pc[0:4, 16 + c:17 + c], lhsT=spl[:, c, :], rhs=ones_f[:, 0:1], start=True, stop=True),
                     reads=["spl", "ones_f"], accw=[pct])
                S.op("pe", lambda e, c=c: e.matmul(out=prw[0:4, c * 128:(c + 1) * 128], lhsT=gsb[:, c, 0:4], rhs=ident_f[:], start=True, stop=False),
                     reads=["gsb", "ident_f"], accw=[prt])
                S.op("pe", lambda e, c=c: e.matmul(out=prw[0:4, c * 128:(c + 1) * 128], lhsT=spl[:, c, :], rhs=triu_f[:], start=False, stop=True),
                     reads=["spl", "triu_f"], accw=[prt])
            S.op("dve", lambda e: e.tensor_tensor(out=a_tm[:], in0=gsb[:, :, 0:4], in1=pc[:, 0:16].rearrange("p (c h) -> p c h", c=4), op=ALU.add),
                 reads=["gsb", pct], writes=["a_tm"])
            S.op("dve", lambda e: e.tensor_copy(out=cum_sb[:], in_=pc[:, 0:16].rearrange("p (c h) -> p c h", c=4)), reads=[pct], writes=["cum_sb"])
            S.op("dve", lambda e: e.tensor_copy(out=rw[:, 4:8], in_=pc[0:4, 16:20]), reads=[pct], writes=["rw_tot"])
            S.op("dve", lambda e: e.tensor_reduce(out=rw[:, 0:4], in_=prw[0:4, :].rearrange("p (c l) -> p c l", c=4), axis=AX.X, op=ALU.max),
                 reads=[prt], writes=["rw_A"])
            for c in range(4):
                S.op("dve", lambda e, c=c: e.tensor_copy(out=rw[:, 12 + c:13 + c], in_=m_st[:]), reads=["m_st"], writes=[("rw_mp", c)])
                S.op("dve", lambda e, c=c: e.tensor_tensor(out=rw[:, 8 + c:9 + c], in0=m_st[:], in1=rw[:, c:c + 1], op=ALU.max), reads=["m_st", "rw_A"], writes=[("rw_G", c)])
                S.op("dve", lambda e, c=c: e.tensor_tensor(out=m_st[:], in0=rw[:, 8 + c:9 + c], in1=rw[:, 4 + c:5 + c], op=ALU.subtract), reads=[("rw_G", c), "rw_tot"], writes=["m_st"])
            S.op("dve", lambda e: e.tensor_tensor(out=rw[:, 16:20], in0=rw[:, 12:16], in1=rw[:, 8:12], op=ALU.subtract),
                 reads=[("rw_G", c) for c in range(4)] + [("rw_mp", c) for c in range(4)], writes=["rw_D"])
            S.op("act", lambda e: e.activation(out=rw[:, 16:20], in_=rw[:, 16:20], func=AF.Exp), reads=["rw_D"], writes=["rw_D"])
            S.op("dve", lambda e: e.tensor_tensor(out=rhs8[:, :, 0:4], in0=ident_f[0:4, 0:4].unsqueeze(1).broadcast_to([4, 4, 4]), in1=rw[:, 8:12].unsqueeze(2).broadcast_to([4, 4, 4]), op=ALU.mult),
                 reads=[("rw_G", c) for c in range(4)] + ["ident_f"], writes=["rhs8a"])
            S.op("dve", lambda e: e.tensor_tensor(out=rhs8[:, :, 4:8], in0=ident_f[0:4, 0:4].unsqueeze(1).broadcast_to([4, 4, 4]), in1=rw[:, 16:20].unsqueeze(2).broadcast_to([4, 4, 4]), op=ALU.mult),
                 reads=["rw_D", "ident_f"], writes=["rhs8b"])
            S.op("pe", lambda e: e.matmul(out=pc[:, 32:64], lhsT=ones_f[0:4, :], rhs=rhs8[:].rearrange("p c g -> p (c g)"), start=True, stop=True),
                 reads=["rhs8a", "rhs8b", "ones_f"], accw=[pct])
            S.op("dve", lambda e: e.tensor_copy(out=gb_sb[:], in_=pc[:, 32:64].rearrange("p (c g) -> p c g", c=4)), reads=[pct], writes=["gb_sb"])
            S.op("dve", lambda e: e.tensor_tensor(out=e_tm[:], in0=a_tm[:], in1=gb_sb[:, :, 0:4], op=ALU.subtract), reads=["a_tm", "gb_sb"], writes=["e_tm"])
            S.op("act", lambda e: e.activation(out=e_tm[:], in_=e_tm[:], func=AF.Exp), reads=["e_tm"], writes=["e_tm"])
            S.op("dve", lambda e: e.tensor_tensor(out=dn_tm[:], in0=cum_sb[:], in1=gb_sb[:, :, 0:4], op=ALU.subtract), reads=["cum_sb", "gb_sb"], writes=["dn_tm"])
            S.op("act", lambda e: e.activation(out=dn_tm[:], in_=dn_tm[:], func=AF.Exp, bias=lnc0_t[:, 0:1]), reads=["dn_tm", "lnc0"], writes=["dn_tm"])

            if gi + 1 < NPRE + NST:
                xload(gi + 1)
            def tile_body(j):
                c = j
                csl = slice(c * 128, (c + 1) * 128)
                tsl = cnt["tl2"] % 2
                cnt["tl2"] += 1
                vea = ve[tsl]
                sm = smask[tsl]
                ktm = ktm2[tsl]
                og = og2[tsl]
                ysl = cnt["y"] % 2
                if main:
                    cnt["y"] += 1
                yt_ = ybuf[ysl]
                def chainA1():
                    pv, pvt = fbankA()
                    for kc in range(8):
                        yield S.op("pe", lambda e, kc=kc, pv=pv: e.matmul(out=pv[:], lhsT=xt[:, kc, csl], rhs=wvo[:, kc, 0:512], start=(kc == 0), stop=(kc == 7)),
                             reads=[("xT", 0), "wvo"], accw=[pvt])
                    yield S.op("dve", lambda e, pv=pv, vea=vea: e.tensor_tensor(out=vea[:, :, 0:128], in0=pv[:].rearrange("p (h d) -> p h d", h=4), in1=e_tm[:, c, :].unsqueeze(2).broadcast_to([128, 4, 128]), op=ALU.mult),
                         reads=[pvt, "e_tm"], writes=[("ve", tsl)])
                    yield S.op("dve", lambda e, vea=vea: e.tensor_copy(out=vea[:, :, 128:129], in_=e_tm[:, c, :].unsqueeze(2)), reads=["e_tm"], writes=[("ve", tsl, "e")])
                    pb, pt = (ptr[0], ("ptr", 0))
                    for h in range(4):
                        yield S.op("pe", lambda e, h=h, pb=pb: e.transpose(out=pb[:, h * 128:(h + 1) * 128], in_=qk[:, 4 + h, csl], identity=ident_b[:]),
                             reads=[("qkT", 0), "ident_b"], accw=[pt])
                    yield S.op("act", lambda e, pb=pb: e.copy(out=ktm[:].rearrange("p h d -> p (h d)"), in_=pb[:, 0:512]), reads=[pt], writes=[("ktm", tsl)])
                    if main:
                        po, pot = fbankA()
                        for kc in range(8):
                            yield S.op("pe", lambda e, kc=kc, po=po: e.matmul(out=po[:], lhsT=xt[:, kc, csl], rhs=wvo[:, kc, 512:1024], start=(kc == 0), stop=(kc == 7)),
                                 reads=[("xT", 0), "wvo"], accw=[pot])
                        yield S.op("act", lambda e, po=po: e.activation(out=og[:], in_=po[:], func=AF.Exp, scale=-1.0), reads=[pot], writes=[("og", tsl)])
                        yield S.op("act", lambda e: e.activation(out=og[:], in_=og[:], func=AF.Ln, bias=1.0), reads=[("og", tsl)], writes=[("og", tsl)])
                        yield S.op("act", lambda e: e.activation(out=og[:], in_=og[:], func=AF.Exp, scale=-1.0), reads=[("og", tsl)], writes=[("og", tsl)])
                        pS, pSt = fbankA()
                        for h in range(4):
                            yield S.op("pe", lambda e, h=h, pS=pS: e.matmul(out=pS[:, h * 128:(h + 1) * 128], lhsT=qk[:, 4 + h, csl], rhs=qk[:, h, csl], start=True, stop=True),
                                 reads=[("qkT", 0)], accw=[pSt])
                        sm = smask[tsl]
                        yield S.op("dve", lambda e, pS=pS, sm=sm: e.tensor_tensor(out=sm[:], in0=pS[:].rearrange("p (h l) -> p h l", h=4), in1=triu_f[:].unsqueeze(1).broadcast_to([128, 4, 128]), op=ALU.mult),
                             reads=[pSt, "triu_f"], writes=[("smask", tsl)])
                    yield None
                def chainA2():
                    if main:
                        for h in range(4):
                            yield S.op("act", lambda e, h=h: e.mul(Csb[:, h, :], C32[:, h, :], gb_sb[:, c, 4 + h:5 + h]), reads=[("C32", h), "gb_sb"], writes=[("Csb", h)])
                        nbanks = []
                        for hp in range(2):
                            pn, pnt = fbankA2()
                            nbanks.append((pn, pnt))
                            for hh in range(2):
                                h = hp * 2 + hh
                                yield S.op("pe", lambda e, h=h, hh=hh, pn=pn, sm=sm, vea=vea: e.matmul(out=pn[:, hh * 129:(hh + 1) * 129], lhsT=sm[:, h, :], rhs=vea[:, h, :], start=True, stop=False),
                                     reads=[("smask", tsl), ("ve", tsl), ("ve", tsl, "e")], accw=[pnt])
                                yield S.op("pe", lambda e, h=h, hh=hh, pn=pn: e.matmul(out=pn[:, hh * 129:(hh + 1) * 129], lhsT=qk[:, h, csl], rhs=Csb[:, h, :], start=False, stop=True),
                                     reads=[("qkT", 0), ("Csb", h)], accw=[pnt])
                        for hp in range(2):
                            pn, pnt = nbanks[hp]
                            yield S.op("act", lambda e, pn=pn, hp=hp: e.activation(out=nqa[:, hp * 2:hp * 2 + 2].unsqueeze(2), in_=pn[:, 0:258].rearrange("p (h d) -> p h d", h=2)[:, :, 128:129], func=AF.Abs),
                                 reads=[pnt], writes=["nqa"])
                        yield S.op("dve", lambda e: e.tensor_tensor(out=nqa[:], in0=nqa[:], in1=dn_tm[:, c, :], op=ALU.max), reads=["nqa", "dn_tm"], writes=["nqa"])
                        yield S.op("dve", lambda e: e.reciprocal(out=nqa[:], in_=nqa[:]), reads=["nqa"], writes=["nqa"])
                        for hp in range(2):
                            pn, pnt = nbanks[hp]
                            yield S.op("dve", lambda e, pn=pn, hp=hp: e.tensor_tensor(out=hs[:, hp * 2:hp * 2 + 2, :], in0=pn[:, 0:258].rearrange("p (h d) -> p h d", h=2)[:, :, 0:128], in1=nqa[:, hp * 2:hp * 2 + 2].unsqueeze(2).broadcast_to([128, 2, 128]), op=ALU.mult),
                                 reads=[pnt, "nqa"], writes=["hs"])
                        yield S.op("dve", lambda e: e.tensor_tensor(out=hs[:].rearrange("p h d -> p (h d)"), in0=hs[:].rearrange("p h d -> p (h d)"), in1=og[:], op=ALU.mult),
                             reads=["hs", ("og", tsl)], writes=["hs"])
                        yield S.op("dve", lambda e: e.tensor_tensor(out=tmpa[:, 0:512], in0=hs[:].rearrange("p h d -> p (h d)"), in1=hs[:].rearrange("p h d -> p (h d)"), op=ALU.mult), reads=["hs"], writes=["tmpa"])
                        yield S.op("dve", lambda e: e.tensor_reduce(out=sm8[:, 0:4], in_=tmpa[:, 0:512].rearrange("p (h d) -> p h d", h=4), axis=AX.X, op=ALU.add), reads=["tmpa"], writes=["sm8a"])
                        yield S.op("act", lambda e: e.activation(out=sm8[:, 0:4], in_=sm8[:, 0:4], func=AF.Ln, scale=1.0 / 128, bias=EPS), reads=["sm8a"], writes=["sm8a"])
                        yield S.op("act", lambda e: e.activation(out=sm8[:, 0:4], in_=sm8[:, 0:4], func=AF.Exp, scale=-0.5), reads=["sm8a"], writes=["sm8a"])
                        yield S.op("dve", lambda e: e.tensor_tensor(out=hs[:], in0=hs[:], in1=sm8[:, 0:4].unsqueeze(2).broadcast_to([128, 4, 128]), op=ALU.mult), reads=["hs", "sm8a"], writes=["hs"])
                        yield S.op("dve", lambda e, yt_=yt_: e.tensor_tensor(out=yt_[:, 0:512], in0=hs[:].rearrange("p h d -> p (h d)"), in1=gml_b[:], op=ALU.mult), reads=["hs", "gml_b"], writes=[("y", ysl, 0)])
                    for hp in range(2):
                        pu_, put = fbankA2()
                        for hh in range(2):
                            h = hp * 2 + hh
                            yield S.op("pe", lambda e, h=h, hh=hh, pu_=pu_, vea=vea: e.matmul(out=pu_[:, hh * 129:(hh + 1) * 129], lhsT=ktm[:, h, :], rhs=vea[:, h, :], start=True, stop=True),
                                 reads=[("ktm", tsl), ("ve", tsl), ("ve", tsl, "e")], accw=[put])
                        for hh in range(2):
                            h = hp * 2 + hh
                            yield S.op("dve", lambda e, h=h, hh=hh, pu_=pu_: e.scalar_tensor_tensor(out=C32[:, h, :], in0=C32[:, h, :], scalar=gb_sb[:, c, 4 + h:5 + h], in1=pu_[:, hh * 129:(hh + 1) * 129], op0=ALU.mult, op1=ALU.add),
                                 reads=[put, "gb_sb", ("C32", h)], writes=[("C32", h)])

                    yield None
                def chainB():
                    pU, pUt = fbankB()
                    pV, pVt = fbankB()
                    for kc in range(8):
                        yield S.op("pe", lambda e, kc=kc, pU=pU: e.matmul(out=pU[:], lhsT=xt[:, kc, csl], rhs=wuv[:, kc, 0:512], start=(kc == 0), stop=(kc == 7)),
                             reads=[("xT", 0), "wuv"], accw=[pUt])
                    for kc in range(8):
                        yield S.op("pe", lambda e, kc=kc, pV=pV: e.matmul(out=pV[:], lhsT=xt[:, kc, csl], rhs=wuv[:, kc, 512:1024], start=(kc == 0), stop=(kc == 7)),
                             reads=[("xT", 0), "wuv"], accw=[pVt])
                    yield S.op("act", lambda e, pU=pU: e.copy(out=ux[:, 0:512], in_=pU[:]), reads=[pUt], writes=["ux"])
                    yield S.op("act", lambda e, pV=pV: e.copy(out=ux[:, 512:1024], in_=pV[:]), reads=[pVt], writes=["ux"])
                    yield S.op("act", lambda e: e.activation(out=t1[:], in_=ux[:], func=AF.Square), reads=["ux"], writes=["t1"])
                    yield S.op("dve", lambda e: e.tensor_scalar(out=t1[:], in0=t1[:], scalar1=0.044715, scalar2=1.0, op0=ALU.mult, op1=ALU.add), reads=["t1"], writes=["t1"])
                    yield S.op("dve", lambda e: e.tensor_tensor(out=t1[:], in0=t1[:], in1=ux[:], op=ALU.mult), reads=["t1", "ux"], writes=["t1"])
                    yield S.op("act", lambda e: e.activation(out=t1[:], in_=t1[:], func=AF.Exp, scale=-2.0 * 0.7978845608028654), reads=["t1"], writes=["t1"])
                    yield S.op("act", lambda e: e.activation(out=t1[:], in_=t1[:], func=AF.Ln, bias=1.0), reads=["t1"], writes=["t1"])
                    yield S.op("act", lambda e: e.activation(out=t1[:], in_=t1[:], func=AF.Exp, scale=-1.0), reads=["t1"], writes=["t1"])
                    yield S.op("dve", lambda e: e.tensor_tensor(out=ux[:], in0=t1[:], in1=ux[:], op=ALU.mult), reads=["t1", "ux"], writes=["ux"])
                    yield S.op("dve", lambda e: e.memset(sm8[:, 4:5], 0.0), writes=["sm8b"])
                    yield S.op("act", lambda e: e.activation(out=junk[:, 0:512], in_=ux[:, 512:1024], func=AF.Square, accum_out=sm8[:, 4:5]), reads=["ux", "sm8b"], writes=["junk", "sm8b"])
                    yield S.op("act", lambda e: e.activation(out=sm8[:, 4:5], in_=sm8[:, 4:5], func=AF.Ln, scale=1.0 / 512, bias=EPS), reads=["sm8b"], writes=["sm8b"])
                    yield S.op("act", lambda e: e.activation(out=sm8[:, 4:5], in_=sm8[:, 4:5], func=AF.Exp, scale=-0.5), reads=["sm8b"], writes=["sm8b"])
                    yield S.op("dve", lambda e: e.scalar_tensor_tensor(out=vn[:], in0=ux[:, 512:1024], scalar=sm8[:, 4:5], in1=ggv_b[:], op0=ALU.mult, op1=ALU.mult), reads=["ux", "sm8b", "ggv_b"], writes=["vn"])
                    pM, pMt = fbankB()
                    for g in range(8):
                        yield S.op("pe", lambda e, g=g, pM=pM: e.matmul(out=pM[:, g * 64:(g + 1) * 64], lhsT=wspT[:, g, :], rhs=vn[:, g * 64:(g + 1) * 64], start=True, stop=True),
                             reads=["vn", "wspT"], accw=[pMt])
                    yield S.op("dve", lambda e, pM=pM: e.tensor_tensor(out=gt[:], in0=pM[:].rearrange("p (g c) -> p g c", g=8), in1=bsp[:].unsqueeze(2).broadcast_to([128, 8, 64]), op=ALU.add),
                         reads=[pMt, "bsp"], writes=["gt"])
                    yield S.op("dve", lambda e: e.tensor_tensor(out=gt[:].rearrange("p g c -> p (g c)"), in0=gt[:].rearrange("p g c -> p (g c)"), in1=ux[:, 0:512], op=ALU.mult), reads=["gt", "ux"], writes=["gt"])
                    yield S.op("dve", lambda e: e.tensor_tensor(out=tmpa[:, 512:1024], in0=gt[:].rearrange("p g c -> p (g c)"), in1=gt[:].rearrange("p g c -> p (g c)"), op=ALU.mult), reads=["gt"], writes=["tmpb"])
                    yield S.op("dve", lambda e: e.tensor_reduce(out=sm8[:, 8:16], in_=tmpa[:, 512:1024].rearrange("p (g c) -> p g c", g=8), axis=AX.X, op=ALU.add), reads=["tmpb"], writes=["sm8c"])
                    yield S.op("act", lambda e: e.activation(out=sm8[:, 8:16], in_=sm8[:, 8:16], func=AF.Ln, scale=1.0 / 64, bias=EPS), reads=["sm8c"], writes=["sm8c"])
                    yield S.op("act", lambda e: e.activation(out=sm8[:, 8:16], in_=sm8[:, 8:16], func=AF.Exp, scale=-0.5), reads=["sm8c"], writes=["sm8c"])
                    yield S.op("dve", lambda e: e.tensor_tensor(out=gt[:], in0=gt[:], in1=sm8[:, 8:16].unsqueeze(2).broadcast_to([128, 8, 64]), op=ALU.mult), reads=["gt", "sm8c"], writes=["gt"])
                    yield S.op("dve", lambda e, yt_=yt_: e.tensor_tensor(out=yt_[:, 512:1024], in0=gt[:].rearrange("p g c -> p (g c)"), in1=ggo_b[:], op=ALU.mult), reads=["gt", "ggo_b"], writes=[("y", ysl, 1)])

                    yield None
                def chainC():
                    if "y" in dbg_t:
                        row0d = st_i * 512 + j * 128
                        finals.append(S.op("pool", lambda e, yt_=yt_, row0d=row0d: e.dma_start(out=dbg_t["y"].ap()[row0d:row0d + 128, :], in_=yt_[:]), reads=[("y", ysl, 0), ("y", ysl, 1)], dma=("dbgy", ysl)))
                    pb, pt = (ptr[1], ("ptr", 1))
                    for ec in range(8):
                        yield S.op("pe", lambda e, ec=ec, pb=pb, yt_=yt_: e.transpose(out=pb[:, ec * 128:(ec + 1) * 128], in_=yt_[:, ec * 128:(ec + 1) * 128], identity=ident_b[:]),
                             reads=[("y", ysl, 0), ("y", ysl, 1), "ident_b"], accw=[pt])
                    yT_ = yT[ysl]
                    yield S.op("act", lambda e, pb=pb, yT_=yT_: e.copy(out=yT_[:].rearrange("p c t -> p (c t)"), in_=pb[:]), reads=[pt], writes=[("yT", ysl)])
                    h1t = h1b[ysl]
                    for hf in range(2):
                        ph, pht = fbankC()
                        for ec in range(8):
                            yield S.op("pe", lambda e, ec=ec, ph=ph, hf=hf, yT_=yT_: e.matmul(out=ph[:], lhsT=yT_[:, ec, :], rhs=wout[:, ec, hf * 512:(hf + 1) * 512], start=(ec == 0), stop=(ec == 7)),
                                 reads=[("yT", ysl), "wout"], accw=[pht])
                        yield S.op("dve", lambda e, ph=ph, hf=hf, h1t=h1t: e.tensor_tensor(out=h1t[:, hf * 512:(hf + 1) * 512], in0=ph[:], in1=xb[:, j, hf * 512:(hf + 1) * 512], op=ALU.add),
                             reads=[pht, ("x", sl)], writes=[("h1", ysl, hf)])
                    row0 = st_i * 512 + j * 128
                    yield S.op("sp", lambda e, h1t=h1t, row0=row0: e.dma_start(out=h1buf.ap()[row0:row0 + 128, :], in_=h1t[:]), reads=[("h1", ysl, 0), ("h1", ysl, 1)], accw=["h1buf"], dma=("h1st", ysl))
                    if "h1" in dbg_t:
                        finals.append(S.op("sp", lambda e, h1t=h1t, row0=row0: e.dma_start(out=dbg_t["h1"].ap()[row0:row0 + 128, :], in_=h1t[:]), reads=[("h1", ysl, 0), ("h1", ysl, 1)], dma=("dbgh1", ysl)))
                    ti = st_i * 4 + j
                    x2 = xn2t[ysl]
                    yield S.op("dve", lambda e: e.memset(sm8[:, 5:6], 0.0), writes=["sm8d"])
                    yield S.op("act", lambda e: e.activation(out=junk[:], in_=h1t[:], func=AF.Square, accum_out=sm8[:, 5:6]), reads=[("h1", ysl, 0), ("h1", ysl, 1), "sm8d"], writes=["junk", "sm8d"])
                    yield S.op("act", lambda e: e.activation(out=sm8[:, 5:6], in_=sm8[:, 5:6], func=AF.Ln, scale=1.0 / D, bias=EPS), reads=["sm8d"], writes=["sm8d"])
                    yield S.op("act", lambda e: e.activation(out=sm8[:, 5:6], in_=sm8[:, 5:6], func=AF.Exp, scale=-0.5), reads=["sm8d"], writes=["sm8d"])
                    yield S.op("dve", lambda e: e.scalar_tensor_tensor(out=x2[:], in0=h1t[:], scalar=sm8[:, 5:6], in1=g2_b[:], op0=ALU.mult, op1=ALU.mult),
                         reads=[("h1", ysl, 0), ("h1", ysl, 1), "sm8d", "g2_b"], writes=[("xn2", ysl)])
                    yield S.op("sp", lambda e: e.dma_start(out=xn2lin.ap()[row0:row0 + 128, :], in_=x2[:]), reads=[("xn2", ysl)], accw=["xn2lin"], dma=("xn2st", ysl))
                    pb2, pt2 = (ptr[1], ("ptr", 1))
                    for kc in range(8):
                        yield S.op("pe", lambda e, kc=kc: e.transpose(out=pb2[:, kc * 128:(kc + 1) * 128], in_=x2[:, kc * 128:(kc + 1) * 128], identity=ident_b[:]),
                             reads=[("xn2", ysl), "ident_b"], accw=[pt2])
                    x2T = xn2T[0]
                    yield S.op("act", lambda e: e.copy(out=x2T[:].rearrange("p c t -> p (c t)"), in_=pb2[:]), reads=[pt2], writes=[("xn2T", 0)])
                    pl, plt = fbankC()
                    for kc in range(8):
                        yield S.op("pe", lambda e, kc=kc: e.matmul(out=pl[:, 0:36], lhsT=x2T[:, kc, :], rhs=wr_b[:, kc, :], start=(kc == 0), stop=(kc == 7)),
                             reads=[("xn2T", 0), "wr_b"], accw=[plt])
                    yield S.op("dve", lambda e: e.tensor_tensor(out=lg[:], in0=pl[:, 0:36], in1=br_b[:], op=ALU.add), reads=[plt, "br_b"], writes=["lg"])
                    R_ = lambda a, b: rt[:, a:b]
                    yield S.op("dve", lambda e: e.tensor_reduce(out=R_(0, 1), in_=lg[:, 0:4], axis=AX.X, op=ALU.max), reads=["lg"], writes=["rt"])
                    yield S.op("dve", lambda e: e.tensor_scalar(out=R_(12, 16), in0=lg[:, 0:4], scalar1=R_(0, 1), scalar2=None, op0=ALU.is_equal), reads=["lg", "rt"], writes=["rt"])
                    yield S.op("dve", lambda e: e.tensor_scalar(out=R_(1, 2), in0=R_(0, 1), scalar1=-1.0, scalar2=None, op0=ALU.mult), reads=["rt"], writes=["rt"])
                    yield S.op("dve", lambda e: e.memset(R_(2, 3), 0.0), reads=["rt"], writes=["rt"])
                    yield S.op("act", lambda e: e.activation(out=le8[:, 8:12], in_=lg[:, 0:4], func=AF.Exp, bias=R_(1, 2), accum_out=R_(2, 3)), reads=["lg", "rt"], writes=["rt", "le8x"])
                    yield S.op("dve", lambda e: e.reciprocal(out=R_(3, 4), in_=R_(2, 3)), reads=["rt"], writes=["rt"])
                    yield S.op("dve", lambda e: e.tensor_tensor(out=t32[:], in0=lg[:, 4:36].rearrange("p (g j) -> p g j", g=4), in1=R_(12, 16).unsqueeze(2).broadcast_to([128, 4, 8]), op=ALU.mult), reads=["lg", "rt"], writes=["t32"])
                    yield S.op("dve", lambda e: e.tensor_reduce(out=le8[:, 0:8], in_=t32[:].rearrange("p g j -> p j g"), axis=AX.X, op=ALU.add), reads=["t32"], writes=["le8"])
                    yield S.op("dve", lambda e: e.tensor_reduce(out=R_(4, 5), in_=le8[:, 0:8], axis=AX.X, op=ALU.max), reads=["le8", "rt"], writes=["rt"])
                    yield S.op("dve", lambda e: e.tensor_scalar(out=R_(16, 24), in0=le8[:, 0:8], scalar1=R_(4, 5), scalar2=None, op0=ALU.is_equal), reads=["le8", "rt"], writes=["rt"])
                    yield S.op("dve", lambda e: e.scalar_tensor_tensor(out=le8[:, 0:8], in0=R_(16, 24), scalar=-1e30, in1=le8[:, 0:8], op0=ALU.mult, op1=ALU.add), reads=["le8", "rt"], writes=["le8"])
                    yield S.op("dve", lambda e: e.tensor_reduce(out=R_(5, 6), in_=le8[:, 0:8], axis=AX.X, op=ALU.max), reads=["le8", "rt"], writes=["rt"])
                    yield S.op("dve", lambda e: e.tensor_scalar(out=R_(24, 32), in0=le8[:, 0:8], scalar1=R_(5, 6), scalar2=None, op0=ALU.is_equal), reads=["le8", "rt"], writes=["rt"])
                    yield S.op("dve", lambda e: e.tensor_tensor(out=R_(6, 7), in0=R_(5, 6), in1=R_(4, 5), op=ALU.subtract), reads=["rt"], writes=["rt"])
                    yield S.op("act", lambda e: e.activation(out=R_(6, 7), in_=R_(6, 7), func=AF.Exp), reads=["rt"], writes=["rt"])
                    yield S.op("dve", lambda e: e.tensor_scalar(out=R_(7, 8), in0=R_(6, 7), scalar1=1.0, scalar2=None, op0=ALU.add), reads=["rt"], writes=["rt"])
                    yield S.op("dve", lambda e: e.reciprocal(out=R_(7, 8), in_=R_(7, 8)), reads=["rt"], writes=["rt"])
                    yield S.op("dve", lambda e: e.tensor_tensor(out=R_(8, 9), in0=R_(6, 7), in1=R_(7, 8), op=ALU.mult), reads=["rt"], writes=["rt"])
                    yield S.op("dve", lambda e: e.tensor_scalar(out=pw[:, ti, 2:4], in0=R_(7, 9), scalar1=R_(3, 4), scalar2=None, op0=ALU.mult), reads=["rt"], accw=["pw"])
                    E1 = E1s[:, ti, :]
                    E2 = E2s[:, ti, :]
                    yield S.op("dve", lambda e: e.tensor_tensor(out=E1.rearrange("p (g j) -> p g j", g=4), in0=R_(12, 16).unsqueeze(2).broadcast_to([128, 4, 8]), in1=R_(16, 24).unsqueeze(1).broadcast_to([128, 4, 8]), op=ALU.mult), reads=["rt"], accw=["E1s"])
                    yield S.op("dve", lambda e: e.tensor_tensor(out=E2.rearrange("p (g j) -> p g j", g=4), in0=R_(12, 16).unsqueeze(2).broadcast_to([128, 4, 8]), in1=R_(24, 32).unsqueeze(1).broadcast_to([128, 4, 8]), op=ALU.mult), reads=["rt"], accw=["E2s"])
                    yield S.op("dve", lambda e: e.tensor_tensor(out=ind_b[:], in0=E1, in1=E2, op=ALU.add), reads=["E1s", "E2s"], writes=["ind_b"])
                    pp, ppt = fbankC()
                    yield S.op("pe", lambda e: e.matmul(out=pp[:, 0:32], lhsT=lstr_b[:], rhs=ind_b[:], start=True, stop=True), reads=["ind_b", "lstr_b"], accw=[ppt])
                    yield S.op("pe", lambda e: e.matmul(out=pp[:, 32:64], lhsT=ones_b[:], rhs=ind_b[:], start=True, stop=True), reads=["ind_b", "ones_b"], accw=[ppt])
                    yield S.op("dve", lambda e: e.tensor_tensor(out=posf[:], in0=pp[:, 0:32], in1=base_b[:], op=ALU.add), reads=[ppt, "base_b"], writes=["posf"])
                    yield S.op("dve", lambda e: e.tensor_tensor(out=base_b[:], in0=pp[:, 32:64], in1=base_b[:], op=ALU.add), reads=[ppt, "base_b"], writes=["base_b"])
                    yield S.op("dve", lambda e: e.tensor_tensor(out=t32[:].rearrange("p g j -> p (g j)"), in0=E1, in1=posf[:], op=ALU.mult), reads=["E1s", "posf"], writes=["t32"])
                    yield S.op("dve", lambda e: e.tensor_reduce(out=pw[:, ti, 0:1], in_=t32[:].rearrange("p g j -> p (g j)"), axis=AX.X, op=ALU.add), reads=["t32"], accw=["pw"])
                    yield S.op("dve", lambda e: e.tensor_tensor(out=t32[:].rearrange("p g j -> p (g j)"), in0=E2, in1=posf[:], op=ALU.mult), reads=["E2s", "posf", "pw"], writes=["t32"])
                    yield S.op("dve", lambda e: e.tensor_reduce(out=pw[:, ti, 1:2], in_=t32[:].rearrange("p g j -> p (g j)"), axis=AX.X, op=ALU.add), reads=["t32"], accw=["pw"])
                    yield None
                return chainA1(), chainA2(), (chainB() if main else None), (chainC() if main else None)

            def run_chains(gens):
                act = [g for g in gens if g is not None]
                while act:
                    for g in list(act):
                        try:
                            next(g)
                        except StopIteration:
                            act.remove(g)

            made = {}

            def get(j):
                if j not in made:
                    made[j] = list(tile_body(j))
                return made[j]

            act = {}
            done = set()
            nxt = {"A1": 0, "A2": 0, "B": 0, "C": 0}
            IDX = {"A1": 0, "A2": 1, "B": 2, "C": 3}

            def fin(kind, j):
                return j < 0 or (kind, j) in done

            def start(kind, j):
                g = get(j)[IDX[kind]]
                if g is None:
                    done.add((kind, j))
                else:
                    act[(kind, j)] = g
                nxt[kind] += 1

            def try_start():
                j = nxt["A1"]
                if j < 4 and fin("A1", j - 1) and fin("A2", j - 2):
                    start("A1", j)
                j = nxt["A2"]
                if j < 4 and fin("A1", j) and j < nxt["A1"] and fin("A2", j - 1) and fin("C", j - 2):
                    start("A2", j)
                j = nxt["B"]
                if j < 4 and j < nxt["A1"] and fin("B", j - 1) and fin("C", j - 2):
                    start("B", j)
                j = nxt["C"]
                if j < 4 and j < nxt["A1"] and fin("A2", j) and fin("B", j) and fin("C", j - 1) and j < nxt["A2"] and j < nxt["B"]:
                    start("C", j)

            ready = {}
            while len(done) < 16:
                try_start()
                if not act:
                    continue
                for k_ in act:
                    ready.setdefault(k_, 0.0)
                k_ = min(act.keys(), key=lambda q: ready[q])
                try:
                    next(act[k_])
                    ready[k_] = S.last_est
                except StopIteration:
                    del act[k_]
                    del ready[k_]
                    done.add(k_)
            if gi == 0 and "qkT" in dbg_t:
                tmpd = S.sb([128, 8, 512], F32, "sdbg_qk")
                S.op("dve", lambda e: e.tensor_copy(out=tmpd[:], in_=qk[:]), reads=[("qkT", 0)], writes=["dbg_qk"])
                finals.append(S.op("sp", lambda e: e.dma_start(out=dbg_t["qkT"].ap(), in_=tmpd[:].rearrange("p a b -> p (a b)")), reads=["dbg_qk"], dma=("dbg", "qkT")))
            if gi == 0 and "xT" in dbg_t:
                tmpx = S.sb([128, 8, 512], F32, "sdbg_xT")
                S.op("dve", lambda e: e.tensor_copy(out=tmpx[:], in_=xt[:]), reads=[("xT", 0)], writes=["dbg_xT"])
                finals.append(S.op("sp", lambda e: e.dma_start(out=dbg_t["xT"].ap(), in_=tmpx[:].rearrange("p a b -> p (a b)")), reads=["dbg_xT"], dma=("dbg", "xT")))

        wcat_bf = nc.dram_tensor("wcat_bf", [NE * 128, 12288], BF16)
        NSUP = NPRE + NST
        conv_rows = NE * 128
        conv_state = {"r": 0, "i": 0}

        def conv_some(gidx, sl):
            tgt = conv_rows * (gidx + 1) // NSUP
            while conv_state["r"] < tgt:
                r0 = conv_state["r"]
                r1 = min(r0 + 32, tgt)
                k = conv_state["i"] % 2
                conv_state["i"] += 1
                conv_state["r"] = r1
                S.op("pool", lambda e, r0=r0, r1=r1: e.dma_start(out=wcat_bf.ap()[r0:r1, :], in_=wcat.ap()[r0:r1, :]),
                     reads=[("x", sl)], writes=[("wck", k)], dma=("wck", k))

        NBLK0 = NT * 2 + NE
        xslots = nc.dram_tensor("xslots", [NBLK0 * 128, D], BF16)
        ztile = junk
        S.op("dve", lambda e: e.memset(ztile[:], 0.0), writes=["junk"])
        for zb in range(0, NBLK0, 8):
            nb_ = min(8, NBLK0 - zb)
            S.op("sp", lambda e, zb=zb, nb_=nb_: e.dma_start(out=xslots.ap()[zb * 128:(zb + nb_) * 128, :].rearrange("(j p) d -> p j d", p=128),
                                                             in_=ztile[:].unsqueeze(1).broadcast_to([128, nb_, 1024])),
                 reads=["junk"], accw=["xslots"], dma=("zinit", zb // 8))
        gi = 0
        for s_i in range(NPRE):
            supertile(x_pre, s_i, "prelast" if s_i == NPRE - 1 else "pre", gi)
            gi += 1
        for s_i in range(NST):
            supertile(x_main, s_i, "main", gi)
            gi += 1


        S.flush()
        stA.close()
        stB = contextlib.ExitStack()
        S.cur = stB
        NBLK = NT * 2 + NE
        NSL = NBLK * 128
        yslots = nc.dram_tensor("yslots", [NSL, D], F32)
        pl_ = S.sb([128, 6, 32], F32, "plan")
        pl_i = S.sb([128, 32], I32, "plan_i")
        p128 = S.sb([128, 1], F32, "p128")
        woff_f = S.sb([128, NBLK], F32, "woff_f")
        woff_i = S.sb([128, NBLK], I32, "woff_i")
        bval = S.sb([128, NBLK], F32, "bval")
        neq = S.sb([128, NBLK], F32, "neq")
        cmpb = S.sb([128, NBLK, 32], F32, "cmpb")
        dst_f = S.sb([128, 2, NT], F32, "dst_f")
        dst_i = S.sb([128, 2, NT], I32, "dst_i")
        big = S.sb([128, NT, 32], F32, "bigtmp")
        S.op("pool", lambda e: e.iota(p128[:], [[0, 1]], base=0, channel_multiplier=1, allow_small_or_imprecise_dtypes=True), writes=["p128"])
        S.op("dve", lambda e: e.tensor_scalar(out=pl_[:, 3, :], in0=base_b[:], scalar1=1.0 / 128, scalar2=63.5 / 128, op0=ALU.mult, op1=ALU.add), reads=["base_b"], writes=["plan"])
        S.op("dve", lambda e: e.tensor_copy(out=pl_i[:], in_=pl_[:, 3, :]), reads=["plan"], writes=["plan_i"])
        S.op("dve", lambda e: e.tensor_copy(out=pl_[:, 0, :], in_=pl_i[:]), reads=["plan_i", "plan"], writes=["plan"])
        S.op("dve", lambda e: e.tensor_scalar(out=pl_[:, 0, :], in0=pl_[:, 0, :], scalar1=128.0, scalar2=None, op0=ALU.mult), reads=["plan"], writes=["plan"])
        S.op("dve", lambda e: e.tensor_tensor_scan(out=pl_[:, 1, :], data0=pl_[:, 0, :], data1=pl_[:, 0, :], initial=0.0, op0=ALU.add, op1=ALU.bypass), reads=["plan"], writes=["plan"])
        S.op("dve", lambda e: e.tensor_tensor(out=pl_[:, 2, :], in0=pl_[:, 1, :], in1=pl_[:, 0, :], op=ALU.subtract), reads=["plan"], writes=["plan"])
        S.op("pool", lambda e: e.iota(bval[:], [[128, NBLK]], base=0, channel_multiplier=0, allow_small_or_imprecise_dtypes=True), writes=["bval"])
        S.op("dve", lambda e: e.tensor_tensor(out=cmpb[:], in0=pl_[:, 1, :].unsqueeze(1).broadcast_to([128, NBLK, 32]), in1=bval[:].unsqueeze(2).broadcast_to([128, NBLK, 32]), op=ALU.is_le), reads=["plan", "bval"], writes=["cmpb"])
        S.op("dve", lambda e: e.tensor_reduce(out=woff_f[:], in_=cmpb[:], axis=AX.X, op=ALU.add), reads=["cmpb"], writes=["woff_f"])
        BIGI = 1000000.0
        S.op("dve", lambda e: e.tensor_scalar(out=woff_f[:], in0=woff_f[:], scalar1=31.0, scalar2=None, op0=ALU.min), reads=["woff_f"], writes=["woff_f"])
        S.op("dve", lambda e: e.memset(neq[:, 0:2], 1.0), writes=["neq0"])
        S.op("dve", lambda e: e.tensor_tensor(out=neq[:, 2:NBLK], in0=woff_f[:, 2:NBLK], in1=woff_f[:, 0:NBLK - 2], op=ALU.not_equal), reads=["woff_f"], writes=["neq"])
        S.op("dve", lambda e: e.tensor_scalar(out=woff_f[:], in0=woff_f[:], scalar1=128.0, scalar2=-BIGI, op0=ALU.mult, op1=ALU.add), reads=["woff_f", "neq"], writes=["woff_f"])
        S.op("dve", lambda e: e.tensor_scalar(out=woff_f[:], in0=woff_f[:], scalar1=p128[:, 0:1], scalar2=None, op0=ALU.add), reads=["woff_f", "p128"], writes=["woff_f"])
        S.op("dve", lambda e: e.tensor_tensor(out=woff_f[:], in0=woff_f[:], in1=neq[:], op=ALU.mult), reads=["woff_f", "neq", "neq0"], writes=["woff_f"])
        S.op("dve", lambda e: e.tensor_scalar(out=woff_f[:], in0=woff_f[:], scalar1=BIGI, scalar2=None, op0=ALU.add), reads=["woff_f"], writes=["woff_f"])
        S.op("dve", lambda e: e.tensor_copy(out=woff_i[:], in_=woff_f[:]), reads=["woff_f"], writes=["woff_i"])
        for k_, Es in ((0, E1s), (1, E2s)):
            S.op("dve", lambda e, Es=Es: e.tensor_tensor(out=big[:], in0=Es[:], in1=pl_[:, 2, :].unsqueeze(1).broadcast_to([128, NT, 32]), op=ALU.mult), reads=["E1s", "E2s", "plan"], writes=["big"])
            S.op("dve", lambda e, k_=k_: e.tensor_reduce(out=dst_f[:, k_, :], in_=big[:], axis=AX.X, op=ALU.add), reads=["big"], writes=[("dst_f", k_)])
            S.op("dve", lambda e, k_=k_: e.tensor_tensor(out=dst_f[:, k_, :], in0=dst_f[:, k_, :], in1=pw[:, :, k_], op=ALU.add), reads=[("dst_f", k_), "pw"], writes=[("dst_f", k_)])
        S.op("dve", lambda e: e.tensor_copy(out=dst_i[:], in_=dst_f[:]), reads=[("dst_f", 0), ("dst_f", 1)], writes=["dst_i"])
        if "plan" in dbg_t:
            finals.append(S.op("sp", lambda e: e.dma_start(out=dbg_t["plan"].ap()[:, 0:192], in_=pl_[:].rearrange("p a b -> p (a b)")), reads=["plan"], dma=("dbg", "plan")))
            finals.append(S.op("sp", lambda e: e.dma_start(out=dbg_t["plan"].ap()[:, 768:768 + NBLK], in_=woff_f[:]), reads=["woff_f"], dma=("dbg", "plan2")))
            finals.append(S.op("sp", lambda e: e.dma_start(out=dbg_t["plan"].ap()[:, 256:256 + 2 * NT], in_=dst_f[:].rearrange("p a b -> p (a b)")), reads=[("dst_f", 0), ("dst_f", 1)], dma=("dbg", "plan3")))
            finals.append(S.op("sp", lambda e: e.dma_start(out=dbg_t["plan"].ap()[:, 512:512 + 4 * NT], in_=pw[:].rearrange("p a b -> p (a b)")), reads=["pw"], dma=("dbg", "plan4")))
        xsc = [S.sb([128, 1024], BF16, f"xsc{i}") for i in range(2)]
        for ti in range(NT):
            bsl = ti % 2
            S.op("sp", lambda e, ti=ti, bsl=bsl: e.dma_start(out=xsc[bsl][:], in_=xn2lin.ap()[ti * 128:(ti + 1) * 128, :]), reads=["xn2lin"], writes=[("xsc", bsl)], dma=("xsc", bsl))
            for k_ in range(2):
                S.op("pool", lambda e, ti=ti, bsl=bsl, k_=k_: e.indirect_dma_start(out=xslots.ap(), out_offset=bass.IndirectOffsetOnAxis(ap=dst_i[:, k_, ti:ti + 1], axis=0), in_=xsc[bsl][:], in_offset=None),
                     reads=[("xsc", bsl), "dst_i"], accw=["xslots"], dma=("scat", bsl, k_))

        wbuf = [S.sb([128, 12288], BF16, f"wbuf{i}") for i in range(2)]
        xs_b = [S.sb([128, 1024], BF16, f"xs_b{i}") for i in range(4)]
        xsT = [S.sb([128, 8, 128], BF16, f"xsT{i}") for i in range(2)]
        eg = [S.sb([128, 512], F32, f"eg{i}") for i in range(2)]
        hid = [S.sb([128, 512], BF16, f"hid{i}") for i in range(2)]
        hidT = [S.sb([128, 4, 128], BF16, f"hidT{i}") for i in range(2)]
        ysb = [S.sb([128, 1024], F32, f"ysb{i}") for i in range(2)]
        regs = {}

        def wgather(e, b, ws):
            if "bnd" not in regs:
                regs["bnd"] = st.enter_context(e.register("wbnd"))
                e.reg_mov(regs["bnd"], NE * 128 - 1)
            return e.indirect_dma_start(out=wbuf[ws][:], out_offset=None, in_=wcat_bf.ap(), in_offset=bass.IndirectOffsetOnAxis(ap=woff_i[:, b:b + 1], axis=0),
                                        bounds_check=regs["bnd"], oob_is_err=False)

        mrr = [0, 0]

        def fbankM(p):
            i = 3 * p + mrr[p] % 3
            mrr[p] += 1
            return pfb[i], ("pf", i)

        def blk(b):
            ws = b % 2
            yield S.op("pool", lambda e, b=b, ws=ws: wgather(e, b, ws), reads=["woff_i"], writes=[("wb", ws)], dma=("wb", ws))
            if b < 2:
                yield S.op("sp", lambda e, b=b: e.dma_start(out=xs_b[b % 4][:], in_=xslots.ap()[b * 128:(b + 1) * 128, :]), reads=["xslots"], writes=[("xs", b % 4)], dma=("xs", b % 4))
            if b + 2 < NBLK:
                yield S.op("sp", lambda e, b=b: e.dma_start(out=xs_b[(b + 2) % 4][:], in_=xslots.ap()[(b + 2) * 128:(b + 3) * 128, :]), reads=["xslots"], writes=[("xs", (b + 2) % 4)], dma=("xs", (b + 2) % 4))
            xq = b % 4
            pb, pt = (ptr[ws], ("ptr", ws))
            for kc in range(8):
                yield S.op("pe", lambda e, kc=kc, pb=pb, xq=xq: e.transpose(out=pb[:, kc * 128:(kc + 1) * 128], in_=xs_b[xq][:, kc * 128:(kc + 1) * 128], identity=ident_b[:]),
                     reads=[("xs", xq), "ident_b"], accw=[pt])
            yield S.op("act", lambda e, pb=pb, ws=ws: e.copy(out=xsT[ws][:].rearrange("p c t -> p (c t)"), in_=pb[:]), reads=[pt], writes=[("xsT", ws)])
            pG, pGt = fbankM(ws)
            pU2, pU2t = fbankM(ws)
            for kc in range(8):
                yield S.op("pe", lambda e, kc=kc, pG=pG, ws=ws: e.matmul(out=pG[:], lhsT=xsT[ws][:, kc, :], rhs=wbuf[ws][:, kc * 1024:kc * 1024 + 512], start=(kc == 0), stop=(kc == 7)),
                     reads=[("xsT", ws), ("wb", ws)], accw=[pGt])
            for kc in range(8):
                yield S.op("pe", lambda e, kc=kc, pU2=pU2, ws=ws: e.matmul(out=pU2[:], lhsT=xsT[ws][:, kc, :], rhs=wbuf[ws][:, kc * 1024 + 512:(kc + 1) * 1024], start=(kc == 0), stop=(kc == 7)),
                     reads=[("xsT", ws), ("wb", ws)], accw=[pU2t])
            yield S.op("act", lambda e, pG=pG, ws=ws: e.activation(out=eg[ws][:], in_=pG[:], func=AF.Exp, scale=-1.0), reads=[pGt], writes=[("eg", ws)])
            yield S.op("act", lambda e, ws=ws: e.activation(out=eg[ws][:], in_=eg[ws][:], func=AF.Ln, bias=1.0), reads=[("eg", ws)], writes=[("eg", ws)])
            yield S.op("act", lambda e, ws=ws: e.activation(out=eg[ws][:], in_=eg[ws][:], func=AF.Exp, scale=-1.0), reads=[("eg", ws)], writes=[("eg", ws)])
            yield S.op("dve", lambda e, pG=pG, ws=ws: e.tensor_tensor(out=eg[ws][:], in0=eg[ws][:], in1=pG[:], op=ALU.mult), reads=[("eg", ws), pGt], writes=[("eg", ws)])
            yield S.op("dve", lambda e, pU2=pU2, ws=ws: e.tensor_tensor(out=hid[ws][:], in0=eg[ws][:], in1=pU2[:], op=ALU.mult), reads=[("eg", ws), pU2t], writes=[("hid", ws)])
            pb, pt = (ptr[ws], ("ptr", ws))
            for fc in range(4):
                yield S.op("pe", lambda e, fc=fc, pb=pb, ws=ws: e.transpose(out=pb[:, fc * 128:(fc + 1) * 128], in_=hid[ws][:, fc * 128:(fc + 1) * 128], identity=ident_b[:]),
                     reads=[("hid", ws), "ident_b"], accw=[pt])
            yield S.op("act", lambda e, pb=pb, ws=ws: e.copy(out=hidT[ws][:].rearrange("p c t -> p (c t)"), in_=pb[:, 0:512]), reads=[pt], writes=[("hidT", ws)])
            for hf in range(2):
                pY, pYt = fbankM(ws)
                for fc in range(4):
                    yield S.op("pe", lambda e, fc=fc, pY=pY, ws=ws, hf=hf: e.matmul(out=pY[:], lhsT=hidT[ws][:, fc, :], rhs=wbuf[ws][:, 8192 + fc * 1024 + hf * 512:8192 + fc * 1024 + (hf + 1) * 512], start=(fc == 0), stop=(fc == 3)),
                         reads=[("hidT", ws), ("wb", ws)], accw=[pYt])
                if hf == 0:
                    yield S.op("act", lambda e, pY=pY, ws=ws: e.copy(out=ysb[ws][:, 0:512], in_=pY[:]), reads=[pYt], writes=[("ysb", ws, 0)])
                else:
                    yield S.op("dve", lambda e, pY=pY, ws=ws: e.tensor_copy(out=ysb[ws][:, 512:1024], in_=pY[:]), reads=[pYt], writes=[("ysb", ws, 1)])
            yield S.op("sp", lambda e, b=b, ws=ws: e.dma_start(out=yslots.ap()[b * 128:(b + 1) * 128, :], in_=ysb[ws][:]), reads=[("ysb", ws, 0), ("ysb", ws, 1)], accw=["yslots"], dma=("yst", ws))


            yield None

        for b in range(NBLK):
            for _ in blk(b):
                pass

        fg_b = bload("fg_b", final_g, 1024)
        NCB = 3
        hc = [S.sb([128, 1024], F32, f"hc{i}") for i in range(NCB)]
        y1 = [S.sb([128, 1024], F32, f"y1_{i}") for i in range(NCB)]
        y2 = [S.sb([128, 1024], F32, f"y2_{i}") for i in range(NCB)]
        fs = S.sb([128, 2], F32, "fs")

        def cloads(ti):
            cs = ti % NCB
            S.op("sp", lambda e, ti=ti, cs=cs: e.dma_start(out=hc[cs][:], in_=h1buf.ap()[ti * 128:(ti + 1) * 128, :]), reads=["h1buf"], writes=[("hc", cs)], dma=("hc", cs))
            S.op("pool", lambda e, ti=ti, cs=cs: e.indirect_dma_start(out=y1[cs][:], out_offset=None, in_=yslots.ap(), in_offset=bass.IndirectOffsetOnAxis(ap=dst_i[:, 0, ti:ti + 1], axis=0)),
                 reads=["yslots", "dst_i"], writes=[("y1", cs)], dma=("y1", cs))
            S.op("pool", lambda e, ti=ti, cs=cs: e.indirect_dma_start(out=y2[cs][:], out_offset=None, in_=yslots.ap(), in_offset=bass.IndirectOffsetOnAxis(ap=dst_i[:, 1, ti:ti + 1], axis=0)),
                 reads=["yslots", "dst_i"], writes=[("y2", cs)], dma=("y2", cs))

        for ti in range(min(NCB - 1, NT)):
            cloads(ti)
        for ti in range(NT):
            cs = ti % NCB
            if ti + NCB - 1 < NT:
                cloads(ti + NCB - 1)
            S.op("dve", lambda e, ti=ti, cs=cs: e.scalar_tensor_tensor(out=hc[cs][:], in0=y1[cs][:], scalar=pw[:, ti, 2:3], in1=hc[cs][:], op0=ALU.mult, op1=ALU.add), reads=[("hc", cs), ("y1", cs), "pw"], writes=[("hc", cs)])
            S.op("dve", lambda e, ti=ti, cs=cs: e.scalar_tensor_tensor(out=hc[cs][:], in0=y2[cs][:], scalar=pw[:, ti, 3:4], in1=hc[cs][:], op0=ALU.mult, op1=ALU.add), reads=[("hc", cs), ("y2", cs), "pw"], writes=[("hc", cs)])
            S.op("dve", lambda e, ti=ti: e.memset(fs[:, (ti % 2):(ti % 2) + 1], 0.0), writes=[("fs", ti % 2)])
            S.op("act", lambda e, ti=ti, cs=cs: e.activation(out=junk[:], in_=hc[cs][:], func=AF.Square, accum_out=fs[:, (ti % 2):(ti % 2) + 1]), reads=[("hc", cs), ("fs", ti % 2)], writes=["junk", ("fs", ti % 2)])
            S.op("act", lambda e, ti=ti: e.activation(out=fs[:, (ti % 2):(ti % 2) + 1], in_=fs[:, (ti % 2):(ti % 2) + 1], func=AF.Ln, scale=1.0 / D, bias=EPS), reads=[("fs", ti % 2)], writes=[("fs", ti % 2)])
            S.op("act", lambda e, ti=ti: e.activation(out=fs[:, (ti % 2):(ti % 2) + 1], in_=fs[:, (ti % 2):(ti % 2) + 1], func=AF.Exp, scale=-0.5), reads=[("fs", ti % 2)], writes=[("fs", ti % 2)])
            S.op("dve", lambda e, ti=ti, cs=cs: e.scalar_tensor_tensor(out=y1[cs][:], in0=hc[cs][:], scalar=fs[:, (ti % 2):(ti % 2) + 1], in1=fg_b[:], op0=ALU.mult, op1=ALU.mult), reads=[("hc", cs), ("fs", ti % 2), "fg_b", ("y1", cs)], writes=[("y1", cs)])
            finals.append(S.op("sp", lambda e, ti=ti, cs=cs: e.dma_start(out=out.ap()[ti * 128:(ti + 1) * 128, :], in_=y1[cs][:]), reads=[("y1", cs)], dma=("ost", cs)))

        S.flush()
        stB.close()
    return nc


def make_wcat(w_gate, w_up, w_down):
    g = w_gate.reshape(NE, 8, 128, 512).transpose(0, 2, 1, 3)
    u = w_up.reshape(NE, 8, 128, 512).transpose(0, 2, 1, 3)
    gu = np.concatenate([g, u], axis=3).reshape(NE, 128, 8192)
    dn = w_down.reshape(NE, 4, 128, 1024).transpose(0, 2, 1, 3).reshape(NE, 128, 4096)
    return np.ascontiguousarray(np.concatenate([gu, dn], axis=2).reshape(NE * 128, 12288))


def kernel(**inputs):
    f = lambda k: np.ascontiguousarray(np.asarray(inputs[k], dtype=np.float32))
    x = f("x")
    com = {
        "norm1_g": f("norm1_g")[0], "w_in": f("w_in")[0], "conv_qk": f("conv_qk")[0],
        "b_if": np.concatenate([f("b_igate")[0], f("b_fgate")[0]]), "g_mlstm_out": f("g_mlstm_out")[0],
        "g_gmlp_v": f("g_gmlp_v")[0], "w_spatial": f("w_spatial")[0], "b_spatial": f("b_spatial")[0],
        "g_gmlp_out": f("g_gmlp_out")[0], "w_out": f("w_out")[0], "norm2_g": f("norm2_g")[0],
        "w_router": np.ascontiguousarray(np.concatenate([f("w_router_group")[0], f("w_router_expert")[0]], axis=1)),
        "b_router": np.concatenate([f("b_router_group")[0], f("b_router_expert")[0]]),
        "wcat": make_wcat(f("w_gate")[0], f("w_up")[0], f("w_down")[0]), "final_g": f("final_g"),
    }
    in_maps = []
    for c in range(8):
        b, half = c // 2, c % 2
        m = dict(com)
        m["x_main"] = np.ascontiguousarray(x[b, half * 4096:(half + 1) * 4096])
        m["x_pre"] = np.ascontiguousarray(x[b, 0:4096]) if half == 1 else np.zeros((4096, D), np.float32)
        in_maps.append(m)
    nc = build(8, 8)
    res = run_bass_kernel_spmd(nc, in_maps, core_ids=list(range(8)))
    out = np.empty((4, 8192, D), np.float32)
    for c in range(8):
        out[c // 2, (c % 2) * 4096:(c % 2 + 1) * 4096] = res.results[c]["out"]
    return out
```

```python
import contextlib
import numpy as np
import concourse.bass as bass
import concourse.mybir as mybir
from concourse.bass_utils import run_bass_kernel_spmd

F32 = mybir.dt.float32
BF16 = mybir.dt.bfloat16
I32 = mybir.dt.int32
AF = mybir.ActivationFunctionType
ALU = mybir.AluOpType
AX = mybir.AxisListType

ENGS = ("pe", "act", "dve", "pool", "sp")
SEM_CH = 8000
D = 1024
EPS = 1e-6
NE = 32
NBLK_MAX = 96
NSLOT = NBLK_MAX * 128


class Sched:
    def __init__(self, nc, stack):
        self.nc = nc
        self.stack = stack
        self.ops = []
        self.last_w = {}
        self.readers = {}
        self.dma_count = {}
        self.nbuf = 0
        self.flushed = 0
        self.sems = {}
        self.eng_seq = {e: 0 for e in ENGS}
        self.waited = {e: {} for e in ENGS}
        self.cur = stack
        self.est_end = []
        self.eng_free = {e: 0.0 for e in ENGS}
        self.last_est = 0.0
        self.know = {}

    def sb(self, shape, dt, name=None, persist=False):
        self.nbuf += 1
        return (self.stack if persist else self.cur).enter_context(self.nc.sbuf_tensor(name or f"sb{self.nbuf}", list(shape), dt))

    def ps(self, shape, dt, name=None):
        self.nbuf += 1
        return self.stack.enter_context(self.nc.psum_tensor(name or f"ps{self.nbuf}", list(shape), dt))

    def op(self, eng, fn, reads=(), writes=(), accw=(), dma=None):
        i = len(self.ops)
        deps = set()
        for t in reads:
            deps.update(self.last_w.get(t, ()))
        for t in writes:
            deps.update(self.last_w.get(t, ()))
            deps.update(self.readers.get(t, ()))
        for t in accw:
            deps.update(self.readers.get(t, ()))
        for t in reads:
            self.readers.setdefault(t, []).append(i)
        for t in writes:
            self.last_w[t] = [i]
            self.readers[t] = []
        for t in accw:
            if self.readers.get(t):
                self.last_w[t] = []
                self.readers[t] = []
            self.last_w.setdefault(t, []).append(i)
        deps.discard(i)
        self.ops.append(dict(eng=eng, fn=fn, deps=sorted(deps), dma=dma, sig=None))
        cost = 2.5 if dma is not None else {"pe": 0.25, "act": 0.7, "dve": 0.6, "pool": 1.0, "sp": 0.1}[eng]
        t0 = max([self.eng_free[eng]] + [self.est_end[d] for d in deps if d < len(self.est_end)])
        if dma is not None:
            self.eng_free[eng] = t0 + 0.1
        else:
            self.eng_free[eng] = t0 + cost
        self.est_end.append(t0 + cost)
        self.last_est = t0 + cost
        return i

    def flush(self):
        nc = self.nc
        ops = self.ops
        lo = self.flushed
        last = {}
        for i in range(lo, len(ops)):
            o = ops[i]
            key = ("dma", o["dma"]) if o["dma"] is not None else ("eng", o["eng"])
            last[key] = i
        bdeps = sorted(last.values())
        for en in ENGS:
            self.ops.append(dict(eng=en, fn=lambda e: e.nop(), deps=list(bdeps), dma=None, sig=None, barrier=True))
            self.est_end.append(max(self.est_end) if self.est_end else 0.0)
        hi = len(ops)

        def pe_pair(a, b):
            return (a["eng"] == "pe" and b["eng"] == "pe" and a["dma"] is None and b["dma"] is None
                    and not b.get("barrier"))

        needed = set()
        for i in range(lo, hi):
            o = ops[i]
            for d in o["deps"]:
                if pe_pair(ops[d], o):
                    continue
                needed.add(d)
        for i in range(lo, hi):
            o = ops[i]
            if o["dma"] is not None:
                k = ("dma", o["dma"])
                self.dma_count[k] = self.dma_count.get(k, 0) + 1
                o["sig"] = (k, 16 * self.dma_count[k])
                self.get_sem(k)
            elif i in needed:
                e = o["eng"]
                n = self.eng_seq[e]
                self.eng_seq[e] += 1
                k = ("eng", e, n // SEM_CH)
                o["sig"] = (k, n % SEM_CH + 1)
                self.get_sem(k)
        sems = self.sems
        waited_all = self.waited

        plan = {}
        for i in range(lo, hi):
            o = ops[i]
            kn = waited_all[o["eng"]]
            wl = []
            for d in o["deps"]:
                od = ops[d]
                if od["sig"] is None or pe_pair(od, o):
                    continue
                k, v = od["sig"]
                if kn.get(k, 0) >= v:
                    continue
                wl.append((k, v))
                kn[k] = v
                for k2, v2 in self.know.get(d, {}).items():
                    if kn.get(k2, 0) < v2:
                        kn[k2] = v2
            plan[i] = wl
            if o["sig"] is not None:
                self.know[i] = dict(kn)

        def run(engname):
            def body(e):
                for i in range(lo, hi):
                    o = ops[i]
                    if o["eng"] != engname:
                        continue
                    for k, v in plan[i]:
                        e.wait_ge(sems[k], v)
                    ins = o["fn"](e)
                    if o["sig"] is not None:
                        k, v = o["sig"]
                        ins.then_inc(sems[k], 16 if o["dma"] is not None else 1)
            return body

        with nc.Block() as block:
            block.tensor(run("pe"))
            block.scalar(run("act"))
            block.vector(run("dve"))
            block.gpsimd(run("pool"))
            block.sync(run("sp"))
        self.flushed = hi
        self.last_w = {}
        self.readers = {}

    def get_sem(self, key):
        if key not in self.sems:
            self.sems[key] = self.stack.enter_context(self.nc.semaphore(f"s_{len(self.sems)}"))
        return self.sems[key]


def build(NST=8, NPRE=8, dbg=None):
    nc = bass.Bass("TRN2", target_bir_lowering=False)
    NT = NST * 4
    TOK = NST * 512

    def din(name, shape, dt=F32):
        return nc.dram_tensor(name, list(shape), dt, kind="ExternalInput")

    x_main = din("x_main", [TOK, D])
    x_pre = din("x_pre", [max(NPRE, 1) * 512, D])
    norm1_g = din("norm1_g", [D])
    w_in = din("w_in", [D, 3080])
    conv_qk = din("conv_qk", [4, 1024])
    b_if = din("b_if", [8])
    g_mlstm = din("g_mlstm_out", [512])
    g_gv = din("g_gmlp_v", [512])
    w_sp = din("w_spatial", [8, 128, 128])
    b_sp = din("b_spatial", [8, 128])
    g_go = din("g_gmlp_out", [512])
    w_out = din("w_out", [D, D])
    norm2_g = din("norm2_g", [D])
    w_r = din("w_router", [D, 36])
    b_r = din("b_router", [36])
    wcat = din("wcat", [NE * 128, 12288])
    final_g = din("final_g", [D])
    out = nc.dram_tensor("out", [TOK, D], F32, kind="ExternalOutput")
    h1buf = nc.dram_tensor("h1buf", [TOK, D], F32)
    dbg_t = {}
    if dbg:
        for k, shp in dbg.items():
            dbg_t[k] = nc.dram_tensor("dbg_" + k, list(shp), F32, kind="ExternalOutput")

    with contextlib.ExitStack() as st:
        S = Sched(nc, st)
        finals = []

        ident_f = S.sb([128, 128], F32, "ident_f")
        ident_b = S.sb([128, 128], BF16, "ident_b")
        triu_f = S.sb([128, 128], F32, "triu_f")
        triu_b = S.sb([128, 128], BF16, "triu_b")
        tril_f = S.sb([128, 128], F32, "tril_f")
        ones_f = S.sb([128, 128], F32, "ones_f")
        S.op("pool", lambda e: e.memset(ident_f[:], 0.0), writes=["ident_f"])
        S.op("pool", lambda e: e.affine_select(out=ident_f[:], in_=ident_f[:], pattern=[[-1, 128]],
                                               compare_op=ALU.not_equal, fill=1.0, base=0, channel_multiplier=1),
             reads=["ident_f"], writes=["ident_f"])
        S.op("pool", lambda e: e.memset(ones_f[:], 1.0), writes=["ones_f"])
        S.op("pool", lambda e: e.affine_select(out=triu_f[:], in_=ones_f[:], pattern=[[1, 128]],
                                               compare_op=ALU.is_ge, fill=0.0, base=0, channel_multiplier=-1),
             reads=["ones_f"], writes=["triu_f"])
        S.op("pool", lambda e: e.affine_select(out=tril_f[:], in_=ones_f[:], pattern=[[-1, 128]],
                                               compare_op=ALU.is_ge, fill=0.0, base=0, channel_multiplier=1),
             reads=["ones_f"], writes=["tril_f"])
        S.op("dve", lambda e: e.tensor_copy(out=ident_b[:], in_=ident_f[:]), reads=["ident_f"], writes=["ident_b"])
        S.op("dve", lambda e: e.tensor_copy(out=triu_b[:], in_=triu_f[:]), reads=["triu_f"], writes=["triu_b"])

        def bload(name, src, n, eng="sp"):
            t = S.sb([128, n], F32, name)
            S.op(eng, lambda e: e.dma_start(out=t[:], in_=bass.AP(src, 0, [[0, 128], [1, n]])),
                 writes=[name], dma=name)
            return t

        gml_b = bload("gml_b", g_mlstm, 512)
        ggv_b = bload("ggv_b", g_gv, 512)
        ggo_b = bload("ggo_b", g_go, 512)
        bif_b = bload("bif_b", b_if, 8)

        g1col = S.sb([128, 8], F32, "g1col")
        cw = S.sb([128, 4, 8], F32, "cw")
        bsp = S.sb([128, 8], F32, "bsp")
        S.op("sp", lambda e: e.dma_start(out=g1col[:], in_=norm1_g.ap().rearrange("(c p) -> p c", p=128),
                                         allow_slow_non_contiguous=True), writes=["g1col"], dma="g1col")
        for i in range(4):
            S.op("sp", lambda e, i=i: e.dma_start(out=cw[:, i, :], in_=conv_qk.ap()[i, :].rearrange("(c p) -> p c", p=128),
                                                  allow_slow_non_contiguous=True), accw=["cw"], dma=("cw", i))
        S.op("sp", lambda e: e.dma_start(out=bsp[:], in_=b_sp.ap().rearrange("g t -> t g"),
                                         allow_slow_non_contiguous=True), writes=["bsp"], dma="bsp")

        ptr = [S.ps([128, 1024], BF16, f"ptr{i}") for i in range(2)]
        pfb = [S.ps([128, 512], F32, f"pf{i}") for i in range(6)]
        rr = {"t": 0, "f": 0, "A": 0, "B": 0, "A2": 0}

        def tbank():
            i = rr["t"] % 2
            rr["t"] += 1
            return ptr[i], ("ptr", i)

        def fbank():
            i = rr["f"] % 6
            rr["f"] += 1
            return pfb[i], ("pf", i)

        def fbankA():
            return pfb[0], ("pf", 0)

        def fbankA2():
            i = 1 + rr["A2"] % 2
            rr["A2"] += 1
            return pfb[i], ("pf", i)

        def fbankB():
            i = 3 + rr["B"] % 2
            rr["B"] += 1
            return pfb[i], ("pf", i)

        def fbankC():
            return pfb[5], ("pf", 5)

        junk = S.sb([128, 1024], BF16, "junk", persist=True)
        base_b = S.sb([128, 32], F32, "base_b", persist=True)
        E1s = S.sb([128, NT, 32], BF16, "E1s", persist=True)
        E2s = S.sb([128, NT, 32], BF16, "E2s", persist=True)
        pw = S.sb([128, NT, 4], F32, "pw", persist=True)
        stA = contextlib.ExitStack()
        S.cur = stA
        xbuf = [S.sb([128, 4, 1024], F32, f"xbuf{i}") for i in range(2)]
        wqk = S.sb([128, 8, 1024], BF16, "wqk")
        wvo = S.sb([128, 8, 1024], BF16, "wvo")
        wuv = S.sb([128, 8, 1024], BF16, "wuv")
        wif = S.sb([128, 8, 8], BF16, "wif")
        wout = S.sb([128, 8, 1024], BF16, "wout")
        for kc in range(8):
            sl = kc % 2
            stg = xbuf[sl][:].rearrange("p a b -> p (a b)")
            S.op("sp", lambda e, stg=stg, kc=kc: e.dma_start(out=stg[:, 0:3080], in_=w_in.ap()[kc * 128:(kc + 1) * 128, :]),
                 writes=[("x", sl)], dma=("x", sl))
            for (dst, c0, n, tok) in ((wqk, 0, 1024, "wqk"), (wvo, 1024, 1024, "wvo"), (wif, 2048, 8, "wif"), (wuv, 2056, 1024, "wuv")):
                S.op("act", lambda e, dst=dst, c0=c0, n=n, kc=kc, stg=stg: e.mul(dst[:, kc, 0:n], stg[:, c0:c0 + n], g1col[:, kc:kc + 1]),
                     reads=[("x", sl), "g1col"], accw=[tok])
        S.op("pool", lambda e: e.dma_start(out=wout[:], in_=w_out.ap().rearrange("(c p) n -> p c n", p=128)),
             writes=["wout"], dma="wout")

        wsp_f = xbuf[1][:, 3, :].rearrange("p (g s) -> p g s", g=8)
        wspT = S.sb([128, 8, 128], BF16, "wspT")
        S.op("sp", lambda e: e.dma_start(out=wsp_f, in_=w_sp.ap().rearrange("g t s -> t g s")), writes=[("x", 1)], dma=("x", 1))
        S.op("dve", lambda e: e.tensor_tensor(out=wsp_f, in0=wsp_f, in1=tril_f[:].unsqueeze(1).broadcast_to([128, 8, 128]), op=ALU.mult),
             reads=[("x", 1), "tril_f"], writes=[("x", 1)])
        for half in range(2):
            pb, pt = fbank()
            for g4 in range(4):
                g = half * 4 + g4
                S.op("pe", lambda e, pb=pb, g=g, g4=g4: e.transpose(out=pb[:, g4 * 128:(g4 + 1) * 128], in_=wsp_f[:, g, :], identity=ident_f[:]),
                     reads=[("x", 1), "ident_f"], accw=[pt])
            S.op("dve", lambda e, pb=pb, half=half: e.tensor_copy(out=wspT[:, half * 4:(half + 1) * 4, :].rearrange("p a b -> p (a b)"), in_=pb[:]),
                 reads=[pt], accw=["wspT"])

        C32 = S.sb([128, 4, 129], F32, "C32")
        Csb = S.sb([128, 4, 129], BF16, "Csb")
        m_st = S.sb([4, 1], F32, "m_st")
        halo = S.sb([128, 8, 3], F32, "halo")
        S.op("pool", lambda e: e.memset(C32[:], 0.0), writes=[("C32", h) for h in range(4)])
        S.op("pool", lambda e: e.memset(m_st[:], 0.0), writes=["m_st"])
        S.op("pool", lambda e: e.memset(halo[:], 0.0), writes=[("halo", c) for c in range(8)])

        xn = [S.sb([128, 1024], BF16, f"xn{i}") for i in range(2)]
        ss1 = S.sb([128, 8], F32, "ss1")
        xT = [S.sb([128, 8, 512], BF16, f"xT{i}") for i in range(1)]
        pre = [S.sb([128, 515], F32, f"pre{i}") for i in range(2)]
        cacc = [S.sb([128, 512], F32, f"cacc{i}") for i in range(2)]
        sg = [S.sb([128, 512], F32, f"sg{i}") for i in range(2)]
        qkT = [S.sb([128, 8, 512], BF16, f"qkT{i}") for i in range(1)]
        ktm2 = [S.sb([128, 4, 128], BF16, f"ktm{i}") for i in range(2)]
        gsb = S.sb([128, 4, 8], F32, "gsb")
        spl = S.sb([128, 4, 4], F32, "spl")
        a_tm = S.sb([128, 4, 4], F32, "a_tm")
        cum_sb = S.sb([128, 4, 4], F32, "cum_sb")
        e_tm = S.sb([128, 4, 4], F32, "e_tm")
        dn_tm = S.sb([128, 4, 4], F32, "dn_tm")
        gb_sb = S.sb([128, 4, 8], F32, "gb_sb")
        rw = S.sb([4, 20], F32, "rw")
        rhs8 = S.sb([4, 4, 8], F32, "rhs8")
        lnc0_t = S.sb([128, 1], F32, "lnc0_t")
        S.op("pool", lambda e: e.memset(lnc0_t[:], -float(np.log(128 ** -0.5))), writes=["lnc0"])
        ve = [S.sb([128, 4, 129], BF16, f"ve{i}") for i in range(2)]
        smask = [S.sb([128, 4, 128], BF16, f"smask{i}") for i in range(2)]
        og2 = [S.sb([128, 512], F32, f"og{i}") for i in range(2)]
        hs = S.sb([128, 4, 128], F32, "hs")
        nqa = S.sb([128, 4], F32, "nqa")
        sm8 = S.sb([128, 16], F32, "sm8")
        tmpa = S.sb([128, 1024], F32, "tmpa")
        ux = S.sb([128, 1024], F32, "ux")
        t1 = S.sb([128, 1024], F32, "t1")
        vn = S.sb([128, 512], BF16, "vn")
        gt = S.sb([128, 8, 64], F32, "gt")
        ybuf = [S.sb([128, 1024], BF16, f"ybuf{i}") for i in range(2)]
        yT = [S.sb([128, 8, 128], BF16, f"yT{i}") for i in range(2)]
        h1b = [S.sb([128, 1024], F32, f"h1b{i}") for i in range(2)]
        g2_b = bload("g2_b", norm2_g, 1024)
        br_b = bload("br_b", b_r, 36)
        wr_f = S.sb([128, 8, 36], F32, "wr_f")
        wr_b = S.sb([128, 8, 36], BF16, "wr_b")
        S.op("sp", lambda e: e.dma_start(out=wr_f[:], in_=w_r.ap().rearrange("(c p) n -> p c n", p=128)), writes=["wr_f"], dma="wr_f")
        S.op("dve", lambda e: e.tensor_copy(out=wr_b[:], in_=wr_f[:]), reads=["wr_f"], writes=["wr_b"])
        lstr_b = S.sb([128, 128], BF16, "lstr_b")
        ones_b = S.sb([128, 128], BF16, "ones_b")
        lstr_f = S.sb([128, 128], F32, "lstr_f")
        S.op("pool", lambda e: e.affine_select(out=lstr_f[:], in_=ones_f[:], pattern=[[1, 128]], compare_op=ALU.is_gt, fill=0.0, base=0, channel_multiplier=-1),
             reads=["ones_f"], writes=["lstr_f"])
        S.op("dve", lambda e: e.tensor_copy(out=lstr_b[:], in_=lstr_f[:]), reads=["lstr_f"], writes=["lstr_b"])
        S.op("dve", lambda e: e.tensor_copy(out=ones_b[:], in_=ones_f[:]), reads=["ones_f"], writes=["ones_b"])
        xn2t = [S.sb([128, 1024], BF16, f"xn2t{i}") for i in range(2)]
        xn2T = [S.sb([128, 8, 128], BF16, f"xn2T{i}") for i in range(1)]
        lg = S.sb([128, 36], F32, "lg")
        rt = S.sb([128, 64], F32, "rt")
        t32 = S.sb([128, 4, 8], F32, "t32")
        le8 = S.sb([128, 16], F32, "le8")
        ind_b = S.sb([128, 32], BF16, "ind_b")
        posf = S.sb([128, 32], F32, "posf")
        S.op("pool", lambda e: e.memset(base_b[:], 0.0), writes=["base_b"])
        xn2lin = nc.dram_tensor("xn2lin", [TOK, D], BF16)
        cnt = {"tile": 0, "st": 0, "ch": 0, "tl2": 0, "y": 0}

        LN_C0 = float(np.log(128 ** -0.5))

        def dump(name, ap_fn, reads, row0=0):
            if name in dbg_t:
                t = dbg_t[name]
                finals.append(S.op("sp", lambda e: e.dma_start(out=t.ap()[row0:row0 + 128, :], in_=ap_fn()), reads=reads, dma=("dbg", name)))

        plan_x = [(x_pre, i) for i in range(NPRE)] + [(x_main, i) for i in range(NST)]

        def xload(g):
            src_, i_ = plan_x[g]
            sl_ = g % 2
            S.op("sp", lambda e: e.dma_start(out=xbuf[sl_][:], in_=src_.ap()[i_ * 512:(i_ + 1) * 512, :].rearrange("(j p) d -> p j d", p=128)),
                 writes=[("x", sl_)], dma=("x", sl_))
            conv_some(g, sl_)

        def supertile(xsrc, st_i, mode, gi):
            main = mode == "main"
            sl = cnt["st"] % 2
            cnt["st"] += 1
            xb = xbuf[sl]
            xt = xT[0]
            qk = qkT[0]
            if gi == 0:
                xload(0)
            for j in range(4):
                tl = cnt["tile"] % 2
                cnt["tile"] += 1
                xnj = xn[tl]
                S.op("dve", lambda e, j=j: e.memset(ss1[:, j:j + 1], 0.0), writes=[("ss1", j)])
                S.op("act", lambda e, j=j: e.activation(out=junk[:], in_=xb[:, j, :], func=AF.Square, accum_out=ss1[:, j:j + 1]),
                     reads=[("x", sl), ("ss1", j)], writes=["junk", ("ss1", j)])
                S.op("act", lambda e, j=j: e.activation(out=ss1[:, 4 + j:5 + j], in_=ss1[:, j:j + 1], func=AF.Ln, scale=1.0 / D, bias=EPS),
                     reads=[("ss1", j)], writes=[("ss1", 4 + j)])
                S.op("act", lambda e, j=j: e.activation(out=ss1[:, 4 + j:5 + j], in_=ss1[:, 4 + j:5 + j], func=AF.Exp, scale=-0.5),
                     reads=[("ss1", 4 + j)], writes=[("ss1", 4 + j)])
                S.op("act", lambda e, j=j, xnj=xnj: e.mul(xnj[:], xb[:, j, :], ss1[:, 4 + j:5 + j]),
                     reads=[("x", sl), ("ss1", 4 + j)], writes=[("xn", tl)])
                pb, pt = tbank()
                for c in range(8):
                    S.op("pe", lambda e, c=c, pb=pb, xnj=xnj: e.transpose(out=pb[:, c * 128:(c + 1) * 128], in_=xnj[:, c * 128:(c + 1) * 128], identity=ident_b[:]),
                         reads=[("xn", tl), "ident_b"], accw=[pt])
                S.op("dve" if j % 2 == 0 else "act",
                     (lambda e, pb=pb, j=j: e.tensor_copy(out=xt[:, :, j * 128:(j + 1) * 128], in_=pb[:].rearrange("p (c t) -> p c t", c=8))) if j % 2 == 0 else
                     (lambda e, pb=pb, j=j: e.copy(out=xt[:, :, j * 128:(j + 1) * 128], in_=pb[:].rearrange("p (c t) -> p c t", c=8))),
                     reads=[pt], accw=[("xT", 0)])
            chunks = range(8) if mode != "pre" else range(4, 8)
            def chunk_s1(ch):
                pb, pt = fbank()
                for kc in range(8):
                    S.op("pe", lambda e, kc=kc, ch=ch, pb=pb: e.matmul(out=pb[:], lhsT=wqk[:, kc, ch * 128:(ch + 1) * 128], rhs=xt[:, kc, :], start=(kc == 0), stop=(kc == 7)),
                         reads=[("xT", 0), "wqk"], accw=[pt])
                ps_ = cnt["ch"] % 2
                cnt["ch"] += 1
                pr = pre[ps_]
                ca = cacc[ps_]
                s_ = sg[ps_]
                S.op("dve", lambda e, pr=pr, ch=ch: e.tensor_copy(out=pr[:, 0:3], in_=halo[:, ch, :]), reads=[("halo", ch)], writes=[("pre", ps_, "h")])
                S.op("act", lambda e, pr=pr, pb=pb: e.copy(out=pr[:, 3:515], in_=pb[:]), reads=[pt], writes=[("pre", ps_)])
                S.op("dve", lambda e, pr=pr, ch=ch: e.tensor_copy(out=halo[:, ch, :], in_=pr[:, 512:515]), reads=[("pre", ps_), ("pre", ps_, "h")], writes=[("halo", ch)])
                S.op("dve", lambda e, pr=pr, ca=ca, ch=ch: e.tensor_scalar(out=ca[:], in0=pr[:, 0:512], scalar1=cw[:, 0, ch:ch + 1], scalar2=None, op0=ALU.mult),
                     reads=[("pre", ps_), ("pre", ps_, "h"), "cw"], writes=[("cacc", ps_)])
                for i in range(1, 4):
                    S.op("dve", lambda e, pr=pr, ca=ca, ch=ch, i=i: e.scalar_tensor_tensor(out=ca[:], in0=pr[:, i:i + 512], scalar=cw[:, i, ch:ch + 1], in1=ca[:], op0=ALU.mult, op1=ALU.add),
                         reads=[("pre", ps_), ("pre", ps_, "h"), ("cacc", ps_), "cw"], writes=[("cacc", ps_)])
                return (ch, ps_, ca, s_)

            def chunk_s2(args):
                ch, ps_, ca, s_ = args
                S.op("act", lambda e, ca=ca, s_=s_: e.activation(out=s_[:], in_=ca[:], func=AF.Exp, scale=-1.0), reads=[("cacc", ps_)], writes=[("sg", ps_)])
                S.op("act", lambda e, s_=s_: e.activation(out=s_[:], in_=s_[:], func=AF.Ln, bias=1.0), reads=[("sg", ps_)], writes=[("sg", ps_)])
                S.op("act", lambda e, s_=s_: e.activation(out=s_[:], in_=s_[:], func=AF.Exp, scale=-1.0), reads=[("sg", ps_)], writes=[("sg", ps_)])
                S.op("dve", lambda e, s_=s_, ca=ca, ch=ch: e.tensor_tensor(out=qk[:, ch, :], in0=s_[:], in1=ca[:], op=ALU.mult),
                     reads=[("sg", ps_), ("cacc", ps_)], accw=[("qkT", 0)])


            pend = None
            for ch in chunks:
                cur = chunk_s1(ch)
                if pend is not None:
                    chunk_s2(pend)
                pend = cur
            chunk_s2(pend)
            pg, pgt = fbank()
            for j in range(4):
                for kc in range(8):
                    S.op("pe", lambda e, j=j, kc=kc: e.matmul(out=pg[:, j * 8:(j + 1) * 8], lhsT=xt[:, kc, j * 128:(j + 1) * 128], rhs=wif[:, kc, :], start=(kc == 0), stop=(kc == 7)),
                         reads=[("xT", 0), "wif"], accw=[pgt])
            S.op("dve", lambda e: e.tensor_tensor(out=gsb[:], in0=pg[:, 0:32].rearrange("p (c g) -> p c g", c=4), in1=bif_b[:].unsqueeze(1).broadcast_to([128, 4, 8]), op=ALU.add),
                 reads=[pgt, "bif_b"], writes=["gsb"])
            S.op("act", lambda e: e.activation(out=spl[:], in_=gsb[:, :, 4:8], func=AF.Exp, scale=-1.0), reads=["gsb"], writes=["spl"])
            S.op("act", lambda e: e.activation(out=spl[:], in_=spl[:], func=AF.Ln, bias=1.0), reads=["spl"], writes=["spl"])
            pc, pct = fbank()
            prw, prt = fbank()
            for c in range(4):
                S.op("pe", lambda e, c=c: e.matmul(out=pc[:, c * 4:(c + 1) * 4], lhsT=triu_f[:], rhs=spl[:, c, :], start=True, stop=True),
                     reads=["spl", "triu_f"], accw=[pct])
                S.op("pe", lambda e, c=c: e.matmul(out=pc[0:4, 16 + c:17 + c], lhsT=spl[:, c, :], rhs=ones_f[:, 0:1], start=True, stop=True),
                     reads=["spl", "ones_f"], accw=[pct])
                S.op("pe", lambda e, c=c: e.matmul(out=prw[0:4, c * 128:(c + 1) * 128], lhsT=gsb[:, c, 0:4], rhs=ident_f[:], start=True, stop=False),
                     reads=["gsb", "ident_f"], accw=[prt])
                S.op("pe", lambda e, c=c: e.matmul(out=prw[0:4, c * 128:(c + 1) * 128], lhsT=spl[:, c, :], rhs=triu_f[:], start=False, stop=True),
                     reads=["spl", "triu_f"], accw=[prt])
            S.op("dve", lambda e: e.tensor_tensor(out=a_tm[:], in0=gsb[:, :, 0:4], in1=pc[:, 0:16].rearrange("p (c h) -> p c h", c=4), op=ALU.add),
                 reads=["gsb", pct], writes=["a_tm"])
            S.op("dve", lambda e: e.tensor_copy(out=cum_sb[:], in_=pc[:, 0:16].rearrange("p (c h) -> p c h", c=4)), reads=[pct], writes=["cum_sb"])
            S.op("dve", lambda e: e.tensor_copy(out=rw[:, 4:8], in_=pc[0:4, 16:20]), reads=[pct], writes=["rw_tot"])
            S.op("dve", lambda e: e.tensor_reduce(out=rw[:, 0:4], in_=prw[0:4, :].rearrange("p (c l) -> p c l", c=4), axis=AX.X, op=ALU.max),
                 reads=[prt], writes=["rw_A"])
            for c in range(4):
                S.op("dve", lambda e, c=c: e.tensor_copy(out=rw[:, 12 + c:13 + c], in_=m_st[:]), reads=["m_st"], writes=[("rw_mp", c)])
                S.op("dve", lambda e, c=c: e.tensor_tensor(out=rw[:, 8 + c:9 + c], in0=m_st[:], in1=rw[:, c:c + 1], op=ALU.max), reads=["m_st", "rw_A"], writes=[("rw_G", c)])
                S.op("dve", lambda e, c=c: e.tensor_tensor(out=m_st[:], in0=rw[:, 8 + c:9 + c], in1=rw[:, 4 + c:5 + c], op=ALU.subtract), reads=[("rw_G", c), "rw_tot"], writes=["m_st"])
            S.op("dve", lambda e: e.tensor_tensor(out=rw[:, 16:20], in0=rw[:, 12:16], in1=rw[:, 8:12], op=ALU.subtract),
                 reads=[("rw_G", c) for c in range(4)] + [("rw_mp", c) for c in range(4)], writes=["rw_D"])
            S.op("act", lambda e: e.activation(out=rw[:, 16:20], in_=rw[:, 16:20], func=AF.Exp), reads=["rw_D"], writes=["rw_D"])
            S.op("dve", lambda e: e.tensor_tensor(out=rhs8[:, :, 0:4], in0=ident_f[0:4, 0:4].unsqueeze(1).broadcast_to([4, 4, 4]), in1=rw[:, 8:12].unsqueeze(2).broadcast_to([4, 4, 4]), op=ALU.mult),
                 reads=[("rw_G", c) for c in range(4)] + ["ident_f"], writes=["rhs8a"])
            S.op("dve", lambda e: e.tensor_tensor(out=rhs8[:, :, 4:8], in0=ident_f[0:4, 0:4].unsqueeze(1).broadcast_to([4, 4, 4]), in1=rw[:, 16:20].unsqueeze(2).broadcast_to([4, 4, 4]), op=ALU.mult),
                 reads=["rw_D", "ident_f"], writes=["rhs8b"])
            S.op("pe", lambda e: e.matmul(out=pc[:, 32:64], lhsT=ones_f[0:4, :], rhs=rhs8[:].rearrange("p c g -> p (c g)"), start=True, stop=True),
                 reads=["rhs8a", "rhs8b", "ones_f"], accw=[pct])
            S.op("dve", lambda e: e.tensor_copy(out=gb_sb[:], in_=pc[:, 32:64].rearrange("p (c g) -> p c g", c=4)), reads=[pct], writes=["gb_sb"])
            S.op("dve", lambda e: e.tensor_tensor(out=e_tm[:], in0=a_tm[:], in1=gb_sb[:, :, 0:4], op=ALU.subtract), reads=["a_tm", "gb_sb"], writes=["e_tm"])
            S.op("act", lambda e: e.activation(out=e_tm[:], in_=e_tm[:], func=AF.Exp), reads=["e_tm"], writes=["e_tm"])
            S.op("dve", lambda e: e.tensor_tensor(out=dn_tm[:], in0=cum_sb[:], in1=gb_sb[:, :, 0:4], op=ALU.subtract), reads=["cum_sb", "gb_sb"], writes=["dn_tm"])
            S.op("act", lambda e: e.activation(out=dn_tm[:], in_=dn_tm[:], func=AF.Exp, bias=lnc0_t[:, 0:1]), reads=["dn_tm", "lnc0"], writes=["dn_tm"])

            if gi + 1 < NPRE + NST:
                xload(gi + 1)
            def tile_body(j):
                c = j
                csl = slice(c * 128, (c + 1) * 128)
                tsl = cnt["tl2"] % 2
                cnt["tl2"] += 1
                vea = ve[tsl]
                sm = smask[tsl]
                ktm = ktm2[tsl]
                og = og2[tsl]
                ysl = cnt["y"] % 2
                if main:
                    cnt["y"] += 1
                yt_ = ybuf[ysl]
                def chainA1():
                    pv, pvt = fbankA()
                    for kc in range(8):
                        yield S.op("pe", lambda e, kc=kc, pv=pv: e.matmul(out=pv[:], lhsT=xt[:, kc, csl], rhs=wvo[:, kc, 0:512], start=(kc == 0), stop=(kc == 7)),
                             reads=[("xT", 0), "wvo"], accw=[pvt])
                    yield S.op("dve", lambda e, pv=pv, vea=vea: e.tensor_tensor(out=vea[:, :, 0:128], in0=pv[:].rearrange("p (h d) -> p h d", h=4), in1=e_tm[:, c, :].unsqueeze(2).broadcast_to([128, 4, 128]), op=ALU.mult),
                         reads=[pvt, "e_tm"], writes=[("ve", tsl)])
                    yield S.op("dve", lambda e, vea=vea: e.tensor_copy(out=vea[:, :, 128:129], in_=e_tm[:, c, :].unsqueeze(2)), reads=["e_tm"], writes=[("ve", tsl, "e")])
                    pb, pt = (ptr[0], ("ptr", 0))
                    for h in range(4):
                        yield S.op("pe", lambda e, h=h, pb=pb: e.transpose(out=pb[:, h * 128:(h + 1) * 128], in_=qk[:, 4 + h, csl], identity=ident_b[:]),
                             reads=[("qkT", 0), "ident_b"], accw=[pt])
                    yield S.op("act", lambda e, pb=pb: e.copy(out=ktm[:].rearrange("p h d -> p (h d)"), in_=pb[:, 0:512]), reads=[pt], writes=[("ktm", tsl)])
                    if main:
                        po, pot = fbankA()
                        for kc in range(8):
                            yield S.op("pe", lambda e, kc=kc, po=po: e.matmul(out=po[:], lhsT=xt[:, kc, csl], rhs=wvo[:, kc, 512:1024], start=(kc == 0), stop=(kc == 7)),
                                 reads=[("xT", 0), "wvo"], accw=[pot])
                        yield S.op("act", lambda e, po=po: e.activation(out=og[:], in_=po[:], func=AF.Exp, scale=-1.0), reads=[pot], writes=[("og", tsl)])
                        yield S.op("act", lambda e: e.activation(out=og[:], in_=og[:], func=AF.Ln, bias=1.0), reads=[("og", tsl)], writes=[("og", tsl)])
                        yield S.op("act", lambda e: e.activation(out=og[:], in_=og[:], func=AF.Exp, scale=-1.0), reads=[("og", tsl)], writes=[("og", tsl)])
                        pS, pSt = fbankA()
                        for h in range(4):
                            yield S.op("pe", lambda e, h=h, pS=pS: e.matmul(out=pS[:, h * 128:(h + 1) * 128], lhsT=qk[:, 4 + h, csl], rhs=qk[:, h, csl], start=True, stop=True),
                                 reads=[("qkT", 0)], accw=[pSt])
                        sm = smask[tsl]
                        yield S.op("dve", lambda e, pS=pS, sm=sm: e.tensor_tensor(out=sm[:], in0=pS[:].rearrange("p (h l) -> p h l", h=4), in1=triu_f[:].unsqueeze(1).broadcast_to([128, 4, 128]), op=ALU.mult),
                             reads=[pSt, "triu_f"], writes=[("smask", tsl)])
                    yield None
                def chainA2():
                    if main:
                        for h in range(4):
                            yield S.op("act", lambda e, h=h: e.mul(Csb[:, h, :], C32[:, h, :], gb_sb[:, c, 4 + h:5 + h]), reads=[("C32", h), "gb_sb"], writes=[("Csb", h)])
                        nbanks = []
                        for hp in range(2):
                            pn, pnt = fbankA2()
                            nbanks.append((pn, pnt))
                            for hh in range(2):
                                h = hp * 2 + hh
                                yield S.op("pe", lambda e, h=h, hh=hh, pn=pn, sm=sm, vea=vea: e.matmul(out=pn[:, hh * 129:(hh + 1) * 129], lhsT=sm[:, h, :], rhs=vea[:, h, :], start=True, stop=False),
                                     reads=[("smask", tsl), ("ve", tsl), ("ve", tsl, "e")], accw=[pnt])
                                yield S.op("pe", lambda e, h=h, hh=hh, pn=pn: e.matmul(out=pn[:, hh * 129:(hh + 1) * 129], lhsT=qk[:, h, csl], rhs=Csb[:, h, :], start=False, stop=True),
                                     reads=[("qkT", 0), ("Csb", h)], accw=[pnt])
                        for hp in range(2):
                            pn, pnt = nbanks[hp]
                            yield S.op("act", lambda e, pn=pn, hp=hp: e.activation(out=nqa[:, hp * 2:hp * 2 + 2].unsqueeze(2), in_=pn[:, 0:258].rearrange("p (h d) -> p h d", h=2)[:, :, 128:129], func=AF.Abs),
                                 reads=[pnt], writes=["nqa"])
                        yield S.op("dve", lambda e: e.tensor_tensor(out=nqa[:], in0=nqa[:], in1=dn_tm[:, c, :], op=ALU.max), reads=["nqa", "dn_tm"], writes=["nqa"])
                        yield S.op("dve", lambda e: e.reciprocal(out=nqa[:], in_=nqa[:]), reads=["nqa"], writes=["nqa"])
                        for hp in range(2):
                            pn, pnt = nbanks[hp]
                            yield S.op("dve", lambda e, pn=pn, hp=hp: e.tensor_tensor(out=hs[:, hp * 2:hp * 2 + 2, :], in0=pn[:, 0:258].rearrange("p (h d) -> p h d", h=2)[:, :, 0:128], in1=nqa[:, hp * 2:hp * 2 + 2].unsqueeze(2).broadcast_to([128, 2, 128]), op=ALU.mult),
                                 reads=[pnt, "nqa"], writes=["hs"])
                        yield S.op("dve", lambda e: e.tensor_tensor(out=hs[:].rearrange("p h d -> p (h d)"), in0=hs[:].rearrange("p h d -> p (h d)"), in1=og[:], op=ALU.mult),
                             reads=["hs", ("og", tsl)], writes=["hs"])
                        yield S.op("dve", lambda e: e.tensor_tensor(out=tmpa[:, 0:512], in0=hs[:].rearrange("p h d -> p (h d)"), in1=hs[:].rearrange("p h d -> p (h d)"), op=ALU.mult), reads=["hs"], writes=["tmpa"])
                        yield S.op("dve", lambda e: e.tensor_reduce(out=sm8[:, 0:4], in_=tmpa[:, 0:512].rearrange("p (h d) -> p h d", h=4), axis=AX.X, op=ALU.add), reads=["tmpa"], writes=["sm8a"])
                        yield S.op("act", lambda e: e.activation(out=sm8[:, 0:4], in_=sm8[:, 0:4], func=AF.Ln, scale=1.0 / 128, bias=EPS), reads=["sm8a"], writes=["sm8a"])
                        yield S.op("act", lambda e: e.activation(out=sm8[:, 0:4], in_=sm8[:, 0:4], func=AF.Exp, scale=-0.5), reads=["sm8a"], writes=["sm8a"])
                        yield S.op("dve", lambda e: e.tensor_tensor(out=hs[:], in0=hs[:], in1=sm8[:, 0:4].unsqueeze(2).broadcast_to([128, 4, 128]), op=ALU.mult), reads=["hs", "sm8a"], writes=["hs"])
                        yield S.op("dve", lambda e, yt_=yt_: e.tensor_tensor(out=yt_[:, 0:512], in0=hs[:].rearrange("p h d -> p (h d)"), in1=gml_b[:], op=ALU.mult), reads=["hs", "gml_b"], writes=[("y", ysl, 0)])
                    for hp in range(2):
                        pu_, put = fbankA2()
                        for hh in range(2):
                            h = hp * 2 + hh
                            yield S.op("pe", lambda e, h=h, hh=hh, pu_=pu_, vea=vea: e.matmul(out=pu_[:, hh * 129:(hh + 1) * 129], lhsT=ktm[:, h, :], rhs=vea[:, h, :], start=True, stop=True),
                                 reads=[("ktm", tsl), ("ve", tsl), ("ve", tsl, "e")], accw=[put])
                        for hh in range(2):
                            h = hp * 2 + hh
                            yield S.op("dve", lambda e, h=h, hh=hh, pu_=pu_: e.scalar_tensor_tensor(out=C32[:, h, :], in0=C32[:, h, :], scalar=gb_sb[:, c, 4 + h:5 + h], in1=pu_[:, hh * 129:(hh + 1) * 129], op0=ALU.mult, op1=ALU.add),
                                 reads=[put, "gb_sb", ("C32", h)], writes=[("C32", h)])

                    yield None
                def chainB():
                    pU, pUt = fbankB()
                    pV, pVt = fbankB()
                    for kc in range(8):
                        yield S.op("pe", lambda e, kc=kc, pU=pU: e.matmul(out=pU[:], lhsT=xt[:, kc, csl], rhs=wuv[:, kc, 0:512], start=(kc == 0), stop=(kc == 7)),
                             reads=[("xT", 0), "wuv"], accw=[pUt])
                    for kc in range(8):
                        yield S.op("pe", lambda e, kc=kc, pV=pV: e.matmul(out=pV[:], lhsT=xt[:, kc, csl], rhs=wuv[:, kc, 512:1024], start=(kc == 0), stop=(kc == 7)),
                             reads=[("xT", 0), "wuv"], accw=[pVt])
                    yield S.op("act", lambda e, pU=pU: e.copy(out=ux[:, 0:512], in_=pU[:]), reads=[pUt], writes=["ux"])
                    yield S.op("act", lambda e, pV=pV: e.copy(out=ux[:, 512:1024], in_=pV[:]), reads=[pVt], writes=["ux"])
                    yield S.op("act", lambda e: e.activation(out=t1[:], in_=ux[:], func=AF.Square), reads=["ux"], writes=["t1"])
                    yield S.op("dve", lambda e: e.tensor_scalar(out=t1[:], in0=t1[:], scalar1=0.044715, scalar2=1.0, op0=ALU.mult, op1=ALU.add), reads=["t1"], writes=["t1"])
                    yield S.op("dve", lambda e: e.tensor_tensor(out=t1[:], in0=t1[:], in1=ux[:], op=ALU.mult), reads=["t1", "ux"], writes=["t1"])
                    yield S.op("act", lambda e: e.activation(out=t1[:], in_=t1[:], func=AF.Exp, scale=-2.0 * 0.7978845608028654), reads=["t1"], writes=["t1"])
                    yield S.op("act", lambda e: e.activation(out=t1[:], in_=t1[:], func=AF.Ln, bias=1.0), reads=["t1"], writes=["t1"])
                    yield S.op("act", lambda e: e.activation(out=t1[:], in_=t1[:], func=AF.Exp, scale=-1.0), reads=["t1"], writes=["t1"])
                    yield S.op("dve", lambda e: e.tensor_tensor(out=ux[:], in0=t1[:], in1=ux[:], op=ALU.mult), reads=["t1", "ux"], writes=["ux"])
                    yield S.op("dve", lambda e: e.memset(sm8[:, 4:5], 0.0), writes=["sm8b"])
                    yield S.op("act", lambda e: e.activation(out=junk[:, 0:512], in_=ux[:, 512:1024], func=AF.Square, accum_out=sm8[:, 4:5]), reads=["ux", "sm8b"], writes=["junk", "sm8b"])
                    yield S.op("act", lambda e: e.activation(out=sm8[:, 4:5], in_=sm8[:, 4:5], func=AF.Ln, scale=1.0 / 512, bias=EPS), reads=["sm8b"], writes=["sm8b"])
                    yield S.op("act", lambda e: e.activation(out=sm8[:, 4:5], in_=sm8[:, 4:5], func=AF.Exp, scale=-0.5), reads=["sm8b"], writes=["sm8b"])
                    yield S.op("dve", lambda e: e.scalar_tensor_tensor(out=vn[:], in0=ux[:, 512:1024], scalar=sm8[:, 4:5], in1=ggv_b[:], op0=ALU.mult, op1=ALU.mult), reads=["ux", "sm8b", "ggv_b"], writes=["vn"])
                    pM, pMt = fbankB()
                    for g in range(8):
                        yield S.op("pe", lambda e, g=g, pM=pM: e.matmul(out=pM[:, g * 64:(g + 1) * 64], lhsT=wspT[:, g, :], rhs=vn[:, g * 64:(g + 1) * 64], start=True, stop=True),
                             reads=["vn", "wspT"], accw=[pMt])
                    yield S.op("dve", lambda e, pM=pM: e.tensor_tensor(out=gt[:], in0=pM[:].rearrange("p (g c) -> p g c", g=8), in1=bsp[:].unsqueeze(2).broadcast_to([128, 8, 64]), op=ALU.add),
                         reads=[pMt, "bsp"], writes=["gt"])
                    yield S.op("dve", lambda e: e.tensor_tensor(out=gt[:].rearrange("p g c -> p (g c)"), in0=gt[:].rearrange("p g c -> p (g c)"), in1=ux[:, 0:512], op=ALU.mult), reads=["gt", "ux"], writes=["gt"])
                    yield S.op("dve", lambda e: e.tensor_tensor(out=tmpa[:, 512:1024], in0=gt[:].rearrange("p g c -> p (g c)"), in1=gt[:].rearrange("p g c -> p (g c)"), op=ALU.mult), reads=["gt"], writes=["tmpb"])
                    yield S.op("dve", lambda e: e.tensor_reduce(out=sm8[:, 8:16], in_=tmpa[:, 512:1024].rearrange("p (g c) -> p g c", g=8), axis=AX.X, op=ALU.add), reads=["tmpb"], writes=["sm8c"])
                    yield S.op("act", lambda e: e.activation(out=sm8[:, 8:16], in_=sm8[:, 8:16], func=AF.Ln, scale=1.0 / 64, bias=EPS), reads=["sm8c"], writes=["sm8c"])
                    yield S.op("act", lambda e: e.activation(out=sm8[:, 8:16], in_=sm8[:, 8:16], func=AF.Exp, scale=-0.5), reads=["sm8c"], writes=["sm8c"])
                    yield S.op("dve", lambda e: e.tensor_tensor(out=gt[:], in0=gt[:], in1=sm8[:, 8:16].unsqueeze(2).broadcast_to([128, 8, 64]), op=ALU.mult), reads=["gt", "sm8c"], writes=["gt"])
                    yield S.op("dve", lambda e, yt_=yt_: e.tensor_tensor(out=yt_[:, 512:1024], in0=gt[:].rearrange("p g c -> p (g c)"), in1=ggo_b[:], op=ALU.mult), reads=["gt", "ggo_b"], writes=[("y", ysl, 1)])

                    yield None
                def chainC():
                    if "y" in dbg_t:
                        row0d = st_i * 512 + j * 128
                        finals.append(S.op("pool", lambda e, yt_=yt_, row0d=row0d: e.dma_start(out=dbg_t["y"].ap()[row0d:row0d + 128, :], in_=yt_[:]), reads=[("y", ysl, 0), ("y", ysl, 1)], dma=("dbgy", ysl)))
                    pb, pt = (ptr[1], ("ptr", 1))
                    for ec in range(8):
                        yield S.op("pe", lambda e, ec=ec, pb=pb, yt_=yt_: e.transpose(out=pb[:, ec * 128:(ec + 1) * 128], in_=yt_[:, ec * 128:(ec + 1) * 128], identity=ident_b[:]),
                             reads=[("y", ysl, 0), ("y", ysl, 1), "ident_b"], accw=[pt])
                    yT_ = yT[ysl]
                    yield S.op("act", lambda e, pb=pb, yT_=yT_: e.copy(out=yT_[:].rearrange("p c t -> p (c t)"), in_=pb[:]), reads=[pt], writes=[("yT", ysl)])
                    h1t = h1b[ysl]
                    for hf in range(2):
                        ph, pht = fbankC()
                        for ec in range(8):
                            yield S.op("pe", lambda e, ec=ec, ph=ph, hf=hf, yT_=yT_: e.matmul(out=ph[:], lhsT=yT_[:, ec, :], rhs=wout[:, ec, hf * 512:(hf + 1) * 512], start=(ec == 0), stop=(ec == 7)),
                                 reads=[("yT", ysl), "wout"], accw=[pht])
                        yield S.op("dve", lambda e, ph=ph, hf=hf, h1t=h1t: e.tensor_tensor(out=h1t[:, hf * 512:(hf + 1) * 512], in0=ph[:], in1=xb[:, j, hf * 512:(hf + 1) * 512], op=ALU.add),
                             reads=[pht, ("x", sl)], writes=[("h1", ysl, hf)])
                    row0 = st_i * 512 + j * 128
                    yield S.op("sp", lambda e, h1t=h1t, row0=row0: e.dma_start(out=h1buf.ap()[row0:row0 + 128, :], in_=h1t[:]), reads=[("h1", ysl, 0), ("h1", ysl, 1)], accw=["h1buf"], dma=("h1st", ysl))
                    if "h1" in dbg_t:
                        finals.append(S.op("sp", lambda e, h1t=h1t, row0=row0: e.dma_start(out=dbg_t["h1"].ap()[row0:row0 + 128, :], in_=h1t[:]), reads=[("h1", ysl, 0), ("h1", ysl, 1)], dma=("dbgh1", ysl)))
                    ti = st_i * 4 + j
                    x2 = xn2t[ysl]
                    yield S.op("dve", lambda e: e.memset(sm8[:, 5:6], 0.0), writes=["sm8d"])
                    yield S.op("act", lambda e: e.activation(out=junk[:], in_=h1t[:], func=AF.Square, accum_out=sm8[:, 5:6]), reads=[("h1", ysl, 0), ("h1", ysl, 1), "sm8d"], writes=["junk", "sm8d"])
                    yield S.op("act", lambda e: e.activation(out=sm8[:, 5:6], in_=sm8[:, 5:6], func=AF.Ln, scale=1.0 / D, bias=EPS), reads=["sm8d"], writes=["sm8d"])
                    yield S.op("act", lambda e: e.activation(out=sm8[:, 5:6], in_=sm8[:, 5:6], func=AF.Exp, scale=-0.5), reads=["sm8d"], writes=["sm8d"])
                    yield S.op("dve", lambda e: e.scalar_tensor_tensor(out=x2[:], in0=h1t[:], scalar=sm8[:, 5:6], in1=g2_b[:], op0=ALU.mult, op1=ALU.mult),
                         reads=[("h1", ysl, 0), ("h1", ysl, 1), "sm8d", "g2_b"], writes=[("xn2", ysl)])
                    yield S.op("sp", lambda e: e.dma_start(out=xn2lin.ap()[row0:row0 + 128, :], in_=x2[:]), reads=[("xn2", ysl)], accw=["xn2lin"], dma=("xn2st", ysl))
                    pb2, pt2 = (ptr[1], ("ptr", 1))
                    for kc in range(8):
                        yield S.op("pe", lambda e, kc=kc: e.transpose(out=pb2[:, kc * 128:(kc + 1) * 128], in_=x2[:, kc * 128:(kc + 1) * 128], identity=ident_b[:]),
                             reads=[("xn2", ysl), "ident_b"], accw=[pt2])
                    x2T = xn2T[0]
                    yield S.op("act", lambda e: e.copy(out=x2T[:].rearrange("p c t -> p (c t)"), in_=pb2[:]), reads=[pt2], writes=[("xn2T", 0)])
                    pl, plt = fbankC()
                    for kc in range(8):
                        yield S.op("pe", lambda e, kc=kc: e.matmul(out=pl[:, 0:36], lhsT=x2T[:, kc, :], rhs=wr_b[:, kc, :], start=(kc == 0), stop=(kc == 7)),
                             reads=[("xn2T", 0), "wr_b"], accw=[plt])
                    yield S.op("dve", lambda e: e.tensor_tensor(out=lg[:], in0=pl[:, 0:36], in1=br_b[:], op=ALU.add), reads=[plt, "br_b"], writes=["lg"])
                    R_ = lambda a, b: rt[:, a:b]
                    yield S.op("dve", lambda e: e.tensor_reduce(out=R_(0, 1), in_=lg[:, 0:4], axis=AX.X, op=ALU.max), reads=["lg"], writes=["rt"])
                    yield S.op("dve", lambda e: e.tensor_scalar(out=R_(12, 16), in0=lg[:, 0:4], scalar1=R_(0, 1), scalar2=None, op0=ALU.is_equal), reads=["lg", "rt"], writes=["rt"])
                    yield S.op("dve", lambda e: e.tensor_scalar(out=R_(1, 2), in0=R_(0, 1), scalar1=-1.0, scalar2=None, op0=ALU.mult), reads=["rt"], writes=["rt"])
                    yield S.op("dve", lambda e: e.memset(R_(2, 3), 0.0), reads=["rt"], writes=["rt"])
                    yield S.op("act", lambda e: e.activation(out=le8[:, 8:12], in_=lg[:, 0:4], func=AF.Exp, bias=R_(1, 2), accum_out=R_(2, 3)), reads=["lg", "rt"], writes=["rt", "le8x"])
                    yield S.op("dve", lambda e: e.reciprocal(out=R_(3, 4), in_=R_(2, 3)), reads=["rt"], writes=["rt"])
                    yield S.op("dve", lambda e: e.tensor_tensor(out=t32[:], in0=lg[:, 4:36].rearrange("p (g j) -> p g j", g=4), in1=R_(12, 16).unsqueeze(2).broadcast_to([128, 4, 8]), op=ALU.mult), reads=["lg", "rt"], writes=["t32"])
                    yield S.op("dve", lambda e: e.tensor_reduce(out=le8[:, 0:8], in_=t32[:].rearrange("p g j -> p j g"), axis=AX.X, op=ALU.add), reads=["t32"], writes=["le8"])
                    yield S.op("dve", lambda e: e.tensor_reduce(out=R_(4, 5), in_=le8[:, 0:8], axis=AX.X, op=ALU.max), reads=["le8", "rt"], writes=["rt"])
                    yield S.op("dve", lambda e: e.tensor_scalar(out=R_(16, 24), in0=le8[:, 0:8], scalar1=R_(4, 5), scalar2=None, op0=ALU.is_equal), reads=["le8", "rt"], writes=["rt"])
                    yield S.op("dve", lambda e: e.scalar_tensor_tensor(out=le8[:, 0:8], in0=R_(16, 24), scalar=-1e30, in1=le8[:, 0:8], op0=ALU.mult, op1=ALU.add), reads=["le8", "rt"], writes=["le8"])
                    yield S.op("dve", lambda e: e.tensor_reduce(out=R_(5, 6), in_=le8[:, 0:8], axis=AX.X, op=ALU.max), reads=["le8", "rt"], writes=["rt"])
                    yield S.op("dve", lambda e: e.tensor_scalar(out=R_(24, 32), in0=le8[:, 0:8], scalar1=R_(5, 6), scalar2=None, op0=ALU.is_equal), reads=["le8", "rt"], writes=["rt"])
                    yield S.op("dve", lambda e: e.tensor_tensor(out=R_(6, 7), in0=R_(5, 6), in1=R_(4, 5), op=ALU.subtract), reads=["rt"], writes=["rt"])
                    yield S.op("act", lambda e: e.activation(out=R_(6, 7), in_=R_(6, 7), func=AF.Exp), reads=["rt"], writes=["rt"])
                    yield S.op("dve", lambda e: e.tensor_scalar(out=R_(7, 8), in0=R_(6, 7), scalar1=1.0, scalar2=None, op0=ALU.add), reads=["rt"], writes=["rt"])
                    yield S.op("dve", lambda e: e.reciprocal(out=R_(7, 8), in_=R_(7, 8)), reads=["rt"], writes=["rt"])
                    yield S.op("dve", lambda e: e.tensor_tensor(out=R_(8, 9), in0=R_(6, 7), in1=R_(7, 8), op=ALU.mult), reads=["rt"], writes=["rt"])
                    yield S.op("dve", lambda e: e.tensor_scalar(out=pw[:, ti, 2:4], in0=R_(7, 9), scalar1=R_(3, 4), scalar2=None, op0=ALU.mult), reads=["rt"], accw=["pw"])
                    E1 = E1s[:, ti, :]
                    E2 = E2s[:, ti, :]
                    yield S.op("dve", lambda e: e.tensor_tensor(out=E1.rearrange("p (g j) -> p g j", g=4), in0=R_(12, 16).unsqueeze(2).broadcast_to([128, 4, 8]), in1=R_(16, 24).unsqueeze(1).broadcast_to([128, 4, 8]), op=ALU.mult), reads=["rt"], accw=["E1s"])
                    yield S.op("dve", lambda e: e.tensor_tensor(out=E2.rearrange("p (g j) -> p g j", g=4), in0=R_(12, 16).unsqueeze(2).broadcast_to([128, 4, 8]), in1=R_(24, 32).unsqueeze(1).broadcast_to([128, 4, 8]), op=ALU.mult), reads=["rt"], accw=["E2s"])
                    yield S.op("dve", lambda e: e.tensor_tensor(out=ind_b[:], in0=E1, in1=E2, op=ALU.add), reads=["E1s", "E2s"], writes=["ind_b"])
                    pp, ppt = fbankC()
                    yield S.op("pe", lambda e: e.matmul(out=pp[:, 0:32], lhsT=lstr_b[:], rhs=ind_b[:], start=True, stop=True), reads=["ind_b", "lstr_b"], accw=[ppt])
                    yield S.op("pe", lambda e: e.matmul(out=pp[:, 32:64], lhsT=ones_b[:], rhs=ind_b[:], start=True, stop=True), reads=["ind_b", "ones_b"], accw=[ppt])
                    yield S.op("dve", lambda e: e.tensor_tensor(out=posf[:], in0=pp[:, 0:32], in1=base_b[:], op=ALU.add), reads=[ppt, "base_b"], writes=["posf"])
                    yield S.op("dve", lambda e: e.tensor_tensor(out=base_b[:], in0=pp[:, 32:64], in1=base_b[:], op=ALU.add), reads=[ppt, "base_b"], writes=["base_b"])
                    yield S.op("dve", lambda e: e.tensor_tensor(out=t32[:].rearrange("p g j -> p (g j)"), in0=E1, in1=posf[:], op=ALU.mult), reads=["E1s", "posf"], writes=["t32"])
                    yield S.op("dve", lambda e: e.tensor_reduce(out=pw[:, ti, 0:1], in_=t32[:].rearrange("p g j -> p (g j)"), axis=AX.X, op=ALU.add), reads=["t32"], accw=["pw"])
                    yield S.op("dve", lambda e: e.tensor_tensor(out=t32[:].rearrange("p g j -> p (g j)"), in0=E2, in1=posf[:], op=ALU.mult), reads=["E2s", "posf", "pw"], writes=["t32"])
                    yield S.op("dve", lambda e: e.tensor_reduce(out=pw[:, ti, 1:2], in_=t32[:].rearrange("p g j -> p (g j)"), axis=AX.X, op=ALU.add), reads=["t32"], accw=["pw"])
                    yield None
                return chainA1(), chainA2(), (chainB() if main else None), (chainC() if main else None)

            def run_chains(gens):
                act = [g for g in gens if g is not None]
                while act:
                    for g in list(act):
                        try:
                            next(g)
                        except StopIteration:
                            act.remove(g)

            made = {}

            def get(j):
                if j not in made:
                    made[j] = list(tile_body(j))
                return made[j]

            act = {}
            done = set()
            nxt = {"A1": 0, "A2": 0, "B": 0, "C": 0}
            IDX = {"A1": 0, "A2": 1, "B": 2, "C": 3}

            def fin(kind, j):
                return j < 0 or (kind, j) in done

            def start(kind, j):
                g = get(j)[IDX[kind]]
                if g is None:
                    done.add((kind, j))
                else:
                    act[(kind, j)] = g
                nxt[kind] += 1

            def try_start():
                j = nxt["A1"]
                if j < 4 and fin("A1", j - 1) and fin("A2", j - 2):
                    start("A1", j)
                j = nxt["A2"]
                if j < 4 and fin("A1", j) and j < nxt["A1"] and fin("A2", j - 1) and fin("C", j - 2):
                    start("A2", j)
                j = nxt["B"]
                if j < 4 and j < nxt["A1"] and fin("B", j - 1) and fin("C", j - 2):
                    start("B", j)
                j = nxt["C"]
                if j < 4 and j < nxt["A1"] and fin("A2", j) and fin("B", j) and fin("C", j - 1) and j < nxt["A2"] and j < nxt["B"]:
                    start("C", j)

            ready = {}
            while len(done) < 16:
                try_start()
                if not act:
                    continue
                for k_ in act:
                    ready.setdefault(k_, 0.0)
                k_ = min(act.keys(), key=lambda q: ready[q])
                try:
                    next(act[k_])
                    ready[k_] = S.last_est
                except StopIteration:
                    del act[k_]
                    del ready[k_]
                    done.add(k_)
            if gi == 0 and "qkT" in dbg_t:
                tmpd = S.sb([128, 8, 512], F32, "sdbg_qk")
                S.op("dve", lambda e: e.tensor_copy(out=tmpd[:], in_=qk[:]), reads=[("qkT", 0)], writes=["dbg_qk"])
                finals.append(S.op("sp", lambda e: e.dma_start(out=dbg_t["qkT"].ap(), in_=tmpd[:].rearrange("p a b -> p (a b)")), reads=["dbg_qk"], dma=("dbg", "qkT")))
            if gi == 0 and "xT" in dbg_t:
                tmpx = S.sb([128, 8, 512], F32, "sdbg_xT")
                S.op("dve", lambda e: e.tensor_copy(out=tmpx[:], in_=xt[:]), reads=[("xT", 0)], writes=["dbg_xT"])
                finals.append(S.op("sp", lambda e: e.dma_start(out=dbg_t["xT"].ap(), in_=tmpx[:].rearrange("p a b -> p (a b)")), reads=["dbg_xT"], dma=("dbg", "xT")))

        wcat_bf = nc.dram_tensor("wcat_bf", [NE * 128, 12288], BF16)
        NSUP = NPRE + NST
        conv_rows = NE * 128
        conv_state = {"r": 0, "i": 0}

        def conv_some(gidx, sl):
            tgt = conv_rows * (gidx + 1) // NSUP
            while conv_state["r"] < tgt:
                r0 = conv_state["r"]
                r1 = min(r0 + 32, tgt)
                k = conv_state["i"] % 2
                conv_state["i"] += 1
                conv_state["r"] = r1
                S.op("pool", lambda e, r0=r0, r1=r1: e.dma_start(out=wcat_bf.ap()[r0:r1, :], in_=wcat.ap()[r0:r1, :]),
                     reads=[("x", sl)], writes=[("wck", k)], dma=("wck", k))

        NBLK0 = NT * 2 + NE
        xslots = nc.dram_tensor("xslots", [NBLK0 * 128, D], BF16)
        ztile = junk
        S.op("dve", lambda e: e.memset(ztile[:], 0.0), writes=["junk"])
        for zb in range(0, NBLK0, 8):
            nb_ = min(8, NBLK0 - zb)
            S.op("sp", lambda e, zb=zb, nb_=nb_: e.dma_start(out=xslots.ap()[zb * 128:(zb + nb_) * 128, :].rearrange("(j p) d -> p j d", p=128),
                                                             in_=ztile[:].unsqueeze(1).broadcast_to([128, nb_, 1024])),
                 reads=["junk"], accw=["xslots"], dma=("zinit", zb // 8))
        gi = 0
        for s_i in range(NPRE):
            supertile(x_pre, s_i, "prelast" if s_i == NPRE - 1 else "pre", gi)
            gi += 1
        for s_i in range(NST):
            supertile(x_main, s_i, "main", gi)
            gi += 1


        S.flush()
        stA.close()
        stB = contextlib.ExitStack()
        S.cur = stB
        NBLK = NT * 2 + NE
        NSL = NBLK * 128
        yslots = nc.dram_tensor("yslots", [NSL, D], F32)
        pl_ = S.sb([128, 6, 32], F32, "plan")
        thr = S.sb([128, 64], F32, "thr")
        cmpn = S.sb([128, 32, 64], F32, "cmpn")
        p128 = S.sb([128, 1], F32, "p128")
        woff_f = S.sb([128, NBLK], F32, "woff_f")
        woff_i = S.sb([128, NBLK], I32, "woff_i")
        bval = S.sb([128, NBLK], F32, "bval")
        neq = S.sb([128, NBLK], F32, "neq")
        cmpb = S.sb([128, NBLK, 32], F32, "cmpb")
        dst_f = S.sb([128, 2, NT], F32, "dst_f")
        dst_i = S.sb([128, 2, NT], I32, "dst_i")
        big = S.sb([128, NT, 32], F32, "bigtmp")
        S.op("pool", lambda e: e.iota(p128[:], [[0, 1]], base=0, channel_multiplier=1, allow_small_or_imprecise_dtypes=True), writes=["p128"])
        S.op("pool", lambda e: e.iota(thr[:], [[128, 64]], base=0, channel_multiplier=0, allow_small_or_imprecise_dtypes=True), writes=["thr"])
        S.op("dve", lambda e: e.tensor_tensor(out=cmpn[:], in0=base_b[:].unsqueeze(2).broadcast_to([128, 32, 64]), in1=thr[:].unsqueeze(1).broadcast_to([128, 32, 64]), op=ALU.is_gt),
             reads=["base_b", "thr"], writes=["cmpn"])
        S.op("dve", lambda e: e.tensor_reduce(out=pl_[:, 0, :], in_=cmpn[:], axis=AX.X, op=ALU.add), reads=["cmpn"], writes=["plan"])
        S.op("dve", lambda e: e.tensor_scalar(out=pl_[:, 0, :], in0=pl_[:, 0, :], scalar1=128.0, scalar2=None, op0=ALU.mult), reads=["plan"], writes=["plan"])
        S.op("dve", lambda e: e.tensor_tensor_scan(out=pl_[:, 1, :], data0=pl_[:, 0, :], data1=pl_[:, 0, :], initial=0.0, op0=ALU.add, op1=ALU.bypass), reads=["plan"], writes=["plan"])
        S.op("dve", lambda e: e.tensor_tensor(out=pl_[:, 2, :], in0=pl_[:, 1, :], in1=pl_[:, 0, :], op=ALU.subtract), reads=["plan"], writes=["plan"])
        S.op("pool", lambda e: e.iota(bval[:], [[128, NBLK]], base=0, channel_multiplier=0, allow_small_or_imprecise_dtypes=True), writes=["bval"])
        S.op("dve", lambda e: e.tensor_tensor(out=cmpb[:], in0=pl_[:, 1, :].unsqueeze(1).broadcast_to([128, NBLK, 32]), in1=bval[:].unsqueeze(2).broadcast_to([128, NBLK, 32]), op=ALU.is_le), reads=["plan", "bval"], writes=["cmpb"])
        S.op("dve", lambda e: e.tensor_reduce(out=woff_f[:], in_=cmpb[:], axis=AX.X, op=ALU.add), reads=["cmpb"], writes=["woff_f"])
        BIGI = 1000000.0
        S.op("dve", lambda e: e.tensor_scalar(out=woff_f[:], in0=woff_f[:], scalar1=31.0, scalar2=None, op0=ALU.min), reads=["woff_f"], writes=["woff_f"])
        S.op("dve", lambda e: e.memset(neq[:, 0:2], 1.0), writes=["neq0"])
        S.op("dve", lambda e: e.tensor_tensor(out=neq[:, 2:NBLK], in0=woff_f[:, 2:NBLK], in1=woff_f[:, 0:NBLK - 2], op=ALU.not_equal), reads=["woff_f"], writes=["neq"])
        S.op("dve", lambda e: e.tensor_scalar(out=woff_f[:], in0=woff_f[:], scalar1=128.0, scalar2=-BIGI, op0=ALU.mult, op1=ALU.add), reads=["woff_f", "neq"], writes=["woff_f"])
        S.op("dve", lambda e: e.tensor_scalar(out=woff_f[:], in0=woff_f[:], scalar1=p128[:, 0:1], scalar2=None, op0=ALU.add), reads=["woff_f", "p128"], writes=["woff_f"])
        S.op("dve", lambda e: e.tensor_tensor(out=woff_f[:], in0=woff_f[:], in1=neq[:], op=ALU.mult), reads=["woff_f", "neq", "neq0"], writes=["woff_f"])
        S.op("dve", lambda e: e.tensor_scalar(out=woff_f[:], in0=woff_f[:], scalar1=BIGI, scalar2=None, op0=ALU.add), reads=["woff_f"], writes=["woff_f"])
        S.op("dve", lambda e: e.tensor_scalar(out=woff_f[:], in0=woff_f[:], scalar1=0.0, scalar2=None, op0=ALU.max), reads=["woff_f"], writes=["woff_f"])
        S.op("dve", lambda e: e.tensor_copy(out=woff_i[:], in_=woff_f[:]), reads=["woff_f"], writes=["woff_i"])
        for k_, Es in ((0, E1s), (1, E2s)):
            S.op("dve", lambda e, Es=Es: e.tensor_tensor(out=big[:], in0=Es[:], in1=pl_[:, 2, :].unsqueeze(1).broadcast_to([128, NT, 32]), op=ALU.mult), reads=["E1s", "E2s", "plan"], writes=["big"])
            S.op("dve", lambda e, k_=k_: e.tensor_reduce(out=dst_f[:, k_, :], in_=big[:], axis=AX.X, op=ALU.add), reads=["big"], writes=[("dst_f", k_)])
            S.op("dve", lambda e, k_=k_: e.tensor_tensor(out=dst_f[:, k_, :], in0=dst_f[:, k_, :], in1=pw[:, :, k_], op=ALU.add), reads=[("dst_f", k_), "pw"], writes=[("dst_f", k_)])
        S.op("dve", lambda e: e.tensor_scalar(out=dst_f[:].rearrange("p a b -> p (a b)"), in0=dst_f[:].rearrange("p a b -> p (a b)"), scalar1=float(NSL - 1), scalar2=0.0, op0=ALU.min, op1=ALU.max),
             reads=[("dst_f", 0), ("dst_f", 1)], writes=[("dst_f", 0), ("dst_f", 1)])
        S.op("dve", lambda e: e.tensor_copy(out=dst_i[:], in_=dst_f[:]), reads=[("dst_f", 0), ("dst_f", 1)], writes=["dst_i"])
        if "plan" in dbg_t:
            finals.append(S.op("sp", lambda e: e.dma_start(out=dbg_t["plan"].ap()[:, 0:192], in_=pl_[:].rearrange("p a b -> p (a b)")), reads=["plan"], dma=("dbg", "plan")))
            finals.append(S.op("sp", lambda e: e.dma_start(out=dbg_t["plan"].ap()[:, 768:768 + NBLK], in_=woff_f[:]), reads=["woff_f"], dma=("dbg", "plan2")))
            finals.append(S.op("sp", lambda e: e.dma_start(out=dbg_t["plan"].ap()[:, 256:256 + 2 * NT], in_=dst_f[:].rearrange("p a b -> p (a b)")), reads=[("dst_f", 0), ("dst_f", 1)], dma=("dbg", "plan3")))
            finals.append(S.op("sp", lambda e: e.dma_start(out=dbg_t["plan"].ap()[:, 512:512 + 4 * NT], in_=pw[:].rearrange("p a b -> p (a b)")), reads=["pw"], dma=("dbg", "plan4")))
        xsc = [S.sb([128, 1024], BF16, f"xsc{i}") for i in range(2)]
        for ti in range(NT):
            bsl = ti % 2
            S.op("sp", lambda e, ti=ti, bsl=bsl: e.dma_start(out=xsc[bsl][:], in_=xn2lin.ap()[ti * 128:(ti + 1) * 128, :]), reads=["xn2lin"], writes=[("xsc", bsl)], dma=("xsc", bsl))
            for k_ in range(2):
                S.op("pool", lambda e, ti=ti, bsl=bsl, k_=k_: e.indirect_dma_start(out=xslots.ap(), out_offset=bass.IndirectOffsetOnAxis(ap=dst_i[:, k_, ti:ti + 1], axis=0), in_=xsc[bsl][:], in_offset=None),
                     reads=[("xsc", bsl), "dst_i"], accw=["xslots"], dma=("scat", bsl, k_))

        wbuf = [S.sb([128, 12288], BF16, f"wbuf{i}") for i in range(2)]
        xs_b = [S.sb([128, 1024], BF16, f"xs_b{i}") for i in range(4)]
        xsT = [S.sb([128, 8, 128], BF16, f"xsT{i}") for i in range(2)]
        eg = [S.sb([128, 512], F32, f"eg{i}") for i in range(2)]
        hid = [S.sb([128, 512], BF16, f"hid{i}") for i in range(2)]
        hidT = [S.sb([128, 4, 128], BF16, f"hidT{i}") for i in range(2)]
        ysb = [S.sb([128, 1024], F32, f"ysb{i}") for i in range(2)]
        regs = {}

        def wgather(e, b, ws):
            if "bnd" not in regs:
                regs["bnd"] = st.enter_context(e.register("wbnd"))
                e.reg_mov(regs["bnd"], NE * 128 - 1)
            return e.indirect_dma_start(out=wbuf[ws][:], out_offset=None, in_=wcat_bf.ap(), in_offset=bass.IndirectOffsetOnAxis(ap=woff_i[:, b:b + 1], axis=0),
                                        bounds_check=regs["bnd"], oob_is_err=False)

        mrr = [0, 0]

        def fbankM(p):
            i = 3 * p + mrr[p] % 3
            mrr[p] += 1
            return pfb[i], ("pf", i)

        def blk(b):
            ws = b % 2
            yield S.op("pool", lambda e, b=b, ws=ws: wgather(e, b, ws), reads=["woff_i"], writes=[("wb", ws)], dma=("wb", ws))
            if b < 2:
                yield S.op("sp", lambda e, b=b: e.dma_start(out=xs_b[b % 4][:], in_=xslots.ap()[b * 128:(b + 1) * 128, :]), reads=["xslots"], writes=[("xs", b % 4)], dma=("xs", b % 4))
            if b + 2 < NBLK:
                yield S.op("sp", lambda e, b=b: e.dma_start(out=xs_b[(b + 2) % 4][:], in_=xslots.ap()[(b + 2) * 128:(b + 3) * 128, :]), reads=["xslots"], writes=[("xs", (b + 2) % 4)], dma=("xs", (b + 2) % 4))
            xq = b % 4
            pb, pt = (ptr[ws], ("ptr", ws))
            for kc in range(8):
                yield S.op("pe", lambda e, kc=kc, pb=pb, xq=xq: e.transpose(out=pb[:, kc * 128:(kc + 1) * 128], in_=xs_b[xq][:, kc * 128:(kc + 1) * 128], identity=ident_b[:]),
                     reads=[("xs", xq), "ident_b"], accw=[pt])
            yield S.op("act", lambda e, pb=pb, ws=ws: e.copy(out=xsT[ws][:].rearrange("p c t -> p (c t)"), in_=pb[:]), reads=[pt], writes=[("xsT", ws)])
            pG, pGt = fbankM(ws)
            pU2, pU2t = fbankM(ws)
            for kc in range(8):
                yield S.op("pe", lambda e, kc=kc, pG=pG, ws=ws: e.matmul(out=pG[:], lhsT=xsT[ws][:, kc, :], rhs=wbuf[ws][:, kc * 1024:kc * 1024 + 512], start=(kc == 0), stop=(kc == 7)),
                     reads=[("xsT", ws), ("wb", ws)], accw=[pGt])
            for kc in range(8):
                yield S.op("pe", lambda e, kc=kc, pU2=pU2, ws=ws: e.matmul(out=pU2[:], lhsT=xsT[ws][:, kc, :], rhs=wbuf[ws][:, kc * 1024 + 512:(kc + 1) * 1024], start=(kc == 0), stop=(kc == 7)),
                     reads=[("xsT", ws), ("wb", ws)], accw=[pU2t])
            yield S.op("act", lambda e, pG=pG, ws=ws: e.activation(out=eg[ws][:], in_=pG[:], func=AF.Exp, scale=-1.0), reads=[pGt], writes=[("eg", ws)])
            yield S.op("act", lambda e, ws=ws: e.activation(out=eg[ws][:], in_=eg[ws][:], func=AF.Ln, bias=1.0), reads=[("eg", ws)], writes=[("eg", ws)])
            yield S.op("act", lambda e, ws=ws: e.activation(out=eg[ws][:], in_=eg[ws][:], func=AF.Exp, scale=-1.0), reads=[("eg", ws)], writes=[("eg", ws)])
            yield S.op("dve", lambda e, pG=pG, ws=ws: e.tensor_tensor(out=eg[ws][:], in0=eg[ws][:], in1=pG[:], op=ALU.mult), reads=[("eg", ws), pGt], writes=[("eg", ws)])
            yield S.op("dve", lambda e, pU2=pU2, ws=ws: e.tensor_tensor(out=hid[ws][:], in0=eg[ws][:], in1=pU2[:], op=ALU.mult), reads=[("eg", ws), pU2t], writes=[("hid", ws)])
            pb, pt = (ptr[ws], ("ptr", ws))
            for fc in range(4):
                yield S.op("pe", lambda e, fc=fc, pb=pb, ws=ws: e.transpose(out=pb[:, fc * 128:(fc + 1) * 128], in_=hid[ws][:, fc * 128:(fc + 1) * 128], identity=ident_b[:]),
                     reads=[("hid", ws), "ident_b"], accw=[pt])
            yield S.op("act", lambda e, pb=pb, ws=ws: e.copy(out=hidT[ws][:].rearrange("p c t -> p (c t)"), in_=pb[:, 0:512]), reads=[pt], writes=[("hidT", ws)])
            for hf in range(2):
                pY, pYt = fbankM(ws)
                for fc in range(4):
                    yield S.op("pe", lambda e, fc=fc, pY=pY, ws=ws, hf=hf: e.matmul(out=pY[:], lhsT=hidT[ws][:, fc, :], rhs=wbuf[ws][:, 8192 + fc * 1024 + hf * 512:8192 + fc * 1024 + (hf + 1) * 512], start=(fc == 0), stop=(fc == 3)),
                         reads=[("hidT", ws), ("wb", ws)], accw=[pYt])
                if hf == 0:
                    yield S.op("act", lambda e, pY=pY, ws=ws: e.copy(out=ysb[ws][:, 0:512], in_=pY[:]), reads=[pYt], writes=[("ysb", ws, 0)])
                else:
                    yield S.op("dve", lambda e, pY=pY, ws=ws: e.tensor_copy(out=ysb[ws][:, 512:1024], in_=pY[:]), reads=[pYt], writes=[("ysb", ws, 1)])
            yield S.op("sp", lambda e, b=b, ws=ws: e.dma_start(out=yslots.ap()[b * 128:(b + 1) * 128, :], in_=ysb[ws][:]), reads=[("ysb", ws, 0), ("ysb", ws, 1)], accw=["yslots"], dma=("yst", ws))


            yield None

        for b in range(NBLK):
            for _ in blk(b):
                pass

        fg_b = bload("fg_b", final_g, 1024)
        NCB = 3
        hc = [S.sb([128, 1024], F32, f"hc{i}") for i in range(NCB)]
        y1 = [S.sb([128, 1024], F32, f"y1_{i}") for i in range(NCB)]
        y2 = [S.sb([128, 1024], F32, f"y2_{i}") for i in range(NCB)]
        fs = S.sb([128, 2], F32, "fs")

        def cloads(ti):
            cs = ti % NCB
            S.op("sp", lambda e, ti=ti, cs=cs: e.dma_start(out=hc[cs][:], in_=h1buf.ap()[ti * 128:(ti + 1) * 128, :]), reads=["h1buf"], writes=[("hc", cs)], dma=("hc", cs))
            S.op("pool", lambda e, ti=ti, cs=cs: e.indirect_dma_start(out=y1[cs][:], out_offset=None, in_=yslots.ap(), in_offset=bass.IndirectOffsetOnAxis(ap=dst_i[:, 0, ti:ti + 1], axis=0)),
                 reads=["yslots", "dst_i"], writes=[("y1", cs)], dma=("y1", cs))
            S.op("pool", lambda e, ti=ti, cs=cs: e.indirect_dma_start(out=y2[cs][:], out_offset=None, in_=yslots.ap(), in_offset=bass.IndirectOffsetOnAxis(ap=dst_i[:, 1, ti:ti + 1], axis=0)),
                 reads=["yslots", "dst_i"], writes=[("y2", cs)], dma=("y2", cs))

        for ti in range(min(NCB - 1, NT)):
            cloads(ti)
        for ti in range(NT):
            cs = ti % NCB
            if ti + NCB - 1 < NT:
                cloads(ti + NCB - 1)
            S.op("dve", lambda e, ti=ti, cs=cs: e.scalar_tensor_tensor(out=hc[cs][:], in0=y1[cs][:], scalar=pw[:, ti, 2:3], in1=hc[cs][:], op0=ALU.mult, op1=ALU.add), reads=[("hc", cs), ("y1", cs), "pw"], writes=[("hc", cs)])
            S.op("dve", lambda e, ti=ti, cs=cs: e.scalar_tensor_tensor(out=hc[cs][:], in0=y2[cs][:], scalar=pw[:, ti, 3:4], in1=hc[cs][:], op0=ALU.mult, op1=ALU.add), reads=[("hc", cs), ("y2", cs), "pw"], writes=[("hc", cs)])
            S.op("dve", lambda e, ti=ti: e.memset(fs[:, (ti % 2):(ti % 2) + 1], 0.0), writes=[("fs", ti % 2)])
            S.op("act", lambda e, ti=ti, cs=cs: e.activation(out=junk[:], in_=hc[cs][:], func=AF.Square, accum_out=fs[:, (ti % 2):(ti % 2) + 1]), reads=[("hc", cs), ("fs", ti % 2)], writes=["junk", ("fs", ti % 2)])
            S.op("act", lambda e, ti=ti: e.activation(out=fs[:, (ti % 2):(ti % 2) + 1], in_=fs[:, (ti % 2):(ti % 2) + 1], func=AF.Ln, scale=1.0 / D, bias=EPS), reads=[("fs", ti % 2)], writes=[("fs", ti % 2)])
            S.op("act", lambda e, ti=ti: e.activation(out=fs[:, (ti % 2):(ti % 2) + 1], in_=fs[:, (ti % 2):(ti % 2) + 1], func=AF.Exp, scale=-0.5), reads=[("fs", ti % 2)], writes=[("fs", ti % 2)])
            S.op("dve", lambda e, ti=ti, cs=cs: e.scalar_tensor_tensor(out=y1[cs][:], in0=hc[cs][:], scalar=fs[:, (ti % 2):(ti % 2) + 1], in1=fg_b[:], op0=ALU.mult, op1=ALU.mult), reads=[("hc", cs), ("fs", ti % 2), "fg_b", ("y1", cs)], writes=[("y1", cs)])
            finals.append(S.op("sp", lambda e, ti=ti, cs=cs: e.dma_start(out=out.ap()[ti * 128:(ti + 1) * 128, :], in_=y1[cs][:]), reads=[("y1", cs)], dma=("ost", cs)))

        S.flush()
        stB.close()
    return nc


def make_wcat(w_gate, w_up, w_down):
    g = w_gate.reshape(NE, 8, 128, 512).transpose(0, 2, 1, 3)
    u = w_up.reshape(NE, 8, 128, 512).transpose(0, 2, 1, 3)
    gu = np.concatenate([g, u], axis=3).reshape(NE, 128, 8192)
    dn = w_down.reshape(NE, 4, 128, 1024).transpose(0, 2, 1, 3).reshape(NE, 128, 4096)
    return np.ascontiguousarray(np.concatenate([gu, dn], axis=2).reshape(NE * 128, 12288))


def kernel(**inputs):
    f = lambda k: np.ascontiguousarray(np.asarray(inputs[k], dtype=np.float32))
    x = f("x")
    com = {
        "norm1_g": f("norm1_g")[0], "w_in": f("w_in")[0], "conv_qk": f("conv_qk")[0],
        "b_if": np.concatenate([f("b_igate")[0], f("b_fgate")[0]]), "g_mlstm_out": f("g_mlstm_out")[0],
        "g_gmlp_v": f("g_gmlp_v")[0], "w_spatial": f("w_spatial")[0], "b_spatial": f("b_spatial")[0],
        "g_gmlp_out": f("g_gmlp_out")[0], "w_out": f("w_out")[0], "norm2_g": f("norm2_g")[0],
        "w_router": np.ascontiguousarray(np.concatenate([f("w_router_group")[0], f("w_router_expert")[0]], axis=1)),
        "b_router": np.concatenate([f("b_router_group")[0], f("b_router_expert")[0]]),
        "wcat": make_wcat(f("w_gate")[0], f("w_up")[0], f("w_down")[0]), "final_g": f("final_g"),
    }
    in_maps = []
    for c in range(8):
        b, half = c // 2, c % 2
        m = dict(com)
        m["x_main"] = np.ascontiguousarray(x[b, half * 4096:(half + 1) * 4096])
        m["x_pre"] = np.ascontiguousarray(x[b, 0:4096]) if half == 1 else np.zeros((4096, D), np.float32)
        in_maps.append(m)
    nc = build(8, 8)
    res = run_bass_kernel_spmd(nc, in_maps, core_ids=list(range(8)))
    out = np.empty((4, 8192, D), np.float32)
    for c in range(8):
        out[c // 2, (c % 2) * 4096:(c % 2 + 1) * 4096] = res.results[c]["out"]
    return out
```

```python
import contextlib
import numpy as np
import concourse.bass as bass
import concourse.mybir as mybir
from concourse.bass_utils import run_bass_kernel_spmd

F32 = mybir.dt.float32
BF16 = mybir.dt.bfloat16
I32 = mybir.dt.int32
AF = mybir.ActivationFunctionType
ALU = mybir.AluOpType
AX = mybir.AxisListType

ENGS = ("pe", "act", "dve", "pool", "sp")
SEM_CH = 8000
D = 1024
EPS = 1e-6
NE = 32
NBLK_MAX = 96
NSLOT = NBLK_MAX * 128


class Sched:
    def __init__(self, nc, stack):
        self.nc = nc
        self.stack = stack
        self.ops = []
        self.last_w = {}
        self.readers = {}
        self.dma_count = {}
        self.nbuf = 0
        self.flushed = 0
        self.sems = {}
        self.eng_seq = {e: 0 for e in ENGS}
        self.waited = {e: {} for e in ENGS}
        self.cur = stack
        self.est_end = []
        self.eng_free = {e: 0.0 for e in ENGS}
        self.last_est = 0.0

    def sb(self, shape, dt, name=None, persist=False):
        self.nbuf += 1
        return (self.stack if persist else self.cur).enter_context(self.nc.sbuf_tensor(name or f"sb{self.nbuf}", list(shape), dt))

    def ps(self, shape, dt, name=None):
        self.nbuf += 1
        return self.stack.enter_context(self.nc.psum_tensor(name or f"ps{self.nbuf}", list(shape), dt))

    def op(self, eng, fn, reads=(), writes=(), accw=(), dma=None):
        i = len(self.ops)
        deps = set()
        for t in reads:
            deps.update(self.last_w.get(t, ()))
        for t in writes:
            deps.update(self.last_w.get(t, ()))
            deps.update(self.readers.get(t, ()))
        for t in accw:
            deps.update(self.readers.get(t, ()))
        for t in reads:
            self.readers.setdefault(t, []).append(i)
        for t in writes:
            self.last_w[t] = [i]
            self.readers[t] = []
        for t in accw:
            if self.readers.get(t):
                self.last_w[t] = []
                self.readers[t] = []
            self.last_w.setdefault(t, []).append(i)
        deps.discard(i)
        latest = {}
        keep = set()
        for d in deps:
            od = self.ops[d]
            if od["dma"] is not None:
                keep.add(d)
            elif d > latest.get(od["eng"], -1):
                latest[od["eng"]] = d
        deps = keep | set(latest.values())
        self.ops.append(dict(eng=eng, fn=fn, deps=sorted(deps), dma=dma, sig=None))
        cost = 2.5 if dma is not None else {"pe": 0.25, "act": 0.7, "dve": 0.6, "pool": 1.0, "sp": 0.1}[eng]
        t0 = max([self.eng_free[eng]] + [self.est_end[d] for d in deps if d < len(self.est_end)])
        if dma is not None:
            self.eng_free[eng] = t0 + 0.1
        else:
            self.eng_free[eng] = t0 + cost
        self.est_end.append(t0 + cost)
        self.last_est = t0 + cost
        return i

    def flush(self):
        nc = self.nc
        ops = self.ops
        lo = self.flushed
        last = {}
        for i in range(lo, len(ops)):
            o = ops[i]
            key = ("dma", o["dma"]) if o["dma"] is not None else ("eng", o["eng"])
            last[key] = i
        bdeps = sorted(last.values())
        for en in ENGS:
            self.ops.append(dict(eng=en, fn=lambda e: e.nop(), deps=list(bdeps), dma=None, sig=None, barrier=True))
            self.est_end.append(max(self.est_end) if self.est_end else 0.0)
        hi = len(ops)

        def pe_pair(a, b):
            return (a["eng"] == "pe" and b["eng"] == "pe" and a["dma"] is None and b["dma"] is None
                    and not b.get("barrier"))

        needed = set()
        for i in range(lo, hi):
            o = ops[i]
            for d in o["deps"]:
                if pe_pair(ops[d], o):
                    continue
                needed.add(d)
        for i in range(lo, hi):
            o = ops[i]
            if o["dma"] is not None:
                k = ("dma", o["dma"])
                self.dma_count[k] = self.dma_count.get(k, 0) + 1
                o["sig"] = (k, 16 * self.dma_count[k])
                self.get_sem(k)
            elif i in needed:
                e = o["eng"]
                n = self.eng_seq[e]
                self.eng_seq[e] += 1
                k = ("eng", e, n // SEM_CH)
                o["sig"] = (k, n % SEM_CH + 1)
                self.get_sem(k)
        sems = self.sems
        waited_all = self.waited

        def run(engname):
            def body(e):
                waited = waited_all[engname]
                for i in range(lo, hi):
                    o = ops[i]
                    if o["eng"] != engname:
                        continue
                    for d in o["deps"]:
                        od = ops[d]
                        if od["sig"] is None or pe_pair(od, o):
                            continue
                        k, v = od["sig"]
                        if waited.get(k, 0) >= v:
                            continue
                        e.wait_ge(sems[k], v)
                        waited[k] = v
                    ins = o["fn"](e)
                    if o["sig"] is not None:
                        k, v = o["sig"]
                        ins.then_inc(sems[k], 16 if o["dma"] is not None else 1)
            return body

        with nc.Block() as block:
            block.tensor(run("pe"))
            block.scalar(run("act"))
            block.vector(run("dve"))
            block.gpsimd(run("pool"))
            block.sync(run("sp"))
        self.flushed = hi
        self.last_w = {}
        self.readers = {}

    def get_sem(self, key):
        if key not in self.sems:
            self.sems[key] = self.stack.enter_context(self.nc.semaphore(f"s_{len(self.sems)}"))
        return self.sems[key]


def build(NST=8, NPRE=8, dbg=None):
    nc = bass.Bass("TRN2", target_bir_lowering=False)
    NT = NST * 4
    TOK = NST * 512

    def din(name, shape, dt=F32):
        return nc.dram_tensor(name, list(shape), dt, kind="ExternalInput")

    x_main = din("x_main", [TOK, D])
    x_pre = din("x_pre", [max(NPRE, 1) * 512, D])
    norm1_g = din("norm1_g", [D])
    w_in = din("w_in", [D, 3080])
    conv_qk = din("conv_qk", [4, 1024])
    b_if = din("b_if", [8])
    g_mlstm = din("g_mlstm_out", [512])
    g_gv = din("g_gmlp_v", [512])
    w_sp = din("w_spatial", [8, 128, 128])
    b_sp = din("b_spatial", [8, 128])
    g_go = din("g_gmlp_out", [512])
    w_out = din("w_out", [D, D])
    norm2_g = din("norm2_g", [D])
    w_r = din("w_router", [D, 36])
    b_r = din("b_router", [36])
    wcat = din("wcat", [NE * 128, 12288])
    final_g = din("final_g", [D])
    out = nc.dram_tensor("out", [TOK, D], F32, kind="ExternalOutput")
    h1buf = nc.dram_tensor("h1buf", [TOK, D], F32)
    dbg_t = {}
    if dbg:
        for k, shp in dbg.items():
            dbg_t[k] = nc.dram_tensor("dbg_" + k, list(shp), F32, kind="ExternalOutput")

    with contextlib.ExitStack() as st:
        S = Sched(nc, st)
        finals = []

        ident_f = S.sb([128, 128], F32, "ident_f")
        ident_b = S.sb([128, 128], BF16, "ident_b")
        triu_f = S.sb([128, 128], F32, "triu_f")
        triu_b = S.sb([128, 128], BF16, "triu_b")
        tril_f = S.sb([128, 128], F32, "tril_f")
        ones_f = S.sb([128, 128], F32, "ones_f")
        S.op("pool", lambda e: e.memset(ident_f[:], 0.0), writes=["ident_f"])
        S.op("pool", lambda e: e.affine_select(out=ident_f[:], in_=ident_f[:], pattern=[[-1, 128]],
                                               compare_op=ALU.not_equal, fill=1.0, base=0, channel_multiplier=1),
             reads=["ident_f"], writes=["ident_f"])
        S.op("pool", lambda e: e.memset(ones_f[:], 1.0), writes=["ones_f"])
        S.op("pool", lambda e: e.affine_select(out=triu_f[:], in_=ones_f[:], pattern=[[1, 128]],
                                               compare_op=ALU.is_ge, fill=0.0, base=0, channel_multiplier=-1),
             reads=["ones_f"], writes=["triu_f"])
        S.op("pool", lambda e: e.affine_select(out=tril_f[:], in_=ones_f[:], pattern=[[-1, 128]],
                                               compare_op=ALU.is_ge, fill=0.0, base=0, channel_multiplier=1),
             reads=["ones_f"], writes=["tril_f"])
        S.op("dve", lambda e: e.tensor_copy(out=ident_b[:], in_=ident_f[:]), reads=["ident_f"], writes=["ident_b"])
        S.op("dve", lambda e: e.tensor_copy(out=triu_b[:], in_=triu_f[:]), reads=["triu_f"], writes=["triu_b"])

        def bload(name, src, n, eng="sp"):
            t = S.sb([128, n], F32, name)
            S.op(eng, lambda e: e.dma_start(out=t[:], in_=bass.AP(src, 0, [[0, 128], [1, n]])),
                 writes=[name], dma=name)
            return t

        gml_b = bload("gml_b", g_mlstm, 512)
        ggv_b = bload("ggv_b", g_gv, 512)
        ggo_b = bload("ggo_b", g_go, 512)
        bif_b = bload("bif_b", b_if, 8)

        g1col = S.sb([128, 8], F32, "g1col")
        cw = S.sb([128, 4, 8], F32, "cw")
        bsp = S.sb([128, 8], F32, "bsp")
        S.op("sp", lambda e: e.dma_start(out=g1col[:], in_=norm1_g.ap().rearrange("(c p) -> p c", p=128),
                                         allow_slow_non_contiguous=True), writes=["g1col"], dma="g1col")
        for i in range(4):
            S.op("sp", lambda e, i=i: e.dma_start(out=cw[:, i, :], in_=conv_qk.ap()[i, :].rearrange("(c p) -> p c", p=128),
                                                  allow_slow_non_contiguous=True), accw=["cw"], dma=("cw", i))
        S.op("sp", lambda e: e.dma_start(out=bsp[:], in_=b_sp.ap().rearrange("g t -> t g"),
                                         allow_slow_non_contiguous=True), writes=["bsp"], dma="bsp")

        ptr = [S.ps([128, 1024], BF16, f"ptr{i}") for i in range(2)]
        pfb = [S.ps([128, 512], F32, f"pf{i}") for i in range(6)]
        rr = {"t": 0, "f": 0, "A": 0, "B": 0, "A2": 0}

        def tbank():
            i = rr["t"] % 2
            rr["t"] += 1
            return ptr[i], ("ptr", i)

        def fbank():
            i = rr["f"] % 6
            rr["f"] += 1
            return pfb[i], ("pf", i)

        def fbankA():
            return pfb[0], ("pf", 0)

        def fbankA2():
            i = 1 + rr["A2"] % 2
            rr["A2"] += 1
            return pfb[i], ("pf", i)

        def fbankB():
            i = 3 + rr["B"] % 2
            rr["B"] += 1
            return pfb[i], ("pf", i)

        def fbankC():
            return pfb[5], ("pf", 5)

        junk = S.sb([128, 1024], BF16, "junk", persist=True)
        base_b = S.sb([128, 32], F32, "base_b", persist=True)
        E1s = S.sb([128, NT, 32], BF16, "E1s", persist=True)
        E2s = S.sb([128, NT, 32], BF16, "E2s", persist=True)
        pw = S.sb([128, NT, 4], F32, "pw", persist=True)
        stA = contextlib.ExitStack()
        S.cur = stA
        xbuf = [S.sb([128, 4, 1024], F32, f"xbuf{i}") for i in range(2)]
        wqk = S.sb([128, 8, 1024], BF16, "wqk")
        wvo = S.sb([128, 8, 1024], BF16, "wvo")
        wuv = S.sb([128, 8, 1024], BF16, "wuv")
        wif = S.sb([128, 8, 8], BF16, "wif")
        wout = S.sb([128, 8, 1024], BF16, "wout")
        for kc in range(8):
            sl = kc % 2
            stg = xbuf[sl][:].rearrange("p a b -> p (a b)")
            S.op("sp", lambda e, stg=stg, kc=kc: e.dma_start(out=stg[:, 0:3080], in_=w_in.ap()[kc * 128:(kc + 1) * 128, :]),
                 writes=[("x", sl)], dma=("x", sl))
            for (dst, c0, n, tok) in ((wqk, 0, 1024, "wqk"), (wvo, 1024, 1024, "wvo"), (wif, 2048, 8, "wif"), (wuv, 2056, 1024, "wuv")):
                S.op("act", lambda e, dst=dst, c0=c0, n=n, kc=kc, stg=stg: e.mul(dst[:, kc, 0:n], stg[:, c0:c0 + n], g1col[:, kc:kc + 1]),
                     reads=[("x", sl), "g1col"], accw=[tok])
        S.op("pool", lambda e: e.dma_start(out=wout[:], in_=w_out.ap().rearrange("(c p) n -> p c n", p=128)),
             writes=["wout"], dma="wout")

        wsp_f = xbuf[1][:, 3, :].rearrange("p (g s) -> p g s", g=8)
        wspT = S.sb([128, 8, 128], BF16, "wspT")
        S.op("sp", lambda e: e.dma_start(out=wsp_f, in_=w_sp.ap().rearrange("g t s -> t g s")), writes=[("x", 1)], dma=("x", 1))
        S.op("dve", lambda e: e.tensor_tensor(out=wsp_f, in0=wsp_f, in1=tril_f[:].unsqueeze(1).broadcast_to([128, 8, 128]), op=ALU.mult),
             reads=[("x", 1), "tril_f"], writes=[("x", 1)])
        for half in range(2):
            pb, pt = fbank()
            for g4 in range(4):
                g = half * 4 + g4
                S.op("pe", lambda e, pb=pb, g=g, g4=g4: e.transpose(out=pb[:, g4 * 128:(g4 + 1) * 128], in_=wsp_f[:, g, :], identity=ident_f[:]),
                     reads=[("x", 1), "ident_f"], accw=[pt])
            S.op("dve", lambda e, pb=pb, half=half: e.tensor_copy(out=wspT[:, half * 4:(half + 1) * 4, :].rearrange("p a b -> p (a b)"), in_=pb[:]),
                 reads=[pt], accw=["wspT"])

        C32 = S.sb([128, 4, 129], F32, "C32")
        Csb = S.sb([128, 4, 129], BF16, "Csb")
        m_st = S.sb([4, 1], F32, "m_st")
        halo = S.sb([128, 8, 3], F32, "halo")
        S.op("pool", lambda e: e.memset(C32[:], 0.0), writes=[("C32", h) for h in range(4)])
        S.op("pool", lambda e: e.memset(m_st[:], 0.0), writes=["m_st"])
        S.op("pool", lambda e: e.memset(halo[:], 0.0), writes=[("halo", c) for c in range(8)])

        xn = [S.sb([128, 1024], BF16, f"xn{i}") for i in range(2)]
        ss1 = S.sb([128, 8], F32, "ss1")
        xT = [S.sb([128, 8, 512], BF16, f"xT{i}") for i in range(1)]
        pre = [S.sb([128, 515], F32, f"pre{i}") for i in range(2)]
        cacc = [S.sb([128, 512], F32, f"cacc{i}") for i in range(2)]
        sg = [S.sb([128, 512], F32, f"sg{i}") for i in range(2)]
        qkT = [S.sb([128, 8, 512], BF16, f"qkT{i}") for i in range(1)]
        ktm2 = [S.sb([128, 4, 128], BF16, f"ktm{i}") for i in range(2)]
        gsb = S.sb([128, 4, 8], F32, "gsb")
        spl = S.sb([128, 4, 4], F32, "spl")
        a_tm = S.sb([128, 4, 4], F32, "a_tm")
        cum_sb = S.sb([128, 4, 4], F32, "cum_sb")
        e_tm = S.sb([128, 4, 4], F32, "e_tm")
        dn_tm = S.sb([128, 4, 4], F32, "dn_tm")
        gb_sb = S.sb([128, 4, 8], F32, "gb_sb")
        rw = S.sb([4, 20], F32, "rw")
        rhs8 = S.sb([4, 4, 8], F32, "rhs8")
        lnc0_t = S.sb([128, 1], F32, "lnc0_t")
        S.op("pool", lambda e: e.memset(lnc0_t[:], -float(np.log(128 ** -0.5))), writes=["lnc0"])
        ve = [S.sb([128, 4, 129], BF16, f"ve{i}") for i in range(2)]
        smask = [S.sb([128, 4, 128], BF16, f"smask{i}") for i in range(2)]
        og2 = [S.sb([128, 512], F32, f"og{i}") for i in range(2)]
        hs = S.sb([128, 4, 128], F32, "hs")
        nqa = S.sb([128, 4], F32, "nqa")
        sm8 = S.sb([128, 16], F32, "sm8")
        tmpa = S.sb([128, 1024], F32, "tmpa")
        ux = S.sb([128, 1024], F32, "ux")
        t1 = S.sb([128, 1024], F32, "t1")
        vn = S.sb([128, 512], BF16, "vn")
        gt = S.sb([128, 8, 64], F32, "gt")
        ybuf = [S.sb([128, 1024], BF16, f"ybuf{i}") for i in range(2)]
        yT = [S.sb([128, 8, 128], BF16, f"yT{i}") for i in range(2)]
        h1b = [S.sb([128, 1024], F32, f"h1b{i}") for i in range(2)]
        g2_b = bload("g2_b", norm2_g, 1024)
        br_b = bload("br_b", b_r, 36)
        wr_f = S.sb([128, 8, 36], F32, "wr_f")
        wr_b = S.sb([128, 8, 36], BF16, "wr_b")
        S.op("sp", lambda e: e.dma_start(out=wr_f[:], in_=w_r.ap().rearrange("(c p) n -> p c n", p=128)), writes=["wr_f"], dma="wr_f")
        S.op("dve", lambda e: e.tensor_copy(out=wr_b[:], in_=wr_f[:]), reads=["wr_f"], writes=["wr_b"])
        lstr_b = S.sb([128, 128], BF16, "lstr_b")
        ones_b = S.sb([128, 128], BF16, "ones_b")
        lstr_f = S.sb([128, 128], F32, "lstr_f")
        S.op("pool", lambda e: e.affine_select(out=lstr_f[:], in_=ones_f[:], pattern=[[1, 128]], compare_op=ALU.is_gt, fill=0.0, base=0, channel_multiplier=-1),
             reads=["ones_f"], writes=["lstr_f"])
        S.op("dve", lambda e: e.tensor_copy(out=lstr_b[:], in_=lstr_f[:]), reads=["lstr_f"], writes=["lstr_b"])
        S.op("dve", lambda e: e.tensor_copy(out=ones_b[:], in_=ones_f[:]), reads=["ones_f"], writes=["ones_b"])
        xn2t = [S.sb([128, 1024], BF16, f"xn2t{i}") for i in range(2)]
        xn2T = [S.sb([128, 8, 128], BF16, f"xn2T{i}") for i in range(1)]
        lg = S.sb([128, 36], F32, "lg")
        rt = S.sb([128, 64], F32, "rt")
        t32 = S.sb([128, 4, 8], F32, "t32")
        le8 = S.sb([128, 16], F32, "le8")
        ind_b = S.sb([128, 32], BF16, "ind_b")
        posf = S.sb([128, 32], F32, "posf")
        S.op("pool", lambda e: e.memset(base_b[:], 0.0), writes=["base_b"])
        xn2lin = nc.dram_tensor("xn2lin", [TOK, D], BF16)
        cnt = {"tile": 0, "st": 0, "ch": 0, "tl2": 0, "y": 0}

        LN_C0 = float(np.log(128 ** -0.5))

        def dump(name, ap_fn, reads, row0=0):
            if name in dbg_t:
                t = dbg_t[name]
                finals.append(S.op("sp", lambda e: e.dma_start(out=t.ap()[row0:row0 + 128, :], in_=ap_fn()), reads=reads, dma=("dbg", name)))

        plan_x = [(x_pre, i) for i in range(NPRE)] + [(x_main, i) for i in range(NST)]

        def xload(g):
            src_, i_ = plan_x[g]
            sl_ = g % 2
            S.op("sp", lambda e: e.dma_start(out=xbuf[sl_][:], in_=src_.ap()[i_ * 512:(i_ + 1) * 512, :].rearrange("(j p) d -> p j d", p=128)),
                 writes=[("x", sl_)], dma=("x", sl_))
            conv_some(g, sl_)

        def supertile(xsrc, st_i, mode, gi):
            main = mode == "main"
            sl = cnt["st"] % 2
            cnt["st"] += 1
            xb = xbuf[sl]
            xt = xT[0]
            qk = qkT[0]
            if gi == 0:
                xload(0)
            for j in range(4):
                tl = cnt["tile"] % 2
                cnt["tile"] += 1
                xnj = xn[tl]
                S.op("dve", lambda e, j=j: e.memset(ss1[:, j:j + 1], 0.0), writes=[("ss1", j)])
                S.op("act", lambda e, j=j: e.activation(out=junk[:], in_=xb[:, j, :], func=AF.Square, accum_out=ss1[:, j:j + 1]),
                     reads=[("x", sl), ("ss1", j)], writes=["junk", ("ss1", j)])
                S.op("act", lambda e, j=j: e.activation(out=ss1[:, 4 + j:5 + j], in_=ss1[:, j:j + 1], func=AF.Ln, scale=1.0 / D, bias=EPS),
                     reads=[("ss1", j)], writes=[("ss1", 4 + j)])
                S.op("act", lambda e, j=j: e.activation(out=ss1[:, 4 + j:5 + j], in_=ss1[:, 4 + j:5 + j], func=AF.Exp, scale=-0.5),
                     reads=[("ss1", 4 + j)], writes=[("ss1", 4 + j)])
                S.op("act", lambda e, j=j, xnj=xnj: e.mul(xnj[:], xb[:, j, :], ss1[:, 4 + j:5 + j]),
                     reads=[("x", sl), ("ss1", 4 + j)], writes=[("xn", tl)])
                pb, pt = tbank()
                for c in range(8):
                    S.op("pe", lambda e, c=c, pb=pb, xnj=xnj: e.transpose(out=pb[:, c * 128:(c + 1) * 128], in_=xnj[:, c * 128:(c + 1) * 128], identity=ident_b[:]),
                         reads=[("xn", tl), "ident_b"], accw=[pt])
                S.op("dve" if j % 2 == 0 else "act",
                     (lambda e, pb=pb, j=j: e.tensor_copy(out=xt[:, :, j * 128:(j + 1) * 128], in_=pb[:].rearrange("p (c t) -> p c t", c=8))) if j % 2 == 0 else
                     (lambda e, pb=pb, j=j: e.copy(out=xt[:, :, j * 128:(j + 1) * 128], in_=pb[:].rearrange("p (c t) -> p c t", c=8))),
                     reads=[pt], accw=[("xT", 0)])
            chunks = range(8) if mode != "pre" else range(4, 8)
            def chunk_s1(ch):
                pb, pt = fbank()
                for kc in range(8):
                    S.op("pe", lambda e, kc=kc, ch=ch, pb=pb: e.matmul(out=pb[:], lhsT=wqk[:, kc, ch * 128:(ch + 1) * 128], rhs=xt[:, kc, :], start=(kc == 0), stop=(kc == 7)),
                         reads=[("xT", 0), "wqk"], accw=[pt])
                ps_ = cnt["ch"] % 2
                cnt["ch"] += 1
                pr = pre[ps_]
                ca = cacc[ps_]
                s_ = sg[ps_]
                S.op("dve", lambda e, pr=pr, ch=ch: e.tensor_copy(out=pr[:, 0:3], in_=halo[:, ch, :]), reads=[("halo", ch)], writes=[("pre", ps_, "h")])
                S.op("act", lambda e, pr=pr, pb=pb: e.copy(out=pr[:, 3:515], in_=pb[:]), reads=[pt], writes=[("pre", ps_)])
                S.op("dve", lambda e, pr=pr, ch=ch: e.tensor_copy(out=halo[:, ch, :], in_=pr[:, 512:515]), reads=[("pre", ps_), ("pre", ps_, "h")], writes=[("halo", ch)])
                S.op("dve", lambda e, pr=pr, ca=ca, ch=ch: e.tensor_scalar(out=ca[:], in0=pr[:, 0:512], scalar1=cw[:, 0, ch:ch + 1], scalar2=None, op0=ALU.mult),
                     reads=[("pre", ps_), ("pre", ps_, "h"), "cw"], writes=[("cacc", ps_)])
                for i in range(1, 4):
                    S.op("dve", lambda e, pr=pr, ca=ca, ch=ch, i=i: e.scalar_tensor_tensor(out=ca[:], in0=pr[:, i:i + 512], scalar=cw[:, i, ch:ch + 1], in1=ca[:], op0=ALU.mult, op1=ALU.add),
                         reads=[("pre", ps_), ("pre", ps_, "h"), ("cacc", ps_), "cw"], writes=[("cacc", ps_)])
                return (ch, ps_, ca, s_)

            def chunk_s2(args):
                ch, ps_, ca, s_ = args
                S.op("act", lambda e, ca=ca, s_=s_: e.activation(out=s_[:], in_=ca[:], func=AF.Exp, scale=-1.0), reads=[("cacc", ps_)], writes=[("sg", ps_)])
                S.op("act", lambda e, s_=s_: e.activation(out=s_[:], in_=s_[:], func=AF.Ln, bias=1.0), reads=[("sg", ps_)], writes=[("sg", ps_)])
                S.op("act", lambda e, s_=s_: e.activation(out=s_[:], in_=s_[:], func=AF.Exp, scale=-1.0), reads=[("sg", ps_)], writes=[("sg", ps_)])
                S.op("dve", lambda e, s_=s_, ca=ca, ch=ch: e.tensor_tensor(out=qk[:, ch, :], in0=s_[:], in1=ca[:], op=ALU.mult),
                     reads=[("sg", ps_), ("cacc", ps_)], accw=[("qkT", 0)])


            pend = None
            for ch in chunks:
                cur = chunk_s1(ch)
                if pend is not None:
                    chunk_s2(pend)
                pend = cur
            chunk_s2(pend)
            pg, pgt = fbank()
            for j in range(4):
                for kc in range(8):
                    S.op("pe", lambda e, j=j, kc=kc: e.matmul(out=pg[:, j * 8:(j + 1) * 8], lhsT=xt[:, kc, j * 128:(j + 1) * 128], rhs=wif[:, kc, :], start=(kc == 0), stop=(kc == 7)),
                         reads=[("xT", 0), "wif"], accw=[pgt])
            S.op("dve", lambda e: e.tensor_tensor(out=gsb[:], in0=pg[:, 0:32].rearrange("p (c g) -> p c g", c=4), in1=bif_b[:].unsqueeze(1).broadcast_to([128, 4, 8]), op=ALU.add),
                 reads=[pgt, "bif_b"], writes=["gsb"])
            S.op("act", lambda e: e.activation(out=spl[:], in_=gsb[:, :, 4:8], func=AF.Exp, scale=-1.0), reads=["gsb"], writes=["spl"])
            S.op("act", lambda e: e.activation(out=spl[:], in_=spl[:], func=AF.Ln, bias=1.0), reads=["spl"], writes=["spl"])
            pc, pct = fbank()
            prw, prt = fbank()
            for c in range(4):
                S.op("pe", lambda e, c=c: e.matmul(out=pc[:, c * 4:(c + 1) * 4], lhsT=triu_f[:], rhs=spl[:, c, :], start=True, stop=True),
                     reads=["spl", "triu_f"], accw=[pct])
                S.op("pe", lambda e, c=c: e.matmul(out=pc[0:4, 16 + c:17 + c], lhsT=spl[:, c, :], rhs=ones_f[:, 0:1], start=True, stop=True),
                     reads=["spl", "ones_f"], accw=[pct])
                S.op("pe", lambda e, c=c: e.matmul(out=prw[0:4, c * 128:(c + 1) * 128], lhsT=gsb[:, c, 0:4], rhs=ident_f[:], start=True, stop=False),
                     reads=["gsb", "ident_f"], accw=[prt])
                S.op("pe", lambda e, c=c: e.matmul(out=prw[0:4, c * 128:(c + 1) * 128], lhsT=spl[:, c, :], rhs=triu_f[:], start=False, stop=True),
                     reads=["spl", "triu_f"], accw=[prt])
            S.op("dve", lambda e: e.tensor_tensor(out=a_tm[:], in0=gsb[:, :, 0:4], in1=pc[:, 0:16].rearrange("p (c h) -> p c h", c=4), op=ALU.add),
                 reads=["gsb", pct], writes=["a_tm"])
            S.op("dve", lambda e: e.tensor_copy(out=cum_sb[:], in_=pc[:, 0:16].rearrange("p (c h) -> p c h", c=4)), reads=[pct], writes=["cum_sb"])
            S.op("dve", lambda e: e.tensor_copy(out=rw[:, 4:8], in_=pc[0:4, 16:20]), reads=[pct], writes=["rw_tot"])
            S.op("dve", lambda e: e.tensor_reduce(out=rw[:, 0:4], in_=prw[0:4, :].rearrange("p (c l) -> p c l", c=4), axis=AX.X, op=ALU.max),
                 reads=[prt], writes=["rw_A"])
            for c in range(4):
                S.op("dve", lambda e, c=c: e.tensor_copy(out=rw[:, 12 + c:13 + c], in_=m_st[:]), reads=["m_st"], writes=[("rw_mp", c)])
                S.op("dve", lambda e, c=c: e.tensor_tensor(out=rw[:, 8 + c:9 + c], in0=m_st[:], in1=rw[:, c:c + 1], op=ALU.max), reads=["m_st", "rw_A"], writes=[("rw_G", c)])
                S.op("dve", lambda e, c=c: e.tensor_tensor(out=m_st[:], in0=rw[:, 8 + c:9 + c], in1=rw[:, 4 + c:5 + c], op=ALU.subtract), reads=[("rw_G", c), "rw_tot"], writes=["m_st"])
            S.op("dve", lambda e: e.tensor_tensor(out=rw[:, 16:20], in0=rw[:, 12:16], in1=rw[:, 8:12], op=ALU.subtract),
                 reads=[("rw_G", c) for c in range(4)] + [("rw_mp", c) for c in range(4)], writes=["rw_D"])
            S.op("act", lambda e: e.activation(out=rw[:, 16:20], in_=rw[:, 16:20], func=AF.Exp), reads=["rw_D"], writes=["rw_D"])
            S.op("dve", lambda e: e.tensor_tensor(out=rhs8[:, :, 0:4], in0=ident_f[0:4, 0:4].unsqueeze(1).broadcast_to([4, 4, 4]), in1=rw[:, 8:12].unsqueeze(2).broadcast_to([4, 4, 4]), op=ALU.mult),
                 reads=[("rw_G", c) for c in range(4)] + ["ident_f"], writes=["rhs8a"])
            S.op("dve", lambda e: e.tensor_tensor(out=rhs8[:, :, 4:8], in0=ident_f[0:4, 0:4].unsqueeze(1).broadcast_to([4, 4, 4]), in1=rw[:, 16:20].unsqueeze(2).broadcast_to([4, 4, 4]), op=ALU.mult),
                 reads=["rw_D", "ident_f"], writes=["rhs8b"])
            S.op("pe", lambda e: e.matmul(out=pc[:, 32:64], lhsT=ones_f[0:4, :], rhs=rhs8[:].rearrange("p c g -> p (c g)"), start=True, stop=True),
                 reads=["rhs8a", "rhs8b", "ones_f"], accw=[pct])
            S.op("dve", lambda e: e.tensor_copy(out=gb_sb[:], in_=pc[:, 32:64].rearrange("p (c g) -> p c g", c=4)), reads=[pct], writes=["gb_sb"])
            S.op("dve", lambda e: e.tensor_tensor(out=e_tm[:], in0=a_tm[:], in1=gb_sb[:, :, 0:4], op=ALU.subtract), reads=["a_tm", "gb_sb"], writes=["e_tm"])
            S.op("act", lambda e: e.activation(out=e_tm[:], in_=e_tm[:], func=AF.Exp), reads=["e_tm"], writes=["e_tm"])
            S.op("dve", lambda e: e.tensor_tensor(out=dn_tm[:], in0=cum_sb[:], in1=gb_sb[:, :, 0:4], op=ALU.subtract), reads=["cum_sb", "gb_sb"], writes=["dn_tm"])
            S.op("act", lambda e: e.activation(out=dn_tm[:], in_=dn_tm[:], func=AF.Exp, bias=lnc0_t[:, 0:1]), reads=["dn_tm", "lnc0"], writes=["dn_tm"])

            if gi + 1 < NPRE + NST:
                xload(gi + 1)
            def tile_body(j):
                c = j
                csl = slice(c * 128, (c + 1) * 128)
                tsl = cnt["tl2"] % 2
                cnt["tl2"] += 1
                vea = ve[tsl]
                sm = smask[tsl]
                ktm = ktm2[tsl]
                og = og2[tsl]
                ysl = cnt["y"] % 2
                if main:
                    cnt["y"] += 1
                yt_ = ybuf[ysl]
                def chainA1():
                    pv, pvt = fbankA()
                    for kc in range(8):
                        yield S.op("pe", lambda e, kc=kc, pv=pv: e.matmul(out=pv[:], lhsT=xt[:, kc, csl], rhs=wvo[:, kc, 0:512], start=(kc == 0), stop=(kc == 7)),
                             reads=[("xT", 0), "wvo"], accw=[pvt])
                    yield S.op("dve", lambda e, pv=pv, vea=vea: e.tensor_tensor(out=vea[:, :, 0:128], in0=pv[:].rearrange("p (h d) -> p h d", h=4), in1=e_tm[:, c, :].unsqueeze(2).broadcast_to([128, 4, 128]), op=ALU.mult),
                         reads=[pvt, "e_tm"], writes=[("ve", tsl)])
                    yield S.op("dve", lambda e, vea=vea: e.tensor_copy(out=vea[:, :, 128:129], in_=e_tm[:, c, :].unsqueeze(2)), reads=["e_tm"], writes=[("ve", tsl, "e")])
                    pb, pt = (ptr[0], ("ptr", 0))
                    for h in range(4):
                        yield S.op("pe", lambda e, h=h, pb=pb: e.transpose(out=pb[:, h * 128:(h + 1) * 128], in_=qk[:, 4 + h, csl], identity=ident_b[:]),
                             reads=[("qkT", 0), "ident_b"], accw=[pt])
                    yield S.op("act", lambda e, pb=pb: e.copy(out=ktm[:].rearrange("p h d -> p (h d)"), in_=pb[:, 0:512]), reads=[pt], writes=[("ktm", tsl)])
                    if main:
                        po, pot = fbankA()
                        for kc in range(8):
                            yield S.op("pe", lambda e, kc=kc, po=po: e.matmul(out=po[:], lhsT=xt[:, kc, csl], rhs=wvo[:, kc, 512:1024], start=(kc == 0), stop=(kc == 7)),
                                 reads=[("xT", 0), "wvo"], accw=[pot])
                        yield S.op("act", lambda e, po=po: e.activation(out=og[:], in_=po[:], func=AF.Exp, scale=-1.0), reads=[pot], writes=[("og", tsl)])
                        yield S.op("act", lambda e: e.activation(out=og[:], in_=og[:], func=AF.Ln, bias=1.0), reads=[("og", tsl)], writes=[("og", tsl)])
                        yield S.op("act", lambda e: e.activation(out=og[:], in_=og[:], func=AF.Exp, scale=-1.0), reads=[("og", tsl)], writes=[("og", tsl)])
                        pS, pSt = fbankA()
                        for h in range(4):
                            yield S.op("pe", lambda e, h=h, pS=pS: e.matmul(out=pS[:, h * 128:(h + 1) * 128], lhsT=qk[:, 4 + h, csl], rhs=qk[:, h, csl], start=True, stop=True),
                                 reads=[("qkT", 0)], accw=[pSt])
                        sm = smask[tsl]
                        yield S.op("dve", lambda e, pS=pS, sm=sm: e.tensor_tensor(out=sm[:], in0=pS[:].rearrange("p (h l) -> p h l", h=4), in1=triu_f[:].unsqueeze(1).broadcast_to([128, 4, 128]), op=ALU.mult),
                             reads=[pSt, "triu_f"], writes=[("smask", tsl)])
                    yield None
                def chainA2():
                    if main:
                        for h in range(4):
                            yield S.op("act", lambda e, h=h: e.mul(Csb[:, h, :], C32[:, h, :], gb_sb[:, c, 4 + h:5 + h]), reads=[("C32", h), "gb_sb"], writes=[("Csb", h)])
                        nbanks = []
                        for hp in range(2):
                            pn, pnt = fbankA2()
                            nbanks.append((pn, pnt))
                            for hh in range(2):
                                h = hp * 2 + hh
                                yield S.op("pe", lambda e, h=h, hh=hh, pn=pn, sm=sm, vea=vea: e.matmul(out=pn[:, hh * 129:(hh + 1) * 129], lhsT=sm[:, h, :], rhs=vea[:, h, :], start=True, stop=False),
                                     reads=[("smask", tsl), ("ve", tsl), ("ve", tsl, "e")], accw=[pnt])
                                yield S.op("pe", lambda e, h=h, hh=hh, pn=pn: e.matmul(out=pn[:, hh * 129:(hh + 1) * 129], lhsT=qk[:, h, csl], rhs=Csb[:, h, :], start=False, stop=True),
                                     reads=[("qkT", 0), ("Csb", h)], accw=[pnt])
                        for hp in range(2):
                            pn, pnt = nbanks[hp]
                            yield S.op("act", lambda e, pn=pn, hp=hp: e.activation(out=nqa[:, hp * 2:hp * 2 + 2].unsqueeze(2), in_=pn[:, 0:258].rearrange("p (h d) -> p h d", h=2)[:, :, 128:129], func=AF.Abs),
                                 reads=[pnt], writes=["nqa"])
                        yield S.op("dve", lambda e: e.tensor_tensor(out=nqa[:], in0=nqa[:], in1=dn_tm[:, c, :], op=ALU.max), reads=["nqa", "dn_tm"], writes=["nqa"])
                        yield S.op("dve", lambda e: e.reciprocal(out=nqa[:], in_=nqa[:]), reads=["nqa"], writes=["nqa"])
                        for hp in range(2):
                            pn, pnt = nbanks[hp]
                            yield S.op("dve", lambda e, pn=pn, hp=hp: e.tensor_tensor(out=hs[:, hp * 2:hp * 2 + 2, :], in0=pn[:, 0:258].rearrange("p (h d) -> p h d", h=2)[:, :, 0:128], in1=nqa[:, hp * 2:hp * 2 + 2].unsqueeze(2).broadcast_to([128, 2, 128]), op=ALU.mult),
                                 reads=[pnt, "nqa"], writes=["hs"])
                        yield S.op("dve", lambda e: e.tensor_tensor(out=hs[:].rearrange("p h d -> p (h d)"), in0=hs[:].rearrange("p h d -> p (h d)"), in1=og[:], op=ALU.mult),
                             reads=["hs", ("og", tsl)], writes=["hs"])
                        yield S.op("dve", lambda e: e.tensor_tensor(out=tmpa[:, 0:512], in0=hs[:].rearrange("p h d -> p (h d)"), in1=hs[:].rearrange("p h d -> p (h d)"), op=ALU.mult), reads=["hs"], writes=["tmpa"])
                        yield S.op("dve", lambda e: e.tensor_reduce(out=sm8[:, 0:4], in_=tmpa[:, 0:512].rearrange("p (h d) -> p h d", h=4), axis=AX.X, op=ALU.add), reads=["tmpa"], writes=["sm8a"])
                        yield S.op("act", lambda e: e.activation(out=sm8[:, 0:4], in_=sm8[:, 0:4], func=AF.Ln, scale=1.0 / 128, bias=EPS), reads=["sm8a"], writes=["sm8a"])
                        yield S.op("act", lambda e: e.activation(out=sm8[:, 0:4], in_=sm8[:, 0:4], func=AF.Exp, scale=-0.5), reads=["sm8a"], writes=["sm8a"])
                        yield S.op("dve", lambda e: e.tensor_tensor(out=hs[:], in0=hs[:], in1=sm8[:, 0:4].unsqueeze(2).broadcast_to([128, 4, 128]), op=ALU.mult), reads=["hs", "sm8a"], writes=["hs"])
                        yield S.op("dve", lambda e, yt_=yt_: e.tensor_tensor(out=yt_[:, 0:512], in0=hs[:].rearrange("p h d -> p (h d)"), in1=gml_b[:], op=ALU.mult), reads=["hs", "gml_b"], writes=[("y", ysl, 0)])
                    for hp in range(2):
                        pu_, put = fbankA2()
                        for hh in range(2):
                            h = hp * 2 + hh
                            yield S.op("pe", lambda e, h=h, hh=hh, pu_=pu_, vea=vea: e.matmul(out=pu_[:, hh * 129:(hh + 1) * 129], lhsT=ktm[:, h, :], rhs=vea[:, h, :], start=True, stop=True),
                                 reads=[("ktm", tsl), ("ve", tsl), ("ve", tsl, "e")], accw=[put])
                        for hh in range(2):
                            h = hp * 2 + hh
                            yield S.op("dve", lambda e, h=h, hh=hh, pu_=pu_: e.scalar_tensor_tensor(out=C32[:, h, :], in0=C32[:, h, :], scalar=gb_sb[:, c, 4 + h:5 + h], in1=pu_[:, hh * 129:(hh + 1) * 129], op0=ALU.mult, op1=ALU.add),
                                 reads=[put, "gb_sb", ("C32", h)], writes=[("C32", h)])

                    yield None
                def chainB():
                    pU, pUt = fbankB()
                    pV, pVt = fbankB()
                    for kc in range(8):
                        yield S.op("pe", lambda e, kc=kc, pU=pU: e.matmul(out=pU[:], lhsT=xt[:, kc, csl], rhs=wuv[:, kc, 0:512], start=(kc == 0), stop=(kc == 7)),
                             reads=[("xT", 0), "wuv"], accw=[pUt])
                    for kc in range(8):
                        yield S.op("pe", lambda e, kc=kc, pV=pV: e.matmul(out=pV[:], lhsT=xt[:, kc, csl], rhs=wuv[:, kc, 512:1024], start=(kc == 0), stop=(kc == 7)),
                             reads=[("xT", 0), "wuv"], accw=[pVt])
                    yield S.op("act", lambda e, pU=pU: e.copy(out=ux[:, 0:512], in_=pU[:]), reads=[pUt], writes=["ux"])
                    yield S.op("act", lambda e, pV=pV: e.copy(out=ux[:, 512:1024], in_=pV[:]), reads=[pVt], writes=["ux"])
                    yield S.op("act", lambda e: e.activation(out=t1[:], in_=ux[:], func=AF.Square), reads=["ux"], writes=["t1"])
                    yield S.op("dve", lambda e: e.tensor_scalar(out=t1[:], in0=t1[:], scalar1=0.044715, scalar2=1.0, op0=ALU.mult, op1=ALU.add), reads=["t1"], writes=["t1"])
                    yield S.op("dve", lambda e: e.tensor_tensor(out=t1[:], in0=t1[:], in1=ux[:], op=ALU.mult), reads=["t1", "ux"], writes=["t1"])
                    yield S.op("act", lambda e: e.activation(out=t1[:], in_=t1[:], func=AF.Exp, scale=-2.0 * 0.7978845608028654), reads=["t1"], writes=["t1"])
                    yield S.op("act", lambda e: e.activation(out=t1[:], in_=t1[:], func=AF.Ln, bias=1.0), reads=["t1"], writes=["t1"])
                    yield S.op("act", lambda e: e.activation(out=t1[:], in_=t1[:], func=AF.Exp, scale=-1.0), reads=["t1"], writes=["t1"])
                    yield S.op("dve", lambda e: e.tensor_tensor(out=ux[:], in0=t1[:], in1=ux[:], op=ALU.mult), reads=["t1", "ux"], writes=["ux"])
                    yield S.op("dve", lambda e: e.memset(sm8[:, 4:5], 0.0), writes=["sm8b"])
                    yield S.op("act", lambda e: e.activation(out=junk[:, 0:512], in_=ux[:, 512:1024], func=AF.Square, accum_out=sm8[:, 4:5]), reads=["ux", "sm8b"], writes=["junk", "sm8b"])
                    yield S.op("act", lambda e: e.activation(out=sm8[:, 4:5], in_=sm8[:, 4:5], func=AF.Ln, scale=1.0 / 512, bias=EPS), reads=["sm8b"], writes=["sm8b"])
                    yield S.op("act", lambda e: e.activation(out=sm8[:, 4:5], in_=sm8[:, 4:5], func=AF.Exp, scale=-0.5), reads=["sm8b"], writes=["sm8b"])
                    yield S.op("dve", lambda e: e.scalar_tensor_tensor(out=vn[:], in0=ux[:, 512:1024], scalar=sm8[:, 4:5], in1=ggv_b[:], op0=ALU.mult, op1=ALU.mult), reads=["ux", "sm8b", "ggv_b"], writes=["vn"])
                    pM, pMt = fbankB()
                    for g in range(8):
                        yield S.op("pe", lambda e, g=g, pM=pM: e.matmul(out=pM[:, g * 64:(g + 1) * 64], lhsT=wspT[:, g, :], rhs=vn[:, g * 64:(g + 1) * 64], start=True, stop=True),
                             reads=["vn", "wspT"], accw=[pMt])
                    yield S.op("dve", lambda e, pM=pM: e.tensor_tensor(out=gt[:], in0=pM[:].rearrange("p (g c) -> p g c", g=8), in1=bsp[:].unsqueeze(2).broadcast_to([128, 8, 64]), op=ALU.add),
                         reads=[pMt, "bsp"], writes=["gt"])
                    yield S.op("dve", lambda e: e.tensor_tensor(out=gt[:].rearrange("p g c -> p (g c)"), in0=gt[:].rearrange("p g c -> p (g c)"), in1=ux[:, 0:512], op=ALU.mult), reads=["gt", "ux"], writes=["gt"])
                    yield S.op("dve", lambda e: e.tensor_tensor(out=tmpa[:, 512:1024], in0=gt[:].rearrange("p g c -> p (g c)"), in1=gt[:].rearrange("p g c -> p (g c)"), op=ALU.mult), reads=["gt"], writes=["tmpb"])
                    yield S.op("dve", lambda e: e.tensor_reduce(out=sm8[:, 8:16], in_=tmpa[:, 512:1024].rearrange("p (g c) -> p g c", g=8), axis=AX.X, op=ALU.add), reads=["tmpb"], writes=["sm8c"])
                    yield S.op("act", lambda e: e.activation(out=sm8[:, 8:16], in_=sm8[:, 8:16], func=AF.Ln, scale=1.0 / 64, bias=EPS), reads=["sm8c"], writes=["sm8c"])
                    yield S.op("act", lambda e: e.activation(out=sm8[:, 8:16], in_=sm8[:, 8:16], func=AF.Exp, scale=-0.5), reads=["sm8c"], writes=["sm8c"])
                    yield S.op("dve", lambda e: e.tensor_tensor(out=gt[:], in0=gt[:], in1=sm8[:, 8:16].unsqueeze(2).broadcast_to([128, 8, 64]), op=ALU.mult), reads=["gt", "sm8c"], writes=["gt"])
                    yield S.op("dve", lambda e, yt_=yt_: e.tensor_tensor(out=yt_[:, 512:1024], in0=gt[:].rearrange("p g c -> p (g c)"), in1=ggo_b[:], op=ALU.mult), reads=["gt", "ggo_b"], writes=[("y", ysl, 1)])

                    yield None
                def chainC():
                    if "y" in dbg_t:
                        row0d = st_i * 512 + j * 128
                        finals.append(S.op("pool", lambda e, yt_=yt_, row0d=row0d: e.dma_start(out=dbg_t["y"].ap()[row0d:row0d + 128, :], in_=yt_[:]), reads=[("y", ysl, 0), ("y", ysl, 1)], dma=("dbgy", ysl)))
                    pb, pt = (ptr[1], ("ptr", 1))
                    for ec in range(8):
                        yield S.op("pe", lambda e, ec=ec, pb=pb, yt_=yt_: e.transpose(out=pb[:, ec * 128:(ec + 1) * 128], in_=yt_[:, ec * 128:(ec + 1) * 128], identity=ident_b[:]),
                             reads=[("y", ysl, 0), ("y", ysl, 1), "ident_b"], accw=[pt])
                    yT_ = yT[ysl]
                    yield S.op("act", lambda e, pb=pb, yT_=yT_: e.copy(out=yT_[:].rearrange("p c t -> p (c t)"), in_=pb[:]), reads=[pt], writes=[("yT", ysl)])
                    h1t = h1b[ysl]
                    for hf in range(2):
                        ph, pht = fbankC()
                        for ec in range(8):
                            yield S.op("pe", lambda e, ec=ec, ph=ph, hf=hf, yT_=yT_: e.matmul(out=ph[:], lhsT=yT_[:, ec, :], rhs=wout[:, ec, hf * 512:(hf + 1) * 512], start=(ec == 0), stop=(ec == 7)),
                                 reads=[("yT", ysl), "wout"], accw=[pht])
                        yield S.op("dve", lambda e, ph=ph, hf=hf, h1t=h1t: e.tensor_tensor(out=h1t[:, hf * 512:(hf + 1) * 512], in0=ph[:], in1=xb[:, j, hf * 512:(hf + 1) * 512], op=ALU.add),
                             reads=[pht, ("x", sl)], writes=[("h1", ysl, hf)])
                    row0 = st_i * 512 + j * 128
                    yield S.op("sp", lambda e, h1t=h1t, row0=row0: e.dma_start(out=h1buf.ap()[row0:row0 + 128, :], in_=h1t[:]), reads=[("h1", ysl, 0), ("h1", ysl, 1)], accw=["h1buf"], dma=("h1st", ysl))
                    if "h1" in dbg_t:
                        finals.append(S.op("sp", lambda e, h1t=h1t, row0=row0: e.dma_start(out=dbg_t["h1"].ap()[row0:row0 + 128, :], in_=h1t[:]), reads=[("h1", ysl, 0), ("h1", ysl, 1)], dma=("dbgh1", ysl)))
                    ti = st_i * 4 + j
                    x2 = xn2t[ysl]
                    yield S.op("dve", lambda e: e.memset(sm8[:, 5:6], 0.0), writes=["sm8d"])
                    yield S.op("act", lambda e: e.activation(out=junk[:], in_=h1t[:], func=AF.Square, accum_out=sm8[:, 5:6]), reads=[("h1", ysl, 0), ("h1", ysl, 1), "sm8d"], writes=["junk", "sm8d"])
                    yield S.op("act", lambda e: e.activation(out=sm8[:, 5:6], in_=sm8[:, 5:6], func=AF.Ln, scale=1.0 / D, bias=EPS), reads=["sm8d"], writes=["sm8d"])
                    yield S.op("act", lambda e: e.activation(out=sm8[:, 5:6], in_=sm8[:, 5:6], func=AF.Exp, scale=-0.5), reads=["sm8d"], writes=["sm8d"])
                    yield S.op("dve", lambda e: e.scalar_tensor_tensor(out=x2[:], in0=h1t[:], scalar=sm8[:, 5:6], in1=g2_b[:], op0=ALU.mult, op1=ALU.mult),
                         reads=[("h1", ysl, 0), ("h1", ysl, 1), "sm8d", "g2_b"], writes=[("xn2", ysl)])
                    yield S.op("sp", lambda e: e.dma_start(out=xn2lin.ap()[row0:row0 + 128, :], in_=x2[:]), reads=[("xn2", ysl)], accw=["xn2lin"], dma=("xn2st", ysl))
                    pb2, pt2 = (ptr[1], ("ptr", 1))
                    for kc in range(8):
                        yield S.op("pe", lambda e, kc=kc: e.transpose(out=pb2[:, kc * 128:(kc + 1) * 128], in_=x2[:, kc * 128:(kc + 1) * 128], identity=ident_b[:]),
                             reads=[("xn2", ysl), "ident_b"], accw=[pt2])
                    x2T = xn2T[0]
                    yield S.op("act", lambda e: e.copy(out=x2T[:].rearrange("p c t -> p (c t)"), in_=pb2[:]), reads=[pt2], writes=[("xn2T", 0)])
                    pl, plt = fbankC()
                    for kc in range(8):
                        yield S.op("pe", lambda e, kc=kc: e.matmul(out=pl[:, 0:36], lhsT=x2T[:, kc, :], rhs=wr_b[:, kc, :], start=(kc == 0), stop=(kc == 7)),
                             reads=[("xn2T", 0), "wr_b"], accw=[plt])
                    yield S.op("dve", lambda e: e.tensor_tensor(out=lg[:], in0=pl[:, 0:36], in1=br_b[:], op=ALU.add), reads=[plt, "br_b"], writes=["lg"])
                    R_ = lambda a, b: rt[:, a:b]
                    yield S.op("dve", lambda e: e.tensor_reduce(out=R_(0, 1), in_=lg[:, 0:4], axis=AX.X, op=ALU.max), reads=["lg"], writes=["rt"])
                    yield S.op("dve", lambda e: e.tensor_scalar(out=R_(12, 16), in0=lg[:, 0:4], scalar1=R_(0, 1), scalar2=None, op0=ALU.is_equal), reads=["lg", "rt"], writes=["rt"])
                    yield S.op("dve", lambda e: e.tensor_scalar(out=R_(1, 2), in0=R_(0, 1), scalar1=-1.0, scalar2=None, op0=ALU.mult), reads=["rt"], writes=["rt"])
                    yield S.op("dve", lambda e: e.memset(R_(2, 3), 0.0), reads=["rt"], writes=["rt"])
                    yield S.op("act", lambda e: e.activation(out=le8[:, 8:12], in_=lg[:, 0:4], func=AF.Exp, bias=R_(1, 2), accum_out=R_(2, 3)), reads=["lg", "rt"], writes=["rt", "le8x"])
                    yield S.op("dve", lambda e: e.reciprocal(out=R_(3, 4), in_=R_(2, 3)), reads=["rt"], writes=["rt"])
                    yield S.op("dve", lambda e: e.tensor_tensor(out=t32[:], in0=lg[:, 4:36].rearrange("p (g j) -> p g j", g=4), in1=R_(12, 16).unsqueeze(2).broadcast_to([128, 4, 8]), op=ALU.mult), reads=["lg", "rt"], writes=["t32"])
                    yield S.op("dve", lambda e: e.tensor_reduce(out=le8[:, 0:8], in_=t32[:].rearrange("p g j -> p j g"), axis=AX.X, op=ALU.add), reads=["t32"], writes=["le8"])
                    yield S.op("dve", lambda e: e.tensor_reduce(out=R_(4, 5), in_=le8[:, 0:8], axis=AX.X, op=ALU.max), reads=["le8", "rt"], writes=["rt"])
                    yield S.op("dve", lambda e: e.tensor_scalar(out=R_(16, 24), in0=le8[:, 0:8], scalar1=R_(4, 5), scalar2=None, op0=ALU.is_equal), reads=["le8", "rt"], writes=["rt"])
                    yield S.op("dve", lambda e: e.scalar_tensor_tensor(out=le8[:, 0:8], in0=R_(16, 24), scalar=-1e30, in1=le8[:, 0:8], op0=ALU.mult, op1=ALU.add), reads=["le8", "rt"], writes=["le8"])
                    yield S.op("dve", lambda e: e.tensor_reduce(out=R_(5, 6), in_=le8[:, 0:8], axis=AX.X, op=ALU.max), reads=["le8", "rt"], writes=["rt"])
                    yield S.op("dve", lambda e: e.tensor_scalar(out=R_(24, 32), in0=le8[:, 0:8], scalar1=R_(5, 6), scalar2=None, op0=ALU.is_equal), reads=["le8", "rt"], writes=["rt"])
                    yield S.op("dve", lambda e: e.tensor_tensor(out=R_(6, 7), in0=R_(5, 6), in1=R_(4, 5), op=ALU.subtract), reads=["rt"], writes=["rt"])
                    yield S.op("act", lambda e: e.activation(out=R_(6, 7), in_=R_(6, 7), func=AF.Exp), reads=["rt"], writes=["rt"])
                    yield S.op("dve", lambda e: e.tensor_scalar(out=R_(7, 8), in0=R_(6, 7), scalar1=1.0, scalar2=None, op0=ALU.add), reads=["rt"], writes=["rt"])
                    yield S.op("dve", lambda e: e.reciprocal(out=R_(7, 8), in_=R_(7, 8)), reads=["rt"], writes=["rt"])
                    yield S.op("dve", lambda e: e.tensor_tensor(out=R_(8, 9), in0=R_(6, 7), in1=R_(7, 8), op=ALU.mult), reads=["rt"], writes=["rt"])
                    yield S.op("dve", lambda e: e.tensor_scalar(out=pw[:, ti, 2:4], in0=R_(7, 9), scalar1=R_(3, 4), scalar2=None, op0=ALU.mult), reads=["rt"], accw=["pw"])
                    E1 = E1s[:, ti, :]
                    E2 = E2s[:, ti, :]
                    yield S.op("dve", lambda e: e.tensor_tensor(out=E1.rearrange("p (g j) -> p g j", g=4), in0=R_(12, 16).unsqueeze(2).broadcast_to([128, 4, 8]), in1=R_(16, 24).unsqueeze(1).broadcast_to([128, 4, 8]), op=ALU.mult), reads=["rt"], accw=["E1s"])
                    yield S.op("dve", lambda e: e.tensor_tensor(out=E2.rearrange("p (g j) -> p g j", g=4), in0=R_(12, 16).unsqueeze(2).broadcast_to([128, 4, 8]), in1=R_(24, 32).unsqueeze(1).broadcast_to([128, 4, 8]), op=ALU.mult), reads=["rt"], accw=["E2s"])
                    yield S.op("dve", lambda e: e.tensor_tensor(out=ind_b[:], in0=E1, in1=E2, op=ALU.add), reads=["E1s", "E2s"], writes=["ind_b"])
                    pp, ppt = fbankC()
                    yield S.op("pe", lambda e: e.matmul(out=pp[:, 0:32], lhsT=lstr_b[:], rhs=ind_b[:], start=True, stop=True), reads=["ind_b", "lstr_b"], accw=[ppt])
                    yield S.op("pe", lambda e: e.matmul(out=pp[:, 32:64], lhsT=ones_b[:], rhs=ind_b[:], start=True, stop=True), reads=["ind_b", "ones_b"], accw=[ppt])
                    yield S.op("dve", lambda e: e.tensor_tensor(out=posf[:], in0=pp[:, 0:32], in1=base_b[:], op=ALU.add), reads=[ppt, "base_b"], writes=["posf"])
                    yield S.op("dve", lambda e: e.tensor_tensor(out=base_b[:], in0=pp[:, 32:64], in1=base_b[:], op=ALU.add), reads=[ppt, "base_b"], writes=["base_b"])
                    yield S.op("dve", lambda e: e.tensor_tensor(out=t32[:].rearrange("p g j -> p (g j)"), in0=E1, in1=posf[:], op=ALU.mult), reads=["E1s", "posf"], writes=["t32"])
                    yield S.op("dve", lambda e: e.tensor_reduce(out=pw[:, ti, 0:1], in_=t32[:].rearrange("p g j -> p (g j)"), axis=AX.X, op=ALU.add), reads=["t32"], accw=["pw"])
                    yield S.op("dve", lambda e: e.tensor_tensor(out=t32[:].rearrange("p g j -> p (g j)"), in0=E2, in1=posf[:], op=ALU.mult), reads=["E2s", "posf", "pw"], writes=["t32"])
                    yield S.op("dve", lambda e: e.tensor_reduce(out=pw[:, ti, 1:2], in_=t32[:].rearrange("p g j -> p (g j)"), axis=AX.X, op=ALU.add), reads=["t32"], accw=["pw"])
                    yield None
                return chainA1(), chainA2(), (chainB() if main else None), (chainC() if main else None)

            def run_chains(gens):
                act = [g for g in gens if g is not None]
                while act:
                    for g in list(act):
                        try:
                            next(g)
                        except StopIteration:
                            act.remove(g)

            made = {}

            def get(j):
                if j not in made:
                    made[j] = list(tile_body(j))
                return made[j]

            act = {}
            done = set()
            nxt = {"A1": 0, "A2": 0, "B": 0, "C": 0}
            IDX = {"A1": 0, "A2": 1, "B": 2, "C": 3}

            def fin(kind, j):
                return j < 0 or (kind, j) in done

            def start(kind, j):
                g = get(j)[IDX[kind]]
                if g is None:
                    done.add((kind, j))
                else:
                    act[(kind, j)] = g
                nxt[kind] += 1

            def try_start():
                j = nxt["A1"]
                if j < 4 and fin("A1", j - 1) and fin("A2", j - 2):
                    start("A1", j)
                j = nxt["A2"]
                if j < 4 and fin("A1", j) and j < nxt["A1"] and fin("A2", j - 1) and fin("C", j - 2):
                    start("A2", j)
                j = nxt["B"]
                if j < 4 and j < nxt["A1"] and fin("B", j - 1) and fin("C", j - 2):
                    start("B", j)
                j = nxt["C"]
                if j < 4 and j < nxt["A1"] and fin("A2", j) and fin("B", j) and fin("C", j - 1) and j < nxt["A2"] and j < nxt["B"]:
                    start("C", j)

            ready = {}
            while len(done) < 16:
                try_start()
                if not act:
                    continue
                for k_ in act:
                    ready.setdefault(k_, 0.0)
                k_ = min(act.keys(), key=lambda q: ready[q])
                try:
                    next(act[k_])
                    ready[k_] = S.last_est
                except StopIteration:
                    del act[k_]
                    del ready[k_]
                    done.add(k_)
            if gi == 0 and "qkT" in dbg_t:
                tmpd = S.sb([128, 8, 512], F32, "sdbg_qk")
                S.op("dve", lambda e: e.tensor_copy(out=tmpd[:], in_=qk[:]), reads=[("qkT", 0)], writes=["dbg_qk"])
                finals.append(S.op("sp", lambda e: e.dma_start(out=dbg_t["qkT"].ap(), in_=tmpd[:].rearrange("p a b -> p (a b)")), reads=["dbg_qk"], dma=("dbg", "qkT")))
            if gi == 0 and "xT" in dbg_t:
                tmpx = S.sb([128, 8, 512], F32, "sdbg_xT")
                S.op("dve", lambda e: e.tensor_copy(out=tmpx[:], in_=xt[:]), reads=[("xT", 0)], writes=["dbg_xT"])
                finals.append(S.op("sp", lambda e: e.dma_start(out=dbg_t["xT"].ap(), in_=tmpx[:].rearrange("p a b -> p (a b)")), reads=["dbg_xT"], dma=("dbg", "xT")))

        wcat_bf = nc.dram_tensor("wcat_bf", [NE * 128, 12288], BF16)
        NSUP = NPRE + NST
        conv_rows = NE * 128
        conv_state = {"r": 0, "i": 0}

        def conv_some(gidx, sl):
            tgt = conv_rows * (gidx + 1) // NSUP
            while conv_state["r"] < tgt:
                r0 = conv_state["r"]
                r1 = min(r0 + 32, tgt)
                k = conv_state["i"] % 2
                conv_state["i"] += 1
                conv_state["r"] = r1
                S.op("pool", lambda e, r0=r0, r1=r1: e.dma_start(out=wcat_bf.ap()[r0:r1, :], in_=wcat.ap()[r0:r1, :]),
                     reads=[("x", sl)], writes=[("wck", k)], dma=("wck", k))

        NBLK0 = NT * 2 + NE
        xslots = nc.dram_tensor("xslots", [NBLK0 * 128, D], BF16)
        ztile = junk
        S.op("dve", lambda e: e.memset(ztile[:], 0.0), writes=["junk"])
        for zb in range(0, NBLK0, 8):
            nb_ = min(8, NBLK0 - zb)
            S.op("sp", lambda e, zb=zb, nb_=nb_: e.dma_start(out=xslots.ap()[zb * 128:(zb + nb_) * 128, :].rearrange("(j p) d -> p j d", p=128),
                                                             in_=ztile[:].unsqueeze(1).broadcast_to([128, nb_, 1024])),
                 reads=["junk"], accw=["xslots"], dma=("zinit", zb // 8))
        gi = 0
        for s_i in range(NPRE):
            supertile(x_pre, s_i, "prelast" if s_i == NPRE - 1 else "pre", gi)
            gi += 1
        for s_i in range(NST):
            supertile(x_main, s_i, "main", gi)
            gi += 1


        S.flush()
        stA.close()
        stB = contextlib.ExitStack()
        S.cur = stB
        NBLK = NT * 2 + NE
        NSL = NBLK * 128
        yslots = nc.dram_tensor("yslots", [NSL, D], F32)
        pl_ = S.sb([128, 6, 32], F32, "plan")
        thr = S.sb([128, 64], F32, "thr")
        cmpn = S.sb([128, 32, 64], F32, "cmpn")
        p128 = S.sb([128, 1], F32, "p128")
        woff_f = S.sb([128, NBLK], F32, "woff_f")
        woff_i = S.sb([128, NBLK], I32, "woff_i")
        bval = S.sb([128, NBLK], F32, "bval")
        neq = S.sb([128, NBLK], F32, "neq")
        cmpb = S.sb([128, NBLK, 32], F32, "cmpb")
        dst_f = S.sb([128, 2, NT], F32, "dst_f")
        dst_i = S.sb([128, 2, NT], I32, "dst_i")
        big = S.sb([128, NT, 32], F32, "bigtmp")
        S.op("pool", lambda e: e.iota(p128[:], [[0, 1]], base=0, channel_multiplier=1, allow_small_or_imprecise_dtypes=True), writes=["p128"])
        S.op("pool", lambda e: e.iota(thr[:], [[128, 64]], base=0, channel_multiplier=0, allow_small_or_imprecise_dtypes=True), writes=["thr"])
        S.op("dve", lambda e: e.tensor_tensor(out=cmpn[:], in0=base_b[:].unsqueeze(2).broadcast_to([128, 32, 64]), in1=thr[:].unsqueeze(1).broadcast_to([128, 32, 64]), op=ALU.is_gt),
             reads=["base_b", "thr"], writes=["cmpn"])
        S.op("dve", lambda e: e.tensor_reduce(out=pl_[:, 0, :], in_=cmpn[:], axis=AX.X, op=ALU.add), reads=["cmpn"], writes=["plan"])
        S.op("dve", lambda e: e.tensor_scalar(out=pl_[:, 0, :], in0=pl_[:, 0, :], scalar1=128.0, scalar2=None, op0=ALU.mult), reads=["plan"], writes=["plan"])
        S.op("dve", lambda e: e.tensor_tensor_scan(out=pl_[:, 1, :], data0=pl_[:, 0, :], data1=pl_[:, 0, :], initial=0.0, op0=ALU.add, op1=ALU.bypass), reads=["plan"], writes=["plan"])
        S.op("dve", lambda e: e.tensor_tensor(out=pl_[:, 2, :], in0=pl_[:, 1, :], in1=pl_[:, 0, :], op=ALU.subtract), reads=["plan"], writes=["plan"])
        S.op("pool", lambda e: e.iota(bval[:], [[128, NBLK]], base=0, channel_multiplier=0, allow_small_or_imprecise_dtypes=True), writes=["bval"])
        S.op("dve", lambda e: e.tensor_tensor(out=cmpb[:], in0=pl_[:, 1, :].unsqueeze(1).broadcast_to([128, NBLK, 32]), in1=bval[:].unsqueeze(2).broadcast_to([128, NBLK, 32]), op=ALU.is_le), reads=["plan", "bval"], writes=["cmpb"])
        S.op("dve", lambda e: e.tensor_reduce(out=woff_f[:], in_=cmpb[:], axis=AX.X, op=ALU.add), reads=["cmpb"], writes=["woff_f"])
        BIGI = 1000000.0
        S.op("dve", lambda e: e.tensor_scalar(out=woff_f[:], in0=woff_f[:], scalar1=31.0, scalar2=None, op0=ALU.min), reads=["woff_f"], writes=["woff_f"])
        S.op("dve", lambda e: e.memset(neq[:, 0:2], 1.0), writes=["neq0"])
        S.op("dve", lambda e: e.tensor_tensor(out=neq[:, 2:NBLK], in0=woff_f[:, 2:NBLK], in1=woff_f[:, 0:NBLK - 2], op=ALU.not_equal), reads=["woff_f"], writes=["neq"])
        S.op("dve", lambda e: e.tensor_scalar(out=woff_f[:], in0=woff_f[:], scalar1=128.0, scalar2=-BIGI, op0=ALU.mult, op1=ALU.add), reads=["woff_f", "neq"], writes=["woff_f"])
        S.op("dve", lambda e: e.tensor_scalar(out=woff_f[:], in0=woff_f[:], scalar1=p128[:, 0:1], scalar2=None, op0=ALU.add), reads=["woff_f", "p128"], writes=["woff_f"])
        S.op("dve", lambda e: e.tensor_tensor(out=woff_f[:], in0=woff_f[:], in1=neq[:], op=ALU.mult), reads=["woff_f", "neq", "neq0"], writes=["woff_f"])
        S.op("dve", lambda e: e.tensor_scalar(out=woff_f[:], in0=woff_f[:], scalar1=BIGI, scalar2=None, op0=ALU.add), reads=["woff_f"], writes=["woff_f"])
        S.op("dve", lambda e: e.tensor_scalar(out=woff_f[:], in0=woff_f[:], scalar1=0.0, scalar2=None, op0=ALU.max), reads=["woff_f"], writes=["woff_f"])
        S.op("dve", lambda e: e.tensor_copy(out=woff_i[:], in_=woff_f[:]), reads=["woff_f"], writes=["woff_i"])
        for k_, Es in ((0, E1s), (1, E2s)):
            S.op("dve", lambda e, Es=Es: e.tensor_tensor(out=big[:], in0=Es[:], in1=pl_[:, 2, :].unsqueeze(1).broadcast_to([128, NT, 32]), op=ALU.mult), reads=["E1s", "E2s", "plan"], writes=["big"])
            S.op("dve", lambda e, k_=k_: e.tensor_reduce(out=dst_f[:, k_, :], in_=big[:], axis=AX.X, op=ALU.add), reads=["big"], writes=[("dst_f", k_)])
            S.op("dve", lambda e, k_=k_: e.tensor_tensor(out=dst_f[:, k_, :], in0=dst_f[:, k_, :], in1=pw[:, :, k_], op=ALU.add), reads=[("dst_f", k_), "pw"], writes=[("dst_f", k_)])
        S.op("dve", lambda e: e.tensor_scalar(out=dst_f[:].rearrange("p a b -> p (a b)"), in0=dst_f[:].rearrange("p a b -> p (a b)"), scalar1=float(NSL - 1), scalar2=0.0, op0=ALU.min, op1=ALU.max),
             reads=[("dst_f", 0), ("dst_f", 1)], writes=[("dst_f", 0), ("dst_f", 1)])
        S.op("dve", lambda e: e.tensor_copy(out=dst_i[:], in_=dst_f[:]), reads=[("dst_f", 0), ("dst_f", 1)], writes=["dst_i"])
        if "plan" in dbg_t:
            finals.append(S.op("sp", lambda e: e.dma_start(out=dbg_t["plan"].ap()[:, 0:192], in_=pl_[:].rearrange("p a b -> p (a b)")), reads=["plan"], dma=("dbg", "plan")))
            finals.append(S.op("sp", lambda e: e.dma_start(out=dbg_t["plan"].ap()[:, 768:768 + NBLK], in_=woff_f[:]), reads=["woff_f"], dma=("dbg", "plan2")))
            finals.append(S.op("sp", lambda e: e.dma_start(out=dbg_t["plan"].ap()[:, 256:256 + 2 * NT], in_=dst_f[:].rearrange("p a b -> p (a b)")), reads=[("dst_f", 0), ("dst_f", 1)], dma=("dbg", "plan3")))
            finals.append(S.op("sp", lambda e: e.dma_start(out=dbg_t["plan"].ap()[:, 512:512 + 4 * NT], in_=pw[:].rearrange("p a b -> p (a b)")), reads=["pw"], dma=("dbg", "plan4")))
        xsc = [S.sb([128, 1024], BF16, f"xsc{i}") for i in range(2)]
        for ti in range(NT):
            bsl = ti % 2
            S.op("sp", lambda e, ti=ti, bsl=bsl: e.dma_start(out=xsc[bsl][:], in_=xn2lin.ap()[ti * 128:(ti + 1) * 128, :]), reads=["xn2lin"], writes=[("xsc", bsl)], dma=("xsc", bsl))
            for k_ in range(2):
                S.op("pool", lambda e, ti=ti, bsl=bsl, k_=k_: e.indirect_dma_start(out=xslots.ap(), out_offset=bass.IndirectOffsetOnAxis(ap=dst_i[:, k_, ti:ti + 1], axis=0), in_=xsc[bsl][:], in_offset=None),
                     reads=[("xsc", bsl), "dst_i"], accw=["xslots"], dma=("scat", bsl, k_))

        wbuf = [S.sb([128, 12288], BF16, f"wbuf{i}") for i in range(2)]
        xs_b = [S.sb([128, 1024], BF16, f"xs_b{i}") for i in range(4)]
        xsT = [S.sb([128, 8, 128], BF16, f"xsT{i}") for i in range(2)]
        eg = [S.sb([128, 512], F32, f"eg{i}") for i in range(2)]
        hid = [S.sb([128, 512], BF16, f"hid{i}") for i in range(2)]
        hidT = [S.sb([128, 4, 128], BF16, f"hidT{i}") for i in range(2)]
        ysb = [S.sb([128, 1024], F32, f"ysb{i}") for i in range(2)]
        regs = {}

        def wgather(e, b, ws):
            if "bnd" not in regs:
                regs["bnd"] = st.enter_context(e.register("wbnd"))
                e.reg_mov(regs["bnd"], NE * 128 - 1)
            return e.indirect_dma_start(out=wbuf[ws][:], out_offset=None, in_=wcat_bf.ap(), in_offset=bass.IndirectOffsetOnAxis(ap=woff_i[:, b:b + 1], axis=0),
                                        bounds_check=regs["bnd"], oob_is_err=False)

        mrr = [0, 0]

        def fbankM(p):
            i = 3 * p + mrr[p] % 3
            mrr[p] += 1
            return pfb[i], ("pf", i)

        def blk(b):
            ws = b % 2
            yield S.op("pool", lambda e, b=b, ws=ws: wgather(e, b, ws), reads=["woff_i"], writes=[("wb", ws)], dma=("wb", ws))
            if b < 2:
                yield S.op("sp", lambda e, b=b: e.dma_start(out=xs_b[b % 4][:], in_=xslots.ap()[b * 128:(b + 1) * 128, :]), reads=["xslots"], writes=[("xs", b % 4)], dma=("xs", b % 4))
            if b + 2 < NBLK:
                yield S.op("sp", lambda e, b=b: e.dma_start(out=xs_b[(b + 2) % 4][:], in_=xslots.ap()[(b + 2) * 128:(b + 3) * 128, :]), reads=["xslots"], writes=[("xs", (b + 2) % 4)], dma=("xs", (b + 2) % 4))
            xq = b % 4
            pb, pt = (ptr[ws], ("ptr", ws))
            for kc in range(8):
                yield S.op("pe", lambda e, kc=kc, pb=pb, xq=xq: e.transpose(out=pb[:, kc * 128:(kc + 1) * 128], in_=xs_b[xq][:, kc * 128:(kc + 1) * 128], identity=ident_b[:]),
                     reads=[("xs", xq), "ident_b"], accw=[pt])
            yield S.op("act", lambda e, pb=pb, ws=ws: e.copy(out=xsT[ws][:].rearrange("p c t -> p (c t)"), in_=pb[:]), reads=[pt], writes=[("xsT", ws)])
            pG, pGt = fbankM(ws)
            pU2, pU2t = fbankM(ws)
            for kc in range(8):
                yield S.op("pe", lambda e, kc=kc, pG=pG, ws=ws: e.matmul(out=pG[:], lhsT=xsT[ws][:, kc, :], rhs=wbuf[ws][:, kc * 1024:kc * 1024 + 512], start=(kc == 0), stop=(kc == 7)),
                     reads=[("xsT", ws), ("wb", ws)], accw=[pGt])
            for kc in range(8):
                yield S.op("pe", lambda e, kc=kc, pU2=pU2, ws=ws: e.matmul(out=pU2[:], lhsT=xsT[ws][:, kc, :], rhs=wbuf[ws][:, kc * 1024 + 512:(kc + 1) * 1024], start=(kc == 0), stop=(kc == 7)),
                     reads=[("xsT", ws), ("wb", ws)], accw=[pU2t])
            yield S.op("act", lambda e, pG=pG, ws=ws: e.activation(out=eg[ws][:], in_=pG[:], func=AF.Exp, scale=-1.0), reads=[pGt], writes=[("eg", ws)])
            yield S.op("act", lambda e, ws=ws: e.activation(out=eg[ws][:], in_=eg[ws][:], func=AF.Ln, bias=1.0), reads=[("eg", ws)], writes=[("eg", ws)])
            yield S.op("act", lambda e, ws=ws: e.activation(out=eg[ws][:], in_=eg[ws][:], func=AF.Exp, scale=-1.0), reads=[("eg", ws)], writes=[("eg", ws)])
            yield S.op("dve", lambda e, pG=pG, ws=ws: e.tensor_tensor(out=eg[ws][:], in0=eg[ws][:], in1=pG[:], op=ALU.mult), reads=[("eg", ws), pGt], writes=[("eg", ws)])
            yield S.op("dve", lambda e, pU2=pU2, ws=ws: e.tensor_tensor(out=hid[ws][:], in0=eg[ws][:], in1=pU2[:], op=ALU.mult), reads=[("eg", ws), pU2t], writes=[("hid", ws)])
            pb, pt = (ptr[ws], ("ptr", ws))
            for fc in range(4):
                yield S.op("pe", lambda e, fc=fc, pb=pb, ws=ws: e.transpose(out=pb[:, fc * 128:(fc + 1) * 128], in_=hid[ws][:, fc * 128:(fc + 1) * 128], identity=ident_b[:]),
                     reads=[("hid", ws), "ident_b"], accw=[pt])
            yield S.op("act", lambda e, pb=pb, ws=ws: e.copy(out=hidT[ws][:].rearrange("p c t -> p (c t)"), in_=pb[:, 0:512]), reads=[pt], writes=[("hidT", ws)])
            for hf in range(2):
                pY, pYt = fbankM(ws)
                for fc in range(4):
                    yield S.op("pe", lambda e, fc=fc, pY=pY, ws=ws, hf=hf: e.matmul(out=pY[:], lhsT=hidT[ws][:, fc, :], rhs=wbuf[ws][:, 8192 + fc * 1024 + hf * 512:8192 + fc * 1024 + (hf + 1) * 512], start=(fc == 0), stop=(fc == 3)),
                         reads=[("hidT", ws), ("wb", ws)], accw=[pYt])
                if hf == 0:
                    yield S.op("act", lambda e, pY=pY, ws=ws: e.copy(out=ysb[ws][:, 0:512], in_=pY[:]), reads=[pYt], writes=[("ysb", ws, 0)])
                else:
                    yield S.op("dve", lambda e, pY=pY, ws=ws: e.tensor_copy(out=ysb[ws][:, 512:1024], in_=pY[:]), reads=[pYt], writes=[("ysb", ws, 1)])
            yield S.op("sp", lambda e, b=b, ws=ws: e.dma_start(out=yslots.ap()[b * 128:(b + 1) * 128, :], in_=ysb[ws][:]), reads=[("ysb", ws, 0), ("ysb", ws, 1)], accw=["yslots"], dma=("yst", ws))


            yield None

        for b in range(NBLK):
            for _ in blk(b):
                pass

        fg_b = bload("fg_b", final_g, 1024)
        NCB = 3
        hc = [S.sb([128, 1024], F32, f"hc{i}") for i in range(NCB)]
        y1 = [S.sb([128, 1024], F32, f"y1_{i}") for i in range(NCB)]
        y2 = [S.sb([128, 1024], F32, f"y2_{i}") for i in range(NCB)]
        fs = S.sb([128, 2], F32, "fs")

        def cloads(ti):
            cs = ti % NCB
            S.op("sp", lambda e, ti=ti, cs=cs: e.dma_start(out=hc[cs][:], in_=h1buf.ap()[ti * 128:(ti + 1) * 128, :]), reads=["h1buf"], writes=[("hc", cs)], dma=("hc", cs))
            S.op("pool", lambda e, ti=ti, cs=cs: e.indirect_dma_start(out=y1[cs][:], out_offset=None, in_=yslots.ap(), in_offset=bass.IndirectOffsetOnAxis(ap=dst_i[:, 0, ti:ti + 1], axis=0)),
                 reads=["yslots", "dst_i"], writes=[("y1", cs)], dma=("y1", cs))
            S.op("pool", lambda e, ti=ti, cs=cs: e.indirect_dma_start(out=y2[cs][:], out_offset=None, in_=yslots.ap(), in_offset=bass.IndirectOffsetOnAxis(ap=dst_i[:, 1, ti:ti + 1], axis=0)),
                 reads=["yslots", "dst_i"], writes=[("y2", cs)], dma=("y2", cs))

        for ti in range(min(NCB - 1, NT)):
            cloads(ti)
        for ti in range(NT):
            cs = ti % NCB
            if ti + NCB - 1 < NT:
                cloads(ti + NCB - 1)
            S.op("dve", lambda e, ti=ti, cs=cs: e.scalar_tensor_tensor(out=hc[cs][:], in0=y1[cs][:], scalar=pw[:, ti, 2:3], in1=hc[cs][:], op0=ALU.mult, op1=ALU.add), reads=[("hc", cs), ("y1", cs), "pw"], writes=[("hc", cs)])
            S.op("dve", lambda e, ti=ti, cs=cs: e.scalar_tensor_tensor(out=hc[cs][:], in0=y2[cs][:], scalar=pw[:, ti, 3:4], in1=hc[cs][:], op0=ALU.mult, op1=ALU.add), reads=[("hc", cs), ("y2", cs), "pw"], writes=[("hc", cs)])
            S.op("dve", lambda e, ti=ti: e.memset(fs[:, (ti % 2):(ti % 2) + 1], 0.0), writes=[("fs", ti % 2)])
            S.op("act", lambda e, ti=ti, cs=cs: e.activation(out=junk[:], in_=hc[cs][:], func=AF.Square, accum_out=fs[:, (ti % 2):(ti % 2) + 1]), reads=[("hc", cs), ("fs", ti % 2)], writes=["junk", ("fs", ti % 2)])
            S.op("act", lambda e, ti=ti: e.activation(out=fs[:, (ti % 2):(ti % 2) + 1], in_=fs[:, (ti % 2):(ti % 2) + 1], func=AF.Ln, scale=1.0 / D, bias=EPS), reads=[("fs", ti % 2)], writes=[("fs", ti % 2)])
            S.op("act", lambda e, ti=ti: e.activation(out=fs[:, (ti % 2):(ti % 2) + 1], in_=fs[:, (ti % 2):(ti % 2) + 1], func=AF.Exp, scale=-0.5), reads=[("fs", ti % 2)], writes=[("fs", ti % 2)])
            S.op("dve", lambda e, ti=ti, cs=cs: e.scalar_tensor_tensor(out=y1[cs][:], in0=hc[cs][:], scalar=fs[:, (ti % 2):(ti % 2) + 1], in1=fg_b[:], op0=ALU.mult, op1=ALU.mult), reads=[("hc", cs), ("fs", ti % 2), "fg_b", ("y1", cs)], writes=[("y1", cs)])
            finals.append(S.op("sp", lambda e, ti=ti, cs=cs: e.dma_start(out=out.ap()[ti * 128:(ti + 1) * 128, :], in_=y1[cs][:]), reads=[("y1", cs)], dma=("ost", cs)))

        S.flush()
        stB.close()
    return nc


def make_wcat(w_gate, w_up, w_down):
    g = w_gate.reshape(NE, 8, 128, 512).transpose(0, 2, 1, 3)
    u = w_up.reshape(NE, 8, 128, 512).transpose(0, 2, 1, 3)
    gu = np.concatenate([g, u], axis=3).reshape(NE, 128, 8192)
    dn = w_down.reshape(NE, 4, 128, 1024).transpose(0, 2, 1, 3).reshape(NE, 128, 4096)
    return np.ascontiguousarray(np.concatenate([gu, dn], axis=2).reshape(NE * 128, 12288))


def kernel(**inputs):
    f = lambda k: np.ascontiguousarray(np.asarray(inputs[k], dtype=np.float32))
    x = f("x")
    com = {
        "norm1_g": f("norm1_g")[0], "w_in": f("w_in")[0], "conv_qk": f("conv_qk")[0],
        "b_if": np.concatenate([f("b_igate")[0], f("b_fgate")[0]]), "g_mlstm_out": f("g_mlstm_out")[0],
        "g_gmlp_v": f("g_gmlp_v")[0], "w_spatial": f("w_spatial")[0], "b_spatial": f("b_spatial")[0],
        "g_gmlp_out": f("g_gmlp_out")[0], "w_out": f("w_out")[0], "norm2_g": f("norm2_g")[0],
        "w_router": np.ascontiguousarray(np.concatenate([f("w_router_group")[0], f("w_router_expert")[0]], axis=1)),
        "b_router": np.concatenate([f("b_router_group")[0], f("b_router_expert")[0]]),
        "wcat": make_wcat(f("w_gate")[0], f("w_up")[0], f("w_down")[0]), "final_g": f("final_g"),
    }
    in_maps = []
    for c in range(8):
        b, half = c // 2, c % 2
        m = dict(com)
        m["x_main"] = np.ascontiguousarray(x[b, half * 4096:(half + 1) * 4096])
        m["x_pre"] = np.ascontiguousarray(x[b, 0:4096]) if half == 1 else np.zeros((4096, D), np.float32)
        in_maps.append(m)
    nc = build(8, 8)
    res = run_bass_kernel_spmd(nc, in_maps, core_ids=list(range(8)))
    out = np.empty((4, 8192, D), np.float32)
    for c in range(8):
        out[c // 2, (c % 2) * 4096:(c % 2 + 1) * 4096] = res.results[c]["out"]
    return out
```
